# Optimizing a Trainium2 kernel written in Bass

```python
import jax, jax.numpy as jnp
from jax import lax
import numpy as np

D_MODEL = 1024
BATCH = 4
SEQ = 8192
DEPTH = 2

N_EVEN = (DEPTH + 1) // 2
N_ODD = DEPTH // 2
RMS_EPS = 1e-6
D_CONV = D_MODEL // 2
CONV_WIDTH = 3
D_POOL = D_MODEL // 2
POOL_WINDOWS = (2, 4, 8, 16)
POOL_GROUPS = len(POOL_WINDOWS)
POOL_GC = D_POOL // POOL_GROUPS
MAX_WIN = max(POOL_WINDOWS)
MIX_IN = 3 * D_CONV + D_POOL
MIX_OUT = D_CONV + D_POOL
HEAD_SIZE = 64
RWKV_HEADS = D_MODEL // HEAD_SIZE
DECAY_LORA = 64
AAA_LORA = 64
GATE_LORA = 160
LNX_EPS = 1e-5 * HEAD_SIZE
D_FF = 2816
N_EXPERTS = 8
TOP_K = 2

kernel_name = 'hybrid_conv_pool_rwkv7_moe'


def rmsnorm(x, g):
    xf = x.astype(jnp.float32)
    y = xf * lax.rsqrt(jnp.mean(xf * xf, axis=-1, keepdims=True) + RMS_EPS)
    return (y * g.astype(jnp.float32)).astype(x.dtype)


def short_conv(u, w):
    S = u.shape[1]
    up = jnp.pad(u, ((0, 0), (CONV_WIDTH - 1, 0), (0, 0)))
    return up[:, 0:S] * w[0] + up[:, 1:S + 1] * w[1] + up[:, 2:S + 2] * w[2]


def multiscale_pool(u, w_pool, scale):
    S = u.shape[1]
    uf = u.astype(jnp.float32)
    cs = jnp.pad(jnp.cumsum(uf, axis=1), ((0, 0), (MAX_WIN, 0), (0, 0)))
    pos = jnp.arange(S)
    outs = []
    for gi, win in enumerate(POOL_WINDOWS):
        sl = slice(gi * POOL_GC, (gi + 1) * POOL_GC)
        total = cs[:, MAX_WIN:MAX_WIN + S, sl] - cs[:, MAX_WIN - win:MAX_WIN - win + S, sl]
        cnt = jnp.minimum(pos + 1, win).astype(jnp.float32)[None, :, None]
        outs.append(total / cnt - uf[:, :, sl])
    p = jnp.stack(outs, axis=2)
    y = jnp.einsum('bsgc,gcd->bsgd', p, w_pool.astype(jnp.float32)).reshape(uf.shape)
    return (y * scale.astype(jnp.float32)).astype(u.dtype)


def conv_pool_mix(h, w_in, conv_w, pool_w, pool_scale, w_out):
    z = h @ w_in
    b_gate, c_gate, v_conv, v_pool = jnp.split(z, [D_CONV, 2 * D_CONV, 3 * D_CONV], axis=-1)
    y_conv = b_gate * short_conv(c_gate * v_conv, conv_w)
    y_pool = multiscale_pool(v_pool, pool_w, pool_scale)
    return jnp.concatenate([y_conv, y_pool], axis=-1) @ w_out


def rwkv7_mix(h, mu, w_r, w_k, w_v, w_o, w0, w1, w2, a0, a1, a2, g1, g2, k_k, k_a, r_k, ln_g, ln_b):
    B, S, D = h.shape
    f32 = jnp.float32
    h_prev = jnp.pad(h, ((0, 0), (1, 0), (0, 0)))[:, :S]
    xx = h_prev - h
    xr, xw, xk, xv, xa, xg = [h + xx * mu[i] for i in range(6)]
    r = xr @ w_r
    k = xk @ w_k
    v = xv @ w_v
    w_log = -jax.nn.softplus(-(w0 + jnp.tanh(xw @ w1) @ w2).astype(f32)) - 0.5
    decay = jnp.exp(-jnp.exp(w_log))
    a = jax.nn.sigmoid((a0 + (xa @ a1) @ a2).astype(f32))
    g = jax.nn.sigmoid(xg @ g1) @ g2

    def heads(t):
        return t.astype(f32).reshape(B, S, RWKV_HEADS, HEAD_SIZE)

    kk = heads(k * k_k)
    kk = kk / jnp.maximum(jnp.sqrt(jnp.sum(kk * kk, axis=-1, keepdims=True)), 1e-12)
    k = k.astype(f32) * (1.0 + (a - 1.0) * k_a.astype(f32))
    rh, wh, kh, vh, ah = heads(r), heads(decay), heads(k), heads(v), heads(a)

    def to_seq(t):
        return jnp.moveaxis(t, 1, 0)

    def step(state, inp):
        r_t, w_t, k_t, v_t, kk_t, a_t = inp
        sa = jnp.einsum('bhij,bhj->bhi', state, -kk_t)
        state = (state * w_t[:, :, None, :] + sa[..., None] * (kk_t * a_t)[:, :, None, :]
                 + v_t[..., None] * k_t[:, :, None, :])
        y_t = jnp.einsum('bhij,bhj->bhi', state, r_t)
        return state, y_t

    state0 = jnp.zeros((B, RWKV_HEADS, HEAD_SIZE, HEAD_SIZE), f32)
    _, ys = lax.scan(step, state0, (to_seq(rh), to_seq(wh), to_seq(kh), to_seq(vh), to_seq(kk), to_seq(ah)))
    y = jnp.moveaxis(ys, 0, 1)
    mean = jnp.mean(y, axis=-1, keepdims=True)
    var = jnp.mean(jnp.square(y - mean), axis=-1, keepdims=True)
    yn = ((y - mean) * lax.rsqrt(var + LNX_EPS)).reshape(B, S, D) * ln_g.astype(f32) + ln_b.astype(f32)
    bonus = (jnp.sum(rh * kh * r_k.astype(f32), axis=-1, keepdims=True) * vh).reshape(B, S, D)
    out = ((yn + bonus) * g.astype(f32)).astype(h.dtype) @ w_o
    return out


def swiglu(t, wg, wu, wd):
    return (jax.nn.silu(t @ wg) * (t @ wu)) @ wd


def moe_swiglu(h, router, wg, wu, wd):
    B, S, D = h.shape
    t = h.reshape(B * S, D)
    logits = (t @ router).astype(jnp.float32)
    top_val, top_idx = lax.top_k(logits, TOP_K)
    top_w = jax.nn.softmax(top_val, axis=-1)
    combine = jnp.sum(top_w[..., None] * jax.nn.one_hot(top_idx, N_EXPERTS, dtype=jnp.float32), axis=1)
    out = jnp.zeros((B * S, D), jnp.float32)
    for e in range(N_EXPERTS):
        out = out + combine[:, e:e + 1] * swiglu(t, wg[e], wu[e], wd[e]).astype(jnp.float32)
    return out.reshape(B, S, D).astype(h.dtype)


def setup_inputs(seed: int = 0) -> dict:
    key = jax.random.key(seed)
    ks = iter(jax.random.split(key, 40))

    def nrm(shape, scale):
        return jax.random.normal(next(ks), shape, jnp.float32) * scale

    def gain(shape):
        return 1.0 + nrm(shape, 0.05)

    D, F, E = D_MODEL, D_FF, N_EXPERTS
    return {
        'x': nrm((BATCH, SEQ, D), 1.0),
        'c': nrm((BATCH, D), 1.0),
        'ada_w': nrm((DEPTH, D, 6 * D), 0.02),
        'ada_b': nrm((DEPTH, 6 * D), 0.02),
        'norm_g': gain((DEPTH, 4, D)),
        'mix_w_in': nrm((N_EVEN, D, MIX_IN), D ** -0.5),
        'conv_w': nrm((N_EVEN, CONV_WIDTH, D_CONV), CONV_WIDTH ** -0.5),
        'pool_w': nrm((N_EVEN, POOL_GROUPS, POOL_GC, POOL_GC), POOL_GC ** -0.5),
        'pool_scale': gain((N_EVEN, D_POOL)),
        'mix_w_out': nrm((N_EVEN, MIX_OUT, D), MIX_OUT ** -0.5),
        'ffn_w_gate': nrm((N_EVEN, D, F), D ** -0.5),
        'ffn_w_up': nrm((N_EVEN, D, F), D ** -0.5),
        'ffn_w_down': nrm((N_EVEN, F, D), F ** -0.5),
        'rwkv_mu': jax.random.uniform(next(ks), (N_ODD, 6, D), jnp.float32),
        'rwkv_w_r': nrm((N_ODD, D, D), D ** -0.5),
        'rwkv_w_k': nrm((N_ODD, D, D), D ** -0.5),
        'rwkv_w_v': nrm((N_ODD, D, D), D ** -0.5),
        'rwkv_w_o': nrm((N_ODD, D, D), D ** -0.5),
        'rwkv_w0': nrm((N_ODD, D), 0.5),
        'rwkv_w1': nrm((N_ODD, D, DECAY_LORA), D ** -0.5),
        'rwkv_w2': nrm((N_ODD, DECAY_LORA, D), DECAY_LORA ** -0.5),
        'rwkv_a0': nrm((N_ODD, D), 0.1),
        'rwkv_a1': nrm((N_ODD, D, AAA_LORA), D ** -0.5),
        'rwkv_a2': nrm((N_ODD, AAA_LORA, D), AAA_LORA ** -0.5),
        'rwkv_g1': nrm((N_ODD, D, GATE_LORA), D ** -0.5),
        'rwkv_g2': nrm((N_ODD, GATE_LORA, D), GATE_LORA ** -0.5),
        'rwkv_k_k': gain((N_ODD, D)),
        'rwkv_k_a': gain((N_ODD, D)),
        'rwkv_r_k': nrm((N_ODD, RWKV_HEADS, HEAD_SIZE), 0.1),
        'rwkv_ln_g': gain((N_ODD, D)),
        'rwkv_ln_b': nrm((N_ODD, D), 0.02),
        'moe_router': nrm((N_ODD, D, E), D ** -0.5),
        'moe_w_gate': nrm((N_ODD, E, D, F), D ** -0.5),
        'moe_w_up': nrm((N_ODD, E, D, F), D ** -0.5),
        'moe_w_down': nrm((N_ODD, E, F, D), F ** -0.5),
    }


def reference(x, c, ada_w, ada_b, norm_g, mix_w_in, conv_w, pool_w, pool_scale, mix_w_out,
              ffn_w_gate, ffn_w_up, ffn_w_down, rwkv_mu, rwkv_w_r, rwkv_w_k, rwkv_w_v, rwkv_w_o,
              rwkv_w0, rwkv_w1, rwkv_w2, rwkv_a0, rwkv_a1, rwkv_a2, rwkv_g1, rwkv_g2,
              rwkv_k_k, rwkv_k_a, rwkv_r_k, rwkv_ln_g, rwkv_ln_b,
              moe_router, moe_w_gate, moe_w_up, moe_w_down):
    cond = jax.nn.silu(c)
    for layer in range(DEPTH):
        mod = cond @ ada_w[layer] + ada_b[layer]
        sh_m, sc_m, gt_m, sh_f, sc_f, gt_f = [m[:, None, :] for m in jnp.split(mod, 6, axis=-1)]
        g_pre_m, g_post_m, g_pre_f, g_post_f = norm_g[layer]
        i = layer // 2
        h = rmsnorm(x, g_pre_m) * (1 + sc_m) + sh_m
        if layer % 2 == 0:
            y = conv_pool_mix(h, mix_w_in[i], conv_w[i], pool_w[i], pool_scale[i], mix_w_out[i])
        else:
            y = rwkv7_mix(h, rwkv_mu[i], rwkv_w_r[i], rwkv_w_k[i], rwkv_w_v[i], rwkv_w_o[i],
                          rwkv_w0[i], rwkv_w1[i], rwkv_w2[i], rwkv_a0[i], rwkv_a1[i], rwkv_a2[i],
                          rwkv_g1[i], rwkv_g2[i], rwkv_k_k[i], rwkv_k_a[i], rwkv_r_k[i],
                          rwkv_ln_g[i], rwkv_ln_b[i])
        x = x + gt_m * rmsnorm(y, g_post_m)
        h = rmsnorm(x, g_pre_f) * (1 + sc_f) + sh_f
        if layer % 2 == 0:
            y = swiglu(h, ffn_w_gate[i], ffn_w_up[i], ffn_w_down[i])
        else:
            y = moe_swiglu(h, moe_router[i], moe_w_gate[i], moe_w_up[i], moe_w_down[i])
        x = x + gt_f * rmsnorm(y, g_post_f)
    return x
```

```python
import contextlib
import numpy as np
import concourse.bass as bass
import concourse.mybir as mybir
from concourse.bass_utils import run_bass_kernel_spmd

F32 = mybir.dt.float32
BF16 = mybir.dt.bfloat16
ALU = mybir.AluOpType
AF = mybir.ActivationFunctionType
AX = mybir.AxisListType

D = 1024
FF = 2816
NE = 8
SEQ = 8192
HALF = 4096
RMS_EPS = 1e-6
LNX_EPS = 1e-5 * 64

COMPUTE = ("pe", "act", "dve", "pool")
NDMA_SEMS = 12
EPOCH = 30000


class _Op:
    __slots__ = ("eng", "fn", "deps", "is_dma", "need_inc", "tick", "dsem", "dval", "dprev")

    def __init__(self, eng, fn, deps, is_dma):
        self.eng = eng
        self.fn = fn
        self.deps = deps
        self.is_dma = is_dma
        self.need_inc = False
        self.tick = 0
        self.dsem = None
        self.dval = 0
        self.dprev = None


class _Rec:
    def __getattr__(self, name):
        return lambda *a, **k: (name, a, k)


_REC = _Rec()


class FW:
    def __init__(self, nc):
        self.nc = nc
        self.ops = []
        self.last_w = {}
        self.readers = {}
        self.bar = set()
        self.last_c = {}
        self.dma_since = []

    def _add(self, eng, fn, reads, writes, is_dma):
        idx = len(self.ops)
        pr = [r for r in reads if r.startswith("ps")]
        if pr:
            reads = [r for r in reads if not r.startswith("ps")]
            writes = list(writes) + pr
        deps = set(self.bar)
        for r in reads:
            w = self.last_w.get(r)
            if w is not None:
                deps.add(w)
        for k in writes:
            w = self.last_w.get(k)
            if w is not None:
                deps.add(w)
            rd = self.readers.get(k)
            if rd is not None:
                deps.update(rd["c"].values())
                deps.update(rd["d"])
        for r in reads:
            rd = self.readers.get(r)
            if rd is None:
                rd = self.readers[r] = {"c": {}, "d": []}
            if is_dma:
                rd["d"].append(idx)
            else:
                rd["c"][eng] = idx
        for k in writes:
            self.last_w[k] = idx
            self.readers[k] = {"c": {}, "d": []}
        deps.discard(idx)
        self.ops.append(_Op(eng, fn, deps, is_dma))
        if is_dma:
            self.dma_since.append(idx)
        else:
            self.last_c[eng] = idx
        return idx

    def op(self, eng, fn, reads=(), writes=()):
        name, a, k = fn(_REC)
        return self._add(eng, lambda e: getattr(e, name)(*a, **k), reads, writes, False)

    def dma(self, q, out, in_, reads=(), writes=()):
        return self._add(q, lambda e: e.dma_start(out=out, in_=in_), reads, writes, True)

    def barrier(self):
        self.bar = set(self.last_c.values()) | set(self.dma_since)
        self.dma_since = []
        self.last_w = {}
        self.readers = {}

    def emit(self):
        nc = self.nc
        ops = self.ops
        for o in ops:
            for d in o.deps:
                p = ops[d]
                if p.is_dma:
                    continue
                if p.eng == o.eng and p.eng == "pe" and not o.is_dma:
                    continue
                p.need_inc = True
        ticks = {e: 0 for e in COMPUTE}
        for o in ops:
            if not o.is_dma and o.need_inc:
                ticks[o.eng] += 1
                o.tick = ticks[o.eng]
        qcount = {}
        qlast = {}
        for i, o in enumerate(ops):
            if o.is_dma:
                n = qcount.get(o.eng, 0)
                qcount[o.eng] = n + 1
                slot = n % NDMA_SEMS
                key = (o.eng, slot)
                o.dsem = key
                o.dval = (n // NDMA_SEMS + 1) * 16
                o.dprev = qlast.get(key)
                qlast[key] = i
        engs = ("pe", "act", "dve", "pool", "sp")
        with contextlib.ExitStack() as st:
            csem = {}
            for e in COMPUTE:
                nep = (ticks[e] + EPOCH - 1) // EPOCH
                for k in range(max(nep, 1)):
                    csem[(e, k)] = st.enter_context(nc.semaphore("c_%s_%d" % (e, k)))
            dsem = {}
            for q in qcount:
                for s in range(min(NDMA_SEMS, qcount[q])):
                    dsem[(q, s)] = st.enter_context(nc.semaphore("d_%s_%d" % (q, s)))
            known = {e: {} for e in engs}
            streams = {e: [] for e in engs}
            for i, o in enumerate(ops):
                e = o.eng
                kn = known[e]
                waits = {}
                for d in o.deps:
                    p = ops[d]
                    if p.is_dma:
                        sk = ("d",) + p.dsem
                        v = p.dval
                    else:
                        if p.eng == e and e == "pe" and not o.is_dma:
                            continue
                        ep = (p.tick - 1) // EPOCH
                        sk = ("c", p.eng, ep)
                        v = p.tick - ep * EPOCH
                    if kn.get(sk, 0) >= v:
                        continue
                    if waits.get(sk, 0) < v:
                        waits[sk] = v
                if o.is_dma and o.dprev is not None:
                    p = ops[o.dprev]
                    sk = ("d",) + p.dsem
                    if kn.get(sk, 0) < p.dval and waits.get(sk, 0) < p.dval:
                        waits[sk] = p.dval
                for sk, v in waits.items():
                    kn[sk] = v
                    sem = csem[(sk[1], sk[2])] if sk[0] == "c" else dsem[(sk[1], sk[2])]
                    streams[e].append(("w", sem, v))
                if o.is_dma:
                    streams[e].append(("i", o.fn, dsem[o.dsem], 16))
                elif o.need_inc:
                    streams[e].append(("i", o.fn, csem[(e, (o.tick - 1) // EPOCH)], 1))
                else:
                    streams[e].append(("i", o.fn, None, 0))
            fin = streams["sp"]
            for key, i in qlast.items():
                fin.append(("w", dsem[key], ops[i].dval))
            for e in COMPUTE:
                if ticks[e] > 0:
                    ep = (ticks[e] - 1) // EPOCH
                    fin.append(("w", csem[(e, ep)], ticks[e] - ep * EPOCH))

            def run(eng_obj, lst):
                for it in lst:
                    if it[0] == "w":
                        eng_obj.wait_ge(it[1], it[2])
                    else:
                        ins = it[1](eng_obj)
                        if it[2] is not None:
                            ins.then_inc(it[2], it[3])

            with nc.Block() as block:
                @block.tensor
                def _(eng):
                    run(eng, streams["pe"])

                @block.scalar
                def _(eng):
                    run(eng, streams["act"])

                @block.vector
                def _(eng):
                    run(eng, streams["dve"])

                @block.gpsimd
                def _(eng):
                    run(eng, streams["pool"])

                @block.sync
                def _(eng):
                    run(eng, streams["sp"])
        return {e: len(streams[e]) for e in streams}


class Arena:
    def __init__(self, t, nwords):
        self.t = t
        self.n = nwords
        self.off = 0

    def reset(self):
        self.off = 0

    def alloc(self, shape, dtype):
        nel = int(np.prod(shape))
        nb = nel * (4 if dtype == F32 else 2)
        nw = (nb + 3) // 4
        ap = self.t[:, self.off:self.off + nw]
        self.off += nw
        assert self.off <= self.n, ("arena overflow", self.off, self.n)
        if dtype != F32:
            ap = ap.bitcast(dtype)[:, 0:nel]
        if len(shape) == 2:
            ap = ap.rearrange("p (a b) -> p a b", a=shape[0])
        elif len(shape) == 3:
            ap = ap.rearrange("p (a b c) -> p a b c", a=shape[0], b=shape[1])
        return ap


def bc(ap, shape):
    return ap.to_broadcast(list(shape))


class Builder:
    def __init__(self, stages=("M", "A", "B", "D", "E", "F"), dbg=False):
        self.stages = stages
        self.dbg = dbg
        self.nc = bass.Bass("TRN2", target_bir_lowering=False)
        self.fw = FW(self.nc)
        self.uid = 0

    def din(self, name, shape, dt=F32):
        return self.nc.dram_tensor(name, list(shape), dt, kind="ExternalInput").ap()

    def dscr(self, name, shape, dt=F32, out=False):
        kind = "ExternalOutput" if out else "Internal"
        return self.nc.dram_tensor(name, list(shape), dt, kind=kind).ap()

    def build(self):
        nc, fw = self.nc, self.fw
        dbg = self.dbg
        I = {}
        I["x8"] = self.din("x8", [SEQ, D])
        I["condB"] = self.din("condB", [128, 8, 128])
        I["flag"] = self.din("flag", [128, 1])
        I["invc"] = self.din("invc", [128, 2, 4, 16])
        I["ident"] = self.din("ident", [128, 128])
        I["ada_w"] = self.din("ada_w", [2, D, 6 * D])
        I["ada_bB"] = self.din("ada_bB", [2, 128, 6 * D])
        I["norm_gB"] = self.din("norm_gB", [2, 128, 4, D])
        I["mix_w_in"] = self.din("mix_w_in", [D, 2048])
        I["mix_w_out"] = self.din("mix_w_out", [D, D])
        I["conv_wT"] = self.din("conv_wT", [128, 4, 3])
        I["pool_w"] = self.din("pool_w", [4, 128, 128])
        I["pool_scT"] = self.din("pool_scT", [128, 4])
        I["ffn_wg"] = self.din("ffn_wg", [D, FF])
        I["ffn_wu"] = self.din("ffn_wu", [D, FF])
        I["ffn_wd"] = self.din("ffn_wd", [FF, D])
        for nm in ("rw_wr", "rw_wk", "rw_wv", "rw_wo"):
            I[nm] = self.din(nm, [D, D])
        I["rw_w1"] = self.din("rw_w1", [D, 64])
        I["rw_a1"] = self.din("rw_a1", [D, 64])
        I["rw_g1"] = self.din("rw_g1", [D, 160])
        I["rw_w2"] = self.din("rw_w2", [64, D])
        I["rw_a2"] = self.din("rw_a2", [64, D])
        I["rw_g2"] = self.din("rw_g2", [160, D])
        I["rw_vec"] = self.din("rw_vec", [128, 13, 8])
        I["moe_router"] = self.din("moe_router", [D, 8])
        I["resetm"] = self.din("resetm", [128, 1024])
        I["mask1"] = self.din("mask1", [128, 2, 128])
        I["maskT"] = self.din("maskT", [128, 128])
        I["bdones"] = self.din("bdones", [128, 128])
        I["moe_wg"] = self.din("moe_wg", [NE, D, FF])
        I["moe_wu"] = self.din("moe_wu", [NE, D, FF])
        I["moe_wd"] = self.din("moe_wd", [NE, FF, D])
        self.I = I
        S = {}
        S["x1"] = self.dscr("x1", [SEQ, D], out=dbg)
        S["hTf0"] = self.dscr("hTf0", [32, 128, 8, 256], BF16)
        S["acc0"] = self.dscr("acc0", [SEQ, D])
        S["x2"] = self.dscr("x2", [SEQ, D], out=("C" in self.stages))
        S["x3"] = self.dscr("x3", [HALF, D], out=dbg)
        S["hTf1"] = self.dscr("hTf1", [16, 128, 8, 256], BF16)
        S["comb"] = self.dscr("comb", [HALF, 8], out=dbg)
        S["acc1"] = self.dscr("acc1", [HALF, D])
        S["y"] = self.dscr("y", [HALF, D], out=True)
        self.S = S

        with contextlib.ExitStack() as st:
            sb = lambda n, sh, dt: st.enter_context(nc.sbuf_tensor("sb_" + n, sh, dt))
            P = {}
            P["ident"] = sb("identf", [128, 128], F32)
            P["identb"] = sb("identb", [128, 128], BF16)
            P["flag"] = sb("flag", [128, 1], F32)
            P["invc"] = sb("invc", [128, 2, 4, 16], F32)
            P["GP"] = sb("GP", [128, 4, D], F32)
            P["modT"] = sb("modT", [128, 8, 8], F32)
            P["small"] = sb("small", [128, 64], F32)
            P["eps"] = sb("eps", [128, 2], F32)
            ARW = 46500
            arena_t = sb("arena", [128, ARW], F32)
            self.P = P
            self.ar = Arena(arena_t, ARW)
            self.ps = [st.enter_context(nc.psum_tensor("ps%d" % i, [128, 512], F32)) for i in range(8)]

            fw.dma("sp", P["ident"][:], I["ident"], writes=["ident"])
            fw.dma("pool", P["identb"][:], I["ident"], writes=["identb"])
            fw.dma("sp", P["flag"][:], I["flag"], writes=["flag"])
            fw.op("pool", lambda e: e.memset(P["eps"][:, 0:1], RMS_EPS), writes=["eps"])
            fw.op("pool", lambda e: e.memset(P["eps"][:, 1:2], LNX_EPS), writes=["eps"])
            fw.dma("sp", P["invc"][:], I["invc"], writes=["invc"])
            if "M" in self.stages:
                self.phase_mod(0)
                self.phase_mod(1)
            if "A" in self.stages:
                self.phase_mixer0()
            if "B" in self.stages:
                fw.barrier()
                self.ar.reset()
                self.ffn_passes(self.I["ffn_wg"], self.I["ffn_wu"], self.I["ffn_wd"], 32, S["hTf0"], S["acc0"], None, [0])
            if "C" in self.stages:
                fw.barrier()
                self.ar.reset()
                self.phase_post0()
            if "D" in self.stages:
                self.phase_rwkv()
            if "E" in self.stages:
                fw.barrier()
                self.ar.reset()
                self.ffn_passes(self.I["moe_wg"], self.I["moe_wu"], self.I["moe_wd"], 16, S["hTf1"], S["acc1"], S["comb"], list(range(NE)))
            if "F" in self.stages:
                fw.barrier()
                self.ar.reset()
                self.phase_final()
            self.stats = fw.emit()
        return nc

    def phase_mod(self, l):
        nc, fw, P, I, ar = self.nc, self.fw, self.P, self.I, self.ar
        ps = self.ps
        fw.barrier()
        ar.reset()
        cond = ar.alloc([8, 128], F32)
        modb = ar.alloc([6 * D], F32)
        adab = ar.alloc([6 * D], F32)
        ng = ar.alloc([4, D], F32)
        wblk = [ar.alloc([8, 512], F32) for _ in range(2)]
        tmpb = ar.alloc([4, D], F32)
        fw.dma("sp", cond, I["condB"], writes=["cond"])
        fw.dma("sp", adab, I["ada_bB"][l], writes=["adab"])
        fw.dma("sp", ng, I["norm_gB"][l], writes=["ng"])
        fw.op("act", lambda e: e.activation(out=cond, in_=cond, func=AF.Silu), reads=["cond"], writes=["cond"])
        for blk in range(12):
            wb = wblk[blk % 2]
            wk = "wblk%d" % (blk % 2)
            fw.dma("sp", wb, I["ada_w"][l, :, blk * 512:(blk + 1) * 512].rearrange("(kc p) n -> p kc n", p=128), writes=[wk])
            pb = ps[blk % 2]
            pk = "ps%d" % (blk % 2)
            for kc in range(8):
                fw.op("pe", lambda e, kc=kc, wb=wb, pb=pb: e.matmul(pb[:, :], cond[:, kc, :], wb[:, kc, :], start=(kc == 0), stop=(kc == 7)),
                      reads=["cond", wk], writes=[pk])
            fw.op("dve", lambda e, blk=blk, pb=pb: e.tensor_tensor(out=modb[:, blk * 512:(blk + 1) * 512], in0=pb[:, :], in1=adab[:, blk * 512:(blk + 1) * 512], op=ALU.add),
                  reads=[pk, "adab"], writes=["modb"])
        sh_m, sc_m, gt_m, sh_f, sc_f, gt_f = [modb[:, i * D:(i + 1) * D] for i in range(6)]
        fw.op("dve", lambda e: e.tensor_tensor(out=P["GP"][:, l * 2 + 0, :], in0=gt_m, in1=ng[:, 1, :], op=ALU.mult), reads=["modb", "ng"], writes=["GP"])
        fw.op("dve", lambda e: e.tensor_tensor(out=P["GP"][:, l * 2 + 1, :], in0=gt_f, in1=ng[:, 3, :], op=ALU.mult), reads=["modb", "ng"], writes=["GP"])
        fw.op("dve", lambda e: e.scalar_tensor_tensor(out=tmpb[:, 0, :], in0=sc_m, scalar=1.0, in1=ng[:, 0, :], op0=ALU.add, op1=ALU.mult), reads=["modb", "ng"], writes=["tmpb"])
        fw.op("dve", lambda e: e.tensor_copy(out=tmpb[:, 1, :], in_=sh_m), reads=["modb"], writes=["tmpb"])
        fw.op("dve", lambda e: e.scalar_tensor_tensor(out=tmpb[:, 2, :], in0=sc_f, scalar=1.0, in1=ng[:, 2, :], op0=ALU.add, op1=ALU.mult), reads=["modb", "ng"], writes=["tmpb"])
        fw.op("dve", lambda e: e.tensor_copy(out=tmpb[:, 3, :], in_=sh_f), reads=["modb"], writes=["tmpb"])
        for v in range(4):
            for kc in range(8):
                pb = ps[2 + kc // 4]
                fw.op("pe", lambda e, v=v, kc=kc, pb=pb: e.transpose(pb[:, (kc % 4) * 128:(kc % 4 + 1) * 128], tmpb[:, v, kc * 128:(kc + 1) * 128], P["ident"][:]),
                      reads=["tmpb", "ident"], writes=["ps%d" % (2 + kc // 4)])
            for hh in range(2):
                fw.op("dve", lambda e, v=v, hh=hh: e.tensor_copy(out=P["modT"][:, l * 4 + v, hh * 4:(hh + 1) * 4],
                                                                 in_=ps[2 + hh][:, :].rearrange("p (k t) -> p k t", t=128)[:, :, 0]),
                      reads=["ps%d" % (2 + hh)], writes=["modT"])

    def prenorm_T(self, xsub, xkey, l, sub, hT, hkey, col0, pbank, scr, f32out=None):
        fw, P, ps = self.fw, self.P, self.ps
        u = self.uid
        self.uid += 1
        junk, ss, rstd, xn, tmp = scr["junk"], scr["ss"], scr["rstd"], scr["xn"], scr["tmp"]
        fw.op("act", lambda e: e.activation(out=junk, in_=xsub, func=AF.Square, accum_out=ss[:, 0:1]), reads=[xkey], writes=["junk", "ss"])
        fw.op("act", lambda e: e.activation(out=rstd[:, 0:1], in_=ss[:, 0:1], func=AF.Sqrt, scale=1.0 / D, bias=P["eps"][:, 0:1]), reads=["ss", "eps"], writes=["rstd"])
        fw.op("dve", lambda e: e.reciprocal(out=rstd[:, 0:1], in_=rstd[:, 0:1]), reads=["rstd"], writes=["rstd"])
        fw.op("act", lambda e: e.activation(out=xn, in_=xsub, func=AF.Copy, scale=rstd[:, 0:1]), reads=[xkey, "rstd"], writes=["xn"])
        pk = "ps%d" % pbank
        pbt = ps[pbank][:, :].bitcast(BF16)
        for kc in range(8):
            fw.op("pe", lambda e, kc=kc: e.transpose(pbt[:, kc * 128:(kc + 1) * 128], xn[:, kc * 128:(kc + 1) * 128], P["identb"][:]),
                  reads=["xn", "identb"], writes=[pk])
        G1 = P["modT"][:, l * 4 + sub * 2 + 0, :]
        sh = P["modT"][:, l * 4 + sub * 2 + 1, :]
        tmp3 = tmp.rearrange("p (k t) -> p k t", t=128)
        fw.op("dve", lambda e: e.tensor_tensor(out=tmp3, in0=pbt.rearrange("p (k t) -> p k t", t=128), in1=bc(G1.unsqueeze(2), [128, 8, 128]), op=ALU.mult),
              reads=[pk, "modT"], writes=["tmp"])
        fw.op("pool", lambda e: e.tensor_tensor(out=hT[:, :, col0:col0 + 128], in0=tmp3, in1=bc(sh.unsqueeze(2), [128, 8, 128]), op=ALU.add),
              reads=["tmp", "modT"], writes=[hkey])

    def postnorm_res(self, psb, xsub, xkey, gp_idx, scr):
        fw, P, ps = self.fw, self.P, self.ps
        junk, ss2, rstd, tmp = scr["junk"], scr["ss2"], scr["rstd2"], scr["tmp"]
        for n in range(2):
            fw.op("act", lambda e, n=n: e.activation(out=junk[:, 0:512], in_=ps[psb[n]][:, :], func=AF.Square, accum_out=ss2[:, n:n + 1]),
                  reads=["ps%d" % psb[n]], writes=["junk", "ss2"])
        fw.op("pool", lambda e: e.tensor_tensor(out=rstd[:, 0:1], in0=ss2[:, 0:1], in1=ss2[:, 1:2], op=ALU.add), reads=["ss2"], writes=["rstd2"])
        fw.op("act", lambda e: e.activation(out=rstd[:, 0:1], in_=rstd[:, 0:1], func=AF.Sqrt, scale=1.0 / D, bias=P["eps"][:, 0:1]), reads=["rstd2", "eps"], writes=["rstd2"])
        fw.op("dve", lambda e: e.reciprocal(out=rstd[:, 0:1], in_=rstd[:, 0:1]), reads=["rstd2"], writes=["rstd2"])
        for n in range(2):
            fw.op("dve", lambda e, n=n: e.scalar_tensor_tensor(out=tmp[:, n * 512:(n + 1) * 512], in0=ps[psb[n]][:, :], scalar=rstd[:, 0:1],
                                                               in1=P["GP"][:, gp_idx, n * 512:(n + 1) * 512], op0=ALU.mult, op1=ALU.mult),
                  reads=["ps%d" % psb[n], "rstd2", "GP"], writes=["tmp"])
        fw.op("pool", lambda e: e.tensor_tensor(out=xsub, in0=xsub, in1=tmp, op=ALU.add), reads=["tmp", xkey], writes=[xkey])

    def mk_scr(self):
        ar = self.ar
        return {"junk": ar.alloc([D], BF16), "ss": ar.alloc([2], F32), "rstd": ar.alloc([2], F32), "xn": ar.alloc([D], BF16),
                "tmp": ar.alloc([D], F32), "ss2": ar.alloc([2], F32), "rstd2": ar.alloc([2], F32)}

    def phase_mixer0(self):
        nc, fw, P, I, S, ar, ps = self.nc, self.fw, self.P, self.I, self.S, self.ar, self.ps
        fw.barrier()
        ar.reset()
        NT = 512
        w_in = ar.alloc([8, 2048], BF16)
        w_out = ar.alloc([8, D], BF16)
        pool_w = ar.alloc([4, 128], BF16)
        convw = ar.alloc([4, 3], F32)
        poolsc = ar.alloc([4], F32)
        xt = [ar.alloc([4, D], F32) for _ in range(2)]
        hT = ar.alloc([8, NT], BF16)
        hT2 = ar.alloc([8, NT], BF16)
        cg = ar.alloc([NT], F32)
        cv = ar.alloc([4, 2 + NT], F32)
        t1 = ar.alloc([NT], F32)
        t2 = ar.alloc([NT], F32)
        up = ar.alloc([4, 16 + NT], F32)
        sA = ar.alloc([16 + NT], F32)
        sB = ar.alloc([16 + NT], F32)
        pg = ar.alloc([NT], BF16)
        t16 = ar.alloc([16], F32)
        ycat = ar.alloc([8, NT], BF16)
        scr = self.mk_scr()
        for kc in range(8):
            fw.dma("pool", w_in[:, kc, :], I["mix_w_in"][kc * 128:(kc + 1) * 128, :], writes=["w_in"])
        fw.dma("pool", w_out, I["mix_w_out"].rearrange("(kc p) n -> p kc n", p=128), writes=["w_out"])
        fw.dma("pool", pool_w, I["pool_w"].rearrange("g c d -> c g d"), writes=["pool_w"])
        fw.dma("sp", convw, I["conv_wT"], writes=["convw"])
        fw.dma("sp", poolsc, I["pool_scT"], writes=["poolsc"])
        fw.op("pool", lambda e: e.memset(cv, 0.0), writes=["cv%d" % j for j in range(4)])
        fw.op("pool", lambda e: e.memset(up, 0.0), writes=["up%d" % g for g in range(4)])
        wins = (2, 4, 8, 16)
        ntiles = SEQ // NT
        for ti in range(ntiles):
            xb = xt[ti % 2]
            xk = "xt%d" % (ti % 2)
            fw.dma("sp", xb, I["x8"][ti * NT:(ti + 1) * NT, :].rearrange("(s p) d -> p s d", p=128), writes=[xk])
            for s in range(4):
                self.prenorm_T(xb[:, s, :], xk, 0, 0, hT, "hT", s * 128, s % 2, scr)
            if ti == ntiles // 2:
                fw.op("pool", lambda e: e.tensor_scalar(out=cv[:, :, 0:2], in0=cv[:, :, 0:2], scalar1=P["flag"][:, 0:1], scalar2=None, op0=ALU.mult),
                      reads=["flag"] + ["cv%d" % j for j in range(4)], writes=["cv%d" % j for j in range(4)])
                fw.op("pool", lambda e: e.tensor_scalar(out=up[:, :, 0:16], in0=up[:, :, 0:16], scalar1=P["flag"][:, 0:1], scalar2=None, op0=ALU.mult),
                      reads=["flag"] + ["up%d" % g for g in range(4)], writes=["up%d" % g for g in range(4)])
            bankrr = [2, 3, 4, 5]
            bi = [0]

            def zchunk(fc):
                b = bankrr[bi[0] % 4]
                bi[0] += 1
                for kc in range(8):
                    fw.op("pe", lambda e, kc=kc, b=b: e.matmul(ps[b][:, :], w_in[:, kc, fc * 128:(fc + 1) * 128], hT[:, kc, :], start=(kc == 0), stop=(kc == 7)),
                          reads=["w_in", "hT"], writes=["ps%d" % b])
                return b
            for j in range(4):
                cvk = "cv%d" % j
                b = zchunk(4 + j)
                fw.op("act", lambda e, b=b: e.activation(out=cg, in_=ps[b][:, :], func=AF.Copy), reads=["ps%d" % b], writes=["cg"])
                b = zchunk(8 + j)
                fw.op("dve", lambda e, b=b, j=j: e.tensor_tensor(out=cv[:, j, 2:2 + NT], in0=ps[b][:, :], in1=cg, op=ALU.mult), reads=["ps%d" % b, "cg"], writes=[cvk])
                fw.op("pool", lambda e, j=j: e.tensor_scalar(out=t1, in0=cv[:, j, 0:NT], scalar1=convw[:, j, 0:1], scalar2=None, op0=ALU.mult), reads=[cvk, "convw"], writes=["t1"])
                fw.op("dve", lambda e, j=j: e.scalar_tensor_tensor(out=t2, in0=cv[:, j, 1:1 + NT], scalar=convw[:, j, 1:2], in1=t1, op0=ALU.mult, op1=ALU.add), reads=[cvk, "convw", "t1"], writes=["t2"])
                fw.op("dve", lambda e, j=j: e.scalar_tensor_tensor(out=t1, in0=cv[:, j, 2:2 + NT], scalar=convw[:, j, 2:3], in1=t2, op0=ALU.mult, op1=ALU.add), reads=[cvk, "convw", "t2"], writes=["t1"])
                b = zchunk(j)
                fw.op("dve", lambda e, b=b, j=j: e.tensor_tensor(out=ycat[:, j, :], in0=ps[b][:, :], in1=t1, op=ALU.mult), reads=["ps%d" % b, "t1"], writes=["ycat"])
                fw.op("pool", lambda e, j=j: e.tensor_copy(out=cv[:, j, 0:2], in_=cv[:, j, NT:NT + 2]), reads=[cvk], writes=[cvk])
            for g in range(4):
                upk = "up%d" % g
                b = zchunk(12 + g)
                fw.op("act", lambda e, b=b, g=g: e.activation(out=up[:, g, 16:16 + NT], in_=ps[b][:, :], func=AF.Copy), reads=["ps%d" % b], writes=[upk])
                W = 16 + NT
                cur = up[:, g, :]
                curk = upk
                bufs = [(sA, "sA"), (sB, "sB")]
                d = 1
                k = 0
                while d < wins[g]:
                    dst, dk = bufs[k % 2]
                    fw.op("pool", lambda e, cur=cur, dst=dst, d=d: e.tensor_tensor(out=dst[:, d:W], in0=cur[:, d:W], in1=cur[:, 0:W - d], op=ALU.add),
                          reads=[curk], writes=[dk])
                    cur, curk = dst, dk
                    d *= 2
                    k += 1
                fw.op("dve", lambda e, cur=cur, g=g: e.scalar_tensor_tensor(out=pg, in0=cur[:, 16:W], scalar=1.0 / wins[g], in1=up[:, g, 16:W], op0=ALU.mult, op1=ALU.subtract),
                      reads=[curk, upk], writes=["pg"])
                if ti == 0 or ti == ntiles // 2:
                    which = 0 if ti == 0 else 1
                    fw.op("pool", lambda e, cur=cur, g=g, which=which: e.tensor_tensor(out=t16, in0=cur[:, 16:32], in1=P["invc"][:, which, g, :], op=ALU.mult),
                          reads=[curk, "invc"], writes=["t16"])
                    fw.op("pool", lambda e, g=g: e.tensor_tensor(out=pg[:, 0:16], in0=t16, in1=up[:, g, 16:32], op=ALU.subtract),
                          reads=["t16", upk], writes=["pg"])
                b2 = bankrr[bi[0] % 4]
                bi[0] += 1
                fw.op("pe", lambda e, g=g, b2=b2: e.matmul(ps[b2][:, :], pool_w[:, g, :], pg, start=True, stop=True), reads=["pool_w", "pg"], writes=["ps%d" % b2])
                fw.op("act", lambda e, g=g, b2=b2: e.activation(out=ycat[:, 4 + g, :], in_=ps[b2][:, :], func=AF.Copy, scale=poolsc[:, g:g + 1]),
                      reads=["ps%d" % b2, "poolsc"], writes=["ycat"])
                fw.op("pool", lambda e, g=g: e.tensor_copy(out=up[:, g, 0:16], in_=up[:, g, NT:NT + 16]), reads=[upk], writes=[upk])
            for s in range(4):
                for n in range(2):
                    b = 6 + n
                    for kc in range(8):
                        fw.op("pe", lambda e, kc=kc, b=b, s=s, n=n: e.matmul(ps[b][:, :], ycat[:, kc, s * 128:(s + 1) * 128], w_out[:, kc, n * 512:(n + 1) * 512], start=(kc == 0), stop=(kc == 7)),
                              reads=["ycat", "w_out"], writes=["ps%d" % b])
                self.postnorm_res((6, 7), xb[:, s, :], xk, 0, scr)
                self.prenorm_T(xb[:, s, :], xk, 0, 1, hT2, "hT2", s * 128, s % 2, scr)
            fw.dma("sp", S["x1"][ti * NT:(ti + 1) * NT, :].rearrange("(s p) d -> p s d", p=128), xb, reads=[xk])
            for hh in range(2):
                fw.dma("sp", S["hTf0"][ti * 2 + hh], hT2[:, :, hh * 256:(hh + 1) * 256], reads=["hT2"])

    def ffn_passes(self, wg_d, wu_d, wd_d, ntiles, hT_d, acc_d, comb_d, experts):
        nc, fw, P, ar, ps = self.nc, self.fw, self.P, self.ar, self.ps
        FH = FF // 2
        NT = 256
        wg = [ar.alloc([8, FH], BF16) for _ in range(2)]
        wu = [ar.alloc([8, FH], BF16) for _ in range(2)]
        wd = [ar.alloc([11, D], BF16) for _ in range(2)]
        hTt = [ar.alloc([8, NT], BF16) for _ in range(2)]
        zt = [ar.alloc([11, NT], BF16) for _ in range(2)]
        acct = [ar.alloc([2, D], F32) for _ in range(2)]
        sg = [ar.alloc([NT], F32) for _ in range(2)]
        combt = [ar.alloc([2, 8], F32) for _ in range(2)]
        npass = 0
        cnt = 0
        for ei, ex in enumerate(experts):
            for hf in range(2):
                wi = npass % 2
                f0 = hf * FH
                if comb_d is None:
                    wgd, wud, wdd = wg_d, wu_d, wd_d
                else:
                    wgd, wud, wdd = wg_d[ex], wu_d[ex], wd_d[ex]
                for kc in range(8):
                    fw.dma("pool", wg[wi][:, kc, :], wgd[kc * 128:(kc + 1) * 128, f0:f0 + FH], writes=["wg%d" % wi])
                    fw.dma("pool", wu[wi][:, kc, :], wud[kc * 128:(kc + 1) * 128, f0:f0 + FH], writes=["wu%d" % wi])
                fw.dma("pool", wd[wi], wdd[f0:f0 + FH, :].rearrange("(fc p) d -> p fc d", p=128), writes=["wd%d" % wi])
                first = (npass == 0)
                for t in range(ntiles):
                    bi = cnt % 2
                    cnt += 1
                    hk, zk, ak, ck = "hTt%d" % bi, "zt%d" % bi, "acct%d" % bi, "combt%d" % bi
                    fw.dma("sp", hTt[bi], hT_d[t], writes=[hk])
                    if not first:
                        fw.dma("sp", acct[bi], acc_d[t * NT:(t + 1) * NT, :].rearrange("(s p) d -> p s d", p=128), reads=["accd%d" % t], writes=[ak])
                    if comb_d is not None:
                        fw.dma("sp", combt[bi], comb_d[t * NT:(t + 1) * NT, :].rearrange("(s p) e -> p s e", p=128), writes=[ck])
                    for fc in range(11):
                        bg = (fc % 2) * 2
                        bu = bg + 1
                        for kc in range(8):
                            fw.op("pe", lambda e, kc=kc, fc=fc, bg=bg, wi=wi, bi=bi: e.matmul(ps[bg][:, 0:NT], wg[wi][:, kc, fc * 128:(fc + 1) * 128], hTt[bi][:, kc, :], start=(kc == 0), stop=(kc == 7)),
                                  reads=["wg%d" % wi, hk], writes=["ps%d" % bg])
                        for kc in range(8):
                            fw.op("pe", lambda e, kc=kc, fc=fc, bu=bu, wi=wi, bi=bi: e.matmul(ps[bu][:, 0:NT], wu[wi][:, kc, fc * 128:(fc + 1) * 128], hTt[bi][:, kc, :], start=(kc == 0), stop=(kc == 7)),
                                  reads=["wu%d" % wi, hk], writes=["ps%d" % bu])
                        sgb = sg[fc % 2]
                        sgk = "sg%d" % (fc % 2)
                        fw.op("act", lambda e, bg=bg, sgb=sgb: e.activation(out=sgb, in_=ps[bg][:, 0:NT], func=AF.Silu), reads=["ps%d" % bg], writes=[sgk])
                        fw.op("dve", lambda e, bu=bu, sgb=sgb, fc=fc, bi=bi: e.tensor_tensor(out=zt[bi][:, fc, :], in0=ps[bu][:, 0:NT], in1=sgb, op=ALU.mult),
                              reads=["ps%d" % bu, sgk], writes=[zk])
                    for s in range(2):
                        for n in range(2):
                            bo = 4 + (s * 2 + n)
                            for fc in range(11):
                                fw.op("pe", lambda e, fc=fc, bo=bo, s=s, n=n, wi=wi, bi=bi: e.matmul(ps[bo][:, :], zt[bi][:, fc, s * 128:(s + 1) * 128], wd[wi][:, fc, n * 512:(n + 1) * 512], start=(fc == 0), stop=(fc == 10)),
                                      reads=[zk, "wd%d" % wi], writes=["ps%d" % bo])
                            dst = acct[bi][:, s, n * 512:(n + 1) * 512]
                            if comb_d is None:
                                if first:
                                    fw.op("act", lambda e, bo=bo, dst=dst: e.activation(out=dst, in_=ps[bo][:, :], func=AF.Copy), reads=["ps%d" % bo], writes=[ak])
                                else:
                                    fw.op("dve", lambda e, bo=bo, dst=dst: e.tensor_tensor(out=dst, in0=ps[bo][:, :], in1=dst, op=ALU.add), reads=["ps%d" % bo, ak], writes=[ak])
                            else:
                                cs = combt[bi][:, s, ex:ex + 1]
                                if first:
                                    fw.op("act", lambda e, bo=bo, dst=dst, cs=cs: e.activation(out=dst, in_=ps[bo][:, :], func=AF.Copy, scale=cs), reads=["ps%d" % bo, ck], writes=[ak])
                                else:
                                    fw.op("dve", lambda e, bo=bo, dst=dst, cs=cs: e.scalar_tensor_tensor(out=dst, in0=ps[bo][:, :], scalar=cs, in1=dst, op0=ALU.mult, op1=ALU.add),
                                          reads=["ps%d" % bo, ak, ck], writes=[ak])
                    fw.dma("sp", acc_d[t * NT:(t + 1) * NT, :].rearrange("(s p) d -> p s d", p=128), acct[bi], reads=[ak], writes=["accd%d" % t])
                npass += 1

    def phase_post0(self):
        nc, fw, P, S, ar, ps = self.nc, self.fw, self.P, self.S, self.ar, self.ps
        NT = 512
        xt = [ar.alloc([4, D], F32) for _ in range(2)]
        at = [ar.alloc([4, D], F32) for _ in range(2)]
        scr = self.mk_scr()
        for ti in range(SEQ // NT):
            xb, ab = xt[ti % 2], at[ti % 2]
            xk, akk = "xt%d" % (ti % 2), "at%d" % (ti % 2)
            fw.dma("sp", xb, S["x1"][ti * NT:(ti + 1) * NT, :].rearrange("(s p) d -> p s d", p=128), writes=[xk])
            fw.dma("sp", ab, S["acc0"][ti * NT:(ti + 1) * NT, :].rearrange("(s p) d -> p s d", p=128), writes=[akk])
            for s in range(4):
                self.postnorm_sb(ab[:, s, :], akk, xb[:, s, :], xk, 1, scr)
            fw.dma("sp", S["x2"][ti * NT:(ti + 1) * NT, :].rearrange("(s p) d -> p s d", p=128), xb, reads=[xk])

    def phase_final(self):
        nc, fw, P, S, ar, ps = self.nc, self.fw, self.P, self.S, self.ar, self.ps
        NT = 512
        xt = [ar.alloc([4, D], F32) for _ in range(2)]
        at = [ar.alloc([4, D], F32) for _ in range(2)]
        scr = self.mk_scr()
        for ti in range(HALF // NT):
            xb, ab = xt[ti % 2], at[ti % 2]
            xk, akk = "xt%d" % (ti % 2), "at%d" % (ti % 2)
            fw.dma("sp", xb, S["x3"][ti * NT:(ti + 1) * NT, :].rearrange("(s p) d -> p s d", p=128), writes=[xk])
            fw.dma("sp", ab, S["acc1"][ti * NT:(ti + 1) * NT, :].rearrange("(s p) d -> p s d", p=128), writes=[akk])
            for s in range(4):
                self.postnorm_sb(ab[:, s, :], akk, xb[:, s, :], xk, 3, scr)
            fw.dma("sp", S["y"][ti * NT:(ti + 1) * NT, :].rearrange("(s p) d -> p s d", p=128), xb, reads=[xk])

    def postnorm_sb(self, y, ykey, xsub, xkey, gp_idx, scr):
        fw, P = self.fw, self.P
        junk, ss, rstd, tmp = scr["junk"], scr["ss2"], scr["rstd2"], scr["tmp"]
        fw.op("act", lambda e: e.activation(out=junk, in_=y, func=AF.Square, accum_out=ss[:, 0:1]), reads=[ykey], writes=["junk", "ss2"])
        fw.op("act", lambda e: e.activation(out=rstd[:, 0:1], in_=ss[:, 0:1], func=AF.Sqrt, scale=1.0 / D, bias=P["eps"][:, 0:1]), reads=["ss2", "eps"], writes=["rstd2"])
        fw.op("dve", lambda e: e.reciprocal(out=rstd[:, 0:1], in_=rstd[:, 0:1]), reads=["rstd2"], writes=["rstd2"])
        fw.op("dve", lambda e: e.scalar_tensor_tensor(out=tmp, in0=y, scalar=rstd[:, 0:1], in1=P["GP"][:, gp_idx, :], op0=ALU.mult, op1=ALU.mult),
              reads=[ykey, "rstd2", "GP"], writes=["tmp"])
        fw.op("pool", lambda e: e.tensor_tensor(out=xsub, in0=xsub, in1=tmp, op=ALU.add), reads=["tmp", xkey], writes=[xkey])


def _fm(v, nch):
    return np.ascontiguousarray(np.asarray(v, np.float32).reshape(nch, 128).T)


def make_in_maps(inp):
    x = np.asarray(inp["x"], np.float32)
    c = np.asarray(inp["c"], np.float32)
    maps = []
    wins = (2, 4, 8, 16)
    pos = np.arange(16)
    invc_start = np.stack([1.0 / np.minimum(pos + 1, w) for w in wins]).astype(np.float32)
    invc_mid = np.stack([np.full(16, 1.0 / w) for w in wins]).astype(np.float32)
    common = {
        "ident": np.eye(128, dtype=np.float32),
        "ada_w": np.ascontiguousarray(inp["ada_w"], np.float32),
        "ada_bB": np.ascontiguousarray(np.broadcast_to(np.asarray(inp["ada_b"], np.float32)[:, None, :], (2, 128, 6 * D))),
        "norm_gB": np.ascontiguousarray(np.broadcast_to(np.asarray(inp["norm_g"], np.float32)[:, None, :, :], (2, 128, 4, D))),
        "mix_w_in": np.ascontiguousarray(inp["mix_w_in"][0], np.float32),
        "mix_w_out": np.ascontiguousarray(inp["mix_w_out"][0], np.float32),
        "conv_wT": np.ascontiguousarray(np.asarray(inp["conv_w"][0], np.float32).reshape(3, 4, 128).transpose(2, 1, 0)),
        "pool_w": np.ascontiguousarray(inp["pool_w"][0], np.float32),
        "pool_scT": _fm(inp["pool_scale"][0], 4),
        "ffn_wg": np.ascontiguousarray(inp["ffn_w_gate"][0], np.float32),
        "ffn_wu": np.ascontiguousarray(inp["ffn_w_up"][0], np.float32),
        "ffn_wd": np.ascontiguousarray(inp["ffn_w_down"][0], np.float32),
    }
    f32 = lambda a: np.ascontiguousarray(a, np.float32)
    common.update({
        "rw_wr": f32(inp["rwkv_w_r"][0]), "rw_wk": f32(inp["rwkv_w_k"][0]), "rw_wv": f32(inp["rwkv_w_v"][0]), "rw_wo": f32(inp["rwkv_w_o"][0]),
        "rw_w1": f32(inp["rwkv_w1"][0]), "rw_a1": f32(inp["rwkv_a1"][0]), "rw_g1": f32(inp["rwkv_g1"][0]),
        "rw_w2": f32(inp["rwkv_w2"][0]), "rw_a2": f32(inp["rwkv_a2"][0]), "rw_g2": f32(inp["rwkv_g2"][0]),
        "moe_router": f32(inp["moe_router"][0]),
        "moe_wg": f32(inp["moe_w_gate"][0]), "moe_wu": f32(inp["moe_w_up"][0]), "moe_wd": f32(inp["moe_w_down"][0]),
    })
    mu = np.asarray(inp["rwkv_mu"][0], np.float32)
    vecs = [mu[i] for i in range(6)] + [inp["rwkv_w0"][0], inp["rwkv_a0"][0], inp["rwkv_k_k"][0], inp["rwkv_k_a"][0],
                                        np.asarray(inp["rwkv_r_k"][0]).reshape(-1), inp["rwkv_ln_g"][0], inp["rwkv_ln_b"][0]]
    common["rw_vec"] = np.ascontiguousarray(np.stack([_fm(v, 8) for v in vecs], axis=1))
    t = np.arange(1024)
    common["resetm"] = np.ascontiguousarray(np.broadcast_to((t % 64 != 0).astype(np.float32)[None], (128, 1024)))
    si = np.arange(128)[:, None]
    tj = np.arange(128)[None, :]
    same = (si // 64) == (tj // 64)
    common["mask1"] = np.ascontiguousarray(np.stack([(same & (si < tj)), (same & (si <= tj))], axis=1).astype(np.float32))
    common["maskT"] = np.ascontiguousarray((same & (si > tj)).astype(np.float32))
    common["bdones"] = np.ascontiguousarray(same.astype(np.float32))
    for core in range(8):
        b, h = core // 2, core % 2
        m = dict(common)
        m["x8"] = np.ascontiguousarray(np.concatenate([x[b, :HALF], x[b, h * HALF:(h + 1) * HALF]], axis=0))
        m["condB"] = np.ascontiguousarray(np.broadcast_to(c[b].reshape(8, 128).T[:, :, None], (128, 8, 128)))
        m["flag"] = np.full((128, 1), float(h), np.float32)
        m["invc"] = np.ascontiguousarray(np.broadcast_to(np.stack([invc_start, invc_start if h == 0 else invc_mid])[None], (128, 2, 4, 16)))
        maps.append(m)
    return maps


_CACHE = {}


def kernel(**inputs):
    if "nc" not in _CACHE:
        _CACHE["nc"] = Builder().build()
    nc = _CACHE["nc"]
    maps = make_in_maps(inputs)
    res = run_bass_kernel_spmd(nc, maps, core_ids=list(range(8)))
    out = np.zeros((4, SEQ, D), np.float32)
    for core in range(8):
        b, h = core // 2, core % 2
        out[b, h * HALF:(h + 1) * HALF] = res.results[core]["y"]
    return out


CDEC = 0.6065306597126334


def _phase_rwkv(self):
    nc, fw, P, I, S, ar, ps = self.nc, self.fw, self.P, self.I, self.S, self.ar, self.ps
    fw.barrier()
    ar.reset()
    op = fw.op
    W = {}
    for nm in ("rw_wr", "rw_wk", "rw_wv", "rw_wo"):
        W[nm] = ar.alloc([8, D], BF16)
        for kc in range(8):
            fw.dma("pool", W[nm][:, kc, :], I[nm][kc * 128:(kc + 1) * 128, :], writes=[nm])
    w1 = ar.alloc([8, 64], BF16)
    a1 = ar.alloc([8, 64], BF16)
    g1 = ar.alloc([8, 160], BF16)
    fw.dma("pool", w1, I["rw_w1"].rearrange("(kc p) n -> p kc n", p=128), writes=["w1"])
    fw.dma("pool", a1, I["rw_a1"].rearrange("(kc p) n -> p kc n", p=128), writes=["a1"])
    fw.dma("pool", g1, I["rw_g1"].rearrange("(kc p) n -> p kc n", p=128), writes=["g1"])
    w2 = ar.alloc([D], BF16)
    a2 = ar.alloc([D], BF16)
    g2a = ar.alloc([D], BF16)
    g2b = ar.alloc([D], BF16)
    fw.dma("pool", w2[0:64, :], I["rw_w2"], writes=["w2"])
    fw.dma("pool", a2[0:64, :], I["rw_a2"], writes=["a2"])
    fw.dma("pool", g2a, I["rw_g2"][0:128, :], writes=["g2a"])
    fw.dma("pool", g2b[0:32, :], I["rw_g2"][128:160, :], writes=["g2b"])
    vec = ar.alloc([14, 8], F32)
    fw.dma("sp", vec[:, 0:13, :], I["rw_vec"], writes=["vec"])
    op("pool", lambda e: e.tensor_scalar(out=vec[:, 13, :], in0=vec[:, 9, :], scalar1=-1.0, scalar2=1.0, op0=ALU.mult, op1=ALU.add), reads=["vec"], writes=["vec"])
    router = ar.alloc([8, 8], F32)
    fw.dma("sp", router, I["moe_router"].rearrange("(kc p) e -> p kc e", p=128), writes=["router"])
    resetm = ar.alloc([1024], F32)
    mask1 = ar.alloc([2, 128], F32)
    maskT = ar.alloc([128], F32)
    bdones = ar.alloc([128], BF16)
    fw.dma("sp", resetm, I["resetm"], writes=["resetm"])
    fw.dma("sp", mask1, I["mask1"], writes=["mask1"])
    fw.dma("sp", maskT, I["maskT"], writes=["maskT"])
    fw.dma("pool", bdones, I["bdones"], writes=["bdones"])

    def vb(i):
        return bc(vec[:, i, :].unsqueeze(2), [128, 8, 128])

    xt = ar.alloc([D], F32)
    at = ar.alloc([D], F32)
    hT = ar.alloc([8, 129], BF16)
    xx = ar.alloc([8, 128], BF16)
    xi = [ar.alloc([8, 128], BF16) for _ in range(2)]
    rS = ar.alloc([8, 128], BF16)
    vS = ar.alloc([8, 128], BF16)
    gS = ar.alloc([8, 128], BF16)
    tw = ar.alloc([128], BF16)
    ta = ar.alloc([128], BF16)
    tg = ar.alloc([128], BF16)
    tg2 = ar.alloc([128], BF16)
    Tf = [ar.alloc([1024], F32) for _ in range(8)]
    T3 = [t.rearrange("p (c t) -> p c t", t=128) for t in Tf]
    PR = ar.alloc([8, 256], BF16)
    Qt = ar.alloc([8, 128], BF16)
    Kt = ar.alloc([8, 128], BF16)
    Qb = ar.alloc([8, 128], BF16)
    Kb = ar.alloc([8, 128], BF16)
    B0 = ar.alloc([8, 128], BF16)
    bonus = ar.alloc([8, 128], BF16)
    yg = ar.alloc([8, 128], BF16)
    GC = ar.alloc([16], F32)
    Ysb = ar.alloc([8, 128], F32)
    Sbd = ar.alloc([8, 128], BF16)
    RHS = ar.alloc([2, 128], BF16)
    SPLA = ar.alloc([3, 128], BF16)
    SPLB = ar.alloc([3, 128], BF16)
    SP2A = ar.alloc([2, 128], BF16)
    SP2B = ar.alloc([2, 128], BF16)
    MA1 = ar.alloc([2, 2, 128], BF16)
    MA2 = ar.alloc([2, 2, 128], BF16)
    MT = ar.alloc([2, 128], BF16)
    Xb_ = [ar.alloc([2, 128], BF16) for _ in range(2)]
    XTb_ = [ar.alloc([2, 128], BF16) for _ in range(2)]
    Tb_ = [ar.alloc([2, 128], BF16) for _ in range(2)]
    Rh = ar.alloc([128], BF16)
    Yloc = ar.alloc([128], F32)
    PQ = ar.alloc([2, 128], BF16)
    Sloc = ar.alloc([2, 128], BF16)
    h32 = ar.alloc([8, 128], F32)
    hb = ar.alloc([8, 128], BF16)
    lg = ar.alloc([64], F32)
    scr = {"junk": Tf[6][:, 0:512].bitcast(BF16), "ss": lg[:, 32:34], "rstd": lg[:, 34:36], "xn": Tf[6][:, 512:1024].bitcast(BF16),
           "tmp": Tf[7], "ss2": lg[:, 36:38], "rstd2": lg[:, 38:40]}
    scr_keys = ["T6", "T7"]

    op("pool", lambda e: e.memset(Sbd, 0.0), writes=["Sbd%d" % c for c in range(8)])
    op("pool", lambda e: e.memset(hT, 0.0), writes=["hT"])
    for t_, k_ in ((SPLA, "SPLA"), (SPLB, "SPLB"), (SP2A, "SP2A"), (SP2B, "SP2B")):
        op("pool", lambda e, t_=t_: e.memset(t_, 0.0), writes=[k_])

    bank_rr = [0]

    def nb():
        b = 4 + bank_rr[0] % 4
        bank_rr[0] += 1
        return b

    evac_rr = [0]

    def cp(dst, src, reads, writes):
        evac_rr[0] += 1
        if evac_rr[0] % 2:
            op("act", lambda e: e.activation(out=dst, in_=src, func=AF.Copy), reads=reads, writes=writes)
        else:
            op("dve", lambda e: e.tensor_copy(out=dst, in_=src), reads=reads, writes=writes)

    NTILE = SEQ // 128
    npre, nown = getattr(self, "rw_tiles", (NTILE // 2, NTILE // 2))
    tiles = list(range(npre)) + list(range(NTILE // 2, NTILE // 2 + nown))
    STOP = getattr(self, "rw_stop", 99)
    for ti in tiles:
        own = ti >= NTILE // 2
        fw.dma("sp", xt, S["x1"][ti * 128:(ti + 1) * 128, :], writes=["xt"])
        fw.dma("sp", at, S["acc0"][ti * 128:(ti + 1) * 128, :], writes=["at"])
        self.postnorm_sb2(at, "at", xt, "xt", 1, scr, scr_keys)
        if ti == NTILE // 2:
            op("pool", lambda e: e.tensor_scalar(out=hT[:, :, 0:1], in0=hT[:, :, 0:1], scalar1=P["flag"][:, 0:1], scalar2=None, op0=ALU.mult), reads=["hT", "flag"], writes=["hT"])
            op("pool", lambda e: e.tensor_scalar(out=Sbd, in0=Sbd, scalar1=P["flag"][:, 0:1], scalar2=None, op0=ALU.mult),
               reads=["flag"] + ["Sbd%d" % c for c in range(8)], writes=["Sbd%d" % c for c in range(8)])
        self.prenorm_T2(xt, "xt", 1, 0, hT, "hT", 1, 4, scr, scr_keys)
        op("pool", lambda e: e.tensor_tensor(out=xx, in0=hT[:, :, 0:128], in1=hT[:, :, 1:129], op=ALU.subtract), reads=["hT"], writes=["xx"])
        if STOP <= 1:
            continue

        def variant(i, buf):
            xb_, xk_ = xi[buf], "xi%d" % buf
            op("pool", lambda e: e.tensor_tensor(out=xb_, in0=xx, in1=vb(i), op=ALU.mult), reads=["xx", "vec"], writes=[xk_])
            op("dve", lambda e: e.tensor_tensor(out=xb_, in0=xb_, in1=hT[:, :, 1:129], op=ALU.add), reads=[xk_, "hT"], writes=[xk_])
            return xb_, xk_

        def proj(wname, xb_, xk_, grp):
            b0 = grp * 2
            for cc in range(8):
                b = b0 + cc // 4
                for kc in range(8):
                    op("pe", lambda e, cc=cc, kc=kc, b=b: e.matmul(ps[b][:, (cc % 4) * 128:(cc % 4 + 1) * 128], W[wname][:, kc, cc * 128:(cc + 1) * 128], xb_[:, kc, :], start=(kc == 0), stop=(kc == 7)),
                       reads=[wname, xk_], writes=["ps%d" % b])
            return b0

        def evac2(b0, fn_eng, mk):
            for hh in range(2):
                mk(hh, ps[b0 + hh][:, :].rearrange("p (c t) -> p c t", t=128), "ps%d" % (b0 + hh))

        kS, kSk = T3[5], "T5"
        sgd, sgk = Tf[0], "T0"
        aS, aSk = T3[6], "T6"
        xb_, xk_ = variant(0, 0)
        b0 = proj("rw_wr", xb_, xk_, 0)
        for hh in range(2):
            op("act", lambda e, hh=hh, b0=b0: e.activation(out=rS[:, hh * 4:(hh + 1) * 4, :], in_=ps[b0 + hh][:, :].rearrange("p (c t) -> p c t", t=128), func=AF.Copy), reads=["ps%d" % (b0 + hh)], writes=["rS"])
        xb_, xk_ = variant(2, 1)
        b0 = proj("rw_wk", xb_, xk_, 1)
        for hh in range(2):
            op("act", lambda e, hh=hh, b0=b0: e.activation(out=kS[:, hh * 4:(hh + 1) * 4, :], in_=ps[b0 + hh][:, :].rearrange("p (c t) -> p c t", t=128), func=AF.Copy), reads=["ps%d" % (b0 + hh)], writes=[kSk])
        xb_, xk_ = variant(3, 0)
        b0 = proj("rw_wv", xb_, xk_, 0)
        for hh in range(2):
            op("act", lambda e, hh=hh, b0=b0: e.activation(out=vS[:, hh * 4:(hh + 1) * 4, :], in_=ps[b0 + hh][:, :].rearrange("p (c t) -> p c t", t=128), func=AF.Copy), reads=["ps%d" % (b0 + hh)], writes=["vS"])
        xb_, xk_ = variant(1, 1)
        b = nb()
        for kc in range(8):
            op("pe", lambda e, kc=kc, b=b, xb_=xb_: e.matmul(ps[b][0:64, 0:128], w1[:, kc, :], xb_[:, kc, :], start=(kc == 0), stop=(kc == 7)), reads=["w1", xk_], writes=["ps%d" % b])
        op("act", lambda e, b=b: e.activation(out=tw[0:64, :], in_=ps[b][0:64, 0:128], func=AF.Tanh), reads=["ps%d" % b], writes=["tw"])
        b0 = 2
        for cc in range(8):
            bb = b0 + cc // 4
            op("pe", lambda e, cc=cc, bb=bb: e.matmul(ps[bb][:, (cc % 4) * 128:(cc % 4 + 1) * 128], w2[0:64, cc * 128:(cc + 1) * 128], tw[0:64, :], start=True, stop=True), reads=["w2", "tw"], writes=["ps%d" % bb])
        for hh in range(2):
            op("dve", lambda e, hh=hh: e.tensor_tensor(out=T3[0][:, hh * 4:(hh + 1) * 4, :], in0=ps[2 + hh][:, :].rearrange("p (c t) -> p c t", t=128),
                                                      in1=bc(vec[:, 6, hh * 4:(hh + 1) * 4].unsqueeze(2), [128, 4, 128]), op=ALU.add), reads=["ps%d" % (2 + hh), "vec"], writes=[sgk])
        op("act", lambda e: e.activation(out=sgd, in_=sgd, func=AF.Sigmoid), reads=[sgk], writes=[sgk])
        xb_, xk_ = variant(4, 0)
        b = nb()
        for kc in range(8):
            op("pe", lambda e, kc=kc, b=b, xb_=xb_: e.matmul(ps[b][0:64, 0:128], a1[:, kc, :], xb_[:, kc, :], start=(kc == 0), stop=(kc == 7)), reads=["a1", xk_], writes=["ps%d" % b])
        op("act", lambda e, b=b: e.activation(out=ta[0:64, :], in_=ps[b][0:64, 0:128], func=AF.Copy), reads=["ps%d" % b], writes=["ta"])
        for cc in range(8):
            bb = cc // 4
            op("pe", lambda e, cc=cc, bb=bb: e.matmul(ps[bb][:, (cc % 4) * 128:(cc % 4 + 1) * 128], a2[0:64, cc * 128:(cc + 1) * 128], ta[0:64, :], start=True, stop=True), reads=["a2", "ta"], writes=["ps%d" % bb])
        for hh in range(2):
            op("dve", lambda e, hh=hh: e.tensor_tensor(out=aS[:, hh * 4:(hh + 1) * 4, :], in0=ps[hh][:, :].rearrange("p (c t) -> p c t", t=128),
                                                      in1=bc(vec[:, 7, hh * 4:(hh + 1) * 4].unsqueeze(2), [128, 4, 128]), op=ALU.add), reads=["ps%d" % hh, "vec"], writes=[aSk])
        op("act", lambda e: e.activation(out=Tf[6], in_=Tf[6], func=AF.Sigmoid), reads=[aSk], writes=[aSk])
        if own:
            xb_, xk_ = variant(5, 1)
            b = nb()
            for kc in range(8):
                op("pe", lambda e, kc=kc, b=b, xb_=xb_: e.matmul(ps[b][:, 0:128], g1[:, kc, 0:128], xb_[:, kc, :], start=(kc == 0), stop=(kc == 7)), reads=["g1", xk_], writes=["ps%d" % b])
            for kc in range(8):
                op("pe", lambda e, kc=kc, b=b, xb_=xb_: e.matmul(ps[b][0:32, 128:256], g1[:, kc, 128:160], xb_[:, kc, :], start=(kc == 0), stop=(kc == 7)), reads=["g1", xk_], writes=["ps%d" % b])
            op("act", lambda e, b=b: e.activation(out=tg, in_=ps[b][:, 0:128], func=AF.Sigmoid), reads=["ps%d" % b], writes=["tg"])
            op("act", lambda e, b=b: e.activation(out=tg2[0:32, :], in_=ps[b][0:32, 128:256], func=AF.Sigmoid), reads=["ps%d" % b], writes=["tg2"])
            for cc in range(8):
                bb = 2 + cc // 4
                op("pe", lambda e, cc=cc, bb=bb: e.matmul(ps[bb][:, (cc % 4) * 128:(cc % 4 + 1) * 128], g2a[:, cc * 128:(cc + 1) * 128], tg, start=True, stop=False), reads=["g2a", "tg"], writes=["ps%d" % bb])
                op("pe", lambda e, cc=cc, bb=bb: e.matmul(ps[bb][:, (cc % 4) * 128:(cc % 4 + 1) * 128], g2b[0:32, cc * 128:(cc + 1) * 128], tg2[0:32, :], start=False, stop=True), reads=["g2b", "tg2"], writes=["ps%d" % bb])
            for hh in range(2):
                op("act", lambda e, hh=hh: e.activation(out=gS[:, hh * 4:(hh + 1) * 4, :], in_=ps[2 + hh][:, :].rearrange("p (c t) -> p c t", t=128), func=AF.Copy), reads=["ps%d" % (2 + hh)], writes=["gS"])

        if STOP <= 2:
            continue
        Lp, Lpk = Tf[1], "T1"
        op("dve", lambda e: e.tensor_tensor_scan(out=Lp, data0=resetm, data1=sgd, initial=0.0, op0=ALU.mult, op1=ALU.add), reads=["resetm", sgk], writes=[Lpk])
        Lm, Lmk = Tf[2], "T2"
        op("pool", lambda e: e.tensor_tensor(out=Lm, in0=Lp, in1=sgd, op=ALU.subtract), reads=[Lpk, sgk], writes=[Lmk])
        Ld, Ldk = Tf[0], "T0"
        Lp64 = Lp.rearrange("p (c t) -> p c t", t=64)
        op("pool", lambda e: e.tensor_tensor(out=Ld.rearrange("p (c t) -> p c t", t=64), in0=bc(Lp64[:, :, 63:64], [128, 16, 64]), in1=Lp64, op=ALU.subtract), reads=[Lpk, Lmk], writes=[Ldk])
        E1, E1k = Tf[3], "T3"
        E2, E2k = Tf[4], "T4"
        op("act", lambda e: e.activation(out=E1, in_=Lp, func=AF.Exp, scale=-CDEC), reads=[Lpk], writes=[E1k])
        op("act", lambda e: e.activation(out=E2, in_=Lp, func=AF.Exp, scale=CDEC), reads=[Lpk], writes=[E2k])
        op("act", lambda e: e.activation(out=Lm, in_=Lm, func=AF.Exp, scale=-CDEC), reads=[Lmk], writes=[Lmk])
        op("act", lambda e: e.activation(out=Ld, in_=Ld, func=AF.Exp, scale=-CDEC), reads=[Ldk], writes=[Ldk])
        E3, E3k, E4, E4k = Lm, Lmk, Ld, Ldk
        op("pool", lambda e: e.tensor_copy(out=GC, in_=E1.rearrange("p (c t) -> p c t", t=64)[:, :, 63]), reads=[E1k], writes=["GC"])
        kk, kkk = T3[1], "T1"
        op("pool", lambda e: e.tensor_tensor(out=kk, in0=kS, in1=vb(8), op=ALU.mult), reads=[kSk, "vec", E1k, E2k], writes=[kkk])
        op("pool", lambda e: e.tensor_tensor(out=B0, in0=kk, in1=kk, op=ALU.mult), reads=[kkk], writes=["B0"])
        for cc in range(8):
            bb = cc // 4
            op("pe", lambda e, cc=cc, bb=bb: e.matmul(ps[bb][:, (cc % 4) * 128:(cc % 4 + 1) * 128], bdones, B0[:, cc, :], start=True, stop=True), reads=["bdones", "B0"], writes=["ps%d" % bb])
        rn, rnk = T3[7], "T7"
        for hh in range(2):
            op("act", lambda e, hh=hh: e.activation(out=rn[:, hh * 4:(hh + 1) * 4, :], in_=ps[hh][:, :].rearrange("p (c t) -> p c t", t=128), func=AF.Sqrt), reads=["ps%d" % hh], writes=[rnk])
        op("dve", lambda e: e.reciprocal(out=Tf[7], in_=Tf[7]), reads=[rnk], writes=[rnk])
        op("pool", lambda e: e.tensor_tensor(out=kk, in0=kk, in1=rn, op=ALU.mult), reads=[kkk, rnk], writes=[kkk])
        km, kmk = T3[7], "T7"
        op("pool", lambda e: e.tensor_tensor(out=km, in0=aS, in1=vb(9), op=ALU.mult), reads=[aSk, "vec", kkk], writes=[kmk])
        op("pool", lambda e: e.tensor_tensor(out=km, in0=km, in1=vb(13), op=ALU.add), reads=[kmk, "vec"], writes=[kmk])
        op("dve", lambda e: e.tensor_tensor(out=km, in0=km, in1=kS, op=ALU.mult), reads=[kmk, kSk], writes=[kmk])
        q, qk = aS, aSk
        op("pool", lambda e: e.tensor_tensor(out=q, in0=aS, in1=kk, op=ALU.mult), reads=[aSk, kkk], writes=[qk])
        op("dve", lambda e: e.scalar_tensor_tensor(out=PR[:, :, 0:128], in0=kk, scalar=-1.0, in1=T3[2], op0=ALU.mult, op1=ALU.mult), reads=[kkk, E3k], writes=["PR"])
        if own:
            op("pool", lambda e: e.tensor_tensor(out=PR[:, :, 128:256], in0=rS, in1=T3[3], op=ALU.mult), reads=["rS", E1k], writes=["PR"])
        else:
            op("pool", lambda e: e.tensor_copy(out=PR[:, :, 128:256], in_=rS), reads=["rS"], writes=["PR"])
        op("dve", lambda e: e.tensor_tensor(out=Qt, in0=q, in1=T3[4], op=ALU.mult), reads=[qk, E2k], writes=["Qt"])
        op("pool", lambda e: e.tensor_tensor(out=Kt, in0=km, in1=T3[4], op=ALU.mult), reads=[kmk, E2k], writes=["Kt"])
        op("dve", lambda e: e.tensor_tensor(out=Qb, in0=q, in1=T3[0], op=ALU.mult), reads=[qk, E4k], writes=["Qb"])
        op("pool", lambda e: e.tensor_tensor(out=Kb, in0=km, in1=T3[0], op=ALU.mult), reads=[kmk, E4k], writes=["Kb"])
        if own:
            op("pool", lambda e: e.tensor_tensor(out=T3[5], in0=km, in1=vb(10), op=ALU.mult), reads=[kmk, "vec", kSk], writes=["T5"])
            op("pool", lambda e: e.tensor_tensor(out=B0, in0=T3[5], in1=rS, op=ALU.mult), reads=["T5", "rS"], writes=["B0"])
            for cc in range(8):
                bb = 2 + cc // 4
                op("pe", lambda e, cc=cc, bb=bb: e.matmul(ps[bb][:, (cc % 4) * 128:(cc % 4 + 1) * 128], bdones, B0[:, cc, :], start=True, stop=True), reads=["bdones", "B0"], writes=["ps%d" % bb])
            for hh in range(2):
                op("dve", lambda e, hh=hh: e.tensor_tensor(out=bonus[:, hh * 4:(hh + 1) * 4, :], in0=ps[2 + hh][:, :].rearrange("p (c t) -> p c t", t=128), in1=vS[:, hh * 4:(hh + 1) * 4, :], op=ALU.mult),
                   reads=["ps%d" % (2 + hh), "vS"], writes=["bonus"])

        if STOP <= 3:
            continue
        for cc in range(8):
            sk = "Sbd%d" % cc
            b = nb()
            tp = ps[b][:, :].bitcast(BF16)
            srcs = (PR[:, cc, 0:128], Qb[:, cc, :], Kb[:, cc, :], vS[:, cc, :])
            skeys = ("PR", "Qb", "Kb", "vS")
            for i4 in range(4):
                op("pe", lambda e, i4=i4, tp=tp, srcs=srcs: e.transpose(tp[:, i4 * 128:(i4 + 1) * 128], srcs[i4], P["identb"][:]), reads=[skeys[i4], "identb"], writes=["ps%d" % b])
            tpv = tp[:, 128:512].rearrange("p (i j) -> p i j", j=128)
            op("act", lambda e, tpv=tpv: e.activation(out=SPLA[:, :, 0:64], in_=tpv[:, :, 0:64], func=AF.Copy), reads=["ps%d" % b], writes=["SPLA"])
            op("dve", lambda e, tpv=tpv: e.tensor_copy(out=SPLB[:, :, 64:128], in_=tpv[:, :, 64:128]), reads=["ps%d" % b], writes=["SPLB"])
            op("act", lambda e, tp=tp: e.activation(out=RHS[:, :, 0:64], in_=tp[:, 0:128].rearrange("p (h j) -> p h j", h=2), func=AF.Copy), reads=["ps%d" % b], writes=["RHS"])
            if STOP <= 4.1:
                continue
            bAh = [nb(), nb()]
            bBh = [nb(), nb()]
            for h in range(2):
                R_ = slice(64 * h, 64 * h + 64)
                op("pe", lambda e, h=h, R_=R_: e.matmul(ps[bAh[h]][:, 0:256], Qt[R_, cc, :], PR[R_, cc, :], start=True, stop=True), reads=["Qt", "PR"], writes=["ps%d" % bAh[h]])
                op("pe", lambda e, h=h, R_=R_: e.matmul(ps[bAh[h]][:, 256:384], PR[R_, cc, 0:128], Qt[R_, cc, :], start=True, stop=True), reads=["Qt", "PR"], writes=["ps%d" % bAh[h]])
                op("pe", lambda e, h=h, R_=R_: e.matmul(ps[bBh[h]][:, 0:256], Kt[R_, cc, :], PR[R_, cc, :], start=True, stop=True), reads=["Kt", "PR"], writes=["ps%d" % bBh[h]])
            for h in range(2):
                op("dve", lambda e, h=h: e.tensor_tensor(out=MA1[:, h, :, :], in0=ps[bAh[h]][:, 0:256].rearrange("p (w t) -> p w t", w=2), in1=mask1, op=ALU.mult), reads=["ps%d" % bAh[h], "mask1"], writes=["MA1"])
                op("dve", lambda e, h=h: e.tensor_tensor(out=MT[:, h, :], in0=ps[bAh[h]][:, 256:384], in1=maskT, op=ALU.mult), reads=["ps%d" % bAh[h], "maskT"], writes=["MT"])
                op("dve", lambda e, h=h: e.tensor_tensor(out=MA2[:, h, :, :], in0=ps[bBh[h]][:, 0:256].rearrange("p (w t) -> p w t", w=2), in1=mask1, op=ALU.mult), reads=["ps%d" % bBh[h], "mask1"], writes=["MA2"])
            if STOP <= 4.2:
                continue
            Tc, Tck = Tb_[0], "Tb0"
            op("pool", lambda e, Tc=Tc: e.tensor_tensor(out=Tc, in0=MA1[:, :, 0, :], in1=bc(P["ident"][:].unsqueeze(1), [128, 2, 128]), op=ALU.add), reads=["MA1", "ident"], writes=[Tck])
            Xc = [MA1[:, 0, 0, :], MA1[:, 1, 0, :]]
            Xck = "MA1"
            XTc = [MT[:, 0, :], MT[:, 1, :]]
            XTck = "MT"
            nlev = 5
            for lv in range(nlev):
                last = (lv == nlev - 1)
                Xn, Xnk = Xb_[lv % 2], "Xb%d" % (lv % 2)
                XTn, XTnk = XTb_[lv % 2], "XTb%d" % (lv % 2)
                Tn, Tnk = Tb_[(lv + 1) % 2], "Tb%d" % ((lv + 1) % 2)
                if not last:
                    bX = nb()
                    for h in range(2):
                        op("pe", lambda e, h=h, bX=bX, XTc=XTc, Xc=Xc: e.matmul(ps[bX][:, h * 128:(h + 1) * 128], XTc[h], Xc[h], start=True, stop=True), reads=[Xck, XTck], writes=["ps%d" % bX])
                    cp(Xn, ps[bX][:, 0:256].rearrange("p (h t) -> p h t", h=2), ["ps%d" % bX], [Xnk])
                bXT = nb()
                for h in range(2):
                    op("pe", lambda e, h=h, bXT=bXT, XTc=XTc, Xc=Xc: e.matmul(ps[bXT][:, h * 128:(h + 1) * 128], Xc[h], XTc[h], start=True, stop=True), reads=[Xck, XTck], writes=["ps%d" % bXT])
                cp(XTn, ps[bXT][:, 0:256].rearrange("p (h t) -> p h t", h=2), ["ps%d" % bXT], [XTnk])
                bT = nb()
                for h in range(2):
                    op("pe", lambda e, h=h, bT=bT, XTn=XTn, Tc=Tc: e.matmul(ps[bT][:, h * 128:(h + 1) * 128], XTn[:, h, :], Tc[:, h, :], start=True, stop=True), reads=[XTnk, Tck], writes=["ps%d" % bT])
                op("dve", lambda e, bT=bT, Tn=Tn, Tc=Tc: e.tensor_tensor(out=Tn, in0=ps[bT][:, 0:256].rearrange("p (h t) -> p h t", h=2), in1=Tc, op=ALU.add), reads=["ps%d" % bT, Tck], writes=[Tnk])
                Tc, Tck = Tn, Tnk
                if not last:
                    Xc, Xck = [Xn[:, 0, :], Xn[:, 1, :]], Xnk
                XTc, XTck = [XTn[:, 0, :], XTn[:, 1, :]], XTnk
            if STOP <= 4.3:
                continue
            bW = nb()
            op("pe", lambda e, bW=bW: e.matmul(ps[bW][:, 0:64], MA2[:, 0, 0, :], SPLA[:, 2, 0:64], start=True, stop=True), reads=["MA2", "SPLA"], writes=["ps%d" % bW])
            op("pe", lambda e, bW=bW: e.matmul(ps[bW][:, 64:128], MA2[:, 1, 0, :], SPLB[:, 2, 64:128], start=True, stop=True), reads=["MA2", "SPLB"], writes=["ps%d" % bW])
            cp(RHS[:, :, 64:128], ps[bW][:, 0:128].rearrange("p (h j) -> p h j", h=2), ["ps%d" % bW], ["RHS"])
            bU = nb()
            for h in range(2):
                op("pe", lambda e, h=h, bU=bU, Tc=Tc: e.matmul(ps[bU][:, h * 128:(h + 1) * 128], Tc[:, h, :], RHS[:, h, :], start=True, stop=True), reads=[Tck, "RHS"], writes=["ps%d" % bU])
            op("act", lambda e, bU=bU: e.activation(out=SP2A[:, :, 0:64], in_=ps[bU][:, 0:128].rearrange("p (w j) -> p w j", w=2), func=AF.Copy), reads=["ps%d" % bU], writes=["SP2A"])
            op("dve", lambda e, bU=bU: e.tensor_copy(out=SP2B[:, :, 64:128], in_=ps[bU][:, 128:256].rearrange("p (w j) -> p w j", w=2)), reads=["ps%d" % bU], writes=["SP2B"])
            if STOP <= 4.5:
                continue
            if own:
                bR = nb()
                op("pe", lambda e, bR=bR: e.matmul(ps[bR][:, 0:128], SP2A[:, 0, :], MA1[:, 0, 1, :], start=True, stop=False), reads=["SP2A", "MA1"], writes=["ps%d" % bR])
                op("pe", lambda e, bR=bR: e.matmul(ps[bR][:, 0:128], SP2B[:, 0, :], MA1[:, 1, 1, :], start=False, stop=True), reads=["SP2B", "MA1"], writes=["ps%d" % bR])
                op("dve", lambda e, bR=bR: e.tensor_tensor(out=Rh, in0=ps[bR][:, 0:128], in1=PR[:, cc, 128:256], op=ALU.add), reads=["ps%d" % bR, "PR"], writes=["Rh"])
                bY = nb()
                op("pe", lambda e, bY=bY: e.matmul(ps[bY][:, 0:128], SP2A[:, 1, :], MA1[:, 0, 1, :], start=True, stop=False), reads=["SP2A", "MA1"], writes=["ps%d" % bY])
                op("pe", lambda e, bY=bY: e.matmul(ps[bY][:, 0:128], SP2B[:, 1, :], MA1[:, 1, 1, :], start=False, stop=False), reads=["SP2B", "MA1"], writes=["ps%d" % bY])
                op("pe", lambda e, bY=bY: e.matmul(ps[bY][:, 0:128], SPLA[:, 2, :], MA2[:, 0, 1, :], start=False, stop=False), reads=["SPLA", "MA2"], writes=["ps%d" % bY])
                op("pe", lambda e, bY=bY: e.matmul(ps[bY][:, 0:128], SPLB[:, 2, :], MA2[:, 1, 1, :], start=False, stop=True), reads=["SPLB", "MA2"], writes=["ps%d" % bY])
                cp(Yloc, ps[bY][:, 0:128], ["ps%d" % bY], ["Yloc"])
            bQc = [nb(), nb()]
            for c in range(2):
                R_ = slice(64 * c, 64 * c + 64)
                bq = bQc[c]
                op("pe", lambda e, R_=R_, bq=bq: e.matmul(ps[bq][:, 0:128], SP2A[R_, 0, :], SPLA[R_, 0, :], start=True, stop=False), reads=["SP2A", "SPLA"], writes=["ps%d" % bq])
                op("pe", lambda e, R_=R_, bq=bq: e.matmul(ps[bq][:, 0:128], SP2B[R_, 0, :], SPLB[R_, 0, :], start=False, stop=True), reads=["SP2B", "SPLB"], writes=["ps%d" % bq])
                op("pe", lambda e, R_=R_, bq=bq: e.matmul(ps[bq][:, 128:256], SPLA[R_, 0, :], SP2A[R_, 1, :], start=True, stop=False), reads=["SP2A", "SPLA"], writes=["ps%d" % bq])
                op("pe", lambda e, R_=R_, bq=bq: e.matmul(ps[bq][:, 128:256], SPLB[R_, 0, :], SP2B[R_, 1, :], start=False, stop=False), reads=["SP2B", "SPLB"], writes=["ps%d" % bq])
                op("pe", lambda e, R_=R_, bq=bq: e.matmul(ps[bq][:, 128:256], SPLA[R_, 1, :], SPLA[R_, 2, :], start=False, stop=False), reads=["SPLA"], writes=["ps%d" % bq])
                op("pe", lambda e, R_=R_, bq=bq: e.matmul(ps[bq][:, 128:256], SPLB[R_, 1, :], SPLB[R_, 2, :], start=False, stop=True), reads=["SPLB"], writes=["ps%d" % bq])
            for c in range(2):
                cp(PQ[:, c, :], ps[bQc[c]][:, 0:128], ["ps%d" % bQc[c]], ["PQ"])
                cp(Sloc[:, c, :], ps[bQc[c]][:, 128:256], ["ps%d" % bQc[c]], ["Sloc"])
            if STOP <= 4.8:
                continue
            for c in range(2):
                if own:
                    bYc = nb()
                    op("pe", lambda e, c=c, bYc=bYc: e.matmul(ps[bYc][:, 0:64], Sbd[:, cc, :], Rh[:, c * 64:(c + 1) * 64], start=True, stop=True), reads=[sk, "Rh"], writes=["ps%d" % bYc])
                    op("dve", lambda e, c=c, bYc=bYc: e.tensor_tensor(out=Ysb[:, cc, c * 64:(c + 1) * 64], in0=ps[bYc][:, 0:64], in1=Yloc[:, c * 64:(c + 1) * 64], op=ALU.add), reads=["ps%d" % bYc, "Yloc"], writes=["Ysb"])
                bS2 = nb()
                op("pe", lambda e, c=c, bS2=bS2: e.matmul(ps[bS2][:, 0:128], PQ[:, c, :], Sbd[:, cc, :], start=True, stop=False), reads=["PQ", sk], writes=["ps%d" % bS2])
                op("pe", lambda e, c=c, bS2=bS2: e.matmul(ps[bS2][:, 0:128], P["identb"][:], Sloc[:, c, :], start=False, stop=True), reads=["identb", "Sloc"], writes=["ps%d" % bS2])
                op("dve", lambda e, c=c, bS2=bS2: e.scalar_tensor_tensor(out=Sbd[:, cc, :], in0=Sbd[:, cc, :], scalar=GC[:, cc * 2 + c:cc * 2 + c + 1], in1=ps[bS2][:, 0:128], op0=ALU.mult, op1=ALU.add),
                   reads=[sk, "GC", "ps%d" % bS2], writes=[sk])
        op("pool", lambda e: e.tensor_copy(out=hT[:, :, 0:1], in_=hT[:, :, 128:129]), reads=["hT"], writes=["hT"])
        if not own:
            continue
        if STOP <= 4:
            continue
        op("act", lambda e: e.activation(out=B0, in_=Ysb, func=AF.Copy), reads=["Ysb"], writes=["B0"])
        for cc in range(8):
            bb = cc // 4
            op("pe", lambda e, cc=cc, bb=bb: e.matmul(ps[bb][:, (cc % 4) * 128:(cc % 4 + 1) * 128], bdones, B0[:, cc, :], start=True, stop=True), reads=["bdones", "B0"], writes=["ps%d" % bb])
        dd, ddk = T3[1], "T1"
        for hh in range(2):
            op("dve", lambda e, hh=hh: e.scalar_tensor_tensor(out=dd[:, hh * 4:(hh + 1) * 4, :], in0=ps[hh][:, :].rearrange("p (c t) -> p c t", t=128), scalar=-1.0 / 64, in1=Ysb[:, hh * 4:(hh + 1) * 4, :], op0=ALU.mult, op1=ALU.add),
               reads=["ps%d" % hh, "Ysb"], writes=[ddk])
        op("pool", lambda e: e.tensor_tensor(out=B0, in0=dd, in1=dd, op=ALU.mult), reads=[ddk], writes=["B0"])
        for cc in range(8):
            bb = 2 + cc // 4
            op("pe", lambda e, cc=cc, bb=bb: e.matmul(ps[bb][:, (cc % 4) * 128:(cc % 4 + 1) * 128], bdones, B0[:, cc, :], start=True, stop=True), reads=["bdones", "B0"], writes=["ps%d" % bb])
        rs, rsk = T3[2], "T2"
        for hh in range(2):
            op("act", lambda e, hh=hh: e.activation(out=rs[:, hh * 4:(hh + 1) * 4, :], in_=ps[2 + hh][:, :].rearrange("p (c t) -> p c t", t=128), func=AF.Sqrt, scale=1.0 / 64, bias=P["eps"][:, 1:2]), reads=["ps%d" % (2 + hh), "eps"], writes=[rsk])
        op("dve", lambda e: e.reciprocal(out=Tf[2], in_=Tf[2]), reads=[rsk], writes=[rsk])
        op("pool", lambda e: e.tensor_tensor(out=dd, in0=dd, in1=rs, op=ALU.mult), reads=[ddk, rsk], writes=[ddk])
        op("pool", lambda e: e.tensor_tensor(out=dd, in0=dd, in1=vb(11), op=ALU.mult), reads=[ddk, "vec"], writes=[ddk])
        op("pool", lambda e: e.tensor_tensor(out=dd, in0=dd, in1=vb(12), op=ALU.add), reads=[ddk, "vec"], writes=[ddk])
        op("dve", lambda e: e.tensor_tensor(out=dd, in0=dd, in1=bonus, op=ALU.add), reads=[ddk, "bonus"], writes=[ddk])
        op("dve", lambda e: e.tensor_tensor(out=yg, in0=dd, in1=gS, op=ALU.mult), reads=[ddk, "gS"], writes=["yg"])
        for n in range(2):
            for kc in range(8):
                op("pe", lambda e, n=n, kc=kc: e.matmul(ps[2 + n][:, :], yg[:, kc, :], W["rw_wo"][:, kc, n * 512:(n + 1) * 512], start=(kc == 0), stop=(kc == 7)), reads=["yg", "rw_wo"], writes=["ps%d" % (2 + n)])
        self.postnorm_res2((2, 3), xt, "xt", 2, scr, scr_keys)
        to = ti - NTILE // 2
        fw.dma("sp", S["x3"][to * 128:(to + 1) * 128, :], xt, reads=["xt"])
        if STOP <= 5:
            continue
        junk, ss, rstd = scr["junk"], scr["ss"], scr["rstd"]
        op("act", lambda e: e.activation(out=junk, in_=xt, func=AF.Square, accum_out=ss[:, 0:1]), reads=["xt"], writes=["T6", "lg"])
        op("act", lambda e: e.activation(out=rstd[:, 0:1], in_=ss[:, 0:1], func=AF.Sqrt, scale=1.0 / D, bias=P["eps"][:, 0:1]), reads=["lg", "eps"], writes=["lg"])
        op("dve", lambda e: e.reciprocal(out=rstd[:, 0:1], in_=rstd[:, 0:1]), reads=["lg"], writes=["lg"])
        xn32 = Tf[7]
        op("act", lambda e: e.activation(out=xn32, in_=xt, func=AF.Copy, scale=rstd[:, 0:1]), reads=["xt", "lg"], writes=["T7"])
        for kc in range(8):
            bb = kc // 4
            op("pe", lambda e, kc=kc, bb=bb: e.transpose(ps[bb][:, (kc % 4) * 128:(kc % 4 + 1) * 128], xn32[:, kc * 128:(kc + 1) * 128], P["ident"][:]), reads=["T7", "ident"], writes=["ps%d" % bb])
        G1 = P["modT"][:, 4 + 2, :]
        sh = P["modT"][:, 4 + 3, :]
        for hh in range(2):
            op("dve", lambda e, hh=hh: e.tensor_tensor(out=h32[:, hh * 4:(hh + 1) * 4, :], in0=ps[hh][:, :].rearrange("p (c t) -> p c t", t=128), in1=bc(G1[:, hh * 4:(hh + 1) * 4].unsqueeze(2), [128, 4, 128]), op=ALU.mult),
               reads=["ps%d" % hh, "modT"], writes=["h32"])
        op("pool", lambda e: e.tensor_tensor(out=h32, in0=h32, in1=bc(sh.unsqueeze(2), [128, 8, 128]), op=ALU.add), reads=["h32", "modT"], writes=["h32"])
        op("act", lambda e: e.activation(out=hb, in_=h32, func=AF.Copy), reads=["h32"], writes=["hb"])
        fw.dma("sp", S["hTf1"][to // 2][:, :, (to % 2) * 128:(to % 2 + 1) * 128], hb, reads=["hb"])
        bL = nb()
        for kc in range(8):
            op("pe", lambda e, kc=kc, bL=bL: e.matmul(ps[bL][:, 0:8], h32[:, kc, :], router[:, kc, :], start=(kc == 0), stop=(kc == 7)), reads=["h32", "router"], writes=["ps%d" % bL])
        L8, m1, m2, eq, ex, sm = lg[:, 0:8], lg[:, 8:9], lg[:, 9:10], lg[:, 10:18], lg[:, 18:26], lg[:, 26:27]
        op("dve", lambda e, bL=bL: e.tensor_copy(out=L8, in_=ps[bL][:, 0:8]), reads=["ps%d" % bL], writes=["lg"])
        op("dve", lambda e: e.reduce_max(out=m1, in_=L8, axis=AX.X), reads=["lg"], writes=["lg"])
        op("dve", lambda e: e.tensor_scalar(out=eq, in0=L8, scalar1=m1, scalar2=-1e30, op0=ALU.is_equal, op1=ALU.mult), reads=["lg"], writes=["lg"])
        op("dve", lambda e: e.tensor_tensor(out=eq, in0=eq, in1=L8, op=ALU.add), reads=["lg"], writes=["lg"])
        op("dve", lambda e: e.reduce_max(out=m2, in_=eq, axis=AX.X), reads=["lg"], writes=["lg"])
        op("dve", lambda e: e.tensor_scalar(out=eq, in0=L8, scalar1=m2, scalar2=None, op0=ALU.is_ge), reads=["lg"], writes=["lg"])
        op("dve", lambda e: e.tensor_scalar(out=ex, in0=L8, scalar1=m1, scalar2=None, op0=ALU.subtract), reads=["lg"], writes=["lg"])
        op("act", lambda e: e.activation(out=ex, in_=ex, func=AF.Exp), reads=["lg"], writes=["lg"])
        op("dve", lambda e: e.tensor_tensor(out=ex, in0=ex, in1=eq, op=ALU.mult), reads=["lg"], writes=["lg"])
        op("dve", lambda e: e.reduce_sum(out=sm, in_=ex, axis=AX.X), reads=["lg"], writes=["lg"])
        op("dve", lambda e: e.reciprocal(out=sm, in_=sm), reads=["lg"], writes=["lg"])
        op("dve", lambda e: e.tensor_scalar(out=ex, in0=ex, scalar1=sm, scalar2=None, op0=ALU.mult), reads=["lg"], writes=["lg"])
        fw.dma("sp", S["comb"][to * 128:(to + 1) * 128, :], ex, reads=["lg"])


def _postnorm_sb2(self, y, ykey, xsub, xkey, gp_idx, scr, skeys):
    fw, P = self.fw, self.P
    junk, ss, rstd, tmp = scr["junk"], scr["ss2"], scr["rstd2"], scr["tmp"]
    fw.op("act", lambda e: e.activation(out=junk, in_=y, func=AF.Square, accum_out=ss[:, 0:1]), reads=[ykey], writes=["T6", "lg"])
    fw.op("act", lambda e: e.activation(out=rstd[:, 0:1], in_=ss[:, 0:1], func=AF.Sqrt, scale=1.0 / D, bias=P["eps"][:, 0:1]), reads=["lg", "eps"], writes=["lg"])
    fw.op("dve", lambda e: e.reciprocal(out=rstd[:, 0:1], in_=rstd[:, 0:1]), reads=["lg"], writes=["lg"])
    fw.op("dve", lambda e: e.scalar_tensor_tensor(out=tmp, in0=y, scalar=rstd[:, 0:1], in1=P["GP"][:, gp_idx, :], op0=ALU.mult, op1=ALU.mult),
          reads=[ykey, "lg", "GP"], writes=["T7"])
    fw.op("pool", lambda e: e.tensor_tensor(out=xsub, in0=xsub, in1=tmp, op=ALU.add), reads=["T7", xkey], writes=[xkey])


def _prenorm_T2(self, xsub, xkey, l, sub, hT, hkey, col0, pbank, scr, skeys):
    fw, P, ps = self.fw, self.P, self.ps
    junk, ss, rstd, xn, tmp = scr["junk"], scr["ss"], scr["rstd"], scr["xn"], scr["tmp"]
    fw.op("act", lambda e: e.activation(out=junk, in_=xsub, func=AF.Square, accum_out=ss[:, 0:1]), reads=[xkey], writes=["T6", "lg"])
    fw.op("act", lambda e: e.activation(out=rstd[:, 0:1], in_=ss[:, 0:1], func=AF.Sqrt, scale=1.0 / D, bias=P["eps"][:, 0:1]), reads=["lg", "eps"], writes=["lg"])
    fw.op("dve", lambda e: e.reciprocal(out=rstd[:, 0:1], in_=rstd[:, 0:1]), reads=["lg"], writes=["lg"])
    fw.op("act", lambda e: e.activation(out=xn, in_=xsub, func=AF.Copy, scale=rstd[:, 0:1]), reads=[xkey, "lg"], writes=["T6"])
    pk = "ps%d" % pbank
    pbt = ps[pbank][:, :].bitcast(BF16)
    for kc in range(8):
        fw.op("pe", lambda e, kc=kc: e.transpose(pbt[:, kc * 128:(kc + 1) * 128], xn[:, kc * 128:(kc + 1) * 128], P["identb"][:]), reads=["T6", "identb"], writes=[pk])
    G1 = P["modT"][:, l * 4 + sub * 2 + 0, :]
    sh = P["modT"][:, l * 4 + sub * 2 + 1, :]
    tmp3 = tmp.rearrange("p (k t) -> p k t", t=128)
    fw.op("dve", lambda e: e.tensor_tensor(out=tmp3, in0=pbt.rearrange("p (k t) -> p k t", t=128), in1=bc(G1.unsqueeze(2), [128, 8, 128]), op=ALU.mult), reads=[pk, "modT"], writes=["T7"])
    fw.op("pool", lambda e: e.tensor_tensor(out=hT[:, :, col0:col0 + 128], in0=tmp3, in1=bc(sh.unsqueeze(2), [128, 8, 128]), op=ALU.add), reads=["T7", "modT"], writes=[hkey])


def _postnorm_res2(self, psb, xsub, xkey, gp_idx, scr, skeys):
    fw, P, ps = self.fw, self.P, self.ps
    junk, ss2, rstd, tmp = scr["junk"], scr["ss2"], scr["rstd2"], scr["tmp"]
    for n in range(2):
        fw.op("act", lambda e, n=n: e.activation(out=junk[:, 0:512], in_=ps[psb[n]][:, :], func=AF.Square, accum_out=ss2[:, n:n + 1]), reads=["ps%d" % psb[n]], writes=["T6", "lg"])
    fw.op("pool", lambda e: e.tensor_tensor(out=rstd[:, 0:1], in0=ss2[:, 0:1], in1=ss2[:, 1:2], op=ALU.add), reads=["lg"], writes=["lg"])
    fw.op("act", lambda e: e.activation(out=rstd[:, 0:1], in_=rstd[:, 0:1], func=AF.Sqrt, scale=1.0 / D, bias=P["eps"][:, 0:1]), reads=["lg", "eps"], writes=["lg"])
    fw.op("dve", lambda e: e.reciprocal(out=rstd[:, 0:1], in_=rstd[:, 0:1]), reads=["lg"], writes=["lg"])
    for n in range(2):
        fw.op("dve", lambda e, n=n: e.scalar_tensor_tensor(out=tmp[:, n * 512:(n + 1) * 512], in0=ps[psb[n]][:, :], scalar=rstd[:, 0:1], in1=P["GP"][:, gp_idx, n * 512:(n + 1) * 512], op0=ALU.mult, op1=ALU.mult),
              reads=["ps%d" % psb[n], "lg", "GP"], writes=["T7"])
    fw.op("pool", lambda e: e.tensor_tensor(out=xsub, in0=xsub, in1=tmp, op=ALU.add), reads=["T7", xkey], writes=[xkey])


Builder.phase_rwkv = _phase_rwkv
Builder.postnorm_sb2 = _postnorm_sb2
Builder.prenorm_T2 = _prenorm_T2
Builder.postnorm_res2 = _postnorm_res2
```

```python
import contextlib
import numpy as np
import concourse.bass as bass
import concourse.mybir as mybir
from concourse.bass_utils import run_bass_kernel_spmd

F32 = mybir.dt.float32
BF16 = mybir.dt.bfloat16
ALU = mybir.AluOpType
AF = mybir.ActivationFunctionType
AX = mybir.AxisListType

D = 1024
FF = 2816
NE = 8
SEQ = 8192
HALF = 4096
RMS_EPS = 1e-6
LNX_EPS = 1e-5 * 64

COMPUTE = ("pe", "act", "dve", "pool")
NDMA_SEMS = 12
EPOCH = 30000


class _Op:
    __slots__ = ("eng", "fn", "deps", "is_dma", "need_inc", "tick", "dsem", "dval", "dprev")

    def __init__(self, eng, fn, deps, is_dma):
        self.eng = eng
        self.fn = fn
        self.deps = deps
        self.is_dma = is_dma
        self.need_inc = False
        self.tick = 0
        self.dsem = None
        self.dval = 0
        self.dprev = None


class _Rec:
    def __getattr__(self, name):
        return lambda *a, **k: (name, a, k)


_REC = _Rec()


class FW:
    def __init__(self, nc):
        self.nc = nc
        self.ops = []
        self.last_w = {}
        self.readers = {}
        self.bar = set()
        self.last_c = {}
        self.dma_since = []

    def _add(self, eng, fn, reads, writes, is_dma):
        idx = len(self.ops)
        pr = [r for r in reads if r.startswith("ps")]
        if pr:
            reads = [r for r in reads if not r.startswith("ps")]
            writes = list(writes) + pr
        deps = set(self.bar)
        for r in reads:
            w = self.last_w.get(r)
            if w is not None:
                deps.add(w)
        for k in writes:
            w = self.last_w.get(k)
            if w is not None:
                deps.add(w)
            rd = self.readers.get(k)
            if rd is not None:
                deps.update(rd["c"].values())
                deps.update(rd["d"])
        for r in reads:
            rd = self.readers.get(r)
            if rd is None:
                rd = self.readers[r] = {"c": {}, "d": []}
            if is_dma:
                rd["d"].append(idx)
            else:
                rd["c"][eng] = idx
        for k in writes:
            self.last_w[k] = idx
            self.readers[k] = {"c": {}, "d": []}
        deps.discard(idx)
        self.ops.append(_Op(eng, fn, deps, is_dma))
        if is_dma:
            self.dma_since.append(idx)
        else:
            self.last_c[eng] = idx
        return idx

    def op(self, eng, fn, reads=(), writes=()):
        name, a, k = fn(_REC)
        return self._add(eng, lambda e: getattr(e, name)(*a, **k), reads, writes, False)

    def dma(self, q, out, in_, reads=(), writes=()):
        return self._add(q, lambda e: e.dma_start(out=out, in_=in_), reads, writes, True)

    def barrier(self):
        self.bar = set(self.last_c.values()) | set(self.dma_since)
        self.dma_since = []
        self.last_w = {}
        self.readers = {}

    def emit(self):
        nc = self.nc
        ops = self.ops
        for o in ops:
            for d in o.deps:
                p = ops[d]
                if p.is_dma:
                    continue
                if p.eng == o.eng and p.eng == "pe" and not o.is_dma:
                    continue
                p.need_inc = True
        ticks = {e: 0 for e in COMPUTE}
        for o in ops:
            if not o.is_dma and o.need_inc:
                ticks[o.eng] += 1
                o.tick = ticks[o.eng]
        qcount = {}
        qlast = {}
        for i, o in enumerate(ops):
            if o.is_dma:
                n = qcount.get(o.eng, 0)
                qcount[o.eng] = n + 1
                slot = n % NDMA_SEMS
                key = (o.eng, slot)
                o.dsem = key
                o.dval = (n // NDMA_SEMS + 1) * 16
                o.dprev = qlast.get(key)
                qlast[key] = i
        engs = ("pe", "act", "dve", "pool", "sp")
        with contextlib.ExitStack() as st:
            csem = {}
            for e in COMPUTE:
                nep = (ticks[e] + EPOCH - 1) // EPOCH
                for k in range(max(nep, 1)):
                    csem[(e, k)] = st.enter_context(nc.semaphore("c_%s_%d" % (e, k)))
            dsem = {}
            for q in qcount:
                for s in range(min(NDMA_SEMS, qcount[q])):
                    dsem[(q, s)] = st.enter_context(nc.semaphore("d_%s_%d" % (q, s)))
            known = {e: {} for e in engs}
            streams = {e: [] for e in engs}
            for i, o in enumerate(ops):
                e = o.eng
                kn = known[e]
                waits = {}
                for d in o.deps:
                    p = ops[d]
                    if p.is_dma:
                        sk = ("d",) + p.dsem
                        v = p.dval
                    else:
                        if p.eng == e and e == "pe" and not o.is_dma:
                            continue
                        ep = (p.tick - 1) // EPOCH
                        sk = ("c", p.eng, ep)
                        v = p.tick - ep * EPOCH
                    if kn.get(sk, 0) >= v:
                        continue
                    if waits.get(sk, 0) < v:
                        waits[sk] = v
                if o.is_dma and o.dprev is not None:
                    p = ops[o.dprev]
                    sk = ("d",) + p.dsem
                    if kn.get(sk, 0) < p.dval and waits.get(sk, 0) < p.dval:
                        waits[sk] = p.dval
                for sk, v in waits.items():
                    kn[sk] = v
                    sem = csem[(sk[1], sk[2])] if sk[0] == "c" else dsem[(sk[1], sk[2])]
                    streams[e].append(("w", sem, v))
                if o.is_dma:
                    streams[e].append(("i", o.fn, dsem[o.dsem], 16))
                elif o.need_inc:
                    streams[e].append(("i", o.fn, csem[(e, (o.tick - 1) // EPOCH)], 1))
                else:
                    streams[e].append(("i", o.fn, None, 0))
            fin = streams["sp"]
            for key, i in qlast.items():
                fin.append(("w", dsem[key], ops[i].dval))
            for e in COMPUTE:
                if ticks[e] > 0:
                    ep = (ticks[e] - 1) // EPOCH
                    fin.append(("w", csem[(e, ep)], ticks[e] - ep * EPOCH))

            def run(eng_obj, lst):
                for it in lst:
                    if it[0] == "w":
                        eng_obj.wait_ge(it[1], it[2])
                    else:
                        ins = it[1](eng_obj)
                        if it[2] is not None:
                            ins.then_inc(it[2], it[3])

            with nc.Block() as block:
                @block.tensor
                def _(eng):
                    run(eng, streams["pe"])

                @block.scalar
                def _(eng):
                    run(eng, streams["act"])

                @block.vector
                def _(eng):
                    run(eng, streams["dve"])

                @block.gpsimd
                def _(eng):
                    run(eng, streams["pool"])

                @block.sync
                def _(eng):
                    run(eng, streams["sp"])
        return {e: len(streams[e]) for e in streams}


class Arena:
    def __init__(self, t, nwords):
        self.t = t
        self.n = nwords
        self.off = 0

    def reset(self):
        self.off = 0

    def alloc(self, shape, dtype):
        nel = int(np.prod(shape))
        nb = nel * (4 if dtype == F32 else 2)
        nw = (nb + 3) // 4
        ap = self.t[:, self.off:self.off + nw]
        self.off += nw
        assert self.off <= self.n, ("arena overflow", self.off, self.n)
        if dtype != F32:
            ap = ap.bitcast(dtype)[:, 0:nel]
        if len(shape) == 2:
            ap = ap.rearrange("p (a b) -> p a b", a=shape[0])
        elif len(shape) == 3:
            ap = ap.rearrange("p (a b c) -> p a b c", a=shape[0], b=shape[1])
        return ap


def bc(ap, shape):
    return ap.to_broadcast(list(shape))


class Builder:
    def __init__(self, stages=("M", "A", "B", "D", "E", "F"), dbg=False):
        self.stages = stages
        self.dbg = dbg
        self.nc = bass.Bass("TRN2", target_bir_lowering=False)
        self.fw = FW(self.nc)
        self.uid = 0

    def din(self, name, shape, dt=F32):
        return self.nc.dram_tensor(name, list(shape), dt, kind="ExternalInput").ap()

    def dscr(self, name, shape, dt=F32, out=False):
        kind = "ExternalOutput" if out else "Internal"
        return self.nc.dram_tensor(name, list(shape), dt, kind=kind).ap()

    def build(self):
        nc, fw = self.nc, self.fw
        dbg = self.dbg
        I = {}
        I["x8"] = self.din("x8", [SEQ, D])
        I["condB"] = self.din("condB", [128, 8, 128])
        I["flag"] = self.din("flag", [128, 1])
        I["invc"] = self.din("invc", [128, 2, 4, 16])
        I["ident"] = self.din("ident", [128, 128])
        I["ada_w"] = self.din("ada_w", [2, D, 6 * D])
        I["ada_bB"] = self.din("ada_bB", [2, 128, 6 * D])
        I["norm_gB"] = self.din("norm_gB", [2, 128, 4, D])
        I["mix_w_in"] = self.din("mix_w_in", [D, 2048])
        I["mix_w_out"] = self.din("mix_w_out", [D, D])
        I["conv_wT"] = self.din("conv_wT", [128, 4, 3])
        I["pool_w"] = self.din("pool_w", [4, 128, 128])
        I["pool_scT"] = self.din("pool_scT", [128, 4])
        I["ffn_wg"] = self.din("ffn_wg", [D, FF])
        I["ffn_wu"] = self.din("ffn_wu", [D, FF])
        I["ffn_wd"] = self.din("ffn_wd", [FF, D])
        for nm in ("rw_wr", "rw_wk", "rw_wv", "rw_wo"):
            I[nm] = self.din(nm, [D, D])
        I["rw_w1"] = self.din("rw_w1", [D, 64])
        I["rw_a1"] = self.din("rw_a1", [D, 64])
        I["rw_g1"] = self.din("rw_g1", [D, 160])
        I["rw_w2"] = self.din("rw_w2", [64, D])
        I["rw_a2"] = self.din("rw_a2", [64, D])
        I["rw_g2"] = self.din("rw_g2", [160, D])
        I["rw_vec"] = self.din("rw_vec", [128, 13, 8])
        I["moe_router"] = self.din("moe_router", [D, 8])
        I["resetm"] = self.din("resetm", [128, 1024])
        I["mask1"] = self.din("mask1", [128, 2, 128])
        I["maskT"] = self.din("maskT", [128, 128])
        I["bdones"] = self.din("bdones", [128, 128])
        I["moe_wg"] = self.din("moe_wg", [NE, D, FF])
        I["moe_wu"] = self.din("moe_wu", [NE, D, FF])
        I["moe_wd"] = self.din("moe_wd", [NE, FF, D])
        self.I = I
        S = {}
        S["x1"] = self.dscr("x1", [SEQ, D], out=dbg)
        S["hTf0"] = self.dscr("hTf0", [32, 128, 8, 256], BF16)
        S["acc0"] = self.dscr("acc0", [SEQ, D])
        S["x2"] = self.dscr("x2", [SEQ, D], out=("C" in self.stages))
        S["x3"] = self.dscr("x3", [HALF, D], out=dbg)
        S["MI"] = self.dscr("MI", [64, 128, 8, 896], BF16)
        S["GCd"] = self.dscr("GCd", [64, 128, 16])
        S["OW"] = self.dscr("OW", [32, 128, 8, 256], BF16)
        S["hTf1"] = self.dscr("hTf1", [16, 128, 8, 256], BF16)
        S["comb"] = self.dscr("comb", [HALF, 8], out=dbg)
        S["acc1"] = self.dscr("acc1", [HALF, D])
        S["y"] = self.dscr("y", [HALF, D], out=True)
        self.S = S

        with contextlib.ExitStack() as st:
            sb = lambda n, sh, dt: st.enter_context(nc.sbuf_tensor("sb_" + n, sh, dt))
            P = {}
            P["ident"] = sb("identf", [128, 128], F32)
            P["identb"] = sb("identb", [128, 128], BF16)
            P["flag"] = sb("flag", [128, 1], F32)
            P["invc"] = sb("invc", [128, 2, 4, 16], F32)
            P["GP"] = sb("GP", [128, 4, D], F32)
            P["modT"] = sb("modT", [128, 8, 8], F32)
            P["small"] = sb("small", [128, 64], F32)
            P["eps"] = sb("eps", [128, 2], F32)
            ARW = 46500
            arena_t = sb("arena", [128, ARW], F32)
            self.P = P
            self.ar = Arena(arena_t, ARW)
            self.ps = [st.enter_context(nc.psum_tensor("ps%d" % i, [128, 512], F32)) for i in range(8)]

            fw.dma("sp", P["ident"][:], I["ident"], writes=["ident"])
            fw.dma("pool", P["identb"][:], I["ident"], writes=["identb"])
            fw.dma("sp", P["flag"][:], I["flag"], writes=["flag"])
            fw.op("pool", lambda e: e.memset(P["eps"][:, 0:1], RMS_EPS), writes=["eps"])
            fw.op("pool", lambda e: e.memset(P["eps"][:, 1:2], LNX_EPS), writes=["eps"])
            fw.dma("sp", P["invc"][:], I["invc"], writes=["invc"])
            if "M" in self.stages:
                self.phase_mod(0)
                self.phase_mod(1)
            if "A" in self.stages:
                self.phase_mixer0()
            if "B" in self.stages:
                fw.barrier()
                self.ar.reset()
                self.ffn_passes(self.I["ffn_wg"], self.I["ffn_wu"], self.I["ffn_wd"], 32, S["hTf0"], S["acc0"], None, [0])
            if "C" in self.stages:
                fw.barrier()
                self.ar.reset()
                self.phase_post0()
            if "D" in self.stages:
                self.phase_rwkv_prep()
                self.phase_rwkv_chain()
            if "E" in self.stages:
                fw.barrier()
                self.ar.reset()
                self.ffn_passes(self.I["moe_wg"], self.I["moe_wu"], self.I["moe_wd"], 16, S["hTf1"], S["acc1"], S["comb"], list(range(NE)))
            if "F" in self.stages:
                fw.barrier()
                self.ar.reset()
                self.phase_final()
            self.stats = fw.emit()
        return nc

    def phase_mod(self, l):
        nc, fw, P, I, ar = self.nc, self.fw, self.P, self.I, self.ar
        ps = self.ps
        fw.barrier()
        ar.reset()
        cond = ar.alloc([8, 128], F32)
        modb = ar.alloc([6 * D], F32)
        adab = ar.alloc([6 * D], F32)
        ng = ar.alloc([4, D], F32)
        wblk = [ar.alloc([8, 512], F32) for _ in range(2)]
        tmpb = ar.alloc([4, D], F32)
        fw.dma("sp", cond, I["condB"], writes=["cond"])
        fw.dma("sp", adab, I["ada_bB"][l], writes=["adab"])
        fw.dma("sp", ng, I["norm_gB"][l], writes=["ng"])
        fw.op("act", lambda e: e.activation(out=cond, in_=cond, func=AF.Silu), reads=["cond"], writes=["cond"])
        for blk in range(12):
            wb = wblk[blk % 2]
            wk = "wblk%d" % (blk % 2)
            fw.dma("sp", wb, I["ada_w"][l, :, blk * 512:(blk + 1) * 512].rearrange("(kc p) n -> p kc n", p=128), writes=[wk])
            pb = ps[blk % 2]
            pk = "ps%d" % (blk % 2)
            for kc in range(8):
                fw.op("pe", lambda e, kc=kc, wb=wb, pb=pb: e.matmul(pb[:, :], cond[:, kc, :], wb[:, kc, :], start=(kc == 0), stop=(kc == 7)),
                      reads=["cond", wk], writes=[pk])
            fw.op("dve", lambda e, blk=blk, pb=pb: e.tensor_tensor(out=modb[:, blk * 512:(blk + 1) * 512], in0=pb[:, :], in1=adab[:, blk * 512:(blk + 1) * 512], op=ALU.add),
                  reads=[pk, "adab"], writes=["modb"])
        sh_m, sc_m, gt_m, sh_f, sc_f, gt_f = [modb[:, i * D:(i + 1) * D] for i in range(6)]
        fw.op("dve", lambda e: e.tensor_tensor(out=P["GP"][:, l * 2 + 0, :], in0=gt_m, in1=ng[:, 1, :], op=ALU.mult), reads=["modb", "ng"], writes=["GP"])
        fw.op("dve", lambda e: e.tensor_tensor(out=P["GP"][:, l * 2 + 1, :], in0=gt_f, in1=ng[:, 3, :], op=ALU.mult), reads=["modb", "ng"], writes=["GP"])
        fw.op("dve", lambda e: e.scalar_tensor_tensor(out=tmpb[:, 0, :], in0=sc_m, scalar=1.0, in1=ng[:, 0, :], op0=ALU.add, op1=ALU.mult), reads=["modb", "ng"], writes=["tmpb"])
        fw.op("dve", lambda e: e.tensor_copy(out=tmpb[:, 1, :], in_=sh_m), reads=["modb"], writes=["tmpb"])
        fw.op("dve", lambda e: e.scalar_tensor_tensor(out=tmpb[:, 2, :], in0=sc_f, scalar=1.0, in1=ng[:, 2, :], op0=ALU.add, op1=ALU.mult), reads=["modb", "ng"], writes=["tmpb"])
        fw.op("dve", lambda e: e.tensor_copy(out=tmpb[:, 3, :], in_=sh_f), reads=["modb"], writes=["tmpb"])
        for v in range(4):
            for kc in range(8):
                pb = ps[2 + kc // 4]
                fw.op("pe", lambda e, v=v, kc=kc, pb=pb: e.transpose(pb[:, (kc % 4) * 128:(kc % 4 + 1) * 128], tmpb[:, v, kc * 128:(kc + 1) * 128], P["ident"][:]),
                      reads=["tmpb", "ident"], writes=["ps%d" % (2 + kc // 4)])
            for hh in range(2):
                fw.op("dve", lambda e, v=v, hh=hh: e.tensor_copy(out=P["modT"][:, l * 4 + v, hh * 4:(hh + 1) * 4],
                                                                 in_=ps[2 + hh][:, :].rearrange("p (k t) -> p k t", t=128)[:, :, 0]),
                      reads=["ps%d" % (2 + hh)], writes=["modT"])

    def prenorm_T(self, xsub, xkey, l, sub, hT, hkey, col0, pbank, scr, f32out=None):
        fw, P, ps = self.fw, self.P, self.ps
        u = self.uid
        self.uid += 1
        junk, ss, rstd, xn, tmp = scr["junk"], scr["ss"], scr["rstd"], scr["xn"], scr["tmp"]
        fw.op("act", lambda e: e.activation(out=junk, in_=xsub, func=AF.Square, accum_out=ss[:, 0:1]), reads=[xkey], writes=["junk", "ss"])
        fw.op("act", lambda e: e.activation(out=rstd[:, 0:1], in_=ss[:, 0:1], func=AF.Sqrt, scale=1.0 / D, bias=P["eps"][:, 0:1]), reads=["ss", "eps"], writes=["rstd"])
        fw.op("dve", lambda e: e.reciprocal(out=rstd[:, 0:1], in_=rstd[:, 0:1]), reads=["rstd"], writes=["rstd"])
        fw.op("act", lambda e: e.activation(out=xn, in_=xsub, func=AF.Copy, scale=rstd[:, 0:1]), reads=[xkey, "rstd"], writes=["xn"])
        pk = "ps%d" % pbank
        pbt = ps[pbank][:, :].bitcast(BF16)
        for kc in range(8):
            fw.op("pe", lambda e, kc=kc: e.transpose(pbt[:, kc * 128:(kc + 1) * 128], xn[:, kc * 128:(kc + 1) * 128], P["identb"][:]),
                  reads=["xn", "identb"], writes=[pk])
        G1 = P["modT"][:, l * 4 + sub * 2 + 0, :]
        sh = P["modT"][:, l * 4 + sub * 2 + 1, :]
        for kc in range(8):
            fw.op("act", lambda e, kc=kc: e.activation(out=hT[:, kc, col0:col0 + 128], in_=pbt[:, kc * 128:(kc + 1) * 128], func=AF.Identity, scale=G1[:, kc:kc + 1], bias=sh[:, kc:kc + 1]),
                  reads=[pk, "modT"], writes=[hkey])

    def postnorm_res(self, psb, xsub, xkey, gp_idx, scr):
        fw, P, ps = self.fw, self.P, self.ps
        junk, ss2, rstd, tmp = scr["junk"], scr["ss2"], scr["rstd2"], scr["tmp"]
        for n in range(2):
            fw.op("act", lambda e, n=n: e.activation(out=junk[:, 0:512], in_=ps[psb[n]][:, :], func=AF.Square, accum_out=ss2[:, n:n + 1]),
                  reads=["ps%d" % psb[n]], writes=["junk", "ss2"])
        fw.op("pool", lambda e: e.tensor_tensor(out=rstd[:, 0:1], in0=ss2[:, 0:1], in1=ss2[:, 1:2], op=ALU.add), reads=["ss2"], writes=["rstd2"])
        fw.op("act", lambda e: e.activation(out=rstd[:, 0:1], in_=rstd[:, 0:1], func=AF.Sqrt, scale=1.0 / D, bias=P["eps"][:, 0:1]), reads=["rstd2", "eps"], writes=["rstd2"])
        fw.op("dve", lambda e: e.reciprocal(out=rstd[:, 0:1], in_=rstd[:, 0:1]), reads=["rstd2"], writes=["rstd2"])
        for n in range(2):
            fw.op("dve", lambda e, n=n: e.scalar_tensor_tensor(out=tmp[:, n * 512:(n + 1) * 512], in0=ps[psb[n]][:, :], scalar=rstd[:, 0:1],
                                                               in1=P["GP"][:, gp_idx, n * 512:(n + 1) * 512], op0=ALU.mult, op1=ALU.mult),
                  reads=["ps%d" % psb[n], "rstd2", "GP"], writes=["tmp"])
        fw.op("dve", lambda e: e.tensor_tensor(out=xsub, in0=xsub, in1=tmp, op=ALU.add), reads=["tmp", xkey], writes=[xkey])

    def mk_scr(self):
        ar = self.ar
        return {"junk": ar.alloc([D], BF16), "ss": ar.alloc([2], F32), "rstd": ar.alloc([2], F32), "xn": ar.alloc([D], BF16),
                "tmp": ar.alloc([D], F32), "ss2": ar.alloc([2], F32), "rstd2": ar.alloc([2], F32)}

    def phase_mixer0(self):
        nc, fw, P, I, S, ar, ps = self.nc, self.fw, self.P, self.I, self.S, self.ar, self.ps
        fw.barrier()
        ar.reset()
        NT = 512
        w_in = ar.alloc([8, 2048], BF16)
        w_out = ar.alloc([8, D], BF16)
        pool_w = ar.alloc([4, 128], BF16)
        convw = ar.alloc([4, 3], F32)
        poolsc = ar.alloc([4], F32)
        xt = [ar.alloc([4, D], F32) for _ in range(2)]
        hT = ar.alloc([8, NT], BF16)
        hT2 = ar.alloc([8, NT], BF16)
        cg = ar.alloc([NT], F32)
        cv = ar.alloc([4, 2 + NT], F32)
        t1 = ar.alloc([NT], F32)
        t2 = ar.alloc([NT], F32)
        up = ar.alloc([4, 16 + NT], F32)
        sA = ar.alloc([16 + NT], F32)
        sB = ar.alloc([16 + NT], F32)
        pg = ar.alloc([NT], BF16)
        t16 = ar.alloc([16], F32)
        ycat = ar.alloc([8, NT], BF16)
        scr = self.mk_scr()
        for kc in range(8):
            fw.dma("pool", w_in[:, kc, :], I["mix_w_in"][kc * 128:(kc + 1) * 128, :], writes=["w_in"])
        fw.dma("pool", w_out, I["mix_w_out"].rearrange("(kc p) n -> p kc n", p=128), writes=["w_out"])
        fw.dma("pool", pool_w, I["pool_w"].rearrange("g c d -> c g d"), writes=["pool_w"])
        fw.dma("sp", convw, I["conv_wT"], writes=["convw"])
        fw.dma("sp", poolsc, I["pool_scT"], writes=["poolsc"])
        fw.op("pool", lambda e: e.memset(cv, 0.0), writes=["cv%d" % j for j in range(4)])
        fw.op("pool", lambda e: e.memset(up, 0.0), writes=["up%d" % g for g in range(4)])
        wins = (2, 4, 8, 16)
        ntiles = SEQ // NT
        for ti in range(ntiles):
            xb = xt[ti % 2]
            xk = "xt%d" % (ti % 2)
            fw.dma("sp", xb, I["x8"][ti * NT:(ti + 1) * NT, :].rearrange("(s p) d -> p s d", p=128), writes=[xk])
            for s in range(4):
                self.prenorm_T(xb[:, s, :], xk, 0, 0, hT, "hT", s * 128, s % 2, scr)
            if ti == ntiles // 2:
                fw.op("pool", lambda e: e.tensor_scalar(out=cv[:, :, 0:2], in0=cv[:, :, 0:2], scalar1=P["flag"][:, 0:1], scalar2=None, op0=ALU.mult),
                      reads=["flag"] + ["cv%d" % j for j in range(4)], writes=["cv%d" % j for j in range(4)])
                fw.op("pool", lambda e: e.tensor_scalar(out=up[:, :, 0:16], in0=up[:, :, 0:16], scalar1=P["flag"][:, 0:1], scalar2=None, op0=ALU.mult),
                      reads=["flag"] + ["up%d" % g for g in range(4)], writes=["up%d" % g for g in range(4)])
            bankrr = [2, 3, 4, 5]
            bi = [0]

            def zchunk(fc):
                b = bankrr[bi[0] % 4]
                bi[0] += 1
                for kc in range(8):
                    fw.op("pe", lambda e, kc=kc, b=b: e.matmul(ps[b][:, :], w_in[:, kc, fc * 128:(fc + 1) * 128], hT[:, kc, :], start=(kc == 0), stop=(kc == 7)),
                          reads=["w_in", "hT"], writes=["ps%d" % b])
                return b
            for j in range(4):
                cvk = "cv%d" % j
                b = zchunk(4 + j)
                fw.op("act", lambda e, b=b: e.activation(out=cg, in_=ps[b][:, :], func=AF.Copy), reads=["ps%d" % b], writes=["cg"])
                b = zchunk(8 + j)
                fw.op("dve", lambda e, b=b, j=j: e.tensor_tensor(out=cv[:, j, 2:2 + NT], in0=ps[b][:, :], in1=cg, op=ALU.mult), reads=["ps%d" % b, "cg"], writes=[cvk])
                fw.op("pool", lambda e, j=j: e.tensor_scalar(out=t1, in0=cv[:, j, 0:NT], scalar1=convw[:, j, 0:1], scalar2=None, op0=ALU.mult), reads=[cvk, "convw"], writes=["t1"])
                fw.op("dve", lambda e, j=j: e.scalar_tensor_tensor(out=t2, in0=cv[:, j, 1:1 + NT], scalar=convw[:, j, 1:2], in1=t1, op0=ALU.mult, op1=ALU.add), reads=[cvk, "convw", "t1"], writes=["t2"])
                fw.op("dve", lambda e, j=j: e.scalar_tensor_tensor(out=t1, in0=cv[:, j, 2:2 + NT], scalar=convw[:, j, 2:3], in1=t2, op0=ALU.mult, op1=ALU.add), reads=[cvk, "convw", "t2"], writes=["t1"])
                b = zchunk(j)
                fw.op("dve", lambda e, b=b, j=j: e.tensor_tensor(out=ycat[:, j, :], in0=ps[b][:, :], in1=t1, op=ALU.mult), reads=["ps%d" % b, "t1"], writes=["ycat"])
                fw.op("pool", lambda e, j=j: e.tensor_copy(out=cv[:, j, 0:2], in_=cv[:, j, NT:NT + 2]), reads=[cvk], writes=[cvk])
            for g in range(4):
                upk = "up%d" % g
                b = zchunk(12 + g)
                fw.op("act", lambda e, b=b, g=g: e.activation(out=up[:, g, 16:16 + NT], in_=ps[b][:, :], func=AF.Copy), reads=["ps%d" % b], writes=[upk])
                W = 16 + NT
                cur = up[:, g, :]
                curk = upk
                bufs = [(sA, "sA"), (sB, "sB")]
                d = 1
                k = 0
                while d < wins[g]:
                    dst, dk = bufs[k % 2]
                    fw.op("dve", lambda e, cur=cur, dst=dst, d=d: e.tensor_tensor(out=dst[:, d:W], in0=cur[:, d:W], in1=cur[:, 0:W - d], op=ALU.add),
                          reads=[curk], writes=[dk])
                    cur, curk = dst, dk
                    d *= 2
                    k += 1
                fw.op("dve", lambda e, cur=cur, g=g: e.scalar_tensor_tensor(out=pg, in0=cur[:, 16:W], scalar=1.0 / wins[g], in1=up[:, g, 16:W], op0=ALU.mult, op1=ALU.subtract),
                      reads=[curk, upk], writes=["pg"])
                if ti == 0 or ti == ntiles // 2:
                    which = 0 if ti == 0 else 1
                    fw.op("pool", lambda e, cur=cur, g=g, which=which: e.tensor_tensor(out=t16, in0=cur[:, 16:32], in1=P["invc"][:, which, g, :], op=ALU.mult),
                          reads=[curk, "invc"], writes=["t16"])
                    fw.op("pool", lambda e, g=g: e.tensor_tensor(out=pg[:, 0:16], in0=t16, in1=up[:, g, 16:32], op=ALU.subtract),
                          reads=["t16", upk], writes=["pg"])
                b2 = bankrr[bi[0] % 4]
                bi[0] += 1
                fw.op("pe", lambda e, g=g, b2=b2: e.matmul(ps[b2][:, :], pool_w[:, g, :], pg, start=True, stop=True), reads=["pool_w", "pg"], writes=["ps%d" % b2])
                fw.op("act", lambda e, g=g, b2=b2: e.activation(out=ycat[:, 4 + g, :], in_=ps[b2][:, :], func=AF.Copy, scale=poolsc[:, g:g + 1]),
                      reads=["ps%d" % b2, "poolsc"], writes=["ycat"])
                fw.op("pool", lambda e, g=g: e.tensor_copy(out=up[:, g, 0:16], in_=up[:, g, NT:NT + 16]), reads=[upk], writes=[upk])
            for s in range(4):
                for n in range(2):
                    b = 6 + n
                    for kc in range(8):
                        fw.op("pe", lambda e, kc=kc, b=b, s=s, n=n: e.matmul(ps[b][:, :], ycat[:, kc, s * 128:(s + 1) * 128], w_out[:, kc, n * 512:(n + 1) * 512], start=(kc == 0), stop=(kc == 7)),
                              reads=["ycat", "w_out"], writes=["ps%d" % b])
                self.postnorm_res((6, 7), xb[:, s, :], xk, 0, scr)
                self.prenorm_T(xb[:, s, :], xk, 0, 1, hT2, "hT2", s * 128, s % 2, scr)
            fw.dma("sp", S["x1"][ti * NT:(ti + 1) * NT, :].rearrange("(s p) d -> p s d", p=128), xb, reads=[xk])
            for hh in range(2):
                fw.dma("sp", S["hTf0"][ti * 2 + hh], hT2[:, :, hh * 256:(hh + 1) * 256], reads=["hT2"])

    def ffn_passes(self, wg_d, wu_d, wd_d, ntiles, hT_d, acc_d, comb_d, experts):
        nc, fw, P, ar, ps = self.nc, self.fw, self.P, self.ar, self.ps
        FH = FF // 2
        NT = 256
        wg = [ar.alloc([8, FH], BF16) for _ in range(2)]
        wu = [ar.alloc([8, FH], BF16) for _ in range(2)]
        wd = [ar.alloc([11, D], BF16) for _ in range(2)]
        hTt = [ar.alloc([8, NT], BF16) for _ in range(2)]
        zt = [ar.alloc([11, NT], BF16) for _ in range(2)]
        acct = [ar.alloc([2, D], F32) for _ in range(2)]
        sg = [ar.alloc([NT], F32) for _ in range(2)]
        combt = [ar.alloc([2, 8], F32) for _ in range(2)]
        npass = 0
        cnt = 0
        for ei, ex in enumerate(experts):
            for hf in range(2):
                wi = npass % 2
                f0 = hf * FH
                if comb_d is None:
                    wgd, wud, wdd = wg_d, wu_d, wd_d
                else:
                    wgd, wud, wdd = wg_d[ex], wu_d[ex], wd_d[ex]
                for kc in range(8):
                    fw.dma("pool", wg[wi][:, kc, :], wgd[kc * 128:(kc + 1) * 128, f0:f0 + FH], writes=["wg%d" % wi])
                    fw.dma("pool", wu[wi][:, kc, :], wud[kc * 128:(kc + 1) * 128, f0:f0 + FH], writes=["wu%d" % wi])
                fw.dma("pool", wd[wi], wdd[f0:f0 + FH, :].rearrange("(fc p) d -> p fc d", p=128), writes=["wd%d" % wi])
                first = (npass == 0)
                for t in range(ntiles):
                    bi = cnt % 2
                    cnt += 1
                    hk, zk, ak, ck = "hTt%d" % bi, "zt%d" % bi, "acct%d" % bi, "combt%d" % bi
                    fw.dma("sp", hTt[bi], hT_d[t], writes=[hk])
                    if not first:
                        fw.dma("sp", acct[bi], acc_d[t * NT:(t + 1) * NT, :].rearrange("(s p) d -> p s d", p=128), reads=["accd%d" % t], writes=[ak])
                    if comb_d is not None:
                        fw.dma("sp", combt[bi], comb_d[t * NT:(t + 1) * NT, :].rearrange("(s p) e -> p s e", p=128), writes=[ck])
                    for fc in range(11):
                        bg = (fc % 2) * 2
                        bu = bg + 1
                        for kc in range(8):
                            fw.op("pe", lambda e, kc=kc, fc=fc, bg=bg, wi=wi, bi=bi: e.matmul(ps[bg][:, 0:NT], wg[wi][:, kc, fc * 128:(fc + 1) * 128], hTt[bi][:, kc, :], start=(kc == 0), stop=(kc == 7)),
                                  reads=["wg%d" % wi, hk], writes=["ps%d" % bg])
                        for kc in range(8):
                            fw.op("pe", lambda e, kc=kc, fc=fc, bu=bu, wi=wi, bi=bi: e.matmul(ps[bu][:, 0:NT], wu[wi][:, kc, fc * 128:(fc + 1) * 128], hTt[bi][:, kc, :], start=(kc == 0), stop=(kc == 7)),
                                  reads=["wu%d" % wi, hk], writes=["ps%d" % bu])
                        sgb = sg[fc % 2]
                        sgk = "sg%d" % (fc % 2)
                        fw.op("act", lambda e, bg=bg, sgb=sgb: e.activation(out=sgb, in_=ps[bg][:, 0:NT], func=AF.Silu), reads=["ps%d" % bg], writes=[sgk])
                        fw.op("dve", lambda e, bu=bu, sgb=sgb, fc=fc, bi=bi: e.tensor_tensor(out=zt[bi][:, fc, :], in0=ps[bu][:, 0:NT], in1=sgb, op=ALU.mult),
                              reads=["ps%d" % bu, sgk], writes=[zk])
                    for s in range(2):
                        for n in range(2):
                            bo = 4 + (s * 2 + n)
                            for fc in range(11):
                                fw.op("pe", lambda e, fc=fc, bo=bo, s=s, n=n, wi=wi, bi=bi: e.matmul(ps[bo][:, :], zt[bi][:, fc, s * 128:(s + 1) * 128], wd[wi][:, fc, n * 512:(n + 1) * 512], start=(fc == 0), stop=(fc == 10)),
                                      reads=[zk, "wd%d" % wi], writes=["ps%d" % bo])
                            dst = acct[bi][:, s, n * 512:(n + 1) * 512]
                            if comb_d is None:
                                if first:
                                    fw.op("act", lambda e, bo=bo, dst=dst: e.activation(out=dst, in_=ps[bo][:, :], func=AF.Copy), reads=["ps%d" % bo], writes=[ak])
                                else:
                                    fw.op("dve", lambda e, bo=bo, dst=dst: e.tensor_tensor(out=dst, in0=ps[bo][:, :], in1=dst, op=ALU.add), reads=["ps%d" % bo, ak], writes=[ak])
                            else:
                                cs = combt[bi][:, s, ex:ex + 1]
                                if first:
                                    fw.op("act", lambda e, bo=bo, dst=dst, cs=cs: e.activation(out=dst, in_=ps[bo][:, :], func=AF.Copy, scale=cs), reads=["ps%d" % bo, ck], writes=[ak])
                                else:
                                    fw.op("dve", lambda e, bo=bo, dst=dst, cs=cs: e.scalar_tensor_tensor(out=dst, in0=ps[bo][:, :], scalar=cs, in1=dst, op0=ALU.mult, op1=ALU.add),
                                          reads=["ps%d" % bo, ak, ck], writes=[ak])
                    fw.dma("sp", acc_d[t * NT:(t + 1) * NT, :].rearrange("(s p) d -> p s d", p=128), acct[bi], reads=[ak], writes=["accd%d" % t])
                npass += 1

    def phase_post0(self):
        nc, fw, P, S, ar, ps = self.nc, self.fw, self.P, self.S, self.ar, self.ps
        NT = 512
        xt = [ar.alloc([4, D], F32) for _ in range(2)]
        at = [ar.alloc([4, D], F32) for _ in range(2)]
        scr = self.mk_scr()
        for ti in range(SEQ // NT):
            xb, ab = xt[ti % 2], at[ti % 2]
            xk, akk = "xt%d" % (ti % 2), "at%d" % (ti % 2)
            fw.dma("sp", xb, S["x1"][ti * NT:(ti + 1) * NT, :].rearrange("(s p) d -> p s d", p=128), writes=[xk])
            fw.dma("sp", ab, S["acc0"][ti * NT:(ti + 1) * NT, :].rearrange("(s p) d -> p s d", p=128), writes=[akk])
            for s in range(4):
                self.postnorm_sb(ab[:, s, :], akk, xb[:, s, :], xk, 1, scr)
            fw.dma("sp", S["x2"][ti * NT:(ti + 1) * NT, :].rearrange("(s p) d -> p s d", p=128), xb, reads=[xk])

    def phase_final(self):
        nc, fw, P, S, ar, ps = self.nc, self.fw, self.P, self.S, self.ar, self.ps
        NT = 512
        xt = [ar.alloc([4, D], F32) for _ in range(2)]
        at = [ar.alloc([4, D], F32) for _ in range(2)]
        scr = self.mk_scr()
        for ti in range(HALF // NT):
            xb, ab = xt[ti % 2], at[ti % 2]
            xk, akk = "xt%d" % (ti % 2), "at%d" % (ti % 2)
            fw.dma("sp", xb, S["x3"][ti * NT:(ti + 1) * NT, :].rearrange("(s p) d -> p s d", p=128), writes=[xk])
            fw.dma("sp", ab, S["acc1"][ti * NT:(ti + 1) * NT, :].rearrange("(s p) d -> p s d", p=128), writes=[akk])
            for s in range(4):
                self.postnorm_sb(ab[:, s, :], akk, xb[:, s, :], xk, 3, scr)
            fw.dma("sp", S["y"][ti * NT:(ti + 1) * NT, :].rearrange("(s p) d -> p s d", p=128), xb, reads=[xk])

    def postnorm_sb(self, y, ykey, xsub, xkey, gp_idx, scr):
        fw, P = self.fw, self.P
        junk, ss, rstd, tmp = scr["junk"], scr["ss2"], scr["rstd2"], scr["tmp"]
        fw.op("act", lambda e: e.activation(out=junk, in_=y, func=AF.Square, accum_out=ss[:, 0:1]), reads=[ykey], writes=["junk", "ss2"])
        fw.op("act", lambda e: e.activation(out=rstd[:, 0:1], in_=ss[:, 0:1], func=AF.Sqrt, scale=1.0 / D, bias=P["eps"][:, 0:1]), reads=["ss2", "eps"], writes=["rstd2"])
        fw.op("dve", lambda e: e.reciprocal(out=rstd[:, 0:1], in_=rstd[:, 0:1]), reads=["rstd2"], writes=["rstd2"])
        fw.op("dve", lambda e: e.scalar_tensor_tensor(out=tmp, in0=y, scalar=rstd[:, 0:1], in1=P["GP"][:, gp_idx, :], op0=ALU.mult, op1=ALU.mult),
              reads=[ykey, "rstd2", "GP"], writes=["tmp"])
        fw.op("dve", lambda e: e.tensor_tensor(out=xsub, in0=xsub, in1=tmp, op=ALU.add), reads=["tmp", xkey], writes=[xkey])


def _fm(v, nch):
    return np.ascontiguousarray(np.asarray(v, np.float32).reshape(nch, 128).T)


def make_in_maps(inp):
    x = np.asarray(inp["x"], np.float32)
    c = np.asarray(inp["c"], np.float32)
    maps = []
    wins = (2, 4, 8, 16)
    pos = np.arange(16)
    invc_start = np.stack([1.0 / np.minimum(pos + 1, w) for w in wins]).astype(np.float32)
    invc_mid = np.stack([np.full(16, 1.0 / w) for w in wins]).astype(np.float32)
    common = {
        "ident": np.eye(128, dtype=np.float32),
        "ada_w": np.ascontiguousarray(inp["ada_w"], np.float32),
        "ada_bB": np.ascontiguousarray(np.broadcast_to(np.asarray(inp["ada_b"], np.float32)[:, None, :], (2, 128, 6 * D))),
        "norm_gB": np.ascontiguousarray(np.broadcast_to(np.asarray(inp["norm_g"], np.float32)[:, None, :, :], (2, 128, 4, D))),
        "mix_w_in": np.ascontiguousarray(inp["mix_w_in"][0], np.float32),
        "mix_w_out": np.ascontiguousarray(inp["mix_w_out"][0], np.float32),
        "conv_wT": np.ascontiguousarray(np.asarray(inp["conv_w"][0], np.float32).reshape(3, 4, 128).transpose(2, 1, 0)),
        "pool_w": np.ascontiguousarray(inp["pool_w"][0], np.float32),
        "pool_scT": _fm(inp["pool_scale"][0], 4),
        "ffn_wg": np.ascontiguousarray(inp["ffn_w_gate"][0], np.float32),
        "ffn_wu": np.ascontiguousarray(inp["ffn_w_up"][0], np.float32),
        "ffn_wd": np.ascontiguousarray(inp["ffn_w_down"][0], np.float32),
    }
    f32 = lambda a: np.ascontiguousarray(a, np.float32)
    common.update({
        "rw_wr": f32(inp["rwkv_w_r"][0]), "rw_wk": f32(inp["rwkv_w_k"][0]), "rw_wv": f32(inp["rwkv_w_v"][0]), "rw_wo": f32(inp["rwkv_w_o"][0]),
        "rw_w1": f32(inp["rwkv_w1"][0]), "rw_a1": f32(inp["rwkv_a1"][0]), "rw_g1": f32(inp["rwkv_g1"][0]),
        "rw_w2": f32(inp["rwkv_w2"][0]), "rw_a2": f32(inp["rwkv_a2"][0]), "rw_g2": f32(inp["rwkv_g2"][0]),
        "moe_router": f32(inp["moe_router"][0]),
        "moe_wg": f32(inp["moe_w_gate"][0]), "moe_wu": f32(inp["moe_w_up"][0]), "moe_wd": f32(inp["moe_w_down"][0]),
    })
    mu = np.asarray(inp["rwkv_mu"][0], np.float32)
    vecs = [mu[i] for i in range(6)] + [inp["rwkv_w0"][0], inp["rwkv_a0"][0], inp["rwkv_k_k"][0], inp["rwkv_k_a"][0],
                                        np.asarray(inp["rwkv_r_k"][0]).reshape(-1), inp["rwkv_ln_g"][0], inp["rwkv_ln_b"][0]]
    common["rw_vec"] = np.ascontiguousarray(np.stack([_fm(v, 8) for v in vecs], axis=1))
    t = np.arange(1024)
    common["resetm"] = np.ascontiguousarray(np.broadcast_to((t % 64 != 0).astype(np.float32)[None], (128, 1024)))
    si = np.arange(128)[:, None]
    tj = np.arange(128)[None, :]
    same = (si // 64) == (tj // 64)
    common["mask1"] = np.ascontiguousarray(np.stack([(same & (si < tj)), (same & (si <= tj))], axis=1).astype(np.float32))
    common["maskT"] = np.ascontiguousarray((same & (si > tj)).astype(np.float32))
    common["bdones"] = np.ascontiguousarray(same.astype(np.float32))
    for core in range(8):
        b, h = core // 2, core % 2
        m = dict(common)
        m["x8"] = np.ascontiguousarray(np.concatenate([x[b, :HALF], x[b, h * HALF:(h + 1) * HALF]], axis=0))
        m["condB"] = np.ascontiguousarray(np.broadcast_to(c[b].reshape(8, 128).T[:, :, None], (128, 8, 128)))
        m["flag"] = np.full((128, 1), float(h), np.float32)
        m["invc"] = np.ascontiguousarray(np.broadcast_to(np.stack([invc_start, invc_start if h == 0 else invc_mid])[None], (128, 2, 4, 16)))
        maps.append(m)
    return maps


_CACHE = {}


def kernel(**inputs):
    if "nc" not in _CACHE:
        _CACHE["nc"] = Builder().build()
    nc = _CACHE["nc"]
    maps = make_in_maps(inputs)
    res = run_bass_kernel_spmd(nc, maps, core_ids=list(range(8)))
    out = np.zeros((4, SEQ, D), np.float32)
    for core in range(8):
        b, h = core // 2, core % 2
        out[b, h * HALF:(h + 1) * HALF] = res.results[core]["y"]
    return out


CDEC = 0.6065306597126334


def _phase_rwkv_prep(self):
    nc, fw, P, I, S, ar, ps = self.nc, self.fw, self.P, self.I, self.S, self.ar, self.ps
    fw.barrier()
    ar.reset()
    op = fw.op
    W = {}
    for nm in ("rw_wr", "rw_wk", "rw_wv"):
        W[nm] = ar.alloc([8, D], BF16)
        for kc in range(8):
            fw.dma("pool", W[nm][:, kc, :], I[nm][kc * 128:(kc + 1) * 128, :], writes=[nm])
    w1 = ar.alloc([8, 64], BF16)
    a1 = ar.alloc([8, 64], BF16)
    g1 = ar.alloc([8, 160], BF16)
    fw.dma("pool", w1, I["rw_w1"].rearrange("(kc p) n -> p kc n", p=128), writes=["w1"])
    fw.dma("pool", a1, I["rw_a1"].rearrange("(kc p) n -> p kc n", p=128), writes=["a1"])
    fw.dma("pool", g1, I["rw_g1"].rearrange("(kc p) n -> p kc n", p=128), writes=["g1"])
    w2 = ar.alloc([D], BF16)
    a2 = ar.alloc([D], BF16)
    g2a = ar.alloc([D], BF16)
    g2b = ar.alloc([D], BF16)
    fw.dma("pool", w2[0:64, :], I["rw_w2"], writes=["w2"])
    fw.dma("pool", a2[0:64, :], I["rw_a2"], writes=["a2"])
    fw.dma("pool", g2a, I["rw_g2"][0:128, :], writes=["g2a"])
    fw.dma("pool", g2b[0:32, :], I["rw_g2"][128:160, :], writes=["g2b"])
    vec = ar.alloc([14, 8], F32)
    fw.dma("sp", vec[:, 0:13, :], I["rw_vec"], writes=["vec"])
    op("pool", lambda e: e.tensor_scalar(out=vec[:, 13, :], in0=vec[:, 9, :], scalar1=-1.0, scalar2=1.0, op0=ALU.mult, op1=ALU.add), reads=["vec"], writes=["vec"])
    resetm = ar.alloc([1024], F32)
    bdones = ar.alloc([128], BF16)
    fw.dma("sp", resetm, I["resetm"], writes=["resetm"])
    fw.dma("pool", bdones, I["bdones"], writes=["bdones"])

    def vb(i):
        return bc(vec[:, i, :].unsqueeze(2), [128, 8, 128])

    xt = ar.alloc([D], F32)
    at = ar.alloc([D], F32)
    hT = ar.alloc([8, 129], BF16)
    xx = ar.alloc([8, 128], BF16)
    xi = [ar.alloc([8, 128], BF16) for _ in range(2)]
    rS = ar.alloc([8, 128], BF16)
    tw = ar.alloc([128], BF16)
    ta = ar.alloc([128], BF16)
    tg = ar.alloc([128], BF16)
    tg2 = ar.alloc([128], BF16)
    Tf = [ar.alloc([1024], F32) for _ in range(8)]
    T3 = [t.rearrange("p (c t) -> p c t", t=128) for t in Tf]
    MIs = [ar.alloc([8, 896], BF16) for _ in range(2)]
    OWs = [ar.alloc([8, 256], BF16) for _ in range(2)]
    GCs = [ar.alloc([16], F32) for _ in range(2)]
    B0 = ar.alloc([8, 128], BF16)
    lg = ar.alloc([64], F32)
    scr = {"junk": Tf[6][:, 0:512].bitcast(BF16), "ss": lg[:, 32:34], "rstd": lg[:, 34:36], "xn": Tf[6][:, 512:1024].bitcast(BF16),
           "tmp": Tf[7], "ss2": lg[:, 36:38], "rstd2": lg[:, 38:40]}
    scr_keys = ["T6", "T7"]

    op("pool", lambda e: e.memset(hT, 0.0), writes=["hT"])

    bank_rr = [0]

    def nb():
        b = 4 + bank_rr[0] % 4
        bank_rr[0] += 1
        return b

    evac_rr = [0]

    def cp(dst, src, reads, writes):
        evac_rr[0] += 1
        if evac_rr[0] % 2:
            op("act", lambda e: e.activation(out=dst, in_=src, func=AF.Copy), reads=reads, writes=writes)
        else:
            op("dve", lambda e: e.tensor_copy(out=dst, in_=src), reads=reads, writes=writes)

    NTILE = SEQ // 128
    npre, nown = getattr(self, "rw_tiles", (NTILE // 2, NTILE // 2))
    tiles = list(range(npre)) + list(range(NTILE // 2, NTILE // 2 + nown))
    STOP = getattr(self, "rw_stop", 99)
    for ti in tiles:
        own = ti >= NTILE // 2
        par = ti % 2
        kx = "_%d" % par
        MIb, OWb, GC = MIs[par], OWs[par], GCs[par]
        PR, Qt, Kt, Qb, Kb, vS = MIb[:, :, 0:256], MIb[:, :, 256:384], MIb[:, :, 384:512], MIb[:, :, 512:640], MIb[:, :, 640:768], MIb[:, :, 768:896]
        gS, bonus = OWb[:, :, 0:128], OWb[:, :, 128:256]
        fw.dma("sp", xt, S["x1"][ti * 128:(ti + 1) * 128, :], writes=["xt"])
        fw.dma("sp", at, S["acc0"][ti * 128:(ti + 1) * 128, :], writes=["at"])
        self.postnorm_sb2(at, "at", xt, "xt", 1, scr, scr_keys)
        if own:
            fw.dma("sp", S["x2"][ti * 128:(ti + 1) * 128, :], xt, reads=["xt"])
        if ti == NTILE // 2:
            op("pool", lambda e: e.tensor_scalar(out=hT[:, :, 0:1], in0=hT[:, :, 0:1], scalar1=P["flag"][:, 0:1], scalar2=None, op0=ALU.mult), reads=["hT", "flag"], writes=["hT"])
        self.prenorm_T2(xt, "xt", 1, 0, hT, "hT", 1, 4, scr, scr_keys)
        op("pool", lambda e: e.tensor_tensor(out=xx, in0=hT[:, :, 0:128], in1=hT[:, :, 1:129], op=ALU.subtract), reads=["hT"], writes=["xx"])
        if STOP <= 1:
            continue

        def variant(i, buf):
            xb_, xk_ = xi[buf], "xi%d" % buf
            op("dve", lambda e: e.tensor_tensor(out=xb_, in0=xx, in1=vb(i), op=ALU.mult), reads=["xx", "vec"], writes=[xk_])
            op("dve", lambda e: e.tensor_tensor(out=xb_, in0=xb_, in1=hT[:, :, 1:129], op=ALU.add), reads=[xk_, "hT"], writes=[xk_])
            return xb_, xk_

        def proj(wname, xb_, xk_, grp):
            b0 = grp * 2
            for cc in range(8):
                b = b0 + cc // 4
                for kc in range(8):
                    op("pe", lambda e, cc=cc, kc=kc, b=b: e.matmul(ps[b][:, (cc % 4) * 128:(cc % 4 + 1) * 128], W[wname][:, kc, cc * 128:(cc + 1) * 128], xb_[:, kc, :], start=(kc == 0), stop=(kc == 7)),
                       reads=[wname, xk_], writes=["ps%d" % b])
            return b0

        def evac2(b0, fn_eng, mk):
            for hh in range(2):
                mk(hh, ps[b0 + hh][:, :].rearrange("p (c t) -> p c t", t=128), "ps%d" % (b0 + hh))

        kS, kSk = T3[5], "T5"
        sgd, sgk = Tf[0], "T0"
        aS, aSk = T3[6], "T6"
        xb_, xk_ = variant(0, 0)
        b0 = proj("rw_wr", xb_, xk_, 0)
        for hh in range(2):
            op("act", lambda e, hh=hh, b0=b0: e.activation(out=rS[:, hh * 4:(hh + 1) * 4, :], in_=ps[b0 + hh][:, :].rearrange("p (c t) -> p c t", t=128), func=AF.Copy), reads=["ps%d" % (b0 + hh)], writes=["rS"])
        xb_, xk_ = variant(2, 1)
        b0 = proj("rw_wk", xb_, xk_, 1)
        for hh in range(2):
            op("act", lambda e, hh=hh, b0=b0: e.activation(out=kS[:, hh * 4:(hh + 1) * 4, :], in_=ps[b0 + hh][:, :].rearrange("p (c t) -> p c t", t=128), func=AF.Copy), reads=["ps%d" % (b0 + hh)], writes=[kSk])
        xb_, xk_ = variant(3, 0)
        b0 = proj("rw_wv", xb_, xk_, 0)
        for hh in range(2):
            op("act", lambda e, hh=hh, b0=b0: e.activation(out=vS[:, hh * 4:(hh + 1) * 4, :], in_=ps[b0 + hh][:, :].rearrange("p (c t) -> p c t", t=128), func=AF.Copy), reads=["ps%d" % (b0 + hh)], writes=["vS" + kx])
        xb_, xk_ = variant(1, 1)
        b = nb()
        for kc in range(8):
            op("pe", lambda e, kc=kc, b=b, xb_=xb_: e.matmul(ps[b][0:64, 0:128], w1[:, kc, :], xb_[:, kc, :], start=(kc == 0), stop=(kc == 7)), reads=["w1", xk_], writes=["ps%d" % b])
        op("act", lambda e, b=b: e.activation(out=tw[0:64, :], in_=ps[b][0:64, 0:128], func=AF.Tanh), reads=["ps%d" % b], writes=["tw"])
        b0 = 2
        for cc in range(8):
            bb = b0 + cc // 4
            op("pe", lambda e, cc=cc, bb=bb: e.matmul(ps[bb][:, (cc % 4) * 128:(cc % 4 + 1) * 128], w2[0:64, cc * 128:(cc + 1) * 128], tw[0:64, :], start=True, stop=True), reads=["w2", "tw"], writes=["ps%d" % bb])
        for hh in range(2):
            op("dve", lambda e, hh=hh: e.tensor_tensor(out=T3[0][:, hh * 4:(hh + 1) * 4, :], in0=ps[2 + hh][:, :].rearrange("p (c t) -> p c t", t=128),
                                                      in1=bc(vec[:, 6, hh * 4:(hh + 1) * 4].unsqueeze(2), [128, 4, 128]), op=ALU.add), reads=["ps%d" % (2 + hh), "vec"], writes=[sgk])
        op("act", lambda e: e.activation(out=sgd, in_=sgd, func=AF.Sigmoid), reads=[sgk], writes=[sgk])
        xb_, xk_ = variant(4, 0)
        b = nb()
        for kc in range(8):
            op("pe", lambda e, kc=kc, b=b, xb_=xb_: e.matmul(ps[b][0:64, 0:128], a1[:, kc, :], xb_[:, kc, :], start=(kc == 0), stop=(kc == 7)), reads=["a1", xk_], writes=["ps%d" % b])
        op("act", lambda e, b=b: e.activation(out=ta[0:64, :], in_=ps[b][0:64, 0:128], func=AF.Copy), reads=["ps%d" % b], writes=["ta"])
        for cc in range(8):
            bb = cc // 4
            op("pe", lambda e, cc=cc, bb=bb: e.matmul(ps[bb][:, (cc % 4) * 128:(cc % 4 + 1) * 128], a2[0:64, cc * 128:(cc + 1) * 128], ta[0:64, :], start=True, stop=True), reads=["a2", "ta"], writes=["ps%d" % bb])
        for hh in range(2):
            op("dve", lambda e, hh=hh: e.tensor_tensor(out=aS[:, hh * 4:(hh + 1) * 4, :], in0=ps[hh][:, :].rearrange("p (c t) -> p c t", t=128),
                                                      in1=bc(vec[:, 7, hh * 4:(hh + 1) * 4].unsqueeze(2), [128, 4, 128]), op=ALU.add), reads=["ps%d" % hh, "vec"], writes=[aSk])
        op("act", lambda e: e.activation(out=Tf[6], in_=Tf[6], func=AF.Sigmoid), reads=[aSk], writes=[aSk])
        if own:
            xb_, xk_ = variant(5, 1)
            b = nb()
            for kc in range(8):
                op("pe", lambda e, kc=kc, b=b, xb_=xb_: e.matmul(ps[b][:, 0:128], g1[:, kc, 0:128], xb_[:, kc, :], start=(kc == 0), stop=(kc == 7)), reads=["g1", xk_], writes=["ps%d" % b])
            for kc in range(8):
                op("pe", lambda e, kc=kc, b=b, xb_=xb_: e.matmul(ps[b][0:32, 128:256], g1[:, kc, 128:160], xb_[:, kc, :], start=(kc == 0), stop=(kc == 7)), reads=["g1", xk_], writes=["ps%d" % b])
            op("act", lambda e, b=b: e.activation(out=tg, in_=ps[b][:, 0:128], func=AF.Sigmoid), reads=["ps%d" % b], writes=["tg"])
            op("act", lambda e, b=b: e.activation(out=tg2[0:32, :], in_=ps[b][0:32, 128:256], func=AF.Sigmoid), reads=["ps%d" % b], writes=["tg2"])
            for cc in range(8):
                bb = 2 + cc // 4
                op("pe", lambda e, cc=cc, bb=bb: e.matmul(ps[bb][:, (cc % 4) * 128:(cc % 4 + 1) * 128], g2a[:, cc * 128:(cc + 1) * 128], tg, start=True, stop=False), reads=["g2a", "tg"], writes=["ps%d" % bb])
                op("pe", lambda e, cc=cc, bb=bb: e.matmul(ps[bb][:, (cc % 4) * 128:(cc % 4 + 1) * 128], g2b[0:32, cc * 128:(cc + 1) * 128], tg2[0:32, :], start=False, stop=True), reads=["g2b", "tg2"], writes=["ps%d" % bb])
            for hh in range(2):
                op("act", lambda e, hh=hh: e.activation(out=gS[:, hh * 4:(hh + 1) * 4, :], in_=ps[2 + hh][:, :].rearrange("p (c t) -> p c t", t=128), func=AF.Copy), reads=["ps%d" % (2 + hh)], writes=["gS" + kx])

        if STOP <= 2:
            continue
        Lp, Lpk = Tf[1], "T1"
        op("dve", lambda e: e.tensor_tensor_scan(out=Lp, data0=resetm, data1=sgd, initial=0.0, op0=ALU.mult, op1=ALU.add), reads=["resetm", sgk], writes=[Lpk])
        Lm, Lmk = Tf[2], "T2"
        op("dve", lambda e: e.tensor_tensor(out=Lm, in0=Lp, in1=sgd, op=ALU.subtract), reads=[Lpk, sgk], writes=[Lmk])
        Ld, Ldk = Tf[0], "T0"
        Lp64 = Lp.rearrange("p (c t) -> p c t", t=64)
        op("pool", lambda e: e.tensor_tensor(out=Ld.rearrange("p (c t) -> p c t", t=64), in0=bc(Lp64[:, :, 63:64], [128, 16, 64]), in1=Lp64, op=ALU.subtract), reads=[Lpk, Lmk], writes=[Ldk])
        E1, E1k = Tf[3], "T3"
        E2, E2k = Tf[4], "T4"
        op("act", lambda e: e.activation(out=E1, in_=Lp, func=AF.Exp, scale=-CDEC), reads=[Lpk], writes=[E1k])
        op("act", lambda e: e.activation(out=E2, in_=Lp, func=AF.Exp, scale=CDEC), reads=[Lpk], writes=[E2k])
        op("act", lambda e: e.activation(out=Lm, in_=Lm, func=AF.Exp, scale=-CDEC), reads=[Lmk], writes=[Lmk])
        op("act", lambda e: e.activation(out=Ld, in_=Ld, func=AF.Exp, scale=-CDEC), reads=[Ldk], writes=[Ldk])
        E3, E3k, E4, E4k = Lm, Lmk, Ld, Ldk
        op("pool", lambda e: e.tensor_copy(out=GC, in_=E1.rearrange("p (c t) -> p c t", t=64)[:, :, 63]), reads=[E1k], writes=["GC" + kx])
        kk, kkk = T3[1], "T1"
        op("dve", lambda e: e.tensor_tensor(out=kk, in0=kS, in1=vb(8), op=ALU.mult), reads=[kSk, "vec", E1k, E2k], writes=[kkk])
        op("pool", lambda e: e.tensor_tensor(out=B0, in0=kk, in1=kk, op=ALU.mult), reads=[kkk], writes=["B0"])
        for cc in range(8):
            bb = cc // 4
            op("pe", lambda e, cc=cc, bb=bb: e.matmul(ps[bb][:, (cc % 4) * 128:(cc % 4 + 1) * 128], bdones, B0[:, cc, :], start=True, stop=True), reads=["bdones", "B0"], writes=["ps%d" % bb])
        rn, rnk = T3[7], "T7"
        for hh in range(2):
            op("act", lambda e, hh=hh: e.activation(out=rn[:, hh * 4:(hh + 1) * 4, :], in_=ps[hh][:, :].rearrange("p (c t) -> p c t", t=128), func=AF.Sqrt), reads=["ps%d" % hh], writes=[rnk])
        op("dve", lambda e: e.reciprocal(out=Tf[7], in_=Tf[7]), reads=[rnk], writes=[rnk])
        op("dve", lambda e: e.tensor_tensor(out=kk, in0=kk, in1=rn, op=ALU.mult), reads=[kkk, rnk], writes=[kkk])
        km, kmk = T3[7], "T7"
        op("dve", lambda e: e.tensor_tensor(out=km, in0=aS, in1=vb(9), op=ALU.mult), reads=[aSk, "vec", kkk], writes=[kmk])
        op("pool", lambda e: e.tensor_tensor(out=km, in0=km, in1=vb(13), op=ALU.add), reads=[kmk, "vec"], writes=[kmk])
        op("dve", lambda e: e.tensor_tensor(out=km, in0=km, in1=kS, op=ALU.mult), reads=[kmk, kSk], writes=[kmk])
        q, qk = aS, aSk
        op("dve", lambda e: e.tensor_tensor(out=q, in0=aS, in1=kk, op=ALU.mult), reads=[aSk, kkk], writes=[qk])
        op("dve", lambda e: e.scalar_tensor_tensor(out=PR[:, :, 0:128], in0=kk, scalar=-1.0, in1=T3[2], op0=ALU.mult, op1=ALU.mult), reads=[kkk, E3k], writes=["PR" + kx])
        if own:
            op("pool", lambda e: e.tensor_tensor(out=PR[:, :, 128:256], in0=rS, in1=T3[3], op=ALU.mult), reads=["rS", E1k], writes=["PR" + kx])
        else:
            op("pool", lambda e: e.tensor_copy(out=PR[:, :, 128:256], in_=rS), reads=["rS"], writes=["PR" + kx])
        op("dve", lambda e: e.tensor_tensor(out=Qt, in0=q, in1=T3[4], op=ALU.mult), reads=[qk, E2k], writes=["Qt" + kx])
        op("pool", lambda e: e.tensor_tensor(out=Kt, in0=km, in1=T3[4], op=ALU.mult), reads=[kmk, E2k], writes=["Kt" + kx])
        op("dve", lambda e: e.tensor_tensor(out=Qb, in0=q, in1=T3[0], op=ALU.mult), reads=[qk, E4k], writes=["Qb" + kx])
        op("pool", lambda e: e.tensor_tensor(out=Kb, in0=km, in1=T3[0], op=ALU.mult), reads=[kmk, E4k], writes=["Kb" + kx])
        if own:
            op("pool", lambda e: e.tensor_tensor(out=T3[5], in0=km, in1=vb(10), op=ALU.mult), reads=[kmk, "vec", kSk], writes=["T5"])
            op("pool", lambda e: e.tensor_tensor(out=B0, in0=T3[5], in1=rS, op=ALU.mult), reads=["T5", "rS"], writes=["B0"])
            for cc in range(8):
                bb = 2 + cc // 4
                op("pe", lambda e, cc=cc, bb=bb: e.matmul(ps[bb][:, (cc % 4) * 128:(cc % 4 + 1) * 128], bdones, B0[:, cc, :], start=True, stop=True), reads=["bdones", "B0"], writes=["ps%d" % bb])
            for hh in range(2):
                op("dve", lambda e, hh=hh: e.tensor_tensor(out=bonus[:, hh * 4:(hh + 1) * 4, :], in0=ps[2 + hh][:, :].rearrange("p (c t) -> p c t", t=128), in1=vS[:, hh * 4:(hh + 1) * 4, :], op=ALU.mult),
                   reads=["ps%d" % (2 + hh), "vS" + kx], writes=["bonus" + kx])

        op("pool", lambda e: e.tensor_copy(out=hT[:, :, 0:1], in_=hT[:, :, 128:129]), reads=["hT"], writes=["hT"])
        fw.dma("sp", S["MI"][ti], MIb, reads=["PR" + kx, "Qt" + kx, "Kt" + kx, "Qb" + kx, "Kb" + kx, "vS" + kx])
        fw.dma("sp", S["GCd"][ti], GC, reads=["GC" + kx])
        if own:
            fw.dma("sp", S["OW"][ti - NTILE // 2], OWb, reads=["gS" + kx, "bonus" + kx])


def _phase_rwkv_chain(self):
    nc, fw, P, I, S, ar, ps = self.nc, self.fw, self.P, self.I, self.S, self.ar, self.ps
    fw.barrier()
    ar.reset()
    op = fw.op
    W = {"rw_wo": ar.alloc([8, D], BF16)}
    for kc in range(8):
        fw.dma("pool", W["rw_wo"][:, kc, :], I["rw_wo"][kc * 128:(kc + 1) * 128, :], writes=["rw_wo"])
    vec = ar.alloc([14, 8], F32)
    fw.dma("sp", vec[:, 0:13, :], I["rw_vec"], writes=["vec"])
    router = ar.alloc([8, 8], F32)
    fw.dma("sp", router, I["moe_router"].rearrange("(kc p) e -> p kc e", p=128), writes=["router"])
    mask1 = ar.alloc([2, 128], F32)
    maskT = ar.alloc([128], F32)
    bdones = ar.alloc([128], BF16)
    fw.dma("sp", mask1, I["mask1"], writes=["mask1"])
    fw.dma("sp", maskT, I["maskT"], writes=["maskT"])
    fw.dma("pool", bdones, I["bdones"], writes=["bdones"])

    def vb(i):
        return bc(vec[:, i, :].unsqueeze(2), [128, 8, 128])

    MIs = [ar.alloc([8, 896], BF16) for _ in range(2)]
    OWs = [ar.alloc([8, 256], BF16) for _ in range(2)]
    GCs = [ar.alloc([16], F32) for _ in range(2)]
    xts = [ar.alloc([D], F32) for _ in range(2)]
    Tf = {i: ar.alloc([1024], F32) for i in (1, 2, 6, 7)}
    T3 = {i: t.rearrange("p (c t) -> p c t", t=128) for i, t in Tf.items()}
    B0 = ar.alloc([8, 128], BF16)
    yg = ar.alloc([8, 128], BF16)
    Ysb = ar.alloc([8, 128], F32)
    Sbd = ar.alloc([8, 128], BF16)
    h32 = ar.alloc([8, 128], F32)
    hb = ar.alloc([8, 128], BF16)
    lg = ar.alloc([64], F32)
    scr = {"junk": Tf[6][:, 0:512].bitcast(BF16), "ss": lg[:, 32:34], "rstd": lg[:, 34:36], "xn": Tf[6][:, 512:1024].bitcast(BF16),
           "tmp": Tf[7], "ss2": lg[:, 36:38], "rstd2": lg[:, 38:40]}
    scr_keys = ["T6", "T7"]
    NSET = 8
    SETS = []
    for si in range(NSET):
        d = {}
        d["RHS"] = ar.alloc([2, 128], BF16)
        d["SPLA"] = ar.alloc([3, 128], BF16)
        d["SPLB"] = ar.alloc([3, 128], BF16)
        d["SP2A"] = ar.alloc([2, 128], BF16)
        d["SP2B"] = ar.alloc([2, 128], BF16)
        d["MA1"] = ar.alloc([2, 2, 128], BF16)
        d["MA2"] = ar.alloc([2, 2, 128], BF16)
        d["MT"] = ar.alloc([2, 128], BF16)
        d["Xb_"] = [ar.alloc([2, 128], BF16) for _ in range(2)]
        d["XTb_"] = [ar.alloc([2, 128], BF16) for _ in range(2)]
        d["Tb_"] = [ar.alloc([2, 128], BF16) for _ in range(2)]
        d["Rh"] = ar.alloc([128], BF16)
        d["Yloc"] = ar.alloc([128], F32)
        d["PQ"] = ar.alloc([2, 128], BF16)
        d["Sloc"] = ar.alloc([2, 128], BF16)
        SETS.append(d)
        for k_ in ("SPLA", "SPLB", "SP2A", "SP2B"):
            op("pool", lambda e, t_=d[k_]: e.memset(t_, 0.0), writes=[k_ + "_s%d" % si])
    op("pool", lambda e: e.memset(Sbd, 0.0), writes=["Sbd%d" % c for c in range(8)])

    bank_rr = [0]

    def nb():
        b = bank_rr[0] % 8
        bank_rr[0] += 1
        return b

    evac_rr = [0]

    def cp(dst, src, reads, writes):
        evac_rr[0] += 1
        if evac_rr[0] % 3:
            op("act", lambda e: e.activation(out=dst, in_=src, func=AF.Copy), reads=reads, writes=writes)
        else:
            op("dve", lambda e: e.tensor_copy(out=dst, in_=src), reads=reads, writes=writes)

    def mach(cc, own, kx, PR, Qt, Kt, Qb, Kb, vS, GC):
        d = SETS[cc % NSET]
        sx = "_s%d" % (cc % NSET)
        RHS, SPLA, SPLB, SP2A, SP2B, MA1, MA2, MT = d["RHS"], d["SPLA"], d["SPLB"], d["SP2A"], d["SP2B"], d["MA1"], d["MA2"], d["MT"]
        Xb_, XTb_, Tb_, Rh, Yloc, PQ, Sloc = d["Xb_"], d["XTb_"], d["Tb_"], d["Rh"], d["Yloc"], d["PQ"], d["Sloc"]
        sk = "Sbd%d" % cc
        b = nb()
        tp = ps[b][:, :].bitcast(BF16)
        srcs = (PR[:, cc, 0:128], Qb[:, cc, :], Kb[:, cc, :], vS[:, cc, :])
        skeys = ("PR" + kx, "Qb" + kx, "Kb" + kx, "vS" + kx)
        for i4 in range(4):
            op("pe", lambda e, i4=i4, tp=tp, srcs=srcs: e.transpose(tp[:, i4 * 128:(i4 + 1) * 128], srcs[i4], P["identb"][:]), reads=[skeys[i4], "identb"], writes=["ps%d" % b])
        tpv = tp[:, 128:512].rearrange("p (i j) -> p i j", j=128)
        op("act", lambda e, tpv=tpv: e.activation(out=SPLA[:, :, 0:64], in_=tpv[:, :, 0:64], func=AF.Copy), reads=["ps%d" % b], writes=["SPLA" + sx])
        op("dve", lambda e, tpv=tpv: e.tensor_copy(out=SPLB[:, :, 64:128], in_=tpv[:, :, 64:128]), reads=["ps%d" % b], writes=["SPLB" + sx])
        op("act", lambda e, tp=tp: e.activation(out=RHS[:, :, 0:64], in_=tp[:, 0:128].rearrange("p (h j) -> p h j", h=2), func=AF.Copy), reads=["ps%d" % b], writes=["RHS" + sx])
        yield
        bAh = [nb(), nb()]
        bBh = [nb(), nb()]
        for h in range(2):
            R_ = slice(64 * h, 64 * h + 64)
            op("pe", lambda e, h=h, R_=R_: e.matmul(ps[bAh[h]][:, 0:256], Qt[R_, cc, :], PR[R_, cc, :], start=True, stop=True), reads=["Qt" + kx, "PR" + kx], writes=["ps%d" % bAh[h]])
            op("pe", lambda e, h=h, R_=R_: e.matmul(ps[bAh[h]][:, 256:384], PR[R_, cc, 0:128], Qt[R_, cc, :], start=True, stop=True), reads=["Qt" + kx, "PR" + kx], writes=["ps%d" % bAh[h]])
            op("pe", lambda e, h=h, R_=R_: e.matmul(ps[bBh[h]][:, 0:256], Kt[R_, cc, :], PR[R_, cc, :], start=True, stop=True), reads=["Kt" + kx, "PR" + kx], writes=["ps%d" % bBh[h]])
        for h in range(2):
            op("dve", lambda e, h=h: e.tensor_tensor(out=MA1[:, h, :, :], in0=ps[bAh[h]][:, 0:256].rearrange("p (w t) -> p w t", w=2), in1=mask1, op=ALU.mult), reads=["ps%d" % bAh[h], "mask1"], writes=["MA1" + sx])
            op("dve", lambda e, h=h: e.tensor_tensor(out=MT[:, h, :], in0=ps[bAh[h]][:, 256:384], in1=maskT, op=ALU.mult), reads=["ps%d" % bAh[h], "maskT"], writes=["MT" + sx])
            op("dve", lambda e, h=h: e.tensor_tensor(out=MA2[:, h, :, :], in0=ps[bBh[h]][:, 0:256].rearrange("p (w t) -> p w t", w=2), in1=mask1, op=ALU.mult), reads=["ps%d" % bBh[h], "mask1"], writes=["MA2" + sx])
        yield
        Tc, Tck = Tb_[0], "Tb0" + sx
        op("pool", lambda e, Tc=Tc: e.tensor_tensor(out=Tc, in0=MA1[:, :, 0, :], in1=bc(P["ident"][:].unsqueeze(1), [128, 2, 128]), op=ALU.add), reads=["MA1" + sx, "ident"], writes=[Tck])
        Xc = [MA1[:, 0, 0, :], MA1[:, 1, 0, :]]
        Xck = "MA1" + sx
        XTc = [MT[:, 0, :], MT[:, 1, :]]
        XTck = "MT" + sx
        nlev = 5
        for lv in range(nlev):
            last = (lv == nlev - 1)
            Xn, Xnk = Xb_[lv % 2], "Xb%d" % (lv % 2) + sx
            XTn, XTnk = XTb_[lv % 2], "XTb%d" % (lv % 2) + sx
            Tn, Tnk = Tb_[(lv + 1) % 2], "Tb%d" % ((lv + 1) % 2) + sx
            if not last:
                bX = nb()
                for h in range(2):
                    op("pe", lambda e, h=h, bX=bX, XTc=XTc, Xc=Xc: e.matmul(ps[bX][:, h * 128:(h + 1) * 128], XTc[h], Xc[h], start=True, stop=True), reads=[Xck, XTck], writes=["ps%d" % bX])
                cp(Xn, ps[bX][:, 0:256].rearrange("p (h t) -> p h t", h=2), ["ps%d" % bX], [Xnk])
            bXT = nb()
            for h in range(2):
                op("pe", lambda e, h=h, bXT=bXT, XTc=XTc, Xc=Xc: e.matmul(ps[bXT][:, h * 128:(h + 1) * 128], Xc[h], XTc[h], start=True, stop=True), reads=[Xck, XTck], writes=["ps%d" % bXT])
            cp(XTn, ps[bXT][:, 0:256].rearrange("p (h t) -> p h t", h=2), ["ps%d" % bXT], [XTnk])
            bT = nb()
            for h in range(2):
                op("pe", lambda e, h=h, bT=bT, XTn=XTn, Tc=Tc: e.matmul(ps[bT][:, h * 128:(h + 1) * 128], XTn[:, h, :], Tc[:, h, :], start=True, stop=True), reads=[XTnk, Tck], writes=["ps%d" % bT])
            op("dve", lambda e, bT=bT, Tn=Tn, Tc=Tc: e.tensor_tensor(out=Tn, in0=ps[bT][:, 0:256].rearrange("p (h t) -> p h t", h=2), in1=Tc, op=ALU.add), reads=["ps%d" % bT, Tck], writes=[Tnk])
            Tc, Tck = Tn, Tnk
            yield
            if not last:
                Xc, Xck = [Xn[:, 0, :], Xn[:, 1, :]], Xnk
            XTc, XTck = [XTn[:, 0, :], XTn[:, 1, :]], XTnk
        yield
        bW = nb()
        op("pe", lambda e, bW=bW: e.matmul(ps[bW][:, 0:64], MA2[:, 0, 0, :], SPLA[:, 2, 0:64], start=True, stop=True), reads=["MA2" + sx, "SPLA" + sx], writes=["ps%d" % bW])
        op("pe", lambda e, bW=bW: e.matmul(ps[bW][:, 64:128], MA2[:, 1, 0, :], SPLB[:, 2, 64:128], start=True, stop=True), reads=["MA2" + sx, "SPLB" + sx], writes=["ps%d" % bW])
        cp(RHS[:, :, 64:128], ps[bW][:, 0:128].rearrange("p (h j) -> p h j", h=2), ["ps%d" % bW], ["RHS" + sx])
        yield
        bU = nb()
        for h in range(2):
            op("pe", lambda e, h=h, bU=bU, Tc=Tc: e.matmul(ps[bU][:, h * 128:(h + 1) * 128], Tc[:, h, :], RHS[:, h, :], start=True, stop=True), reads=[Tck, "RHS" + sx], writes=["ps%d" % bU])
        op("act", lambda e, bU=bU: e.activation(out=SP2A[:, :, 0:64], in_=ps[bU][:, 0:128].rearrange("p (w j) -> p w j", w=2), func=AF.Copy), reads=["ps%d" % bU], writes=["SP2A" + sx])
        op("dve", lambda e, bU=bU: e.tensor_copy(out=SP2B[:, :, 64:128], in_=ps[bU][:, 128:256].rearrange("p (w j) -> p w j", w=2)), reads=["ps%d" % bU], writes=["SP2B" + sx])
        yield
        if own:
            bR = nb()
            op("pe", lambda e, bR=bR: e.matmul(ps[bR][:, 0:128], SP2A[:, 0, :], MA1[:, 0, 1, :], start=True, stop=False), reads=["SP2A" + sx, "MA1" + sx], writes=["ps%d" % bR])
            op("pe", lambda e, bR=bR: e.matmul(ps[bR][:, 0:128], SP2B[:, 0, :], MA1[:, 1, 1, :], start=False, stop=True), reads=["SP2B" + sx, "MA1" + sx], writes=["ps%d" % bR])
            op("dve", lambda e, bR=bR: e.tensor_tensor(out=Rh, in0=ps[bR][:, 0:128], in1=PR[:, cc, 128:256], op=ALU.add), reads=["ps%d" % bR, "PR" + kx], writes=["Rh" + sx])
            bY = nb()
            op("pe", lambda e, bY=bY: e.matmul(ps[bY][:, 0:128], SP2A[:, 1, :], MA1[:, 0, 1, :], start=True, stop=False), reads=["SP2A" + sx, "MA1" + sx], writes=["ps%d" % bY])
            op("pe", lambda e, bY=bY: e.matmul(ps[bY][:, 0:128], SP2B[:, 1, :], MA1[:, 1, 1, :], start=False, stop=False), reads=["SP2B" + sx, "MA1" + sx], writes=["ps%d" % bY])
            op("pe", lambda e, bY=bY: e.matmul(ps[bY][:, 0:128], SPLA[:, 2, :], MA2[:, 0, 1, :], start=False, stop=False), reads=["SPLA" + sx, "MA2" + sx], writes=["ps%d" % bY])
            op("pe", lambda e, bY=bY: e.matmul(ps[bY][:, 0:128], SPLB[:, 2, :], MA2[:, 1, 1, :], start=False, stop=True), reads=["SPLB" + sx, "MA2" + sx], writes=["ps%d" % bY])
            cp(Yloc, ps[bY][:, 0:128], ["ps%d" % bY], ["Yloc" + sx])
        yield
        bQc = [nb(), nb()]
        for c in range(2):
            R_ = slice(64 * c, 64 * c + 64)
            bq = bQc[c]
            op("pe", lambda e, R_=R_, bq=bq: e.matmul(ps[bq][:, 0:128], SP2A[R_, 0, :], SPLA[R_, 0, :], start=True, stop=False), reads=["SP2A" + sx, "SPLA" + sx], writes=["ps%d" % bq])
            op("pe", lambda e, R_=R_, bq=bq: e.matmul(ps[bq][:, 0:128], SP2B[R_, 0, :], SPLB[R_, 0, :], start=False, stop=True), reads=["SP2B" + sx, "SPLB" + sx], writes=["ps%d" % bq])
            op("pe", lambda e, R_=R_, bq=bq: e.matmul(ps[bq][:, 128:256], SPLA[R_, 0, :], SP2A[R_, 1, :], start=True, stop=False), reads=["SP2A" + sx, "SPLA" + sx], writes=["ps%d" % bq])
            op("pe", lambda e, R_=R_, bq=bq: e.matmul(ps[bq][:, 128:256], SPLB[R_, 0, :], SP2B[R_, 1, :], start=False, stop=False), reads=["SP2B" + sx, "SPLB" + sx], writes=["ps%d" % bq])
            op("pe", lambda e, R_=R_, bq=bq: e.matmul(ps[bq][:, 128:256], SPLA[R_, 1, :], SPLA[R_, 2, :], start=False, stop=False), reads=["SPLA" + sx], writes=["ps%d" % bq])
            op("pe", lambda e, R_=R_, bq=bq: e.matmul(ps[bq][:, 128:256], SPLB[R_, 1, :], SPLB[R_, 2, :], start=False, stop=True), reads=["SPLB" + sx], writes=["ps%d" % bq])
        for c in range(2):
            cp(PQ[:, c, :], ps[bQc[c]][:, 0:128], ["ps%d" % bQc[c]], ["PQ" + sx])
            cp(Sloc[:, c, :], ps[bQc[c]][:, 128:256], ["ps%d" % bQc[c]], ["Sloc" + sx])
        yield
        for c in range(2):
            yield
            if own:
                bYc = nb()
                op("pe", lambda e, c=c, bYc=bYc: e.matmul(ps[bYc][:, 0:64], Sbd[:, cc, :], Rh[:, c * 64:(c + 1) * 64], start=True, stop=True), reads=[sk, "Rh" + sx], writes=["ps%d" % bYc])
                op("dve", lambda e, c=c, bYc=bYc: e.tensor_tensor(out=Ysb[:, cc, c * 64:(c + 1) * 64], in0=ps[bYc][:, 0:64], in1=Yloc[:, c * 64:(c + 1) * 64], op=ALU.add), reads=["ps%d" % bYc, "Yloc" + sx], writes=["Ysb"])
            bS2 = nb()
            op("pe", lambda e, c=c, bS2=bS2: e.matmul(ps[bS2][:, 0:128], PQ[:, c, :], Sbd[:, cc, :], start=True, stop=False), reads=["PQ" + sx, sk], writes=["ps%d" % bS2])
            op("pe", lambda e, c=c, bS2=bS2: e.matmul(ps[bS2][:, 0:128], P["identb"][:], Sloc[:, c, :], start=False, stop=True), reads=["identb", "Sloc" + sx], writes=["ps%d" % bS2])
            op("dve", lambda e, c=c, bS2=bS2: e.scalar_tensor_tensor(out=Sbd[:, cc, :], in0=Sbd[:, cc, :], scalar=GC[:, cc * 2 + c:cc * 2 + c + 1], in1=ps[bS2][:, 0:128], op0=ALU.mult, op1=ALU.add),
               reads=[sk, "GC" + kx, "ps%d" % bS2], writes=[sk])

    NTILE = SEQ // 128
    npre, nown = getattr(self, "rw_tiles", (NTILE // 2, NTILE // 2))
    tiles = list(range(npre)) + list(range(NTILE // 2, NTILE // 2 + nown))
    for tn, ti in enumerate(tiles):
        own = ti >= NTILE // 2
        par = tn % 2
        kx = "_%d" % par
        MIb, OWb, GC, xt = MIs[par], OWs[par], GCs[par], xts[par]
        xk = "xt" + kx
        PR, Qt, Kt, Qb, Kb, vS = MIb[:, :, 0:256], MIb[:, :, 256:384], MIb[:, :, 384:512], MIb[:, :, 512:640], MIb[:, :, 640:768], MIb[:, :, 768:896]
        gS, bonus = OWb[:, :, 0:128], OWb[:, :, 128:256]
        fw.dma("sp", MIb, S["MI"][ti], writes=["PR" + kx, "Qt" + kx, "Kt" + kx, "Qb" + kx, "Kb" + kx, "vS" + kx])
        fw.dma("sp", GC, S["GCd"][ti], writes=["GC" + kx])
        if own:
            fw.dma("sp", OWb, S["OW"][ti - NTILE // 2], writes=["gS" + kx, "bonus" + kx])
            fw.dma("sp", xt, S["x2"][ti * 128:(ti + 1) * 128, :], writes=[xk])
        if ti == NTILE // 2:
            op("pool", lambda e: e.tensor_scalar(out=Sbd, in0=Sbd, scalar1=P["flag"][:, 0:1], scalar2=None, op0=ALU.mult),
               reads=["flag"] + ["Sbd%d" % c for c in range(8)], writes=["Sbd%d" % c for c in range(8)])
        gens = [mach(cc, own, kx, PR, Qt, Kt, Qb, Kb, vS, GC) for cc in range(8)]
        while gens:
            for g in list(gens):
                try:
                    next(g)
                except StopIteration:
                    gens.remove(g)
        if not own:
            continue
        op("act", lambda e: e.activation(out=B0, in_=Ysb, func=AF.Copy), reads=["Ysb"], writes=["B0"])
        for cc in range(8):
            bb = cc // 4
            op("pe", lambda e, cc=cc, bb=bb: e.matmul(ps[bb][:, (cc % 4) * 128:(cc % 4 + 1) * 128], bdones, B0[:, cc, :], start=True, stop=True), reads=["bdones", "B0"], writes=["ps%d" % bb])
        dd, ddk = T3[1], "T1"
        for hh in range(2):
            op("dve", lambda e, hh=hh: e.scalar_tensor_tensor(out=dd[:, hh * 4:(hh + 1) * 4, :], in0=ps[hh][:, :].rearrange("p (c t) -> p c t", t=128), scalar=-1.0 / 64, in1=Ysb[:, hh * 4:(hh + 1) * 4, :], op0=ALU.mult, op1=ALU.add),
               reads=["ps%d" % hh, "Ysb"], writes=[ddk])
        op("pool", lambda e: e.tensor_tensor(out=B0, in0=dd, in1=dd, op=ALU.mult), reads=[ddk], writes=["B0"])
        for cc in range(8):
            bb = 2 + cc // 4
            op("pe", lambda e, cc=cc, bb=bb: e.matmul(ps[bb][:, (cc % 4) * 128:(cc % 4 + 1) * 128], bdones, B0[:, cc, :], start=True, stop=True), reads=["bdones", "B0"], writes=["ps%d" % bb])
        rs, rsk = T3[2], "T2"
        for hh in range(2):
            op("act", lambda e, hh=hh: e.activation(out=rs[:, hh * 4:(hh + 1) * 4, :], in_=ps[2 + hh][:, :].rearrange("p (c t) -> p c t", t=128), func=AF.Sqrt, scale=1.0 / 64, bias=P["eps"][:, 1:2]), reads=["ps%d" % (2 + hh), "eps"], writes=[rsk])
        op("dve", lambda e: e.reciprocal(out=Tf[2], in_=Tf[2]), reads=[rsk], writes=[rsk])
        op("pool", lambda e: e.tensor_tensor(out=dd, in0=dd, in1=rs, op=ALU.mult), reads=[ddk, rsk], writes=[ddk])
        op("pool", lambda e: e.tensor_tensor(out=dd, in0=dd, in1=vb(11), op=ALU.mult), reads=[ddk, "vec"], writes=[ddk])
        op("pool", lambda e: e.tensor_tensor(out=dd, in0=dd, in1=vb(12), op=ALU.add), reads=[ddk, "vec"], writes=[ddk])
        op("dve", lambda e: e.tensor_tensor(out=dd, in0=dd, in1=bonus, op=ALU.add), reads=[ddk, "bonus" + kx], writes=[ddk])
        op("dve", lambda e: e.tensor_tensor(out=yg, in0=dd, in1=gS, op=ALU.mult), reads=[ddk, "gS" + kx], writes=["yg"])
        for n in range(2):
            for kc in range(8):
                op("pe", lambda e, n=n, kc=kc: e.matmul(ps[2 + n][:, :], yg[:, kc, :], W["rw_wo"][:, kc, n * 512:(n + 1) * 512], start=(kc == 0), stop=(kc == 7)), reads=["yg", "rw_wo"], writes=["ps%d" % (2 + n)])
        self.postnorm_res2((2, 3), xt, xk, 2, scr, scr_keys)
        to = ti - NTILE // 2
        fw.dma("sp", S["x3"][to * 128:(to + 1) * 128, :], xt, reads=[xk])
        junk, ss, rstd = scr["junk"], scr["ss"], scr["rstd"]
        op("act", lambda e: e.activation(out=junk, in_=xt, func=AF.Square, accum_out=ss[:, 0:1]), reads=[xk], writes=["T6", "lg"])
        op("act", lambda e: e.activation(out=rstd[:, 0:1], in_=ss[:, 0:1], func=AF.Sqrt, scale=1.0 / D, bias=P["eps"][:, 0:1]), reads=["lg", "eps"], writes=["lg"])
        op("dve", lambda e: e.reciprocal(out=rstd[:, 0:1], in_=rstd[:, 0:1]), reads=["lg"], writes=["lg"])
        xn32 = Tf[7]
        op("act", lambda e: e.activation(out=xn32, in_=xt, func=AF.Copy, scale=rstd[:, 0:1]), reads=[xk, "lg"], writes=["T7"])
        for kc in range(8):
            bb = kc // 4
            op("pe", lambda e, kc=kc, bb=bb: e.transpose(ps[bb][:, (kc % 4) * 128:(kc % 4 + 1) * 128], xn32[:, kc * 128:(kc + 1) * 128], P["ident"][:]), reads=["T7", "ident"], writes=["ps%d" % bb])
        G1 = P["modT"][:, 4 + 2, :]
        sh = P["modT"][:, 4 + 3, :]
        for hh in range(2):
            op("dve", lambda e, hh=hh: e.tensor_tensor(out=h32[:, hh * 4:(hh + 1) * 4, :], in0=ps[hh][:, :].rearrange("p (c t) -> p c t", t=128), in1=bc(G1[:, hh * 4:(hh + 1) * 4].unsqueeze(2), [128, 4, 128]), op=ALU.mult),
               reads=["ps%d" % hh, "modT"], writes=["h32"])
        op("pool", lambda e: e.tensor_tensor(out=h32, in0=h32, in1=bc(sh.unsqueeze(2), [128, 8, 128]), op=ALU.add), reads=["h32", "modT"], writes=["h32"])
        op("act", lambda e: e.activation(out=hb, in_=h32, func=AF.Copy), reads=["h32"], writes=["hb"])
        fw.dma("sp", S["hTf1"][to // 2][:, :, (to % 2) * 128:(to % 2 + 1) * 128], hb, reads=["hb"])
        bL = nb()
        for kc in range(8):
            op("pe", lambda e, kc=kc, bL=bL: e.matmul(ps[bL][:, 0:8], h32[:, kc, :], router[:, kc, :], start=(kc == 0), stop=(kc == 7)), reads=["h32", "router"], writes=["ps%d" % bL])
        L8, m1, m2, eq, ex, sm = lg[:, 0:8], lg[:, 8:9], lg[:, 9:10], lg[:, 10:18], lg[:, 18:26], lg[:, 26:27]
        op("dve", lambda e, bL=bL: e.tensor_copy(out=L8, in_=ps[bL][:, 0:8]), reads=["ps%d" % bL], writes=["lg"])
        op("dve", lambda e: e.reduce_max(out=m1, in_=L8, axis=AX.X), reads=["lg"], writes=["lg"])
        op("dve", lambda e: e.tensor_scalar(out=eq, in0=L8, scalar1=m1, scalar2=-1e30, op0=ALU.is_equal, op1=ALU.mult), reads=["lg"], writes=["lg"])
        op("dve", lambda e: e.tensor_tensor(out=eq, in0=eq, in1=L8, op=ALU.add), reads=["lg"], writes=["lg"])
        op("dve", lambda e: e.reduce_max(out=m2, in_=eq, axis=AX.X), reads=["lg"], writes=["lg"])
        op("dve", lambda e: e.tensor_scalar(out=eq, in0=L8, scalar1=m2, scalar2=None, op0=ALU.is_ge), reads=["lg"], writes=["lg"])
        op("dve", lambda e: e.tensor_scalar(out=ex, in0=L8, scalar1=m1, scalar2=None, op0=ALU.subtract), reads=["lg"], writes=["lg"])
        op("act", lambda e: e.activation(out=ex, in_=ex, func=AF.Exp), reads=["lg"], writes=["lg"])
        op("dve", lambda e: e.tensor_tensor(out=ex, in0=ex, in1=eq, op=ALU.mult), reads=["lg"], writes=["lg"])
        op("dve", lambda e: e.reduce_sum(out=sm, in_=ex, axis=AX.X), reads=["lg"], writes=["lg"])
        op("dve", lambda e: e.reciprocal(out=sm, in_=sm), reads=["lg"], writes=["lg"])
        op("dve", lambda e: e.tensor_scalar(out=ex, in0=ex, scalar1=sm, scalar2=None, op0=ALU.mult), reads=["lg"], writes=["lg"])
        fw.dma("sp", S["comb"][to * 128:(to + 1) * 128, :], ex, reads=["lg"])


def _postnorm_sb2(self, y, ykey, xsub, xkey, gp_idx, scr, skeys):
    fw, P = self.fw, self.P
    junk, ss, rstd, tmp = scr["junk"], scr["ss2"], scr["rstd2"], scr["tmp"]
    fw.op("act", lambda e: e.activation(out=junk, in_=y, func=AF.Square, accum_out=ss[:, 0:1]), reads=[ykey], writes=["T6", "lg"])
    fw.op("act", lambda e: e.activation(out=rstd[:, 0:1], in_=ss[:, 0:1], func=AF.Sqrt, scale=1.0 / D, bias=P["eps"][:, 0:1]), reads=["lg", "eps"], writes=["lg"])
    fw.op("dve", lambda e: e.reciprocal(out=rstd[:, 0:1], in_=rstd[:, 0:1]), reads=["lg"], writes=["lg"])
    fw.op("dve", lambda e: e.scalar_tensor_tensor(out=tmp, in0=y, scalar=rstd[:, 0:1], in1=P["GP"][:, gp_idx, :], op0=ALU.mult, op1=ALU.mult),
          reads=[ykey, "lg", "GP"], writes=["T7"])
    fw.op("dve", lambda e: e.tensor_tensor(out=xsub, in0=xsub, in1=tmp, op=ALU.add), reads=["T7", xkey], writes=[xkey])


def _prenorm_T2(self, xsub, xkey, l, sub, hT, hkey, col0, pbank, scr, skeys):
    fw, P, ps = self.fw, self.P, self.ps
    junk, ss, rstd, xn, tmp = scr["junk"], scr["ss"], scr["rstd"], scr["xn"], scr["tmp"]
    fw.op("act", lambda e: e.activation(out=junk, in_=xsub, func=AF.Square, accum_out=ss[:, 0:1]), reads=[xkey], writes=["T6", "lg"])
    fw.op("act", lambda e: e.activation(out=rstd[:, 0:1], in_=ss[:, 0:1], func=AF.Sqrt, scale=1.0 / D, bias=P["eps"][:, 0:1]), reads=["lg", "eps"], writes=["lg"])
    fw.op("dve", lambda e: e.reciprocal(out=rstd[:, 0:1], in_=rstd[:, 0:1]), reads=["lg"], writes=["lg"])
    fw.op("act", lambda e: e.activation(out=xn, in_=xsub, func=AF.Copy, scale=rstd[:, 0:1]), reads=[xkey, "lg"], writes=["T6"])
    pk = "ps%d" % pbank
    pbt = ps[pbank][:, :].bitcast(BF16)
    for kc in range(8):
        fw.op("pe", lambda e, kc=kc: e.transpose(pbt[:, kc * 128:(kc + 1) * 128], xn[:, kc * 128:(kc + 1) * 128], P["identb"][:]), reads=["T6", "identb"], writes=[pk])
    G1 = P["modT"][:, l * 4 + sub * 2 + 0, :]
    sh = P["modT"][:, l * 4 + sub * 2 + 1, :]
    for kc in range(8):
        fw.op("act", lambda e, kc=kc: e.activation(out=hT[:, kc, col0:col0 + 128], in_=pbt[:, kc * 128:(kc + 1) * 128], func=AF.Identity, scale=G1[:, kc:kc + 1], bias=sh[:, kc:kc + 1]),
              reads=[pk, "modT"], writes=[hkey])


def _postnorm_res2(self, psb, xsub, xkey, gp_idx, scr, skeys):
    fw, P, ps = self.fw, self.P, self.ps
    junk, ss2, rstd, tmp = scr["junk"], scr["ss2"], scr["rstd2"], scr["tmp"]
    for n in range(2):
        fw.op("act", lambda e, n=n: e.activation(out=junk[:, 0:512], in_=ps[psb[n]][:, :], func=AF.Square, accum_out=ss2[:, n:n + 1]), reads=["ps%d" % psb[n]], writes=["T6", "lg"])
    fw.op("pool", lambda e: e.tensor_tensor(out=rstd[:, 0:1], in0=ss2[:, 0:1], in1=ss2[:, 1:2], op=ALU.add), reads=["lg"], writes=["lg"])
    fw.op("act", lambda e: e.activation(out=rstd[:, 0:1], in_=rstd[:, 0:1], func=AF.Sqrt, scale=1.0 / D, bias=P["eps"][:, 0:1]), reads=["lg", "eps"], writes=["lg"])
    fw.op("dve", lambda e: e.reciprocal(out=rstd[:, 0:1], in_=rstd[:, 0:1]), reads=["lg"], writes=["lg"])
    for n in range(2):
        fw.op("dve", lambda e, n=n: e.scalar_tensor_tensor(out=tmp[:, n * 512:(n + 1) * 512], in0=ps[psb[n]][:, :], scalar=rstd[:, 0:1], in1=P["GP"][:, gp_idx, n * 512:(n + 1) * 512], op0=ALU.mult, op1=ALU.mult),
              reads=["ps%d" % psb[n], "lg", "GP"], writes=["T7"])
    fw.op("dve", lambda e: e.tensor_tensor(out=xsub, in0=xsub, in1=tmp, op=ALU.add), reads=["T7", xkey], writes=[xkey])


Builder.phase_rwkv_prep = _phase_rwkv_prep
Builder.phase_rwkv_chain = _phase_rwkv_chain
Builder.postnorm_sb2 = _postnorm_sb2
Builder.prenorm_T2 = _prenorm_T2
Builder.postnorm_res2 = _postnorm_res2
```

```python
import contextlib
import numpy as np
import concourse.bass as bass
import concourse.mybir as mybir
from concourse.bass_utils import run_bass_kernel_spmd

F32 = mybir.dt.float32
BF16 = mybir.dt.bfloat16
ALU = mybir.AluOpType
AF = mybir.ActivationFunctionType
AX = mybir.AxisListType

D = 1024
FF = 2816
NE = 8
SEQ = 8192
HALF = 4096
RMS_EPS = 1e-6
LNX_EPS = 1e-5 * 64

COMPUTE = ("pe", "act", "dve", "pool")
NDMA_SEMS = 12
EPOCH = 30000


class _Op:
    __slots__ = ("eng", "fn", "deps", "is_dma", "need_inc", "tick", "dsem", "dval", "dprev")

    def __init__(self, eng, fn, deps, is_dma):
        self.eng = eng
        self.fn = fn
        self.deps = deps
        self.is_dma = is_dma
        self.need_inc = False
        self.tick = 0
        self.dsem = None
        self.dval = 0
        self.dprev = None


class _Rec:
    def __getattr__(self, name):
        return lambda *a, **k: (name, a, k)


_REC = _Rec()


class FW:
    def __init__(self, nc):
        self.nc = nc
        self.ops = []
        self.last_w = {}
        self.readers = {}
        self.bar = set()
        self.last_c = {}
        self.dma_since = []

    def _add(self, eng, fn, reads, writes, is_dma):
        idx = len(self.ops)
        pr = [r for r in reads if r.startswith("ps")]
        if pr:
            reads = [r for r in reads if not r.startswith("ps")]
            writes = list(writes) + pr
        deps = set(self.bar)
        for r in reads:
            w = self.last_w.get(r)
            if w is not None:
                deps.add(w)
        for k in writes:
            w = self.last_w.get(k)
            if w is not None:
                deps.add(w)
            rd = self.readers.get(k)
            if rd is not None:
                deps.update(rd["c"].values())
                deps.update(rd["d"])
        for r in reads:
            rd = self.readers.get(r)
            if rd is None:
                rd = self.readers[r] = {"c": {}, "d": []}
            if is_dma:
                rd["d"].append(idx)
            else:
                rd["c"][eng] = idx
        for k in writes:
            self.last_w[k] = idx
            self.readers[k] = {"c": {}, "d": []}
        deps.discard(idx)
        self.ops.append(_Op(eng, fn, deps, is_dma))
        if is_dma:
            self.dma_since.append(idx)
        else:
            self.last_c[eng] = idx
        return idx

    def op(self, eng, fn, reads=(), writes=()):
        name, a, k = fn(_REC)
        return self._add(eng, lambda e: getattr(e, name)(*a, **k), reads, writes, False)

    def dma(self, q, out, in_, reads=(), writes=()):
        return self._add(q, lambda e: e.dma_start(out=out, in_=in_), reads, writes, True)

    def barrier(self):
        self.bar = set(self.last_c.values()) | set(self.dma_since)
        self.dma_since = []
        self.last_w = {}
        self.readers = {}

    def emit(self):
        nc = self.nc
        ops = self.ops
        for o in ops:
            for d in o.deps:
                p = ops[d]
                if p.is_dma:
                    continue
                if p.eng == o.eng and p.eng == "pe" and not o.is_dma:
                    continue
                p.need_inc = True
        ticks = {e: 0 for e in COMPUTE}
        for o in ops:
            if not o.is_dma and o.need_inc:
                ticks[o.eng] += 1
                o.tick = ticks[o.eng]
        qcount = {}
        qlast = {}
        for i, o in enumerate(ops):
            if o.is_dma:
                n = qcount.get(o.eng, 0)
                qcount[o.eng] = n + 1
                slot = n % NDMA_SEMS
                key = (o.eng, slot)
                o.dsem = key
                o.dval = (n // NDMA_SEMS + 1) * 16
                o.dprev = qlast.get(key)
                qlast[key] = i
        engs = ("pe", "act", "dve", "pool", "sp")
        with contextlib.ExitStack() as st:
            csem = {}
            for e in COMPUTE:
                nep = (ticks[e] + EPOCH - 1) // EPOCH
                for k in range(max(nep, 1)):
                    csem[(e, k)] = st.enter_context(nc.semaphore("c_%s_%d" % (e, k)))
            dsem = {}
            for q in qcount:
                for s in range(min(NDMA_SEMS, qcount[q])):
                    dsem[(q, s)] = st.enter_context(nc.semaphore("d_%s_%d" % (q, s)))
            known = {e: {} for e in engs}
            streams = {e: [] for e in engs}
            for i, o in enumerate(ops):
                e = o.eng
                kn = known[e]
                waits = {}
                for d in o.deps:
                    p = ops[d]
                    if p.is_dma:
                        sk = ("d",) + p.dsem
                        v = p.dval
                    else:
                        if p.eng == e and e == "pe" and not o.is_dma:
                            continue
                        ep = (p.tick - 1) // EPOCH
                        sk = ("c", p.eng, ep)
                        v = p.tick - ep * EPOCH
                    if kn.get(sk, 0) >= v:
                        continue
                    if waits.get(sk, 0) < v:
                        waits[sk] = v
                if o.is_dma and o.dprev is not None:
                    p = ops[o.dprev]
                    sk = ("d",) + p.dsem
                    if kn.get(sk, 0) < p.dval and waits.get(sk, 0) < p.dval:
                        waits[sk] = p.dval
                for sk, v in waits.items():
                    kn[sk] = v
                    sem = csem[(sk[1], sk[2])] if sk[0] == "c" else dsem[(sk[1], sk[2])]
                    streams[e].append(("w", sem, v))
                if o.is_dma:
                    streams[e].append(("i", o.fn, dsem[o.dsem], 16))
                elif o.need_inc:
                    streams[e].append(("i", o.fn, csem[(e, (o.tick - 1) // EPOCH)], 1))
                else:
                    streams[e].append(("i", o.fn, None, 0))
            fin = streams["sp"]
            for key, i in qlast.items():
                fin.append(("w", dsem[key], ops[i].dval))
            for e in COMPUTE:
                if ticks[e] > 0:
                    ep = (ticks[e] - 1) // EPOCH
                    fin.append(("w", csem[(e, ep)], ticks[e] - ep * EPOCH))

            def run(eng_obj, lst):
                for it in lst:
                    if it[0] == "w":
                        eng_obj.wait_ge(it[1], it[2])
                    else:
                        ins = it[1](eng_obj)
                        if it[2] is not None:
                            ins.then_inc(it[2], it[3])

            with nc.Block() as block:
                @block.tensor
                def _(eng):
                    run(eng, streams["pe"])

                @block.scalar
                def _(eng):
                    run(eng, streams["act"])

                @block.vector
                def _(eng):
                    run(eng, streams["dve"])

                @block.gpsimd
                def _(eng):
                    run(eng, streams["pool"])

                @block.sync
                def _(eng):
                    run(eng, streams["sp"])
        return {e: len(streams[e]) for e in streams}


class Arena:
    def __init__(self, t, nwords):
        self.t = t
        self.n = nwords
        self.off = 0

    def reset(self):
        self.off = 0

    def alloc(self, shape, dtype):
        nel = int(np.prod(shape))
        nb = nel * (4 if dtype == F32 else 2)
        nw = (nb + 3) // 4
        ap = self.t[:, self.off:self.off + nw]
        self.off += nw
        assert self.off <= self.n, ("arena overflow", self.off, self.n)
        if dtype != F32:
            ap = ap.bitcast(dtype)[:, 0:nel]
        if len(shape) == 2:
            ap = ap.rearrange("p (a b) -> p a b", a=shape[0])
        elif len(shape) == 3:
            ap = ap.rearrange("p (a b c) -> p a b c", a=shape[0], b=shape[1])
        return ap


def bc(ap, shape):
    return ap.to_broadcast(list(shape))


class Builder:
    def __init__(self, stages=("M", "A", "B", "D", "E", "F"), dbg=False):
        self.stages = stages
        self.dbg = dbg
        self.nc = bass.Bass("TRN2", target_bir_lowering=False)
        self.fw = FW(self.nc)
        self.uid = 0

    def din(self, name, shape, dt=F32):
        return self.nc.dram_tensor(name, list(shape), dt, kind="ExternalInput").ap()

    def dscr(self, name, shape, dt=F32, out=False):
        kind = "ExternalOutput" if out else "Internal"
        return self.nc.dram_tensor(name, list(shape), dt, kind=kind).ap()

    def build(self):
        nc, fw = self.nc, self.fw
        dbg = self.dbg
        I = {}
        I["x8"] = self.din("x8", [SEQ, D])
        I["condB"] = self.din("condB", [128, 8, 128])
        I["flag"] = self.din("flag", [128, 1])
        I["invc"] = self.din("invc", [128, 2, 4, 16])
        I["ident"] = self.din("ident", [128, 128])
        I["ada_w"] = self.din("ada_w", [2, D, 6 * D])
        I["ada_bB"] = self.din("ada_bB", [2, 128, 6 * D])
        I["norm_gB"] = self.din("norm_gB", [2, 128, 4, D])
        I["mix_w_in"] = self.din("mix_w_in", [D, 2048])
        I["mix_w_out"] = self.din("mix_w_out", [D, D])
        I["conv_wT"] = self.din("conv_wT", [128, 4, 3])
        I["pool_w"] = self.din("pool_w", [4, 128, 128])
        I["pool_scT"] = self.din("pool_scT", [128, 4])
        I["ffn_wg"] = self.din("ffn_wg", [D, FF])
        I["ffn_wu"] = self.din("ffn_wu", [D, FF])
        I["ffn_wd"] = self.din("ffn_wd", [FF, D])
        for nm in ("rw_wr", "rw_wk", "rw_wv", "rw_wo"):
            I[nm] = self.din(nm, [D, D])
        I["rw_w1"] = self.din("rw_w1", [D, 64])
        I["rw_a1"] = self.din("rw_a1", [D, 64])
        I["rw_g1"] = self.din("rw_g1", [D, 160])
        I["rw_w2"] = self.din("rw_w2", [64, D])
        I["rw_a2"] = self.din("rw_a2", [64, D])
        I["rw_g2"] = self.din("rw_g2", [160, D])
        I["rw_vec"] = self.din("rw_vec", [128, 13, 8])
        I["moe_router"] = self.din("moe_router", [D, 8])
        I["resetm"] = self.din("resetm", [128, 1024])
        I["mask1"] = self.din("mask1", [128, 2, 128])
        I["maskT"] = self.din("maskT", [128, 128])
        I["bdones"] = self.din("bdones", [128, 128])
        I["moe_wg"] = self.din("moe_wg", [NE, D, FF])
        I["moe_wu"] = self.din("moe_wu", [NE, D, FF])
        I["moe_wd"] = self.din("moe_wd", [NE, FF, D])
        self.I = I
        S = {}
        S["x1"] = self.dscr("x1", [SEQ, D], out=dbg)
        S["hTf0"] = self.dscr("hTf0", [32, 128, 8, 256], BF16)
        S["acc0"] = self.dscr("acc0", [SEQ, D])
        S["x2"] = self.dscr("x2", [SEQ, D], out=("C" in self.stages))
        S["x3"] = self.dscr("x3", [HALF, D], out=dbg)
        S["MI"] = self.dscr("MI", [64, 128, 8, 896], BF16)
        S["GCd"] = self.dscr("GCd", [64, 128, 16])
        S["OW"] = self.dscr("OW", [32, 128, 8, 256], BF16)
        S["hTf1"] = self.dscr("hTf1", [16, 128, 8, 256], BF16)
        S["comb"] = self.dscr("comb", [HALF, 8], out=dbg)
        S["acc1"] = self.dscr("acc1", [HALF, D])
        S["y"] = self.dscr("y", [HALF, D], out=True)
        self.S = S

        with contextlib.ExitStack() as st:
            sb = lambda n, sh, dt: st.enter_context(nc.sbuf_tensor("sb_" + n, sh, dt))
            P = {}
            P["ident"] = sb("identf", [128, 128], F32)
            P["identb"] = sb("identb", [128, 128], BF16)
            P["flag"] = sb("flag", [128, 1], F32)
            P["invc"] = sb("invc", [128, 2, 4, 16], F32)
            P["GP"] = sb("GP", [128, 4, D], F32)
            P["modT"] = sb("modT", [128, 8, 8], F32)
            P["small"] = sb("small", [128, 64], F32)
            P["eps"] = sb("eps", [128, 2], F32)
            ARW = 48000
            arena_t = sb("arena", [128, ARW], F32)
            self.P = P
            self.ar = Arena(arena_t, ARW)
            self.ps = [st.enter_context(nc.psum_tensor("ps%d" % i, [128, 512], F32)) for i in range(8)]

            fw.dma("sp", P["ident"][:], I["ident"], writes=["ident"])
            fw.dma("pool", P["identb"][:], I["ident"], writes=["identb"])
            fw.dma("sp", P["flag"][:], I["flag"], writes=["flag"])
            fw.op("pool", lambda e: e.memset(P["eps"][:, 0:1], RMS_EPS), writes=["eps"])
            fw.op("pool", lambda e: e.memset(P["eps"][:, 1:2], LNX_EPS), writes=["eps"])
            fw.dma("sp", P["invc"][:], I["invc"], writes=["invc"])
            if "M" in self.stages:
                self.phase_mod(0)
                self.phase_mod(1)
            if "A" in self.stages:
                self.phase_mixer0()
            if "B" in self.stages:
                fw.barrier()
                self.ar.reset()
                self.ffn_passes(self.I["ffn_wg"], self.I["ffn_wu"], self.I["ffn_wd"], 32, S["hTf0"], S["acc0"], None, [0])
            if "C" in self.stages:
                fw.barrier()
                self.ar.reset()
                self.phase_post0()
            if "D" in self.stages:
                self.phase_rwkv_prep()
                self.phase_rwkv_chain()
            if "E" in self.stages:
                fw.barrier()
                self.ar.reset()
                self.ffn_passes(self.I["moe_wg"], self.I["moe_wu"], self.I["moe_wd"], 16, S["hTf1"], S["acc1"], S["comb"], list(range(NE)))
            if "F" in self.stages:
                fw.barrier()
                self.ar.reset()
                self.phase_final()
            self.stats = fw.emit()
        return nc

    def phase_mod(self, l):
        nc, fw, P, I, ar = self.nc, self.fw, self.P, self.I, self.ar
        ps = self.ps
        fw.barrier()
        ar.reset()
        cond = ar.alloc([8, 128], F32)
        modb = ar.alloc([6 * D], F32)
        adab = ar.alloc([6 * D], F32)
        ng = ar.alloc([4, D], F32)
        wblk = [ar.alloc([8, 512], F32) for _ in range(2)]
        tmpb = ar.alloc([4, D], F32)
        fw.dma("sp", cond, I["condB"], writes=["cond"])
        fw.dma("sp", adab, I["ada_bB"][l], writes=["adab"])
        fw.dma("sp", ng, I["norm_gB"][l], writes=["ng"])
        fw.op("act", lambda e: e.activation(out=cond, in_=cond, func=AF.Silu), reads=["cond"], writes=["cond"])
        for blk in range(12):
            wb = wblk[blk % 2]
            wk = "wblk%d" % (blk % 2)
            fw.dma("sp", wb, I["ada_w"][l, :, blk * 512:(blk + 1) * 512].rearrange("(kc p) n -> p kc n", p=128), writes=[wk])
            pb = ps[blk % 2]
            pk = "ps%d" % (blk % 2)
            for kc in range(8):
                fw.op("pe", lambda e, kc=kc, wb=wb, pb=pb: e.matmul(pb[:, :], cond[:, kc, :], wb[:, kc, :], start=(kc == 0), stop=(kc == 7)),
                      reads=["cond", wk], writes=[pk])
            fw.op("dve", lambda e, blk=blk, pb=pb: e.tensor_tensor(out=modb[:, blk * 512:(blk + 1) * 512], in0=pb[:, :], in1=adab[:, blk * 512:(blk + 1) * 512], op=ALU.add),
                  reads=[pk, "adab"], writes=["modb"])
        sh_m, sc_m, gt_m, sh_f, sc_f, gt_f = [modb[:, i * D:(i + 1) * D] for i in range(6)]
        fw.op("dve", lambda e: e.tensor_tensor(out=P["GP"][:, l * 2 + 0, :], in0=gt_m, in1=ng[:, 1, :], op=ALU.mult), reads=["modb", "ng"], writes=["GP"])
        fw.op("dve", lambda e: e.tensor_tensor(out=P["GP"][:, l * 2 + 1, :], in0=gt_f, in1=ng[:, 3, :], op=ALU.mult), reads=["modb", "ng"], writes=["GP"])
        fw.op("dve", lambda e: e.scalar_tensor_tensor(out=tmpb[:, 0, :], in0=sc_m, scalar=1.0, in1=ng[:, 0, :], op0=ALU.add, op1=ALU.mult), reads=["modb", "ng"], writes=["tmpb"])
        fw.op("dve", lambda e: e.tensor_copy(out=tmpb[:, 1, :], in_=sh_m), reads=["modb"], writes=["tmpb"])
        fw.op("dve", lambda e: e.scalar_tensor_tensor(out=tmpb[:, 2, :], in0=sc_f, scalar=1.0, in1=ng[:, 2, :], op0=ALU.add, op1=ALU.mult), reads=["modb", "ng"], writes=["tmpb"])
        fw.op("dve", lambda e: e.tensor_copy(out=tmpb[:, 3, :], in_=sh_f), reads=["modb"], writes=["tmpb"])
        for v in range(4):
            for kc in range(8):
                pb = ps[2 + kc // 4]
                fw.op("pe", lambda e, v=v, kc=kc, pb=pb: e.transpose(pb[:, (kc % 4) * 128:(kc % 4 + 1) * 128], tmpb[:, v, kc * 128:(kc + 1) * 128], P["ident"][:]),
                      reads=["tmpb", "ident"], writes=["ps%d" % (2 + kc // 4)])
            for hh in range(2):
                fw.op("dve", lambda e, v=v, hh=hh: e.tensor_copy(out=P["modT"][:, l * 4 + v, hh * 4:(hh + 1) * 4],
                                                                 in_=ps[2 + hh][:, :].rearrange("p (k t) -> p k t", t=128)[:, :, 0]),
                      reads=["ps%d" % (2 + hh)], writes=["modT"])

    def prenorm_T(self, xsub, xkey, l, sub, hT, hkey, col0, pbank, scr, f32out=None):
        fw, P, ps = self.fw, self.P, self.ps
        u = self.uid
        self.uid += 1
        junk, ss, rstd, xn, tmp = scr["junk"], scr["ss"], scr["rstd"], scr["xn"], scr["tmp"]
        fw.op("act", lambda e: e.activation(out=junk, in_=xsub, func=AF.Square, accum_out=ss[:, 0:1]), reads=[xkey], writes=["junk", "ss"])
        fw.op("act", lambda e: e.activation(out=rstd[:, 0:1], in_=ss[:, 0:1], func=AF.Sqrt, scale=1.0 / D, bias=P["eps"][:, 0:1]), reads=["ss", "eps"], writes=["rstd"])
        fw.op("dve", lambda e: e.reciprocal(out=rstd[:, 0:1], in_=rstd[:, 0:1]), reads=["rstd"], writes=["rstd"])
        fw.op("act", lambda e: e.activation(out=xn, in_=xsub, func=AF.Copy, scale=rstd[:, 0:1]), reads=[xkey, "rstd"], writes=["xn"])
        pk = "ps%d" % pbank
        pbt = ps[pbank][:, :].bitcast(BF16)
        for kc in range(8):
            fw.op("pe", lambda e, kc=kc: e.transpose(pbt[:, kc * 128:(kc + 1) * 128], xn[:, kc * 128:(kc + 1) * 128], P["identb"][:]),
                  reads=["xn", "identb"], writes=[pk])
        G1 = P["modT"][:, l * 4 + sub * 2 + 0, :]
        sh = P["modT"][:, l * 4 + sub * 2 + 1, :]
        for kc in range(8):
            fw.op("act", lambda e, kc=kc: e.activation(out=hT[:, kc, col0:col0 + 128], in_=pbt[:, kc * 128:(kc + 1) * 128], func=AF.Identity, scale=G1[:, kc:kc + 1], bias=sh[:, kc:kc + 1]),
                  reads=[pk, "modT"], writes=[hkey])

    def postnorm_res(self, psb, xsub, xkey, gp_idx, scr):
        fw, P, ps = self.fw, self.P, self.ps
        junk, ss2, rstd, tmp = scr["junk"], scr["ss2"], scr["rstd2"], scr["tmp"]
        for n in range(2):
            fw.op("act", lambda e, n=n: e.activation(out=junk[:, 0:512], in_=ps[psb[n]][:, :], func=AF.Square, accum_out=ss2[:, n:n + 1]),
                  reads=["ps%d" % psb[n]], writes=["junk", "ss2"])
        fw.op("pool", lambda e: e.tensor_tensor(out=rstd[:, 0:1], in0=ss2[:, 0:1], in1=ss2[:, 1:2], op=ALU.add), reads=["ss2"], writes=["rstd2"])
        fw.op("act", lambda e: e.activation(out=rstd[:, 0:1], in_=rstd[:, 0:1], func=AF.Sqrt, scale=1.0 / D, bias=P["eps"][:, 0:1]), reads=["rstd2", "eps"], writes=["rstd2"])
        fw.op("dve", lambda e: e.reciprocal(out=rstd[:, 0:1], in_=rstd[:, 0:1]), reads=["rstd2"], writes=["rstd2"])
        for n in range(2):
            fw.op("dve", lambda e, n=n: e.scalar_tensor_tensor(out=tmp[:, n * 512:(n + 1) * 512], in0=ps[psb[n]][:, :], scalar=rstd[:, 0:1],
                                                               in1=P["GP"][:, gp_idx, n * 512:(n + 1) * 512], op0=ALU.mult, op1=ALU.mult),
                  reads=["ps%d" % psb[n], "rstd2", "GP"], writes=["tmp"])
        fw.op("dve", lambda e: e.tensor_tensor(out=xsub, in0=xsub, in1=tmp, op=ALU.add), reads=["tmp", xkey], writes=[xkey])

    def mk_scr(self):
        ar = self.ar
        return {"junk": ar.alloc([D], BF16), "ss": ar.alloc([2], F32), "rstd": ar.alloc([2], F32), "xn": ar.alloc([D], BF16),
                "tmp": ar.alloc([D], F32), "ss2": ar.alloc([2], F32), "rstd2": ar.alloc([2], F32)}

    def phase_mixer0(self):
        nc, fw, P, I, S, ar, ps = self.nc, self.fw, self.P, self.I, self.S, self.ar, self.ps
        fw.barrier()
        ar.reset()
        NT = 512
        w_in = ar.alloc([8, 2048], BF16)
        w_out = ar.alloc([8, D], BF16)
        pool_w = ar.alloc([4, 128], BF16)
        convw = ar.alloc([4, 3], F32)
        poolsc = ar.alloc([4], F32)
        xt = [ar.alloc([4, D], F32) for _ in range(2)]
        hT = ar.alloc([8, NT], BF16)
        hT2 = ar.alloc([8, NT], BF16)
        cg = ar.alloc([NT], F32)
        cv = ar.alloc([4, 2 + NT], F32)
        t1 = ar.alloc([NT], F32)
        t2 = ar.alloc([NT], F32)
        up = ar.alloc([4, 16 + NT], F32)
        sA = ar.alloc([16 + NT], F32)
        sB = ar.alloc([16 + NT], F32)
        pg = ar.alloc([NT], BF16)
        t16 = ar.alloc([16], F32)
        ycat = ar.alloc([8, NT], BF16)
        scr = self.mk_scr()
        for kc in range(8):
            fw.dma("pool", w_in[:, kc, :], I["mix_w_in"][kc * 128:(kc + 1) * 128, :], writes=["w_in"])
        fw.dma("pool", w_out, I["mix_w_out"].rearrange("(kc p) n -> p kc n", p=128), writes=["w_out"])
        fw.dma("pool", pool_w, I["pool_w"].rearrange("g c d -> c g d"), writes=["pool_w"])
        fw.dma("sp", convw, I["conv_wT"], writes=["convw"])
        fw.dma("sp", poolsc, I["pool_scT"], writes=["poolsc"])
        fw.op("pool", lambda e: e.memset(cv, 0.0), writes=["cv%d" % j for j in range(4)])
        fw.op("pool", lambda e: e.memset(up, 0.0), writes=["up%d" % g for g in range(4)])
        fw.op("pool", lambda e: e.memset(sA, 0.0), writes=["sA"])
        fw.op("pool", lambda e: e.memset(sB, 0.0), writes=["sB"])
        wins = (2, 4, 8, 16)
        ntiles = SEQ // NT
        for ti in range(ntiles):
            xb = xt[ti % 2]
            xk = "xt%d" % (ti % 2)
            fw.dma("sp", xb, I["x8"][ti * NT:(ti + 1) * NT, :].rearrange("(s p) d -> p s d", p=128), writes=[xk])
            for s in range(4):
                self.prenorm_T(xb[:, s, :], xk, 0, 0, hT, "hT", s * 128, s % 2, scr)
            if ti == ntiles // 2:
                fw.op("pool", lambda e: e.tensor_scalar(out=cv[:, :, 0:2], in0=cv[:, :, 0:2], scalar1=P["flag"][:, 0:1], scalar2=None, op0=ALU.mult),
                      reads=["flag"] + ["cv%d" % j for j in range(4)], writes=["cv%d" % j for j in range(4)])
                fw.op("pool", lambda e: e.tensor_scalar(out=up[:, :, 0:16], in0=up[:, :, 0:16], scalar1=P["flag"][:, 0:1], scalar2=None, op0=ALU.mult),
                      reads=["flag"] + ["up%d" % g for g in range(4)], writes=["up%d" % g for g in range(4)])
            bankrr = [2, 3, 4, 5]
            bi = [0]

            def zchunk(fc):
                b = bankrr[bi[0] % 4]
                bi[0] += 1
                for kc in range(8):
                    fw.op("pe", lambda e, kc=kc, b=b: e.matmul(ps[b][:, :], w_in[:, kc, fc * 128:(fc + 1) * 128], hT[:, kc, :], start=(kc == 0), stop=(kc == 7)),
                          reads=["w_in", "hT"], writes=["ps%d" % b])
                return b
            for j in range(4):
                cvk = "cv%d" % j
                b = zchunk(4 + j)
                fw.op("act", lambda e, b=b: e.activation(out=cg, in_=ps[b][:, :], func=AF.Copy), reads=["ps%d" % b], writes=["cg"])
                b = zchunk(8 + j)
                fw.op("dve", lambda e, b=b, j=j: e.tensor_tensor(out=cv[:, j, 2:2 + NT], in0=ps[b][:, :], in1=cg, op=ALU.mult), reads=["ps%d" % b, "cg"], writes=[cvk])
                fw.op("pool", lambda e, j=j: e.tensor_scalar(out=t1, in0=cv[:, j, 0:NT], scalar1=convw[:, j, 0:1], scalar2=None, op0=ALU.mult), reads=[cvk, "convw"], writes=["t1"])
                fw.op("dve", lambda e, j=j: e.scalar_tensor_tensor(out=t2, in0=cv[:, j, 1:1 + NT], scalar=convw[:, j, 1:2], in1=t1, op0=ALU.mult, op1=ALU.add), reads=[cvk, "convw", "t1"], writes=["t2"])
                fw.op("dve", lambda e, j=j: e.scalar_tensor_tensor(out=t1, in0=cv[:, j, 2:2 + NT], scalar=convw[:, j, 2:3], in1=t2, op0=ALU.mult, op1=ALU.add), reads=[cvk, "convw", "t2"], writes=["t1"])
                b = zchunk(j)
                fw.op("dve", lambda e, b=b, j=j: e.tensor_tensor(out=ycat[:, j, :], in0=ps[b][:, :], in1=t1, op=ALU.mult), reads=["ps%d" % b, "t1"], writes=["ycat"])
                fw.op("pool", lambda e, j=j: e.tensor_copy(out=cv[:, j, 0:2], in_=cv[:, j, NT:NT + 2]), reads=[cvk], writes=[cvk])
            for g in range(4):
                upk = "up%d" % g
                b = zchunk(12 + g)
                fw.op("act", lambda e, b=b, g=g: e.activation(out=up[:, g, 16:16 + NT], in_=ps[b][:, :], func=AF.Copy), reads=["ps%d" % b], writes=[upk])
                W = 16 + NT
                cur = up[:, g, :]
                curk = upk
                bufs = [(sA, "sA"), (sB, "sB")]
                d = 1
                k = 0
                while d < wins[g]:
                    dst, dk = bufs[k % 2]
                    fw.op("dve", lambda e, cur=cur, dst=dst, d=d: e.tensor_tensor(out=dst[:, d:W], in0=cur[:, d:W], in1=cur[:, 0:W - d], op=ALU.add),
                          reads=[curk], writes=[dk])
                    cur, curk = dst, dk
                    d *= 2
                    k += 1
                fw.op("dve", lambda e, cur=cur, g=g: e.scalar_tensor_tensor(out=pg, in0=cur[:, 16:W], scalar=1.0 / wins[g], in1=up[:, g, 16:W], op0=ALU.mult, op1=ALU.subtract),
                      reads=[curk, upk], writes=["pg"])
                if ti == 0 or ti == ntiles // 2:
                    which = 0 if ti == 0 else 1
                    fw.op("pool", lambda e, cur=cur, g=g, which=which: e.tensor_tensor(out=t16, in0=cur[:, 16:32], in1=P["invc"][:, which, g, :], op=ALU.mult),
                          reads=[curk, "invc"], writes=["t16"])
                    fw.op("pool", lambda e, g=g: e.tensor_tensor(out=pg[:, 0:16], in0=t16, in1=up[:, g, 16:32], op=ALU.subtract),
                          reads=["t16", upk], writes=["pg"])
                b2 = bankrr[bi[0] % 4]
                bi[0] += 1
                fw.op("pe", lambda e, g=g, b2=b2: e.matmul(ps[b2][:, :], pool_w[:, g, :], pg, start=True, stop=True), reads=["pool_w", "pg"], writes=["ps%d" % b2])
                fw.op("act", lambda e, g=g, b2=b2: e.activation(out=ycat[:, 4 + g, :], in_=ps[b2][:, :], func=AF.Copy, scale=poolsc[:, g:g + 1]),
                      reads=["ps%d" % b2, "poolsc"], writes=["ycat"])
                fw.op("pool", lambda e, g=g: e.tensor_copy(out=up[:, g, 0:16], in_=up[:, g, NT:NT + 16]), reads=[upk], writes=[upk])
            for s in range(4):
                for n in range(2):
                    b = 6 + n
                    for kc in range(8):
                        fw.op("pe", lambda e, kc=kc, b=b, s=s, n=n: e.matmul(ps[b][:, :], ycat[:, kc, s * 128:(s + 1) * 128], w_out[:, kc, n * 512:(n + 1) * 512], start=(kc == 0), stop=(kc == 7)),
                              reads=["ycat", "w_out"], writes=["ps%d" % b])
                self.postnorm_res((6, 7), xb[:, s, :], xk, 0, scr)
                self.prenorm_T(xb[:, s, :], xk, 0, 1, hT2, "hT2", s * 128, s % 2, scr)
            fw.dma("sp", S["x1"][ti * NT:(ti + 1) * NT, :].rearrange("(s p) d -> p s d", p=128), xb, reads=[xk])
            for hh in range(2):
                fw.dma("sp", S["hTf0"][ti * 2 + hh], hT2[:, :, hh * 256:(hh + 1) * 256], reads=["hT2"])

    def ffn_passes(self, wg_d, wu_d, wd_d, ntiles, hT_d, acc_d, comb_d, experts):
        nc, fw, P, ar, ps = self.nc, self.fw, self.P, self.ar, self.ps
        FH = FF // 2
        NT = 256
        wg = [ar.alloc([8, FH], BF16) for _ in range(2)]
        wu = [ar.alloc([8, FH], BF16) for _ in range(2)]
        wd = [ar.alloc([11, D], BF16) for _ in range(2)]
        hTt = [ar.alloc([8, NT], BF16) for _ in range(2)]
        zt = [ar.alloc([11, NT], BF16) for _ in range(2)]
        acct = [ar.alloc([2, D], F32) for _ in range(2)]
        sg = [ar.alloc([NT], F32) for _ in range(2)]
        combt = [ar.alloc([2, 8], F32) for _ in range(2)]
        npass = 0
        cnt = 0
        for ei, ex in enumerate(experts):
            for hf in range(2):
                wi = npass % 2
                f0 = hf * FH
                if comb_d is None:
                    wgd, wud, wdd = wg_d, wu_d, wd_d
                else:
                    wgd, wud, wdd = wg_d[ex], wu_d[ex], wd_d[ex]
                for kc in range(8):
                    fw.dma("pool", wg[wi][:, kc, :], wgd[kc * 128:(kc + 1) * 128, f0:f0 + FH], writes=["wg%d" % wi])
                    fw.dma("pool", wu[wi][:, kc, :], wud[kc * 128:(kc + 1) * 128, f0:f0 + FH], writes=["wu%d" % wi])
                fw.dma("pool", wd[wi], wdd[f0:f0 + FH, :].rearrange("(fc p) d -> p fc d", p=128), writes=["wd%d" % wi])
                first = (npass == 0)
                for t in range(ntiles):
                    bi = cnt % 2
                    cnt += 1
                    hk, zk, ak, ck = "hTt%d" % bi, "zt%d" % bi, "acct%d" % bi, "combt%d" % bi
                    fw.dma("sp", hTt[bi], hT_d[t], writes=[hk])
                    if not first:
                        fw.dma("sp", acct[bi], acc_d[t * NT:(t + 1) * NT, :].rearrange("(s p) d -> p s d", p=128), reads=["accd%d" % t], writes=[ak])
                    if comb_d is not None:
                        fw.dma("sp", combt[bi], comb_d[t * NT:(t + 1) * NT, :].rearrange("(s p) e -> p s e", p=128), writes=[ck])
                    for fc in range(11):
                        bg = (fc % 2) * 2
                        bu = bg + 1
                        for kc in range(8):
                            fw.op("pe", lambda e, kc=kc, fc=fc, bg=bg, wi=wi, bi=bi: e.matmul(ps[bg][:, 0:NT], wg[wi][:, kc, fc * 128:(fc + 1) * 128], hTt[bi][:, kc, :], start=(kc == 0), stop=(kc == 7)),
                                  reads=["wg%d" % wi, hk], writes=["ps%d" % bg])
                        for kc in range(8):
                            fw.op("pe", lambda e, kc=kc, fc=fc, bu=bu, wi=wi, bi=bi: e.matmul(ps[bu][:, 0:NT], wu[wi][:, kc, fc * 128:(fc + 1) * 128], hTt[bi][:, kc, :], start=(kc == 0), stop=(kc == 7)),
                                  reads=["wu%d" % wi, hk], writes=["ps%d" % bu])
                        sgb = sg[fc % 2]
                        sgk = "sg%d" % (fc % 2)
                        fw.op("act", lambda e, bg=bg, sgb=sgb: e.activation(out=sgb, in_=ps[bg][:, 0:NT], func=AF.Silu), reads=["ps%d" % bg], writes=[sgk])
                        fw.op("dve", lambda e, bu=bu, sgb=sgb, fc=fc, bi=bi: e.tensor_tensor(out=zt[bi][:, fc, :], in0=ps[bu][:, 0:NT], in1=sgb, op=ALU.mult),
                              reads=["ps%d" % bu, sgk], writes=[zk])
                    for s in range(2):
                        for n in range(2):
                            bo = 4 + (s * 2 + n)
                            for fc in range(11):
                                fw.op("pe", lambda e, fc=fc, bo=bo, s=s, n=n, wi=wi, bi=bi: e.matmul(ps[bo][:, :], zt[bi][:, fc, s * 128:(s + 1) * 128], wd[wi][:, fc, n * 512:(n + 1) * 512], start=(fc == 0), stop=(fc == 10)),
                                      reads=[zk, "wd%d" % wi], writes=["ps%d" % bo])
                            dst = acct[bi][:, s, n * 512:(n + 1) * 512]
                            if comb_d is None:
                                if first:
                                    fw.op("act", lambda e, bo=bo, dst=dst: e.activation(out=dst, in_=ps[bo][:, :], func=AF.Copy), reads=["ps%d" % bo], writes=[ak])
                                else:
                                    fw.op("dve", lambda e, bo=bo, dst=dst: e.tensor_tensor(out=dst, in0=ps[bo][:, :], in1=dst, op=ALU.add), reads=["ps%d" % bo, ak], writes=[ak])
                            else:
                                cs = combt[bi][:, s, ex:ex + 1]
                                if first:
                                    fw.op("act", lambda e, bo=bo, dst=dst, cs=cs: e.activation(out=dst, in_=ps[bo][:, :], func=AF.Copy, scale=cs), reads=["ps%d" % bo, ck], writes=[ak])
                                else:
                                    fw.op("dve", lambda e, bo=bo, dst=dst, cs=cs: e.scalar_tensor_tensor(out=dst, in0=ps[bo][:, :], scalar=cs, in1=dst, op0=ALU.mult, op1=ALU.add),
                                          reads=["ps%d" % bo, ak, ck], writes=[ak])
                    fw.dma("sp", acc_d[t * NT:(t + 1) * NT, :].rearrange("(s p) d -> p s d", p=128), acct[bi], reads=[ak], writes=["accd%d" % t])
                npass += 1

    def phase_post0(self):
        nc, fw, P, S, ar, ps = self.nc, self.fw, self.P, self.S, self.ar, self.ps
        NT = 512
        xt = [ar.alloc([4, D], F32) for _ in range(2)]
        at = [ar.alloc([4, D], F32) for _ in range(2)]
        scr = self.mk_scr()
        for ti in range(SEQ // NT):
            xb, ab = xt[ti % 2], at[ti % 2]
            xk, akk = "xt%d" % (ti % 2), "at%d" % (ti % 2)
            fw.dma("sp", xb, S["x1"][ti * NT:(ti + 1) * NT, :].rearrange("(s p) d -> p s d", p=128), writes=[xk])
            fw.dma("sp", ab, S["acc0"][ti * NT:(ti + 1) * NT, :].rearrange("(s p) d -> p s d", p=128), writes=[akk])
            for s in range(4):
                self.postnorm_sb(ab[:, s, :], akk, xb[:, s, :], xk, 1, scr)
            fw.dma("sp", S["x2"][ti * NT:(ti + 1) * NT, :].rearrange("(s p) d -> p s d", p=128), xb, reads=[xk])

    def phase_final(self):
        nc, fw, P, S, ar, ps = self.nc, self.fw, self.P, self.S, self.ar, self.ps
        NT = 512
        xt = [ar.alloc([4, D], F32) for _ in range(2)]
        at = [ar.alloc([4, D], F32) for _ in range(2)]
        scr = self.mk_scr()
        for ti in range(HALF // NT):
            xb, ab = xt[ti % 2], at[ti % 2]
            xk, akk = "xt%d" % (ti % 2), "at%d" % (ti % 2)
            fw.dma("sp", xb, S["x3"][ti * NT:(ti + 1) * NT, :].rearrange("(s p) d -> p s d", p=128), writes=[xk])
            fw.dma("sp", ab, S["acc1"][ti * NT:(ti + 1) * NT, :].rearrange("(s p) d -> p s d", p=128), writes=[akk])
            for s in range(4):
                self.postnorm_sb(ab[:, s, :], akk, xb[:, s, :], xk, 3, scr)
            fw.dma("sp", S["y"][ti * NT:(ti + 1) * NT, :].rearrange("(s p) d -> p s d", p=128), xb, reads=[xk])

    def postnorm_sb(self, y, ykey, xsub, xkey, gp_idx, scr):
        fw, P = self.fw, self.P
        junk, ss, rstd, tmp = scr["junk"], scr["ss2"], scr["rstd2"], scr["tmp"]
        fw.op("act", lambda e: e.activation(out=junk, in_=y, func=AF.Square, accum_out=ss[:, 0:1]), reads=[ykey], writes=["junk", "ss2"])
        fw.op("act", lambda e: e.activation(out=rstd[:, 0:1], in_=ss[:, 0:1], func=AF.Sqrt, scale=1.0 / D, bias=P["eps"][:, 0:1]), reads=["ss2", "eps"], writes=["rstd2"])
        fw.op("dve", lambda e: e.reciprocal(out=rstd[:, 0:1], in_=rstd[:, 0:1]), reads=["rstd2"], writes=["rstd2"])
        fw.op("dve", lambda e: e.scalar_tensor_tensor(out=tmp, in0=y, scalar=rstd[:, 0:1], in1=P["GP"][:, gp_idx, :], op0=ALU.mult, op1=ALU.mult),
              reads=[ykey, "rstd2", "GP"], writes=["tmp"])
        fw.op("dve", lambda e: e.tensor_tensor(out=xsub, in0=xsub, in1=tmp, op=ALU.add), reads=["tmp", xkey], writes=[xkey])


def _fm(v, nch):
    return np.ascontiguousarray(np.asarray(v, np.float32).reshape(nch, 128).T)


def make_in_maps(inp):
    x = np.asarray(inp["x"], np.float32)
    c = np.asarray(inp["c"], np.float32)
    maps = []
    wins = (2, 4, 8, 16)
    pos = np.arange(16)
    invc_start = np.stack([1.0 / np.minimum(pos + 1, w) for w in wins]).astype(np.float32)
    invc_mid = np.stack([np.full(16, 1.0 / w) for w in wins]).astype(np.float32)
    common = {
        "ident": np.eye(128, dtype=np.float32),
        "ada_w": np.ascontiguousarray(inp["ada_w"], np.float32),
        "ada_bB": np.ascontiguousarray(np.broadcast_to(np.asarray(inp["ada_b"], np.float32)[:, None, :], (2, 128, 6 * D))),
        "norm_gB": np.ascontiguousarray(np.broadcast_to(np.asarray(inp["norm_g"], np.float32)[:, None, :, :], (2, 128, 4, D))),
        "mix_w_in": np.ascontiguousarray(inp["mix_w_in"][0], np.float32),
        "mix_w_out": np.ascontiguousarray(inp["mix_w_out"][0], np.float32),
        "conv_wT": np.ascontiguousarray(np.asarray(inp["conv_w"][0], np.float32).reshape(3, 4, 128).transpose(2, 1, 0)),
        "pool_w": np.ascontiguousarray(inp["pool_w"][0], np.float32),
        "pool_scT": _fm(inp["pool_scale"][0], 4),
        "ffn_wg": np.ascontiguousarray(inp["ffn_w_gate"][0], np.float32),
        "ffn_wu": np.ascontiguousarray(inp["ffn_w_up"][0], np.float32),
        "ffn_wd": np.ascontiguousarray(inp["ffn_w_down"][0], np.float32),
    }
    f32 = lambda a: np.ascontiguousarray(a, np.float32)
    common.update({
        "rw_wr": f32(inp["rwkv_w_r"][0]), "rw_wk": f32(inp["rwkv_w_k"][0]), "rw_wv": f32(inp["rwkv_w_v"][0]), "rw_wo": f32(inp["rwkv_w_o"][0]),
        "rw_w1": f32(inp["rwkv_w1"][0]), "rw_a1": f32(inp["rwkv_a1"][0]), "rw_g1": f32(inp["rwkv_g1"][0]),
        "rw_w2": f32(inp["rwkv_w2"][0]), "rw_a2": f32(inp["rwkv_a2"][0]), "rw_g2": f32(inp["rwkv_g2"][0]),
        "moe_router": f32(inp["moe_router"][0]),
        "moe_wg": f32(inp["moe_w_gate"][0]), "moe_wu": f32(inp["moe_w_up"][0]), "moe_wd": f32(inp["moe_w_down"][0]),
    })
    mu = np.asarray(inp["rwkv_mu"][0], np.float32)
    vecs = [mu[i] for i in range(6)] + [inp["rwkv_w0"][0], inp["rwkv_a0"][0], inp["rwkv_k_k"][0], inp["rwkv_k_a"][0],
                                        np.asarray(inp["rwkv_r_k"][0]).reshape(-1), inp["rwkv_ln_g"][0], inp["rwkv_ln_b"][0]]
    common["rw_vec"] = np.ascontiguousarray(np.stack([_fm(v, 8) for v in vecs], axis=1))
    t = np.arange(1024)
    common["resetm"] = np.ascontiguousarray(np.broadcast_to((t % 64 != 0).astype(np.float32)[None], (128, 1024)))
    si = np.arange(128)[:, None]
    tj = np.arange(128)[None, :]
    same = (si // 64) == (tj // 64)
    common["mask1"] = np.ascontiguousarray(np.stack([(same & (si < tj)), (same & (si <= tj))], axis=1).astype(np.float32))
    common["maskT"] = np.ascontiguousarray((same & (si > tj)).astype(np.float32))
    common["bdones"] = np.ascontiguousarray(same.astype(np.float32))
    for core in range(8):
        b, h = core // 2, core % 2
        m = dict(common)
        m["x8"] = np.ascontiguousarray(np.concatenate([x[b, :HALF], x[b, h * HALF:(h + 1) * HALF]], axis=0))
        m["condB"] = np.ascontiguousarray(np.broadcast_to(c[b].reshape(8, 128).T[:, :, None], (128, 8, 128)))
        m["flag"] = np.full((128, 1), float(h), np.float32)
        m["invc"] = np.ascontiguousarray(np.broadcast_to(np.stack([invc_start, invc_start if h == 0 else invc_mid])[None], (128, 2, 4, 16)))
        maps.append(m)
    return maps


_CACHE = {}


def kernel(**inputs):
    if "nc" not in _CACHE:
        _CACHE["nc"] = Builder().build()
    nc = _CACHE["nc"]
    maps = make_in_maps(inputs)
    res = run_bass_kernel_spmd(nc, maps, core_ids=list(range(8)))
    out = np.zeros((4, SEQ, D), np.float32)
    for core in range(8):
        b, h = core // 2, core % 2
        out[b, h * HALF:(h + 1) * HALF] = res.results[core]["y"]
    return out


CDEC = 0.6065306597126334


def _phase_rwkv_prep(self):
    nc, fw, P, I, S, ar, ps = self.nc, self.fw, self.P, self.I, self.S, self.ar, self.ps
    fw.barrier()
    ar.reset()
    op = fw.op
    W = {}
    for nm in ("rw_wr", "rw_wk", "rw_wv"):
        W[nm] = ar.alloc([8, D], BF16)
        for kc in range(8):
            fw.dma("pool", W[nm][:, kc, :], I[nm][kc * 128:(kc + 1) * 128, :], writes=[nm])
    w1 = ar.alloc([8, 64], BF16)
    a1 = ar.alloc([8, 64], BF16)
    g1 = ar.alloc([8, 160], BF16)
    fw.dma("pool", w1, I["rw_w1"].rearrange("(kc p) n -> p kc n", p=128), writes=["w1"])
    fw.dma("pool", a1, I["rw_a1"].rearrange("(kc p) n -> p kc n", p=128), writes=["a1"])
    fw.dma("pool", g1, I["rw_g1"].rearrange("(kc p) n -> p kc n", p=128), writes=["g1"])
    w2 = ar.alloc([D], BF16)
    a2 = ar.alloc([D], BF16)
    g2a = ar.alloc([D], BF16)
    g2b = ar.alloc([D], BF16)
    fw.dma("pool", w2[0:64, :], I["rw_w2"], writes=["w2"])
    fw.dma("pool", a2[0:64, :], I["rw_a2"], writes=["a2"])
    fw.dma("pool", g2a, I["rw_g2"][0:128, :], writes=["g2a"])
    fw.dma("pool", g2b[0:32, :], I["rw_g2"][128:160, :], writes=["g2b"])
    vec = ar.alloc([14, 8], F32)
    fw.dma("sp", vec[:, 0:13, :], I["rw_vec"], writes=["vec"])
    op("pool", lambda e: e.tensor_scalar(out=vec[:, 13, :], in0=vec[:, 9, :], scalar1=-1.0, scalar2=1.0, op0=ALU.mult, op1=ALU.add), reads=["vec"], writes=["vec"])
    resetm = ar.alloc([1024], F32)
    bdones = ar.alloc([128], BF16)
    fw.dma("sp", resetm, I["resetm"], writes=["resetm"])
    fw.dma("pool", bdones, I["bdones"], writes=["bdones"])

    def vb(i):
        return bc(vec[:, i, :].unsqueeze(2), [128, 8, 128])

    xt = ar.alloc([D], F32)
    at = ar.alloc([D], F32)
    hT = ar.alloc([8, 129], BF16)
    xx = ar.alloc([8, 128], BF16)
    xi = [ar.alloc([8, 128], BF16) for _ in range(2)]
    rSs = [ar.alloc([8, 128], BF16) for _ in range(2)]
    kSs = [ar.alloc([1024], F32) for _ in range(2)]
    sgds = [ar.alloc([1024], F32) for _ in range(2)]
    aSs = [ar.alloc([1024], F32) for _ in range(2)]
    ftmp = ar.alloc([1024], F32)
    tw = ar.alloc([128], BF16)
    ta = ar.alloc([128], BF16)
    tg = ar.alloc([128], BF16)
    tg2 = ar.alloc([128], BF16)
    Tf = [ar.alloc([1024], F32) for _ in range(8)]
    T3 = [t.rearrange("p (c t) -> p c t", t=128) for t in Tf]
    MIs = [ar.alloc([8, 896], BF16) for _ in range(2)]
    OWs = [ar.alloc([8, 256], BF16) for _ in range(2)]
    GCs = [ar.alloc([16], F32) for _ in range(2)]
    B0 = ar.alloc([8, 128], BF16)
    lg = ar.alloc([64], F32)
    scr = {"junk": Tf[6][:, 0:512].bitcast(BF16), "ss": lg[:, 32:34], "rstd": lg[:, 34:36], "xn": Tf[6][:, 512:1024].bitcast(BF16),
           "tmp": ftmp, "ss2": lg[:, 36:38], "rstd2": lg[:, 38:40]}
    scr_keys = ["T6", "FT"]

    op("pool", lambda e: e.memset(hT, 0.0), writes=["hT"])

    bank_rr = [0]

    def nb():
        b = 4 + bank_rr[0] % 2
        bank_rr[0] += 1
        return b

    evac_rr = [0]

    def cp(dst, src, reads, writes):
        evac_rr[0] += 1
        if evac_rr[0] % 2:
            op("act", lambda e: e.activation(out=dst, in_=src, func=AF.Copy), reads=reads, writes=writes)
        else:
            op("dve", lambda e: e.tensor_copy(out=dst, in_=src), reads=reads, writes=writes)

    NTILE = SEQ // 128
    npre, nown = getattr(self, "rw_tiles", (NTILE // 2, NTILE // 2))
    tiles = list(range(npre)) + list(range(NTILE // 2, NTILE // 2 + nown))
    STOP = getattr(self, "rw_stop", 99)
    def front(ti):
        own = ti >= NTILE // 2
        par = ti % 2
        kx = "_%d" % par
        MIb, OWb, GC = MIs[par], OWs[par], GCs[par]
        PR, Qt, Kt, Qb, Kb, vS = MIb[:, :, 0:256], MIb[:, :, 256:384], MIb[:, :, 384:512], MIb[:, :, 512:640], MIb[:, :, 640:768], MIb[:, :, 768:896]
        gS, bonus = OWb[:, :, 0:128], OWb[:, :, 128:256]
        rS, rSk = rSs[par], "rS" + kx
        kSf, kSk = kSs[par], "kS" + kx
        kS = kSf.rearrange("p (c t) -> p c t", t=128)
        sgd, sgk = sgds[par], "sg" + kx
        sgd3 = sgd.rearrange("p (c t) -> p c t", t=128)
        aSf, aSk = aSs[par], "aS" + kx
        aS = aSf.rearrange("p (c t) -> p c t", t=128)
        fw.dma("sp", xt, S["x1"][ti * 128:(ti + 1) * 128, :], writes=["xt"])
        fw.dma("sp", at, S["acc0"][ti * 128:(ti + 1) * 128, :], writes=["at"])
        self.postnorm_sb2(at, "at", xt, "xt", 1, scr, scr_keys)
        if own:
            fw.dma("sp", S["x2"][ti * 128:(ti + 1) * 128, :], xt, reads=["xt"])
        if ti == NTILE // 2:
            op("pool", lambda e: e.tensor_scalar(out=hT[:, :, 0:1], in0=hT[:, :, 0:1], scalar1=P["flag"][:, 0:1], scalar2=None, op0=ALU.mult), reads=["hT", "flag"], writes=["hT"])
        self.prenorm_T2(xt, "xt", 1, 0, hT, "hT", 1, 4, scr, scr_keys)
        op("pool", lambda e: e.tensor_tensor(out=xx, in0=hT[:, :, 0:128], in1=hT[:, :, 1:129], op=ALU.subtract), reads=["hT"], writes=["xx"])

        def variant(i, buf):
            xb_, xk_ = xi[buf], "xi%d" % buf
            op("dve", lambda e: e.tensor_tensor(out=xb_, in0=xx, in1=vb(i), op=ALU.mult), reads=["xx", "vec"], writes=[xk_])
            op("dve", lambda e: e.tensor_tensor(out=xb_, in0=xb_, in1=hT[:, :, 1:129], op=ALU.add), reads=[xk_, "hT"], writes=[xk_])
            return xb_, xk_

        def proj(wname, xb_, xk_, grp):
            b0 = grp * 2
            for cc in range(8):
                b = b0 + cc // 4
                for kc in range(8):
                    op("pe", lambda e, cc=cc, kc=kc, b=b: e.matmul(ps[b][:, (cc % 4) * 128:(cc % 4 + 1) * 128], W[wname][:, kc, cc * 128:(cc + 1) * 128], xb_[:, kc, :], start=(kc == 0), stop=(kc == 7)),
                       reads=[wname, xk_], writes=["ps%d" % b])
            return b0

        def evac2(b0, fn_eng, mk):
            for hh in range(2):
                mk(hh, ps[b0 + hh][:, :].rearrange("p (c t) -> p c t", t=128), "ps%d" % (b0 + hh))

        xb_, xk_ = variant(0, 0)
        b0 = proj("rw_wr", xb_, xk_, 0)
        for hh in range(2):
            op("act", lambda e, hh=hh, b0=b0: e.activation(out=rS[:, hh * 4:(hh + 1) * 4, :], in_=ps[b0 + hh][:, :].rearrange("p (c t) -> p c t", t=128), func=AF.Copy), reads=["ps%d" % (b0 + hh)], writes=[rSk])
        yield
        xb_, xk_ = variant(2, 1)
        b0 = proj("rw_wk", xb_, xk_, 1)
        for hh in range(2):
            op("act", lambda e, hh=hh, b0=b0: e.activation(out=kS[:, hh * 4:(hh + 1) * 4, :], in_=ps[b0 + hh][:, :].rearrange("p (c t) -> p c t", t=128), func=AF.Copy), reads=["ps%d" % (b0 + hh)], writes=[kSk])
        yield
        xb_, xk_ = variant(3, 0)
        b0 = proj("rw_wv", xb_, xk_, 0)
        for hh in range(2):
            op("act", lambda e, hh=hh, b0=b0: e.activation(out=vS[:, hh * 4:(hh + 1) * 4, :], in_=ps[b0 + hh][:, :].rearrange("p (c t) -> p c t", t=128), func=AF.Copy), reads=["ps%d" % (b0 + hh)], writes=["vS" + kx])
        yield
        xb_, xk_ = variant(1, 1)
        b = nb()
        for kc in range(8):
            op("pe", lambda e, kc=kc, b=b, xb_=xb_: e.matmul(ps[b][0:64, 0:128], w1[:, kc, :], xb_[:, kc, :], start=(kc == 0), stop=(kc == 7)), reads=["w1", xk_], writes=["ps%d" % b])
        op("act", lambda e, b=b: e.activation(out=tw[0:64, :], in_=ps[b][0:64, 0:128], func=AF.Tanh), reads=["ps%d" % b], writes=["tw"])
        b0 = 2
        for cc in range(8):
            bb = b0 + cc // 4
            op("pe", lambda e, cc=cc, bb=bb: e.matmul(ps[bb][:, (cc % 4) * 128:(cc % 4 + 1) * 128], w2[0:64, cc * 128:(cc + 1) * 128], tw[0:64, :], start=True, stop=True), reads=["w2", "tw"], writes=["ps%d" % bb])
        for hh in range(2):
            op("dve", lambda e, hh=hh: e.tensor_tensor(out=sgd3[:, hh * 4:(hh + 1) * 4, :], in0=ps[2 + hh][:, :].rearrange("p (c t) -> p c t", t=128),
                                                      in1=bc(vec[:, 6, hh * 4:(hh + 1) * 4].unsqueeze(2), [128, 4, 128]), op=ALU.add), reads=["ps%d" % (2 + hh), "vec"], writes=[sgk])
        op("act", lambda e: e.activation(out=sgd, in_=sgd, func=AF.Sigmoid), reads=[sgk], writes=[sgk])
        yield
        xb_, xk_ = variant(4, 0)
        b = nb()
        for kc in range(8):
            op("pe", lambda e, kc=kc, b=b, xb_=xb_: e.matmul(ps[b][0:64, 0:128], a1[:, kc, :], xb_[:, kc, :], start=(kc == 0), stop=(kc == 7)), reads=["a1", xk_], writes=["ps%d" % b])
        op("act", lambda e, b=b: e.activation(out=ta[0:64, :], in_=ps[b][0:64, 0:128], func=AF.Copy), reads=["ps%d" % b], writes=["ta"])
        for cc in range(8):
            bb = cc // 4
            op("pe", lambda e, cc=cc, bb=bb: e.matmul(ps[bb][:, (cc % 4) * 128:(cc % 4 + 1) * 128], a2[0:64, cc * 128:(cc + 1) * 128], ta[0:64, :], start=True, stop=True), reads=["a2", "ta"], writes=["ps%d" % bb])
        for hh in range(2):
            op("dve", lambda e, hh=hh: e.tensor_tensor(out=aS[:, hh * 4:(hh + 1) * 4, :], in0=ps[hh][:, :].rearrange("p (c t) -> p c t", t=128),
                                                      in1=bc(vec[:, 7, hh * 4:(hh + 1) * 4].unsqueeze(2), [128, 4, 128]), op=ALU.add), reads=["ps%d" % hh, "vec"], writes=[aSk])
        op("act", lambda e: e.activation(out=aSf, in_=aSf, func=AF.Sigmoid), reads=[aSk], writes=[aSk])
        yield
        if own:
            xb_, xk_ = variant(5, 1)
            b = nb()
            for kc in range(8):
                op("pe", lambda e, kc=kc, b=b, xb_=xb_: e.matmul(ps[b][:, 0:128], g1[:, kc, 0:128], xb_[:, kc, :], start=(kc == 0), stop=(kc == 7)), reads=["g1", xk_], writes=["ps%d" % b])
            for kc in range(8):
                op("pe", lambda e, kc=kc, b=b, xb_=xb_: e.matmul(ps[b][0:32, 128:256], g1[:, kc, 128:160], xb_[:, kc, :], start=(kc == 0), stop=(kc == 7)), reads=["g1", xk_], writes=["ps%d" % b])
            op("act", lambda e, b=b: e.activation(out=tg, in_=ps[b][:, 0:128], func=AF.Sigmoid), reads=["ps%d" % b], writes=["tg"])
            op("act", lambda e, b=b: e.activation(out=tg2[0:32, :], in_=ps[b][0:32, 128:256], func=AF.Sigmoid), reads=["ps%d" % b], writes=["tg2"])
            for cc in range(8):
                bb = 2 + cc // 4
                op("pe", lambda e, cc=cc, bb=bb: e.matmul(ps[bb][:, (cc % 4) * 128:(cc % 4 + 1) * 128], g2a[:, cc * 128:(cc + 1) * 128], tg, start=True, stop=False), reads=["g2a", "tg"], writes=["ps%d" % bb])
                op("pe", lambda e, cc=cc, bb=bb: e.matmul(ps[bb][:, (cc % 4) * 128:(cc % 4 + 1) * 128], g2b[0:32, cc * 128:(cc + 1) * 128], tg2[0:32, :], start=False, stop=True), reads=["g2b", "tg2"], writes=["ps%d" % bb])
            for hh in range(2):
                op("act", lambda e, hh=hh: e.activation(out=gS[:, hh * 4:(hh + 1) * 4, :], in_=ps[2 + hh][:, :].rearrange("p (c t) -> p c t", t=128), func=AF.Copy), reads=["ps%d" % (2 + hh)], writes=["gS" + kx])

        op("pool", lambda e: e.tensor_copy(out=hT[:, :, 0:1], in_=hT[:, :, 128:129]), reads=["hT"], writes=["hT"])
        yield


    def back(ti):
        own = ti >= NTILE // 2
        par = ti % 2
        kx = "_%d" % par
        MIb, OWb, GC = MIs[par], OWs[par], GCs[par]
        PR, Qt, Kt, Qb, Kb, vS = MIb[:, :, 0:256], MIb[:, :, 256:384], MIb[:, :, 384:512], MIb[:, :, 512:640], MIb[:, :, 640:768], MIb[:, :, 768:896]
        gS, bonus = OWb[:, :, 0:128], OWb[:, :, 128:256]
        rS, rSk = rSs[par], "rS" + kx
        kSf, kSk = kSs[par], "kS" + kx
        kS = kSf.rearrange("p (c t) -> p c t", t=128)
        sgd, sgk = sgds[par], "sg" + kx
        sgd3 = sgd.rearrange("p (c t) -> p c t", t=128)
        aSf, aSk = aSs[par], "aS" + kx
        aS = aSf.rearrange("p (c t) -> p c t", t=128)
        Lp, Lpk = Tf[1], "T1"
        op("dve", lambda e: e.tensor_tensor_scan(out=Lp, data0=resetm, data1=sgd, initial=0.0, op0=ALU.mult, op1=ALU.add), reads=["resetm", sgk], writes=[Lpk])
        Lm, Lmk = Tf[2], "T2"
        op("dve", lambda e: e.tensor_tensor(out=Lm, in0=Lp, in1=sgd, op=ALU.subtract), reads=[Lpk, sgk], writes=[Lmk])
        Ld, Ldk = Tf[0], "T0"
        Lp64 = Lp.rearrange("p (c t) -> p c t", t=64)
        yield
        op("pool", lambda e: e.tensor_tensor(out=Ld.rearrange("p (c t) -> p c t", t=64), in0=bc(Lp64[:, :, 63:64], [128, 16, 64]), in1=Lp64, op=ALU.subtract), reads=[Lpk, Lmk], writes=[Ldk])
        E1, E1k = Tf[3], "T3"
        E2, E2k = Tf[4], "T4"
        op("act", lambda e: e.activation(out=E1, in_=Lp, func=AF.Exp, scale=-CDEC), reads=[Lpk], writes=[E1k])
        op("act", lambda e: e.activation(out=E2, in_=Lp, func=AF.Exp, scale=CDEC), reads=[Lpk], writes=[E2k])
        yield
        op("act", lambda e: e.activation(out=Lm, in_=Lm, func=AF.Exp, scale=-CDEC), reads=[Lmk], writes=[Lmk])
        op("act", lambda e: e.activation(out=Ld, in_=Ld, func=AF.Exp, scale=-CDEC), reads=[Ldk], writes=[Ldk])
        E3, E3k, E4, E4k = Lm, Lmk, Ld, Ldk
        op("pool", lambda e: e.tensor_copy(out=GC, in_=E1.rearrange("p (c t) -> p c t", t=64)[:, :, 63]), reads=[E1k], writes=["GC" + kx])
        kk, kkk = T3[1], "T1"
        yield
        op("dve", lambda e: e.tensor_tensor(out=kk, in0=kS, in1=vb(8), op=ALU.mult), reads=[kSk, "vec", E1k, E2k], writes=[kkk])
        op("pool", lambda e: e.tensor_tensor(out=B0, in0=kk, in1=kk, op=ALU.mult), reads=[kkk], writes=["B0"])
        for cc in range(8):
            bb = 6 + cc // 4
            op("pe", lambda e, cc=cc, bb=bb: e.matmul(ps[bb][:, (cc % 4) * 128:(cc % 4 + 1) * 128], bdones, B0[:, cc, :], start=True, stop=True), reads=["bdones", "B0"], writes=["ps%d" % bb])
        rn, rnk = T3[7], "T7"
        yield
        for hh in range(2):
            op("act", lambda e, hh=hh: e.activation(out=rn[:, hh * 4:(hh + 1) * 4, :], in_=ps[6 + hh][:, :].rearrange("p (c t) -> p c t", t=128), func=AF.Sqrt), reads=["ps%d" % (6 + hh)], writes=[rnk])
        op("dve", lambda e: e.reciprocal(out=Tf[7], in_=Tf[7]), reads=[rnk], writes=[rnk])
        op("dve", lambda e: e.tensor_tensor(out=kk, in0=kk, in1=rn, op=ALU.mult), reads=[kkk, rnk], writes=[kkk])
        km, kmk = T3[7], "T7"
        yield
        op("dve", lambda e: e.tensor_tensor(out=km, in0=aS, in1=vb(9), op=ALU.mult), reads=[aSk, "vec", kkk], writes=[kmk])
        op("pool", lambda e: e.tensor_tensor(out=km, in0=km, in1=vb(13), op=ALU.add), reads=[kmk, "vec"], writes=[kmk])
        op("dve", lambda e: e.tensor_tensor(out=km, in0=km, in1=kS, op=ALU.mult), reads=[kmk, kSk], writes=[kmk])
        q, qk = aS, aSk
        yield
        op("dve", lambda e: e.tensor_tensor(out=q, in0=aS, in1=kk, op=ALU.mult), reads=[aSk, kkk], writes=[qk])
        op("dve", lambda e: e.scalar_tensor_tensor(out=PR[:, :, 0:128], in0=kk, scalar=-1.0, in1=T3[2], op0=ALU.mult, op1=ALU.mult), reads=[kkk, E3k], writes=["PR" + kx])
        if own:
            op("pool", lambda e: e.tensor_tensor(out=PR[:, :, 128:256], in0=rS, in1=T3[3], op=ALU.mult), reads=[rSk, E1k], writes=["PR" + kx])
        else:
            op("pool", lambda e: e.tensor_copy(out=PR[:, :, 128:256], in_=rS), reads=[rSk], writes=["PR" + kx])
        op("dve", lambda e: e.tensor_tensor(out=Qt, in0=q, in1=T3[4], op=ALU.mult), reads=[qk, E2k], writes=["Qt" + kx])
        yield
        op("pool", lambda e: e.tensor_tensor(out=Kt, in0=km, in1=T3[4], op=ALU.mult), reads=[kmk, E2k], writes=["Kt" + kx])
        op("dve", lambda e: e.tensor_tensor(out=Qb, in0=q, in1=T3[0], op=ALU.mult), reads=[qk, E4k], writes=["Qb" + kx])
        op("pool", lambda e: e.tensor_tensor(out=Kb, in0=km, in1=T3[0], op=ALU.mult), reads=[kmk, E4k], writes=["Kb" + kx])
        if own:
            op("pool", lambda e: e.tensor_tensor(out=T3[5], in0=km, in1=vb(10), op=ALU.mult), reads=[kmk, "vec", kSk], writes=["T5"])
            op("pool", lambda e: e.tensor_tensor(out=B0, in0=T3[5], in1=rS, op=ALU.mult), reads=["T5", rSk], writes=["B0"])
            for cc in range(8):
                bb = 6 + cc // 4
                op("pe", lambda e, cc=cc, bb=bb: e.matmul(ps[bb][:, (cc % 4) * 128:(cc % 4 + 1) * 128], bdones, B0[:, cc, :], start=True, stop=True), reads=["bdones", "B0"], writes=["ps%d" % bb])
            for hh in range(2):
                op("dve", lambda e, hh=hh: e.tensor_tensor(out=bonus[:, hh * 4:(hh + 1) * 4, :], in0=ps[6 + hh][:, :].rearrange("p (c t) -> p c t", t=128), in1=vS[:, hh * 4:(hh + 1) * 4, :], op=ALU.mult),
                   reads=["ps%d" % (6 + hh), "vS" + kx], writes=["bonus" + kx])

        fw.dma("sp", S["MI"][ti], MIb, reads=["PR" + kx, "Qt" + kx, "Kt" + kx, "Qb" + kx, "Kb" + kx, "vS" + kx])
        fw.dma("sp", S["GCd"][ti], GC, reads=["GC" + kx])
        if own:
            fw.dma("sp", S["OW"][ti - NTILE // 2], OWb, reads=["gS" + kx, "bonus" + kx])


        yield


    prev = None
    for ti in tiles:
        gens = [front(ti)]
        if prev is not None:
            gens.append(back(prev))
        while gens:
            for g in list(gens):
                try:
                    next(g)
                except StopIteration:
                    gens.remove(g)
        prev = ti
    if prev is not None:
        for _ in back(prev):
            pass


def _phase_rwkv_chain(self):
    nc, fw, P, I, S, ar, ps = self.nc, self.fw, self.P, self.I, self.S, self.ar, self.ps
    fw.barrier()
    ar.reset()
    op = fw.op
    W = {"rw_wo": ar.alloc([8, D], BF16)}
    for kc in range(8):
        fw.dma("pool", W["rw_wo"][:, kc, :], I["rw_wo"][kc * 128:(kc + 1) * 128, :], writes=["rw_wo"])
    vec = ar.alloc([14, 8], F32)
    fw.dma("sp", vec[:, 0:13, :], I["rw_vec"], writes=["vec"])
    router = ar.alloc([8, 8], F32)
    fw.dma("sp", router, I["moe_router"].rearrange("(kc p) e -> p kc e", p=128), writes=["router"])
    mask1 = ar.alloc([2, 128], F32)
    maskT = ar.alloc([128], F32)
    bdones = ar.alloc([128], BF16)
    fw.dma("sp", mask1, I["mask1"], writes=["mask1"])
    fw.dma("sp", maskT, I["maskT"], writes=["maskT"])
    fw.dma("pool", bdones, I["bdones"], writes=["bdones"])

    def vb(i):
        return bc(vec[:, i, :].unsqueeze(2), [128, 8, 128])

    MIs = [ar.alloc([8, 896], BF16) for _ in range(2)]
    OWs = [ar.alloc([8, 256], BF16) for _ in range(2)]
    GCs = [ar.alloc([16], F32) for _ in range(2)]
    xts = [ar.alloc([D], F32) for _ in range(2)]
    Tf = {i: ar.alloc([1024], F32) for i in (1, 2, 6, 7)}
    T3 = {i: t.rearrange("p (c t) -> p c t", t=128) for i, t in Tf.items()}
    B0 = ar.alloc([8, 128], BF16)
    yg = ar.alloc([8, 128], BF16)
    Ysb = ar.alloc([8, 128], F32)
    Sbd = ar.alloc([8, 128], BF16)
    h32 = ar.alloc([8, 128], F32)
    hb = ar.alloc([8, 128], BF16)
    lg = ar.alloc([64], F32)
    scr = {"junk": Tf[6][:, 0:512].bitcast(BF16), "ss": lg[:, 32:34], "rstd": lg[:, 34:36], "xn": Tf[6][:, 512:1024].bitcast(BF16),
           "tmp": Tf[7], "ss2": lg[:, 36:38], "rstd2": lg[:, 38:40]}
    scr_keys = ["T6", "T7"]
    NSET = 8
    SETS = []
    for si in range(NSET):
        d = {}
        d["RHS"] = ar.alloc([2, 128], BF16)
        d["SPLA"] = ar.alloc([3, 128], BF16)
        d["SPLB"] = ar.alloc([3, 128], BF16)
        d["SP2A"] = ar.alloc([2, 128], BF16)
        d["SP2B"] = ar.alloc([2, 128], BF16)
        d["MA1"] = ar.alloc([2, 2, 128], BF16)
        d["MA2"] = ar.alloc([2, 2, 128], BF16)
        d["MT"] = ar.alloc([2, 128], BF16)
        d["Xb_"] = [ar.alloc([2, 128], BF16) for _ in range(2)]
        d["XTb_"] = [ar.alloc([2, 128], BF16) for _ in range(2)]
        d["Tb_"] = [ar.alloc([2, 128], BF16) for _ in range(2)]
        d["Rh"] = ar.alloc([128], BF16)
        d["Yloc"] = ar.alloc([128], F32)
        d["PQ"] = ar.alloc([2, 128], BF16)
        d["Sloc"] = ar.alloc([2, 128], BF16)
        SETS.append(d)
        for k_ in ("SPLA", "SPLB", "SP2A", "SP2B"):
            op("pool", lambda e, t_=d[k_]: e.memset(t_, 0.0), writes=[k_ + "_s%d" % si])
    op("pool", lambda e: e.memset(Sbd, 0.0), writes=["Sbd%d" % c for c in range(8)])

    bank_rr = [0]

    def nb():
        b = bank_rr[0] % 8
        bank_rr[0] += 1
        return b

    evac_rr = [0]

    def cp(dst, src, reads, writes):
        evac_rr[0] += 1
        if evac_rr[0] % 3:
            op("act", lambda e: e.activation(out=dst, in_=src, func=AF.Copy), reads=reads, writes=writes)
        else:
            op("dve", lambda e: e.tensor_copy(out=dst, in_=src), reads=reads, writes=writes)

    def mach(cc, own, kx, PR, Qt, Kt, Qb, Kb, vS, GC):
        d = SETS[cc % NSET]
        sx = "_s%d" % (cc % NSET)
        RHS, SPLA, SPLB, SP2A, SP2B, MA1, MA2, MT = d["RHS"], d["SPLA"], d["SPLB"], d["SP2A"], d["SP2B"], d["MA1"], d["MA2"], d["MT"]
        Xb_, XTb_, Tb_, Rh, Yloc, PQ, Sloc = d["Xb_"], d["XTb_"], d["Tb_"], d["Rh"], d["Yloc"], d["PQ"], d["Sloc"]
        sk = "Sbd%d" % cc
        b = nb()
        tp = ps[b][:, :].bitcast(BF16)
        srcs = (PR[:, cc, 0:128], Qb[:, cc, :], Kb[:, cc, :], vS[:, cc, :])
        skeys = ("PR" + kx, "Qb" + kx, "Kb" + kx, "vS" + kx)
        for i4 in range(4):
            op("pe", lambda e, i4=i4, tp=tp, srcs=srcs: e.transpose(tp[:, i4 * 128:(i4 + 1) * 128], srcs[i4], P["identb"][:]), reads=[skeys[i4], "identb"], writes=["ps%d" % b])
        tpv = tp[:, 128:512].rearrange("p (i j) -> p i j", j=128)
        op("act", lambda e, tpv=tpv: e.activation(out=SPLA[:, :, 0:64], in_=tpv[:, :, 0:64], func=AF.Copy), reads=["ps%d" % b], writes=["SPLA" + sx])
        op("dve", lambda e, tpv=tpv: e.tensor_copy(out=SPLB[:, :, 64:128], in_=tpv[:, :, 64:128]), reads=["ps%d" % b], writes=["SPLB" + sx])
        op("act", lambda e, tp=tp: e.activation(out=RHS[:, :, 0:64], in_=tp[:, 0:128].rearrange("p (h j) -> p h j", h=2), func=AF.Copy), reads=["ps%d" % b], writes=["RHS" + sx])
        yield
        bAh = [nb(), nb()]
        bBh = [nb(), nb()]
        for h in range(2):
            R_ = slice(64 * h, 64 * h + 64)
            op("pe", lambda e, h=h, R_=R_: e.matmul(ps[bAh[h]][:, 0:256], Qt[R_, cc, :], PR[R_, cc, :], start=True, stop=True), reads=["Qt" + kx, "PR" + kx], writes=["ps%d" % bAh[h]])
            op("pe", lambda e, h=h, R_=R_: e.matmul(ps[bAh[h]][:, 256:384], PR[R_, cc, 0:128], Qt[R_, cc, :], start=True, stop=True), reads=["Qt" + kx, "PR" + kx], writes=["ps%d" % bAh[h]])
            op("pe", lambda e, h=h, R_=R_: e.matmul(ps[bBh[h]][:, 0:256], Kt[R_, cc, :], PR[R_, cc, :], start=True, stop=True), reads=["Kt" + kx, "PR" + kx], writes=["ps%d" % bBh[h]])
        for h in range(2):
            op("dve", lambda e, h=h: e.tensor_tensor(out=MA1[:, h, :, :], in0=ps[bAh[h]][:, 0:256].rearrange("p (w t) -> p w t", w=2), in1=mask1, op=ALU.mult), reads=["ps%d" % bAh[h], "mask1"], writes=["MA1" + sx])
            op("dve", lambda e, h=h: e.tensor_tensor(out=MT[:, h, :], in0=ps[bAh[h]][:, 256:384], in1=maskT, op=ALU.mult), reads=["ps%d" % bAh[h], "maskT"], writes=["MT" + sx])
            op("dve", lambda e, h=h: e.tensor_tensor(out=MA2[:, h, :, :], in0=ps[bBh[h]][:, 0:256].rearrange("p (w t) -> p w t", w=2), in1=mask1, op=ALU.mult), reads=["ps%d" % bBh[h], "mask1"], writes=["MA2" + sx])
        yield
        Tc, Tck = Tb_[0], "Tb0" + sx
        op("pool", lambda e, Tc=Tc: e.tensor_tensor(out=Tc, in0=MA1[:, :, 0, :], in1=bc(P["ident"][:].unsqueeze(1), [128, 2, 128]), op=ALU.add), reads=["MA1" + sx, "ident"], writes=[Tck])
        Xc = [MA1[:, 0, 0, :], MA1[:, 1, 0, :]]
        Xck = "MA1" + sx
        XTc = [MT[:, 0, :], MT[:, 1, :]]
        XTck = "MT" + sx
        nlev = 5
        for lv in range(nlev):
            last = (lv == nlev - 1)
            Xn, Xnk = Xb_[lv % 2], "Xb%d" % (lv % 2) + sx
            XTn, XTnk = XTb_[lv % 2], "XTb%d" % (lv % 2) + sx
            Tn, Tnk = Tb_[(lv + 1) % 2], "Tb%d" % ((lv + 1) % 2) + sx
            if not last:
                bX = nb()
                for h in range(2):
                    op("pe", lambda e, h=h, bX=bX, XTc=XTc, Xc=Xc: e.matmul(ps[bX][:, h * 128:(h + 1) * 128], XTc[h], Xc[h], start=True, stop=True), reads=[Xck, XTck], writes=["ps%d" % bX])
                cp(Xn, ps[bX][:, 0:256].rearrange("p (h t) -> p h t", h=2), ["ps%d" % bX], [Xnk])
            bXT = nb()
            for h in range(2):
                op("pe", lambda e, h=h, bXT=bXT, XTc=XTc, Xc=Xc: e.matmul(ps[bXT][:, h * 128:(h + 1) * 128], Xc[h], XTc[h], start=True, stop=True), reads=[Xck, XTck], writes=["ps%d" % bXT])
            cp(XTn, ps[bXT][:, 0:256].rearrange("p (h t) -> p h t", h=2), ["ps%d" % bXT], [XTnk])
            bT = nb()
            for h in range(2):
                op("pe", lambda e, h=h, bT=bT, XTn=XTn, Tc=Tc: e.matmul(ps[bT][:, h * 128:(h + 1) * 128], XTn[:, h, :], Tc[:, h, :], start=True, stop=True), reads=[XTnk, Tck], writes=["ps%d" % bT])
            op("dve", lambda e, bT=bT, Tn=Tn, Tc=Tc: e.tensor_tensor(out=Tn, in0=ps[bT][:, 0:256].rearrange("p (h t) -> p h t", h=2), in1=Tc, op=ALU.add), reads=["ps%d" % bT, Tck], writes=[Tnk])
            Tc, Tck = Tn, Tnk
            yield
            if not last:
                Xc, Xck = [Xn[:, 0, :], Xn[:, 1, :]], Xnk
            XTc, XTck = [XTn[:, 0, :], XTn[:, 1, :]], XTnk
        yield
        bW = nb()
        op("pe", lambda e, bW=bW: e.matmul(ps[bW][:, 0:64], MA2[:, 0, 0, :], SPLA[:, 2, 0:64], start=True, stop=True), reads=["MA2" + sx, "SPLA" + sx], writes=["ps%d" % bW])
        op("pe", lambda e, bW=bW: e.matmul(ps[bW][:, 64:128], MA2[:, 1, 0, :], SPLB[:, 2, 64:128], start=True, stop=True), reads=["MA2" + sx, "SPLB" + sx], writes=["ps%d" % bW])
        cp(RHS[:, :, 64:128], ps[bW][:, 0:128].rearrange("p (h j) -> p h j", h=2), ["ps%d" % bW], ["RHS" + sx])
        yield
        bU = nb()
        for h in range(2):
            op("pe", lambda e, h=h, bU=bU, Tc=Tc: e.matmul(ps[bU][:, h * 128:(h + 1) * 128], Tc[:, h, :], RHS[:, h, :], start=True, stop=True), reads=[Tck, "RHS" + sx], writes=["ps%d" % bU])
        op("act", lambda e, bU=bU: e.activation(out=SP2A[:, :, 0:64], in_=ps[bU][:, 0:128].rearrange("p (w j) -> p w j", w=2), func=AF.Copy), reads=["ps%d" % bU], writes=["SP2A" + sx])
        op("dve", lambda e, bU=bU: e.tensor_copy(out=SP2B[:, :, 64:128], in_=ps[bU][:, 128:256].rearrange("p (w j) -> p w j", w=2)), reads=["ps%d" % bU], writes=["SP2B" + sx])
        yield
        if own:
            bR = nb()
            op("pe", lambda e, bR=bR: e.matmul(ps[bR][:, 0:128], SP2A[:, 0, :], MA1[:, 0, 1, :], start=True, stop=False), reads=["SP2A" + sx, "MA1" + sx], writes=["ps%d" % bR])
            op("pe", lambda e, bR=bR: e.matmul(ps[bR][:, 0:128], SP2B[:, 0, :], MA1[:, 1, 1, :], start=False, stop=True), reads=["SP2B" + sx, "MA1" + sx], writes=["ps%d" % bR])
            op("dve", lambda e, bR=bR: e.tensor_tensor(out=Rh, in0=ps[bR][:, 0:128], in1=PR[:, cc, 128:256], op=ALU.add), reads=["ps%d" % bR, "PR" + kx], writes=["Rh" + sx])
            bY = nb()
            op("pe", lambda e, bY=bY: e.matmul(ps[bY][:, 0:128], SP2A[:, 1, :], MA1[:, 0, 1, :], start=True, stop=False), reads=["SP2A" + sx, "MA1" + sx], writes=["ps%d" % bY])
            op("pe", lambda e, bY=bY: e.matmul(ps[bY][:, 0:128], SP2B[:, 1, :], MA1[:, 1, 1, :], start=False, stop=False), reads=["SP2B" + sx, "MA1" + sx], writes=["ps%d" % bY])
            op("pe", lambda e, bY=bY: e.matmul(ps[bY][:, 0:128], SPLA[:, 2, :], MA2[:, 0, 1, :], start=False, stop=False), reads=["SPLA" + sx, "MA2" + sx], writes=["ps%d" % bY])
            op("pe", lambda e, bY=bY: e.matmul(ps[bY][:, 0:128], SPLB[:, 2, :], MA2[:, 1, 1, :], start=False, stop=True), reads=["SPLB" + sx, "MA2" + sx], writes=["ps%d" % bY])
            cp(Yloc, ps[bY][:, 0:128], ["ps%d" % bY], ["Yloc" + sx])
        yield
        bQc = [nb(), nb()]
        for c in range(2):
            R_ = slice(64 * c, 64 * c + 64)
            bq = bQc[c]
            op("pe", lambda e, R_=R_, bq=bq: e.matmul(ps[bq][:, 0:128], SP2A[R_, 0, :], SPLA[R_, 0, :], start=True, stop=False), reads=["SP2A" + sx, "SPLA" + sx], writes=["ps%d" % bq])
            op("pe", lambda e, R_=R_, bq=bq: e.matmul(ps[bq][:, 0:128], SP2B[R_, 0, :], SPLB[R_, 0, :], start=False, stop=True), reads=["SP2B" + sx, "SPLB" + sx], writes=["ps%d" % bq])
            op("pe", lambda e, R_=R_, bq=bq: e.matmul(ps[bq][:, 128:256], SPLA[R_, 0, :], SP2A[R_, 1, :], start=True, stop=False), reads=["SP2A" + sx, "SPLA" + sx], writes=["ps%d" % bq])
            op("pe", lambda e, R_=R_, bq=bq: e.matmul(ps[bq][:, 128:256], SPLB[R_, 0, :], SP2B[R_, 1, :], start=False, stop=False), reads=["SP2B" + sx, "SPLB" + sx], writes=["ps%d" % bq])
            op("pe", lambda e, R_=R_, bq=bq: e.matmul(ps[bq][:, 128:256], SPLA[R_, 1, :], SPLA[R_, 2, :], start=False, stop=False), reads=["SPLA" + sx], writes=["ps%d" % bq])
            op("pe", lambda e, R_=R_, bq=bq: e.matmul(ps[bq][:, 128:256], SPLB[R_, 1, :], SPLB[R_, 2, :], start=False, stop=True), reads=["SPLB" + sx], writes=["ps%d" % bq])
        for c in range(2):
            cp(PQ[:, c, :], ps[bQc[c]][:, 0:128], ["ps%d" % bQc[c]], ["PQ" + sx])
            cp(Sloc[:, c, :], ps[bQc[c]][:, 128:256], ["ps%d" % bQc[c]], ["Sloc" + sx])
        yield
        for c in range(2):
            yield
            if own:
                bYc = nb()
                op("pe", lambda e, c=c, bYc=bYc: e.matmul(ps[bYc][:, 0:64], Sbd[:, cc, :], Rh[:, c * 64:(c + 1) * 64], start=True, stop=True), reads=[sk, "Rh" + sx], writes=["ps%d" % bYc])
                op("dve", lambda e, c=c, bYc=bYc: e.tensor_tensor(out=Ysb[:, cc, c * 64:(c + 1) * 64], in0=ps[bYc][:, 0:64], in1=Yloc[:, c * 64:(c + 1) * 64], op=ALU.add), reads=["ps%d" % bYc, "Yloc" + sx], writes=["Ysb"])
            bS2 = nb()
            op("pe", lambda e, c=c, bS2=bS2: e.matmul(ps[bS2][:, 0:128], PQ[:, c, :], Sbd[:, cc, :], start=True, stop=False), reads=["PQ" + sx, sk], writes=["ps%d" % bS2])
            op("pe", lambda e, c=c, bS2=bS2: e.matmul(ps[bS2][:, 0:128], P["identb"][:], Sloc[:, c, :], start=False, stop=True), reads=["identb", "Sloc" + sx], writes=["ps%d" % bS2])
            op("dve", lambda e, c=c, bS2=bS2: e.scalar_tensor_tensor(out=Sbd[:, cc, :], in0=Sbd[:, cc, :], scalar=GC[:, cc * 2 + c:cc * 2 + c + 1], in1=ps[bS2][:, 0:128], op0=ALU.mult, op1=ALU.add),
               reads=[sk, "GC" + kx, "ps%d" % bS2], writes=[sk])

    NTILE = SEQ // 128
    npre, nown = getattr(self, "rw_tiles", (NTILE // 2, NTILE // 2))
    tiles = list(range(npre)) + list(range(NTILE // 2, NTILE // 2 + nown))
    for tn, ti in enumerate(tiles):
        own = ti >= NTILE // 2
        par = tn % 2
        kx = "_%d" % par
        MIb, OWb, GC, xt = MIs[par], OWs[par], GCs[par], xts[par]
        xk = "xt" + kx
        PR, Qt, Kt, Qb, Kb, vS = MIb[:, :, 0:256], MIb[:, :, 256:384], MIb[:, :, 384:512], MIb[:, :, 512:640], MIb[:, :, 640:768], MIb[:, :, 768:896]
        gS, bonus = OWb[:, :, 0:128], OWb[:, :, 128:256]
        fw.dma("sp", MIb, S["MI"][ti], writes=["PR" + kx, "Qt" + kx, "Kt" + kx, "Qb" + kx, "Kb" + kx, "vS" + kx])
        fw.dma("sp", GC, S["GCd"][ti], writes=["GC" + kx])
        if own:
            fw.dma("sp", OWb, S["OW"][ti - NTILE // 2], writes=["gS" + kx, "bonus" + kx])
            fw.dma("sp", xt, S["x2"][ti * 128:(ti + 1) * 128, :], writes=[xk])
        if ti == NTILE // 2:
            op("pool", lambda e: e.tensor_scalar(out=Sbd, in0=Sbd, scalar1=P["flag"][:, 0:1], scalar2=None, op0=ALU.mult),
               reads=["flag"] + ["Sbd%d" % c for c in range(8)], writes=["Sbd%d" % c for c in range(8)])
        gens = [mach(cc, own, kx, PR, Qt, Kt, Qb, Kb, vS, GC) for cc in range(8)]
        while gens:
            for g in list(gens):
                try:
                    next(g)
                except StopIteration:
                    gens.remove(g)
        if not own:
            continue
        op("act", lambda e: e.activation(out=B0, in_=Ysb, func=AF.Copy), reads=["Ysb"], writes=["B0"])
        for cc in range(8):
            bb = cc // 4
            op("pe", lambda e, cc=cc, bb=bb: e.matmul(ps[bb][:, (cc % 4) * 128:(cc % 4 + 1) * 128], bdones, B0[:, cc, :], start=True, stop=True), reads=["bdones", "B0"], writes=["ps%d" % bb])
        dd, ddk = T3[1], "T1"
        for hh in range(2):
            op("dve", lambda e, hh=hh: e.scalar_tensor_tensor(out=dd[:, hh * 4:(hh + 1) * 4, :], in0=ps[hh][:, :].rearrange("p (c t) -> p c t", t=128), scalar=-1.0 / 64, in1=Ysb[:, hh * 4:(hh + 1) * 4, :], op0=ALU.mult, op1=ALU.add),
               reads=["ps%d" % hh, "Ysb"], writes=[ddk])
        op("pool", lambda e: e.tensor_tensor(out=B0, in0=dd, in1=dd, op=ALU.mult), reads=[ddk], writes=["B0"])
        for cc in range(8):
            bb = 2 + cc // 4
            op("pe", lambda e, cc=cc, bb=bb: e.matmul(ps[bb][:, (cc % 4) * 128:(cc % 4 + 1) * 128], bdones, B0[:, cc, :], start=True, stop=True), reads=["bdones", "B0"], writes=["ps%d" % bb])
        rs, rsk = T3[2], "T2"
        for hh in range(2):
            op("act", lambda e, hh=hh: e.activation(out=rs[:, hh * 4:(hh + 1) * 4, :], in_=ps[2 + hh][:, :].rearrange("p (c t) -> p c t", t=128), func=AF.Sqrt, scale=1.0 / 64, bias=P["eps"][:, 1:2]), reads=["ps%d" % (2 + hh), "eps"], writes=[rsk])
        op("dve", lambda e: e.reciprocal(out=Tf[2], in_=Tf[2]), reads=[rsk], writes=[rsk])
        op("pool", lambda e: e.tensor_tensor(out=dd, in0=dd, in1=rs, op=ALU.mult), reads=[ddk, rsk], writes=[ddk])
        op("pool", lambda e: e.tensor_tensor(out=dd, in0=dd, in1=vb(11), op=ALU.mult), reads=[ddk, "vec"], writes=[ddk])
        op("pool", lambda e: e.tensor_tensor(out=dd, in0=dd, in1=vb(12), op=ALU.add), reads=[ddk, "vec"], writes=[ddk])
        op("dve", lambda e: e.tensor_tensor(out=dd, in0=dd, in1=bonus, op=ALU.add), reads=[ddk, "bonus" + kx], writes=[ddk])
        op("dve", lambda e: e.tensor_tensor(out=yg, in0=dd, in1=gS, op=ALU.mult), reads=[ddk, "gS" + kx], writes=["yg"])
        for n in range(2):
            for kc in range(8):
                op("pe", lambda e, n=n, kc=kc: e.matmul(ps[2 + n][:, :], yg[:, kc, :], W["rw_wo"][:, kc, n * 512:(n + 1) * 512], start=(kc == 0), stop=(kc == 7)), reads=["yg", "rw_wo"], writes=["ps%d" % (2 + n)])
        self.postnorm_res2((2, 3), xt, xk, 2, scr, scr_keys)
        to = ti - NTILE // 2
        fw.dma("sp", S["x3"][to * 128:(to + 1) * 128, :], xt, reads=[xk])
        junk, ss, rstd = scr["junk"], scr["ss"], scr["rstd"]
        op("act", lambda e: e.activation(out=junk, in_=xt, func=AF.Square, accum_out=ss[:, 0:1]), reads=[xk], writes=["T6", "lg"])
        op("act", lambda e: e.activation(out=rstd[:, 0:1], in_=ss[:, 0:1], func=AF.Sqrt, scale=1.0 / D, bias=P["eps"][:, 0:1]), reads=["lg", "eps"], writes=["lg"])
        op("dve", lambda e: e.reciprocal(out=rstd[:, 0:1], in_=rstd[:, 0:1]), reads=["lg"], writes=["lg"])
        xn32 = Tf[7]
        op("act", lambda e: e.activation(out=xn32, in_=xt, func=AF.Copy, scale=rstd[:, 0:1]), reads=[xk, "lg"], writes=["T7"])
        for kc in range(8):
            bb = kc // 4
            op("pe", lambda e, kc=kc, bb=bb: e.transpose(ps[bb][:, (kc % 4) * 128:(kc % 4 + 1) * 128], xn32[:, kc * 128:(kc + 1) * 128], P["ident"][:]), reads=["T7", "ident"], writes=["ps%d" % bb])
        G1 = P["modT"][:, 4 + 2, :]
        sh = P["modT"][:, 4 + 3, :]
        for hh in range(2):
            op("dve", lambda e, hh=hh: e.tensor_tensor(out=h32[:, hh * 4:(hh + 1) * 4, :], in0=ps[hh][:, :].rearrange("p (c t) -> p c t", t=128), in1=bc(G1[:, hh * 4:(hh + 1) * 4].unsqueeze(2), [128, 4, 128]), op=ALU.mult),
               reads=["ps%d" % hh, "modT"], writes=["h32"])
        op("pool", lambda e: e.tensor_tensor(out=h32, in0=h32, in1=bc(sh.unsqueeze(2), [128, 8, 128]), op=ALU.add), reads=["h32", "modT"], writes=["h32"])
        op("act", lambda e: e.activation(out=hb, in_=h32, func=AF.Copy), reads=["h32"], writes=["hb"])
        fw.dma("sp", S["hTf1"][to // 2][:, :, (to % 2) * 128:(to % 2 + 1) * 128], hb, reads=["hb"])
        bL = nb()
        for kc in range(8):
            op("pe", lambda e, kc=kc, bL=bL: e.matmul(ps[bL][:, 0:8], h32[:, kc, :], router[:, kc, :], start=(kc == 0), stop=(kc == 7)), reads=["h32", "router"], writes=["ps%d" % bL])
        L8, m1, m2, eq, ex, sm = lg[:, 0:8], lg[:, 8:9], lg[:, 9:10], lg[:, 10:18], lg[:, 18:26], lg[:, 26:27]
        op("dve", lambda e, bL=bL: e.tensor_copy(out=L8, in_=ps[bL][:, 0:8]), reads=["ps%d" % bL], writes=["lg"])
        op("dve", lambda e: e.reduce_max(out=m1, in_=L8, axis=AX.X), reads=["lg"], writes=["lg"])
        op("dve", lambda e: e.tensor_scalar(out=eq, in0=L8, scalar1=m1, scalar2=-1e30, op0=ALU.is_equal, op1=ALU.mult), reads=["lg"], writes=["lg"])
        op("dve", lambda e: e.tensor_tensor(out=eq, in0=eq, in1=L8, op=ALU.add), reads=["lg"], writes=["lg"])
        op("dve", lambda e: e.reduce_max(out=m2, in_=eq, axis=AX.X), reads=["lg"], writes=["lg"])
        op("dve", lambda e: e.tensor_scalar(out=eq, in0=L8, scalar1=m2, scalar2=None, op0=ALU.is_ge), reads=["lg"], writes=["lg"])
        op("dve", lambda e: e.tensor_scalar(out=ex, in0=L8, scalar1=m1, scalar2=None, op0=ALU.subtract), reads=["lg"], writes=["lg"])
        op("act", lambda e: e.activation(out=ex, in_=ex, func=AF.Exp), reads=["lg"], writes=["lg"])
        op("dve", lambda e: e.tensor_tensor(out=ex, in0=ex, in1=eq, op=ALU.mult), reads=["lg"], writes=["lg"])
        op("dve", lambda e: e.reduce_sum(out=sm, in_=ex, axis=AX.X), reads=["lg"], writes=["lg"])
        op("dve", lambda e: e.reciprocal(out=sm, in_=sm), reads=["lg"], writes=["lg"])
        op("dve", lambda e: e.tensor_scalar(out=ex, in0=ex, scalar1=sm, scalar2=None, op0=ALU.mult), reads=["lg"], writes=["lg"])
        fw.dma("sp", S["comb"][to * 128:(to + 1) * 128, :], ex, reads=["lg"])


def _postnorm_sb2(self, y, ykey, xsub, xkey, gp_idx, scr, skeys):
    fw, P = self.fw, self.P
    junk, ss, rstd, tmp = scr["junk"], scr["ss2"], scr["rstd2"], scr["tmp"]
    fw.op("act", lambda e: e.activation(out=junk, in_=y, func=AF.Square, accum_out=ss[:, 0:1]), reads=[ykey], writes=[skeys[0], "lg"])
    fw.op("act", lambda e: e.activation(out=rstd[:, 0:1], in_=ss[:, 0:1], func=AF.Sqrt, scale=1.0 / D, bias=P["eps"][:, 0:1]), reads=["lg", "eps"], writes=["lg"])
    fw.op("dve", lambda e: e.reciprocal(out=rstd[:, 0:1], in_=rstd[:, 0:1]), reads=["lg"], writes=["lg"])
    fw.op("dve", lambda e: e.scalar_tensor_tensor(out=tmp, in0=y, scalar=rstd[:, 0:1], in1=P["GP"][:, gp_idx, :], op0=ALU.mult, op1=ALU.mult),
          reads=[ykey, "lg", "GP"], writes=[skeys[1]])
    fw.op("dve", lambda e: e.tensor_tensor(out=xsub, in0=xsub, in1=tmp, op=ALU.add), reads=[skeys[1], xkey], writes=[xkey])


def _prenorm_T2(self, xsub, xkey, l, sub, hT, hkey, col0, pbank, scr, skeys):
    fw, P, ps = self.fw, self.P, self.ps
    junk, ss, rstd, xn, tmp = scr["junk"], scr["ss"], scr["rstd"], scr["xn"], scr["tmp"]
    fw.op("act", lambda e: e.activation(out=junk, in_=xsub, func=AF.Square, accum_out=ss[:, 0:1]), reads=[xkey], writes=[skeys[0], "lg"])
    fw.op("act", lambda e: e.activation(out=rstd[:, 0:1], in_=ss[:, 0:1], func=AF.Sqrt, scale=1.0 / D, bias=P["eps"][:, 0:1]), reads=["lg", "eps"], writes=["lg"])
    fw.op("dve", lambda e: e.reciprocal(out=rstd[:, 0:1], in_=rstd[:, 0:1]), reads=["lg"], writes=["lg"])
    fw.op("act", lambda e: e.activation(out=xn, in_=xsub, func=AF.Copy, scale=rstd[:, 0:1]), reads=[xkey, "lg"], writes=[skeys[0]])
    pk = "ps%d" % pbank
    pbt = ps[pbank][:, :].bitcast(BF16)
    for kc in range(8):
        fw.op("pe", lambda e, kc=kc: e.transpose(pbt[:, kc * 128:(kc + 1) * 128], xn[:, kc * 128:(kc + 1) * 128], P["identb"][:]), reads=[skeys[0], "identb"], writes=[pk])
    G1 = P["modT"][:, l * 4 + sub * 2 + 0, :]
    sh = P["modT"][:, l * 4 + sub * 2 + 1, :]
    for kc in range(8):
        fw.op("act", lambda e, kc=kc: e.activation(out=hT[:, kc, col0:col0 + 128], in_=pbt[:, kc * 128:(kc + 1) * 128], func=AF.Identity, scale=G1[:, kc:kc + 1], bias=sh[:, kc:kc + 1]),
              reads=[pk, "modT"], writes=[hkey])


def _postnorm_res2(self, psb, xsub, xkey, gp_idx, scr, skeys):
    fw, P, ps = self.fw, self.P, self.ps
    junk, ss2, rstd, tmp = scr["junk"], scr["ss2"], scr["rstd2"], scr["tmp"]
    for n in range(2):
        fw.op("act", lambda e, n=n: e.activation(out=junk[:, 0:512], in_=ps[psb[n]][:, :], func=AF.Square, accum_out=ss2[:, n:n + 1]), reads=["ps%d" % psb[n]], writes=[skeys[0], "lg"])
    fw.op("pool", lambda e: e.tensor_tensor(out=rstd[:, 0:1], in0=ss2[:, 0:1], in1=ss2[:, 1:2], op=ALU.add), reads=["lg"], writes=["lg"])
    fw.op("act", lambda e: e.activation(out=rstd[:, 0:1], in_=rstd[:, 0:1], func=AF.Sqrt, scale=1.0 / D, bias=P["eps"][:, 0:1]), reads=["lg", "eps"], writes=["lg"])
    fw.op("dve", lambda e: e.reciprocal(out=rstd[:, 0:1], in_=rstd[:, 0:1]), reads=["lg"], writes=["lg"])
    for n in range(2):
        fw.op("dve", lambda e, n=n: e.scalar_tensor_tensor(out=tmp[:, n * 512:(n + 1) * 512], in0=ps[psb[n]][:, :], scalar=rstd[:, 0:1], in1=P["GP"][:, gp_idx, n * 512:(n + 1) * 512], op0=ALU.mult, op1=ALU.mult),
              reads=["ps%d" % psb[n], "lg", "GP"], writes=[skeys[1]])
    fw.op("dve", lambda e: e.tensor_tensor(out=xsub, in0=xsub, in1=tmp, op=ALU.add), reads=[skeys[1], xkey], writes=[xkey])


Builder.phase_rwkv_prep = _phase_rwkv_prep
Builder.phase_rwkv_chain = _phase_rwkv_chain
Builder.postnorm_sb2 = _postnorm_sb2
Builder.prenorm_T2 = _prenorm_T2
Builder.postnorm_res2 = _postnorm_res2
```

```python
import contextlib
import numpy as np
import concourse.bass as bass
import concourse.mybir as mybir
from concourse.bass_utils import run_bass_kernel_spmd

F32 = mybir.dt.float32
BF16 = mybir.dt.bfloat16
ALU = mybir.AluOpType
AF = mybir.ActivationFunctionType
AX = mybir.AxisListType

D = 1024
FF = 2816
NE = 8
SEQ = 8192
HALF = 4096
RMS_EPS = 1e-6
LNX_EPS = 1e-5 * 64

COMPUTE = ("pe", "act", "dve", "pool")
NDMA_SEMS = 12
EPOCH = 30000


class _Op:
    __slots__ = ("eng", "fn", "deps", "is_dma", "need_inc", "tick", "dsem", "dval", "dprev")

    def __init__(self, eng, fn, deps, is_dma):
        self.eng = eng
        self.fn = fn
        self.deps = deps
        self.is_dma = is_dma
        self.need_inc = False
        self.tick = 0
        self.dsem = None
        self.dval = 0
        self.dprev = None


class _Rec:
    def __getattr__(self, name):
        return lambda *a, **k: (name, a, k)


_REC = _Rec()


class FW:
    def __init__(self, nc):
        self.nc = nc
        self.ops = []
        self.last_w = {}
        self.readers = {}
        self.bar = set()
        self.last_c = {}
        self.dma_since = []

    def _add(self, eng, fn, reads, writes, is_dma):
        idx = len(self.ops)
        pr = [r for r in reads if r.startswith("ps")]
        if pr:
            reads = [r for r in reads if not r.startswith("ps")]
            writes = list(writes) + pr
        deps = set(self.bar)
        for r in reads:
            w = self.last_w.get(r)
            if w is not None:
                deps.add(w)
        for k in writes:
            w = self.last_w.get(k)
            if w is not None:
                deps.add(w)
            rd = self.readers.get(k)
            if rd is not None:
                deps.update(rd["c"].values())
                deps.update(rd["d"])
        for r in reads:
            rd = self.readers.get(r)
            if rd is None:
                rd = self.readers[r] = {"c": {}, "d": []}
            if is_dma:
                rd["d"].append(idx)
            else:
                rd["c"][eng] = idx
        for k in writes:
            self.last_w[k] = idx
            self.readers[k] = {"c": {}, "d": []}
        deps.discard(idx)
        self.ops.append(_Op(eng, fn, deps, is_dma))
        if is_dma:
            self.dma_since.append(idx)
        else:
            self.last_c[eng] = idx
        return idx

    def op(self, eng, fn, reads=(), writes=()):
        name, a, k = fn(_REC)
        return self._add(eng, lambda e: getattr(e, name)(*a, **k), reads, writes, False)

    def dma(self, q, out, in_, reads=(), writes=()):
        return self._add(q, lambda e: e.dma_start(out=out, in_=in_), reads, writes, True)

    def barrier(self):
        self.bar = set(self.last_c.values()) | set(self.dma_since)
        self.dma_since = []
        self.last_w = {}
        self.readers = {}

    def emit(self):
        nc = self.nc
        ops = self.ops
        for o in ops:
            for d in o.deps:
                p = ops[d]
                if p.is_dma:
                    continue
                if p.eng == o.eng and p.eng == "pe" and not o.is_dma:
                    continue
                p.need_inc = True
        ticks = {e: 0 for e in COMPUTE}
        for o in ops:
            if not o.is_dma and o.need_inc:
                ticks[o.eng] += 1
                o.tick = ticks[o.eng]
        qcount = {}
        qlast = {}
        for i, o in enumerate(ops):
            if o.is_dma:
                n = qcount.get(o.eng, 0)
                qcount[o.eng] = n + 1
                slot = n % NDMA_SEMS
                key = (o.eng, slot)
                o.dsem = key
                o.dval = (n // NDMA_SEMS + 1) * 16
                o.dprev = qlast.get(key)
                qlast[key] = i
        engs = ("pe", "act", "dve", "pool", "sp")
        with contextlib.ExitStack() as st:
            csem = {}
            for e in COMPUTE:
                nep = (ticks[e] + EPOCH - 1) // EPOCH
                for k in range(max(nep, 1)):
                    csem[(e, k)] = st.enter_context(nc.semaphore("c_%s_%d" % (e, k)))
            dsem = {}
            for q in qcount:
                for s in range(min(NDMA_SEMS, qcount[q])):
                    dsem[(q, s)] = st.enter_context(nc.semaphore("d_%s_%d" % (q, s)))
            known = {e: {} for e in engs}
            streams = {e: [] for e in engs}
            for i, o in enumerate(ops):
                e = o.eng
                kn = known[e]
                waits = {}
                for d in o.deps:
                    p = ops[d]
                    if p.is_dma:
                        sk = ("d",) + p.dsem
                        v = p.dval
                    else:
                        if p.eng == e and e == "pe" and not o.is_dma:
                            continue
                        ep = (p.tick - 1) // EPOCH
                        sk = ("c", p.eng, ep)
                        v = p.tick - ep * EPOCH
                    if kn.get(sk, 0) >= v:
                        continue
                    if waits.get(sk, 0) < v:
                        waits[sk] = v
                if o.is_dma and o.dprev is not None:
                    p = ops[o.dprev]
                    sk = ("d",) + p.dsem
                    if kn.get(sk, 0) < p.dval and waits.get(sk, 0) < p.dval:
                        waits[sk] = p.dval
                for sk, v in waits.items():
                    kn[sk] = v
                    sem = csem[(sk[1], sk[2])] if sk[0] == "c" else dsem[(sk[1], sk[2])]
                    streams[e].append(("w", sem, v))
                if o.is_dma:
                    streams[e].append(("i", o.fn, dsem[o.dsem], 16))
                elif o.need_inc:
                    streams[e].append(("i", o.fn, csem[(e, (o.tick - 1) // EPOCH)], 1))
                else:
                    streams[e].append(("i", o.fn, None, 0))
            fin = streams["sp"]
            for key, i in qlast.items():
                fin.append(("w", dsem[key], ops[i].dval))
            for e in COMPUTE:
                if ticks[e] > 0:
                    ep = (ticks[e] - 1) // EPOCH
                    fin.append(("w", csem[(e, ep)], ticks[e] - ep * EPOCH))

            def run(eng_obj, lst):
                for it in lst:
                    if it[0] == "w":
                        eng_obj.wait_ge(it[1], it[2])
                    else:
                        ins = it[1](eng_obj)
                        if it[2] is not None:
                            ins.then_inc(it[2], it[3])

            with nc.Block() as block:
                @block.tensor
                def _(eng):
                    run(eng, streams["pe"])

                @block.scalar
                def _(eng):
                    run(eng, streams["act"])

                @block.vector
                def _(eng):
                    run(eng, streams["dve"])

                @block.gpsimd
                def _(eng):
                    run(eng, streams["pool"])

                @block.sync
                def _(eng):
                    run(eng, streams["sp"])
        return {e: len(streams[e]) for e in streams}


class Arena:
    def __init__(self, t, nwords):
        self.t = t
        self.n = nwords
        self.off = 0

    def reset(self):
        self.off = 0

    def alloc(self, shape, dtype):
        nel = int(np.prod(shape))
        nb = nel * (4 if dtype == F32 else 2)
        nw = (nb + 3) // 4
        ap = self.t[:, self.off:self.off + nw]
        self.off += nw
        assert self.off <= self.n, ("arena overflow", self.off, self.n)
        if dtype != F32:
            ap = ap.bitcast(dtype)[:, 0:nel]
        if len(shape) == 2:
            ap = ap.rearrange("p (a b) -> p a b", a=shape[0])
        elif len(shape) == 3:
            ap = ap.rearrange("p (a b c) -> p a b c", a=shape[0], b=shape[1])
        return ap


def bc(ap, shape):
    return ap.to_broadcast(list(shape))


class Builder:
    def __init__(self, stages=("M", "A", "B", "D", "E", "F"), dbg=False):
        self.stages = stages
        self.dbg = dbg
        self.nc = bass.Bass("TRN2", target_bir_lowering=False)
        self.fw = FW(self.nc)
        self.uid = 0

    def din(self, name, shape, dt=F32):
        return self.nc.dram_tensor(name, list(shape), dt, kind="ExternalInput").ap()

    def dscr(self, name, shape, dt=F32, out=False):
        kind = "ExternalOutput" if out else "Internal"
        return self.nc.dram_tensor(name, list(shape), dt, kind=kind).ap()

    def build(self):
        nc, fw = self.nc, self.fw
        dbg = self.dbg
        I = {}
        I["x8"] = self.din("x8", [SEQ, D])
        I["condB"] = self.din("condB", [128, 8, 128])
        I["flag"] = self.din("flag", [128, 1])
        I["invc"] = self.din("invc", [128, 2, 4, 16])
        I["ident"] = self.din("ident", [128, 128])
        I["ada_w"] = self.din("ada_w", [2, D, 6 * D])
        I["ada_bB"] = self.din("ada_bB", [2, 128, 6 * D])
        I["norm_gB"] = self.din("norm_gB", [2, 128, 4, D])
        I["mix_w_in"] = self.din("mix_w_in", [D, 2048])
        I["mix_w_out"] = self.din("mix_w_out", [D, D])
        I["conv_wT"] = self.din("conv_wT", [128, 4, 3])
        I["pool_w"] = self.din("pool_w", [4, 128, 128])
        I["pool_scT"] = self.din("pool_scT", [128, 4])
        I["ffn_wg"] = self.din("ffn_wg", [D, FF])
        I["ffn_wu"] = self.din("ffn_wu", [D, FF])
        I["ffn_wd"] = self.din("ffn_wd", [FF, D])
        for nm in ("rw_wr", "rw_wk", "rw_wv", "rw_wo"):
            I[nm] = self.din(nm, [D, D])
        I["rw_w1"] = self.din("rw_w1", [D, 64])
        I["rw_a1"] = self.din("rw_a1", [D, 64])
        I["rw_g1"] = self.din("rw_g1", [D, 160])
        I["rw_w2"] = self.din("rw_w2", [64, D])
        I["rw_a2"] = self.din("rw_a2", [64, D])
        I["rw_g2"] = self.din("rw_g2", [160, D])
        I["rw_vec"] = self.din("rw_vec", [128, 13, 8])
        I["moe_router"] = self.din("moe_router", [D, 8])
        I["resetm"] = self.din("resetm", [128, 1024])
        I["mask1"] = self.din("mask1", [128, 2, 128])
        I["maskT"] = self.din("maskT", [128, 128])
        I["bdones"] = self.din("bdones", [128, 128])
        I["moe_wg"] = self.din("moe_wg", [NE, D, FF])
        I["moe_wu"] = self.din("moe_wu", [NE, D, FF])
        I["moe_wd"] = self.din("moe_wd", [NE, FF, D])
        self.I = I
        S = {}
        S["x1"] = self.dscr("x1", [SEQ, D], out=dbg)
        S["hTf0"] = self.dscr("hTf0", [32, 128, 8, 256], BF16)
        S["acc0"] = self.dscr("acc0", [SEQ, D])
        S["x2"] = self.dscr("x2", [SEQ, D], out=("C" in self.stages))
        S["x3"] = self.dscr("x3", [HALF, D], out=dbg)
        S["MI"] = self.dscr("MI", [64, 128, 8, 896], BF16)
        S["GCd"] = self.dscr("GCd", [64, 128, 16])
        S["OW"] = self.dscr("OW", [32, 128, 8, 256], BF16)
        S["hTf1"] = self.dscr("hTf1", [16, 128, 8, 256], BF16)
        S["comb"] = self.dscr("comb", [HALF, 8], out=dbg)
        S["acc1"] = self.dscr("acc1", [HALF, D])
        S["y"] = self.dscr("y", [HALF, D], out=True)
        self.S = S

        with contextlib.ExitStack() as st:
            sb = lambda n, sh, dt: st.enter_context(nc.sbuf_tensor("sb_" + n, sh, dt))
            P = {}
            P["ident"] = sb("identf", [128, 128], F32)
            P["identb"] = sb("identb", [128, 128], BF16)
            P["flag"] = sb("flag", [128, 1], F32)
            P["invc"] = sb("invc", [128, 2, 4, 16], F32)
            P["GP"] = sb("GP", [128, 4, D], F32)
            P["modT"] = sb("modT", [128, 8, 8], F32)
            P["small"] = sb("small", [128, 64], F32)
            P["eps"] = sb("eps", [128, 2], F32)
            ARW = 48000
            arena_t = sb("arena", [128, ARW], F32)
            self.P = P
            self.ar = Arena(arena_t, ARW)
            self.ps = [st.enter_context(nc.psum_tensor("ps%d" % i, [128, 512], F32)) for i in range(8)]

            fw.dma("sp", P["ident"][:], I["ident"], writes=["ident"])
            fw.dma("pool", P["identb"][:], I["ident"], writes=["identb"])
            fw.dma("sp", P["flag"][:], I["flag"], writes=["flag"])
            fw.op("pool", lambda e: e.memset(P["eps"][:, 0:1], RMS_EPS), writes=["eps"])
            fw.op("pool", lambda e: e.memset(P["eps"][:, 1:2], LNX_EPS), writes=["eps"])
            fw.dma("sp", P["invc"][:], I["invc"], writes=["invc"])
            if "M" in self.stages:
                self.phase_mod(0)
                self.phase_mod(1)
            if "A" in self.stages:
                self.phase_mixer0()
            if "B" in self.stages:
                fw.barrier()
                self.ar.reset()
                self.ffn_passes(self.I["ffn_wg"], self.I["ffn_wu"], self.I["ffn_wd"], 32, S["hTf0"], S["acc0"], None, [0])
            if "C" in self.stages:
                fw.barrier()
                self.ar.reset()
                self.phase_post0()
            if "D" in self.stages:
                self.phase_rwkv_prep()
                self.phase_rwkv_chain()
            if "E" in self.stages:
                fw.barrier()
                self.ar.reset()
                self.ffn_passes(self.I["moe_wg"], self.I["moe_wu"], self.I["moe_wd"], 16, S["hTf1"], S["acc1"], S["comb"], list(range(NE)))
            if "F" in self.stages:
                fw.barrier()
                self.ar.reset()
                self.phase_final()
            self.stats = fw.emit()
        return nc

    def phase_mod(self, l):
        nc, fw, P, I, ar = self.nc, self.fw, self.P, self.I, self.ar
        ps = self.ps
        fw.barrier()
        ar.reset()
        cond = ar.alloc([8, 128], F32)
        modb = ar.alloc([6 * D], F32)
        adab = ar.alloc([6 * D], F32)
        ng = ar.alloc([4, D], F32)
        wblk = [ar.alloc([8, 512], F32) for _ in range(2)]
        tmpb = ar.alloc([4, D], F32)
        fw.dma("sp", cond, I["condB"], writes=["cond"])
        fw.dma("sp", adab, I["ada_bB"][l], writes=["adab"])
        fw.dma("sp", ng, I["norm_gB"][l], writes=["ng"])
        fw.op("act", lambda e: e.activation(out=cond, in_=cond, func=AF.Silu), reads=["cond"], writes=["cond"])
        for blk in range(12):
            wb = wblk[blk % 2]
            wk = "wblk%d" % (blk % 2)
            fw.dma("sp", wb, I["ada_w"][l, :, blk * 512:(blk + 1) * 512].rearrange("(kc p) n -> p kc n", p=128), writes=[wk])
            pb = ps[blk % 2]
            pk = "ps%d" % (blk % 2)
            for kc in range(8):
                fw.op("pe", lambda e, kc=kc, wb=wb, pb=pb: e.matmul(pb[:, :], cond[:, kc, :], wb[:, kc, :], start=(kc == 0), stop=(kc == 7)),
                      reads=["cond", wk], writes=[pk])
            fw.op("dve", lambda e, blk=blk, pb=pb: e.tensor_tensor(out=modb[:, blk * 512:(blk + 1) * 512], in0=pb[:, :], in1=adab[:, blk * 512:(blk + 1) * 512], op=ALU.add),
                  reads=[pk, "adab"], writes=["modb"])
        sh_m, sc_m, gt_m, sh_f, sc_f, gt_f = [modb[:, i * D:(i + 1) * D] for i in range(6)]
        fw.op("dve", lambda e: e.tensor_tensor(out=P["GP"][:, l * 2 + 0, :], in0=gt_m, in1=ng[:, 1, :], op=ALU.mult), reads=["modb", "ng"], writes=["GP"])
        fw.op("dve", lambda e: e.tensor_tensor(out=P["GP"][:, l * 2 + 1, :], in0=gt_f, in1=ng[:, 3, :], op=ALU.mult), reads=["modb", "ng"], writes=["GP"])
        fw.op("dve", lambda e: e.scalar_tensor_tensor(out=tmpb[:, 0, :], in0=sc_m, scalar=1.0, in1=ng[:, 0, :], op0=ALU.add, op1=ALU.mult), reads=["modb", "ng"], writes=["tmpb"])
        fw.op("dve", lambda e: e.tensor_copy(out=tmpb[:, 1, :], in_=sh_m), reads=["modb"], writes=["tmpb"])
        fw.op("dve", lambda e: e.scalar_tensor_tensor(out=tmpb[:, 2, :], in0=sc_f, scalar=1.0, in1=ng[:, 2, :], op0=ALU.add, op1=ALU.mult), reads=["modb", "ng"], writes=["tmpb"])
        fw.op("dve", lambda e: e.tensor_copy(out=tmpb[:, 3, :], in_=sh_f), reads=["modb"], writes=["tmpb"])
        for v in range(4):
            for kc in range(8):
                pb = ps[2 + kc // 4]
                fw.op("pe", lambda e, v=v, kc=kc, pb=pb: e.transpose(pb[:, (kc % 4) * 128:(kc % 4 + 1) * 128], tmpb[:, v, kc * 128:(kc + 1) * 128], P["ident"][:]),
                      reads=["tmpb", "ident"], writes=["ps%d" % (2 + kc // 4)])
            for hh in range(2):
                fw.op("dve", lambda e, v=v, hh=hh: e.tensor_copy(out=P["modT"][:, l * 4 + v, hh * 4:(hh + 1) * 4],
                                                                 in_=ps[2 + hh][:, :].rearrange("p (k t) -> p k t", t=128)[:, :, 0]),
                      reads=["ps%d" % (2 + hh)], writes=["modT"])

    def prenorm_T(self, xsub, xkey, l, sub, hT, hkey, col0, pbank, scr, f32out=None):
        fw, P, ps = self.fw, self.P, self.ps
        u = self.uid
        self.uid += 1
        junk, ss, rstd, xn, tmp = scr["junk"], scr["ss"], scr["rstd"], scr["xn"], scr["tmp"]
        fw.op("act", lambda e: e.activation(out=junk, in_=xsub, func=AF.Square, accum_out=ss[:, 0:1]), reads=[xkey], writes=["junk", "ss"])
        fw.op("act", lambda e: e.activation(out=rstd[:, 0:1], in_=ss[:, 0:1], func=AF.Sqrt, scale=1.0 / D, bias=P["eps"][:, 0:1]), reads=["ss", "eps"], writes=["rstd"])
        fw.op("dve", lambda e: e.reciprocal(out=rstd[:, 0:1], in_=rstd[:, 0:1]), reads=["rstd"], writes=["rstd"])
        fw.op("act", lambda e: e.activation(out=xn, in_=xsub, func=AF.Copy, scale=rstd[:, 0:1]), reads=[xkey, "rstd"], writes=["xn"])
        pk = "ps%d" % pbank
        pbt = ps[pbank][:, :].bitcast(BF16)
        for kc in range(8):
            fw.op("pe", lambda e, kc=kc: e.transpose(pbt[:, kc * 128:(kc + 1) * 128], xn[:, kc * 128:(kc + 1) * 128], P["identb"][:]),
                  reads=["xn", "identb"], writes=[pk])
        G1 = P["modT"][:, l * 4 + sub * 2 + 0, :]
        sh = P["modT"][:, l * 4 + sub * 2 + 1, :]
        for kc in range(8):
            fw.op("act", lambda e, kc=kc: e.activation(out=hT[:, kc, col0:col0 + 128], in_=pbt[:, kc * 128:(kc + 1) * 128], func=AF.Identity, scale=G1[:, kc:kc + 1], bias=sh[:, kc:kc + 1]),
                  reads=[pk, "modT"], writes=[hkey])

    def postnorm_res(self, psb, xsub, xkey, gp_idx, scr):
        fw, P, ps = self.fw, self.P, self.ps
        junk, ss2, rstd, tmp = scr["junk"], scr["ss2"], scr["rstd2"], scr["tmp"]
        for n in range(2):
            fw.op("act", lambda e, n=n: e.activation(out=junk[:, 0:512], in_=ps[psb[n]][:, :], func=AF.Square, accum_out=ss2[:, n:n + 1]),
                  reads=["ps%d" % psb[n]], writes=["junk", "ss2"])
        fw.op("pool", lambda e: e.tensor_tensor(out=rstd[:, 0:1], in0=ss2[:, 0:1], in1=ss2[:, 1:2], op=ALU.add), reads=["ss2"], writes=["rstd2"])
        fw.op("act", lambda e: e.activation(out=rstd[:, 0:1], in_=rstd[:, 0:1], func=AF.Sqrt, scale=1.0 / D, bias=P["eps"][:, 0:1]), reads=["rstd2", "eps"], writes=["rstd2"])
        fw.op("dve", lambda e: e.reciprocal(out=rstd[:, 0:1], in_=rstd[:, 0:1]), reads=["rstd2"], writes=["rstd2"])
        for n in range(2):
            fw.op("dve", lambda e, n=n: e.scalar_tensor_tensor(out=tmp[:, n * 512:(n + 1) * 512], in0=ps[psb[n]][:, :], scalar=rstd[:, 0:1],
                                                               in1=P["GP"][:, gp_idx, n * 512:(n + 1) * 512], op0=ALU.mult, op1=ALU.mult),
                  reads=["ps%d" % psb[n], "rstd2", "GP"], writes=["tmp"])
        fw.op("dve", lambda e: e.tensor_tensor(out=xsub, in0=xsub, in1=tmp, op=ALU.add), reads=["tmp", xkey], writes=[xkey])

    def mk_scr(self):
        ar = self.ar
        return {"junk": ar.alloc([D], BF16), "ss": ar.alloc([2], F32), "rstd": ar.alloc([2], F32), "xn": ar.alloc([D], BF16),
                "tmp": ar.alloc([D], F32), "ss2": ar.alloc([2], F32), "rstd2": ar.alloc([2], F32)}

    def phase_mixer0(self):
        nc, fw, P, I, S, ar, ps = self.nc, self.fw, self.P, self.I, self.S, self.ar, self.ps
        fw.barrier()
        ar.reset()
        NT = 512
        w_in = ar.alloc([8, 2048], BF16)
        w_out = ar.alloc([8, D], BF16)
        pool_w = ar.alloc([4, 128], BF16)
        convw = ar.alloc([4, 3], F32)
        poolsc = ar.alloc([4], F32)
        xt = [ar.alloc([4, D], F32) for _ in range(2)]
        hT = ar.alloc([8, NT], BF16)
        hT2 = ar.alloc([8, NT], BF16)
        cg = ar.alloc([NT], F32)
        cv = ar.alloc([4, 2 + NT], F32)
        t1 = ar.alloc([NT], F32)
        t2 = ar.alloc([NT], F32)
        up = ar.alloc([4, 16 + NT], F32)
        sA = ar.alloc([16 + NT], F32)
        sB = ar.alloc([16 + NT], F32)
        pg = ar.alloc([NT], BF16)
        t16 = ar.alloc([16], F32)
        ycat = ar.alloc([8, NT], BF16)
        scr = self.mk_scr()
        for kc in range(8):
            fw.dma("pool", w_in[:, kc, :], I["mix_w_in"][kc * 128:(kc + 1) * 128, :], writes=["w_in"])
        fw.dma("pool", w_out, I["mix_w_out"].rearrange("(kc p) n -> p kc n", p=128), writes=["w_out"])
        fw.dma("pool", pool_w, I["pool_w"].rearrange("g c d -> c g d"), writes=["pool_w"])
        fw.dma("sp", convw, I["conv_wT"], writes=["convw"])
        fw.dma("sp", poolsc, I["pool_scT"], writes=["poolsc"])
        fw.op("pool", lambda e: e.memset(cv, 0.0), writes=["cv%d" % j for j in range(4)])
        fw.op("pool", lambda e: e.memset(up, 0.0), writes=["up%d" % g for g in range(4)])
        fw.op("pool", lambda e: e.memset(sA, 0.0), writes=["sA"])
        fw.op("pool", lambda e: e.memset(sB, 0.0), writes=["sB"])
        wins = (2, 4, 8, 16)
        ntiles = SEQ // NT
        for ti in range(ntiles):
            xb = xt[ti % 2]
            xk = "xt%d" % (ti % 2)
            fw.dma("sp", xb, I["x8"][ti * NT:(ti + 1) * NT, :].rearrange("(s p) d -> p s d", p=128), writes=[xk])
            for s in range(4):
                self.prenorm_T(xb[:, s, :], xk, 0, 0, hT, "hT", s * 128, s % 2, scr)
            if ti == ntiles // 2:
                fw.op("pool", lambda e: e.tensor_scalar(out=cv[:, :, 0:2], in0=cv[:, :, 0:2], scalar1=P["flag"][:, 0:1], scalar2=None, op0=ALU.mult),
                      reads=["flag"] + ["cv%d" % j for j in range(4)], writes=["cv%d" % j for j in range(4)])
                fw.op("pool", lambda e: e.tensor_scalar(out=up[:, :, 0:16], in0=up[:, :, 0:16], scalar1=P["flag"][:, 0:1], scalar2=None, op0=ALU.mult),
                      reads=["flag"] + ["up%d" % g for g in range(4)], writes=["up%d" % g for g in range(4)])
            bankrr = [2, 3, 4, 5]
            bi = [0]

            def zchunk(fc):
                b = bankrr[bi[0] % 4]
                bi[0] += 1
                for kc in range(8):
                    fw.op("pe", lambda e, kc=kc, b=b: e.matmul(ps[b][:, :], w_in[:, kc, fc * 128:(fc + 1) * 128], hT[:, kc, :], start=(kc == 0), stop=(kc == 7)),
                          reads=["w_in", "hT"], writes=["ps%d" % b])
                return b
            for j in range(4):
                cvk = "cv%d" % j
                b = zchunk(4 + j)
                fw.op("act", lambda e, b=b: e.activation(out=cg, in_=ps[b][:, :], func=AF.Copy), reads=["ps%d" % b], writes=["cg"])
                b = zchunk(8 + j)
                fw.op("dve", lambda e, b=b, j=j: e.tensor_tensor(out=cv[:, j, 2:2 + NT], in0=ps[b][:, :], in1=cg, op=ALU.mult), reads=["ps%d" % b, "cg"], writes=[cvk])
                fw.op("pool", lambda e, j=j: e.tensor_scalar(out=t1, in0=cv[:, j, 0:NT], scalar1=convw[:, j, 0:1], scalar2=None, op0=ALU.mult), reads=[cvk, "convw"], writes=["t1"])
                fw.op("dve", lambda e, j=j: e.scalar_tensor_tensor(out=t2, in0=cv[:, j, 1:1 + NT], scalar=convw[:, j, 1:2], in1=t1, op0=ALU.mult, op1=ALU.add), reads=[cvk, "convw", "t1"], writes=["t2"])
                fw.op("dve", lambda e, j=j: e.scalar_tensor_tensor(out=t1, in0=cv[:, j, 2:2 + NT], scalar=convw[:, j, 2:3], in1=t2, op0=ALU.mult, op1=ALU.add), reads=[cvk, "convw", "t2"], writes=["t1"])
                b = zchunk(j)
                fw.op("dve", lambda e, b=b, j=j: e.tensor_tensor(out=ycat[:, j, :], in0=ps[b][:, :], in1=t1, op=ALU.mult), reads=["ps%d" % b, "t1"], writes=["ycat"])
                fw.op("pool", lambda e, j=j: e.tensor_copy(out=cv[:, j, 0:2], in_=cv[:, j, NT:NT + 2]), reads=[cvk], writes=[cvk])
            for g in range(4):
                upk = "up%d" % g
                b = zchunk(12 + g)
                fw.op("act", lambda e, b=b, g=g: e.activation(out=up[:, g, 16:16 + NT], in_=ps[b][:, :], func=AF.Copy), reads=["ps%d" % b], writes=[upk])
                W = 16 + NT
                cur = up[:, g, :]
                curk = upk
                bufs = [(sA, "sA"), (sB, "sB")]
                d = 1
                k = 0
                while d < wins[g]:
                    dst, dk = bufs[k % 2]
                    fw.op("dve", lambda e, cur=cur, dst=dst, d=d: e.tensor_tensor(out=dst[:, d:W], in0=cur[:, d:W], in1=cur[:, 0:W - d], op=ALU.add),
                          reads=[curk], writes=[dk])
                    cur, curk = dst, dk
                    d *= 2
                    k += 1
                fw.op("dve", lambda e, cur=cur, g=g: e.scalar_tensor_tensor(out=pg, in0=cur[:, 16:W], scalar=1.0 / wins[g], in1=up[:, g, 16:W], op0=ALU.mult, op1=ALU.subtract),
                      reads=[curk, upk], writes=["pg"])
                if ti == 0 or ti == ntiles // 2:
                    which = 0 if ti == 0 else 1
                    fw.op("pool", lambda e, cur=cur, g=g, which=which: e.tensor_tensor(out=t16, in0=cur[:, 16:32], in1=P["invc"][:, which, g, :], op=ALU.mult),
                          reads=[curk, "invc"], writes=["t16"])
                    fw.op("pool", lambda e, g=g: e.tensor_tensor(out=pg[:, 0:16], in0=t16, in1=up[:, g, 16:32], op=ALU.subtract),
                          reads=["t16", upk], writes=["pg"])
                b2 = bankrr[bi[0] % 4]
                bi[0] += 1
                fw.op("pe", lambda e, g=g, b2=b2: e.matmul(ps[b2][:, :], pool_w[:, g, :], pg, start=True, stop=True), reads=["pool_w", "pg"], writes=["ps%d" % b2])
                fw.op("act", lambda e, g=g, b2=b2: e.activation(out=ycat[:, 4 + g, :], in_=ps[b2][:, :], func=AF.Copy, scale=poolsc[:, g:g + 1]),
                      reads=["ps%d" % b2, "poolsc"], writes=["ycat"])
                fw.op("pool", lambda e, g=g: e.tensor_copy(out=up[:, g, 0:16], in_=up[:, g, NT:NT + 16]), reads=[upk], writes=[upk])
            for s in range(4):
                for n in range(2):
                    b = 6 + n
                    for kc in range(8):
                        fw.op("pe", lambda e, kc=kc, b=b, s=s, n=n: e.matmul(ps[b][:, :], ycat[:, kc, s * 128:(s + 1) * 128], w_out[:, kc, n * 512:(n + 1) * 512], start=(kc == 0), stop=(kc == 7)),
                              reads=["ycat", "w_out"], writes=["ps%d" % b])
                self.postnorm_res((6, 7), xb[:, s, :], xk, 0, scr)
                self.prenorm_T(xb[:, s, :], xk, 0, 1, hT2, "hT2", s * 128, s % 2, scr)
            fw.dma("sp", S["x1"][ti * NT:(ti + 1) * NT, :].rearrange("(s p) d -> p s d", p=128), xb, reads=[xk])
            for hh in range(2):
                fw.dma("sp", S["hTf0"][ti * 2 + hh], hT2[:, :, hh * 256:(hh + 1) * 256], reads=["hT2"])

    def ffn_passes(self, wg_d, wu_d, wd_d, ntiles, hT_d, acc_d, comb_d, experts):
        nc, fw, P, ar, ps = self.nc, self.fw, self.P, self.ar, self.ps
        FH = FF // 2
        NT = 256
        wg = [ar.alloc([8, FH], BF16) for _ in range(2)]
        wu = [ar.alloc([8, FH], BF16) for _ in range(2)]
        wd = [ar.alloc([11, D], BF16) for _ in range(2)]
        hTt = [ar.alloc([8, NT], BF16) for _ in range(2)]
        zt = [ar.alloc([11, NT], BF16) for _ in range(2)]
        acct = [ar.alloc([2, D], F32) for _ in range(2)]
        sg = [ar.alloc([NT], F32) for _ in range(2)]
        combt = [ar.alloc([2, 8], F32) for _ in range(2)]
        npass = 0
        cnt = 0
        for ei, ex in enumerate(experts):
            for hf in range(2):
                wi = npass % 2
                f0 = hf * FH
                if comb_d is None:
                    wgd, wud, wdd = wg_d, wu_d, wd_d
                else:
                    wgd, wud, wdd = wg_d[ex], wu_d[ex], wd_d[ex]
                for kc in range(8):
                    fw.dma("pool", wg[wi][:, kc, :], wgd[kc * 128:(kc + 1) * 128, f0:f0 + FH], writes=["wg%d" % wi])
                    fw.dma("pool", wu[wi][:, kc, :], wud[kc * 128:(kc + 1) * 128, f0:f0 + FH], writes=["wu%d" % wi])
                fw.dma("pool", wd[wi], wdd[f0:f0 + FH, :].rearrange("(fc p) d -> p fc d", p=128), writes=["wd%d" % wi])
                first = (npass == 0)
                pending = None
                for t in range(ntiles):
                    bi = cnt % 2
                    cnt += 1
                    hk, zk, ak, ck = "hTt%d" % bi, "zt%d" % bi, "acct%d" % bi, "combt%d" % bi
                    fw.dma("sp", hTt[bi], hT_d[t], writes=[hk])
                    if not first:
                        fw.dma("sp", acct[bi], acc_d[t * NT:(t + 1) * NT, :].rearrange("(s p) d -> p s d", p=128), reads=["accd%d" % t], writes=[ak])
                    if comb_d is not None:
                        fw.dma("sp", combt[bi], comb_d[t * NT:(t + 1) * NT, :].rearrange("(s p) e -> p s e", p=128), writes=[ck])
                    for fc in range(11):
                        bg = (fc % 2) * 2
                        bu = bg + 1
                        for kc in range(8):
                            fw.op("pe", lambda e, kc=kc, fc=fc, bg=bg, wi=wi, bi=bi: e.matmul(ps[bg][:, 0:NT], wg[wi][:, kc, fc * 128:(fc + 1) * 128], hTt[bi][:, kc, :], start=(kc == 0), stop=(kc == 7)),
                                  reads=["wg%d" % wi, hk], writes=["ps%d" % bg])
                        for kc in range(8):
                            fw.op("pe", lambda e, kc=kc, fc=fc, bu=bu, wi=wi, bi=bi: e.matmul(ps[bu][:, 0:NT], wu[wi][:, kc, fc * 128:(fc + 1) * 128], hTt[bi][:, kc, :], start=(kc == 0), stop=(kc == 7)),
                                  reads=["wu%d" % wi, hk], writes=["ps%d" % bu])
                        sgb = sg[fc % 2]
                        sgk = "sg%d" % (fc % 2)
                        fw.op("act", lambda e, bg=bg, sgb=sgb: e.activation(out=sgb, in_=ps[bg][:, 0:NT], func=AF.Silu), reads=["ps%d" % bg], writes=[sgk])
                        fw.op("dve", lambda e, bu=bu, sgb=sgb, fc=fc, bi=bi: e.tensor_tensor(out=zt[bi][:, fc, :], in0=ps[bu][:, 0:NT], in1=sgb, op=ALU.mult),
                              reads=["ps%d" % bu, sgk], writes=[zk])
                    def down(t=t, bi=bi, hk=hk, zk=zk, ak=ak, ck=ck, wi=wi, first=first, ex=ex):
                        for s in range(2):
                            for n in range(2):
                                bo = 4 + (s * 2 + n)
                                for fc in range(11):
                                    fw.op("pe", lambda e, fc=fc, bo=bo, s=s, n=n, wi=wi, bi=bi: e.matmul(ps[bo][:, :], zt[bi][:, fc, s * 128:(s + 1) * 128], wd[wi][:, fc, n * 512:(n + 1) * 512], start=(fc == 0), stop=(fc == 10)),
                                          reads=[zk, "wd%d" % wi], writes=["ps%d" % bo])
                                dst = acct[bi][:, s, n * 512:(n + 1) * 512]
                                if comb_d is None:
                                    if first:
                                        fw.op("act", lambda e, bo=bo, dst=dst: e.activation(out=dst, in_=ps[bo][:, :], func=AF.Copy), reads=["ps%d" % bo], writes=[ak])
                                    else:
                                        fw.op("dve", lambda e, bo=bo, dst=dst: e.tensor_tensor(out=dst, in0=ps[bo][:, :], in1=dst, op=ALU.add), reads=["ps%d" % bo, ak], writes=[ak])
                                else:
                                    cs = combt[bi][:, s, ex:ex + 1]
                                    if first:
                                        fw.op("act", lambda e, bo=bo, dst=dst, cs=cs: e.activation(out=dst, in_=ps[bo][:, :], func=AF.Copy, scale=cs), reads=["ps%d" % bo, ck], writes=[ak])
                                    else:
                                        fw.op("dve", lambda e, bo=bo, dst=dst, cs=cs: e.scalar_tensor_tensor(out=dst, in0=ps[bo][:, :], scalar=cs, in1=dst, op0=ALU.mult, op1=ALU.add),
                                              reads=["ps%d" % bo, ak, ck], writes=[ak])
                        fw.dma("sp", acc_d[t * NT:(t + 1) * NT, :].rearrange("(s p) d -> p s d", p=128), acct[bi], reads=[ak], writes=["accd%d" % t])
                    if pending is not None:
                        pending()
                    pending = down
                if pending is not None:
                    pending()
                    pending = None
                npass += 1

    def phase_post0(self):
        nc, fw, P, S, ar, ps = self.nc, self.fw, self.P, self.S, self.ar, self.ps
        NT = 512
        xt = [ar.alloc([4, D], F32) for _ in range(2)]
        at = [ar.alloc([4, D], F32) for _ in range(2)]
        scr = self.mk_scr()
        for ti in range(SEQ // NT):
            xb, ab = xt[ti % 2], at[ti % 2]
            xk, akk = "xt%d" % (ti % 2), "at%d" % (ti % 2)
            fw.dma("sp", xb, S["x1"][ti * NT:(ti + 1) * NT, :].rearrange("(s p) d -> p s d", p=128), writes=[xk])
            fw.dma("sp", ab, S["acc0"][ti * NT:(ti + 1) * NT, :].rearrange("(s p) d -> p s d", p=128), writes=[akk])
            for s in range(4):
                self.postnorm_sb(ab[:, s, :], akk, xb[:, s, :], xk, 1, scr)
            fw.dma("sp", S["x2"][ti * NT:(ti + 1) * NT, :].rearrange("(s p) d -> p s d", p=128), xb, reads=[xk])

    def phase_final(self):
        nc, fw, P, S, ar, ps = self.nc, self.fw, self.P, self.S, self.ar, self.ps
        NT = 512
        xt = [ar.alloc([4, D], F32) for _ in range(2)]
        at = [ar.alloc([4, D], F32) for _ in range(2)]
        scr = self.mk_scr()
        for ti in range(HALF // NT):
            xb, ab = xt[ti % 2], at[ti % 2]
            xk, akk = "xt%d" % (ti % 2), "at%d" % (ti % 2)
            fw.dma("sp", xb, S["x3"][ti * NT:(ti + 1) * NT, :].rearrange("(s p) d -> p s d", p=128), writes=[xk])
            fw.dma("sp", ab, S["acc1"][ti * NT:(ti + 1) * NT, :].rearrange("(s p) d -> p s d", p=128), writes=[akk])
            for s in range(4):
                self.postnorm_sb(ab[:, s, :], akk, xb[:, s, :], xk, 3, scr)
            fw.dma("sp", S["y"][ti * NT:(ti + 1) * NT, :].rearrange("(s p) d -> p s d", p=128), xb, reads=[xk])

    def postnorm_sb(self, y, ykey, xsub, xkey, gp_idx, scr):
        fw, P = self.fw, self.P
        junk, ss, rstd, tmp = scr["junk"], scr["ss2"], scr["rstd2"], scr["tmp"]
        fw.op("act", lambda e: e.activation(out=junk, in_=y, func=AF.Square, accum_out=ss[:, 0:1]), reads=[ykey], writes=["junk", "ss2"])
        fw.op("act", lambda e: e.activation(out=rstd[:, 0:1], in_=ss[:, 0:1], func=AF.Sqrt, scale=1.0 / D, bias=P["eps"][:, 0:1]), reads=["ss2", "eps"], writes=["rstd2"])
        fw.op("dve", lambda e: e.reciprocal(out=rstd[:, 0:1], in_=rstd[:, 0:1]), reads=["rstd2"], writes=["rstd2"])
        fw.op("dve", lambda e: e.scalar_tensor_tensor(out=tmp, in0=y, scalar=rstd[:, 0:1], in1=P["GP"][:, gp_idx, :], op0=ALU.mult, op1=ALU.mult),
              reads=[ykey, "rstd2", "GP"], writes=["tmp"])
        fw.op("dve", lambda e: e.tensor_tensor(out=xsub, in0=xsub, in1=tmp, op=ALU.add), reads=["tmp", xkey], writes=[xkey])


def _fm(v, nch):
    return np.ascontiguousarray(np.asarray(v, np.float32).reshape(nch, 128).T)


def make_in_maps(inp):
    x = np.asarray(inp["x"], np.float32)
    c = np.asarray(inp["c"], np.float32)
    maps = []
    wins = (2, 4, 8, 16)
    pos = np.arange(16)
    invc_start = np.stack([1.0 / np.minimum(pos + 1, w) for w in wins]).astype(np.float32)
    invc_mid = np.stack([np.full(16, 1.0 / w) for w in wins]).astype(np.float32)
    common = {
        "ident": np.eye(128, dtype=np.float32),
        "ada_w": np.ascontiguousarray(inp["ada_w"], np.float32),
        "ada_bB": np.ascontiguousarray(np.broadcast_to(np.asarray(inp["ada_b"], np.float32)[:, None, :], (2, 128, 6 * D))),
        "norm_gB": np.ascontiguousarray(np.broadcast_to(np.asarray(inp["norm_g"], np.float32)[:, None, :, :], (2, 128, 4, D))),
        "mix_w_in": np.ascontiguousarray(inp["mix_w_in"][0], np.float32),
        "mix_w_out": np.ascontiguousarray(inp["mix_w_out"][0], np.float32),
        "conv_wT": np.ascontiguousarray(np.asarray(inp["conv_w"][0], np.float32).reshape(3, 4, 128).transpose(2, 1, 0)),
        "pool_w": np.ascontiguousarray(inp["pool_w"][0], np.float32),
        "pool_scT": _fm(inp["pool_scale"][0], 4),
        "ffn_wg": np.ascontiguousarray(inp["ffn_w_gate"][0], np.float32),
        "ffn_wu": np.ascontiguousarray(inp["ffn_w_up"][0], np.float32),
        "ffn_wd": np.ascontiguousarray(inp["ffn_w_down"][0], np.float32),
    }
    f32 = lambda a: np.ascontiguousarray(a, np.float32)
    common.update({
        "rw_wr": f32(inp["rwkv_w_r"][0]), "rw_wk": f32(inp["rwkv_w_k"][0]), "rw_wv": f32(inp["rwkv_w_v"][0]), "rw_wo": f32(inp["rwkv_w_o"][0]),
        "rw_w1": f32(inp["rwkv_w1"][0]), "rw_a1": f32(inp["rwkv_a1"][0]), "rw_g1": f32(inp["rwkv_g1"][0]),
        "rw_w2": f32(inp["rwkv_w2"][0]), "rw_a2": f32(inp["rwkv_a2"][0]), "rw_g2": f32(inp["rwkv_g2"][0]),
        "moe_router": f32(inp["moe_router"][0]),
        "moe_wg": f32(inp["moe_w_gate"][0]), "moe_wu": f32(inp["moe_w_up"][0]), "moe_wd": f32(inp["moe_w_down"][0]),
    })
    mu = np.asarray(inp["rwkv_mu"][0], np.float32)
    vecs = [mu[i] for i in range(6)] + [inp["rwkv_w0"][0], inp["rwkv_a0"][0], inp["rwkv_k_k"][0], inp["rwkv_k_a"][0],
                                        np.asarray(inp["rwkv_r_k"][0]).reshape(-1), inp["rwkv_ln_g"][0], inp["rwkv_ln_b"][0]]
    common["rw_vec"] = np.ascontiguousarray(np.stack([_fm(v, 8) for v in vecs], axis=1))
    t = np.arange(1024)
    common["resetm"] = np.ascontiguousarray(np.broadcast_to((t % 64 != 0).astype(np.float32)[None], (128, 1024)))
    si = np.arange(128)[:, None]
    tj = np.arange(128)[None, :]
    same = (si // 64) == (tj // 64)
    common["mask1"] = np.ascontiguousarray(np.stack([(same & (si < tj)), (same & (si <= tj))], axis=1).astype(np.float32))
    common["maskT"] = np.ascontiguousarray((same & (si > tj)).astype(np.float32))
    common["bdones"] = np.ascontiguousarray(same.astype(np.float32))
    for core in range(8):
        b, h = core // 2, core % 2
        m = dict(common)
        m["x8"] = np.ascontiguousarray(np.concatenate([x[b, :HALF], x[b, h * HALF:(h + 1) * HALF]], axis=0))
        m["condB"] = np.ascontiguousarray(np.broadcast_to(c[b].reshape(8, 128).T[:, :, None], (128, 8, 128)))
        m["flag"] = np.full((128, 1), float(h), np.float32)
        m["invc"] = np.ascontiguousarray(np.broadcast_to(np.stack([invc_start, invc_start if h == 0 else invc_mid])[None], (128, 2, 4, 16)))
        maps.append(m)
    return maps


_CACHE = {}


def kernel(**inputs):
    if "nc" not in _CACHE:
        _CACHE["nc"] = Builder().build()
    nc = _CACHE["nc"]
    maps = make_in_maps(inputs)
    res = run_bass_kernel_spmd(nc, maps, core_ids=list(range(8)))
    out = np.zeros((4, SEQ, D), np.float32)
    for core in range(8):
        b, h = core // 2, core % 2
        out[b, h * HALF:(h + 1) * HALF] = res.results[core]["y"]
    return out


CDEC = 0.6065306597126334


def _phase_rwkv_prep(self):
    nc, fw, P, I, S, ar, ps = self.nc, self.fw, self.P, self.I, self.S, self.ar, self.ps
    fw.barrier()
    ar.reset()
    op = fw.op
    W = {}
    for nm in ("rw_wr", "rw_wk", "rw_wv"):
        W[nm] = ar.alloc([8, D], BF16)
        for kc in range(8):
            fw.dma("pool", W[nm][:, kc, :], I[nm][kc * 128:(kc + 1) * 128, :], writes=[nm])
    w1 = ar.alloc([8, 64], BF16)
    a1 = ar.alloc([8, 64], BF16)
    g1 = ar.alloc([8, 160], BF16)
    fw.dma("pool", w1, I["rw_w1"].rearrange("(kc p) n -> p kc n", p=128), writes=["w1"])
    fw.dma("pool", a1, I["rw_a1"].rearrange("(kc p) n -> p kc n", p=128), writes=["a1"])
    fw.dma("pool", g1, I["rw_g1"].rearrange("(kc p) n -> p kc n", p=128), writes=["g1"])
    w2 = ar.alloc([D], BF16)
    a2 = ar.alloc([D], BF16)
    g2a = ar.alloc([D], BF16)
    g2b = ar.alloc([D], BF16)
    fw.dma("pool", w2[0:64, :], I["rw_w2"], writes=["w2"])
    fw.dma("pool", a2[0:64, :], I["rw_a2"], writes=["a2"])
    fw.dma("pool", g2a, I["rw_g2"][0:128, :], writes=["g2a"])
    fw.dma("pool", g2b[0:32, :], I["rw_g2"][128:160, :], writes=["g2b"])
    vec = ar.alloc([14, 8], F32)
    fw.dma("sp", vec[:, 0:13, :], I["rw_vec"], writes=["vec"])
    op("pool", lambda e: e.tensor_scalar(out=vec[:, 13, :], in0=vec[:, 9, :], scalar1=-1.0, scalar2=1.0, op0=ALU.mult, op1=ALU.add), reads=["vec"], writes=["vec"])
    resetm = ar.alloc([1024], F32)
    bdones = ar.alloc([128], BF16)
    fw.dma("sp", resetm, I["resetm"], writes=["resetm"])
    fw.dma("pool", bdones, I["bdones"], writes=["bdones"])

    def vb(i):
        return bc(vec[:, i, :].unsqueeze(2), [128, 8, 128])

    xt = ar.alloc([D], F32)
    at = ar.alloc([D], F32)
    hT = ar.alloc([8, 129], BF16)
    xx = ar.alloc([8, 128], BF16)
    xi = [ar.alloc([8, 128], BF16) for _ in range(2)]
    rSs = [ar.alloc([8, 128], BF16) for _ in range(2)]
    kSs = [ar.alloc([1024], F32) for _ in range(2)]
    sgds = [ar.alloc([1024], F32) for _ in range(2)]
    aSs = [ar.alloc([1024], F32) for _ in range(2)]
    ftmp = ar.alloc([1024], F32)
    tw = ar.alloc([128], BF16)
    ta = ar.alloc([128], BF16)
    tg = ar.alloc([128], BF16)
    tg2 = ar.alloc([128], BF16)
    Tf = [ar.alloc([1024], F32) for _ in range(8)]
    T3 = [t.rearrange("p (c t) -> p c t", t=128) for t in Tf]
    MIs = [ar.alloc([8, 896], BF16) for _ in range(2)]
    OWs = [ar.alloc([8, 256], BF16) for _ in range(2)]
    GCs = [ar.alloc([16], F32) for _ in range(2)]
    B0 = ar.alloc([8, 128], BF16)
    lg = ar.alloc([64], F32)
    scr = {"junk": Tf[6][:, 0:512].bitcast(BF16), "ss": lg[:, 32:34], "rstd": lg[:, 34:36], "xn": Tf[6][:, 512:1024].bitcast(BF16),
           "tmp": ftmp, "ss2": lg[:, 36:38], "rstd2": lg[:, 38:40]}
    scr_keys = ["T6", "FT"]

    op("pool", lambda e: e.memset(hT, 0.0), writes=["hT"])

    bank_rr = [0]

    def nb():
        b = 4 + bank_rr[0] % 2
        bank_rr[0] += 1
        return b

    evac_rr = [0]

    def cp(dst, src, reads, writes):
        evac_rr[0] += 1
        if evac_rr[0] % 2:
            op("act", lambda e: e.activation(out=dst, in_=src, func=AF.Copy), reads=reads, writes=writes)
        else:
            op("dve", lambda e: e.tensor_copy(out=dst, in_=src), reads=reads, writes=writes)

    NTILE = SEQ // 128
    npre, nown = getattr(self, "rw_tiles", (NTILE // 2, NTILE // 2))
    tiles = list(range(npre)) + list(range(NTILE // 2, NTILE // 2 + nown))
    STOP = getattr(self, "rw_stop", 99)
    def front(ti):
        own = ti >= NTILE // 2
        par = ti % 2
        kx = "_%d" % par
        MIb, OWb, GC = MIs[par], OWs[par], GCs[par]
        PR, Qt, Kt, Qb, Kb, vS = MIb[:, :, 0:256], MIb[:, :, 256:384], MIb[:, :, 384:512], MIb[:, :, 512:640], MIb[:, :, 640:768], MIb[:, :, 768:896]
        gS, bonus = OWb[:, :, 0:128], OWb[:, :, 128:256]
        rS, rSk = rSs[par], "rS" + kx
        kSf, kSk = kSs[par], "kS" + kx
        kS = kSf.rearrange("p (c t) -> p c t", t=128)
        sgd, sgk = sgds[par], "sg" + kx
        sgd3 = sgd.rearrange("p (c t) -> p c t", t=128)
        aSf, aSk = aSs[par], "aS" + kx
        aS = aSf.rearrange("p (c t) -> p c t", t=128)
        fw.dma("sp", xt, S["x1"][ti * 128:(ti + 1) * 128, :], writes=["xt"])
        fw.dma("sp", at, S["acc0"][ti * 128:(ti + 1) * 128, :], writes=["at"])
        self.postnorm_sb2(at, "at", xt, "xt", 1, scr, scr_keys)
        if own:
            fw.dma("sp", S["x2"][ti * 128:(ti + 1) * 128, :], xt, reads=["xt"])
        if ti == NTILE // 2:
            op("pool", lambda e: e.tensor_scalar(out=hT[:, :, 0:1], in0=hT[:, :, 0:1], scalar1=P["flag"][:, 0:1], scalar2=None, op0=ALU.mult), reads=["hT", "flag"], writes=["hT"])
        self.prenorm_T2(xt, "xt", 1, 0, hT, "hT", 1, 4, scr, scr_keys)
        op("pool", lambda e: e.tensor_tensor(out=xx, in0=hT[:, :, 0:128], in1=hT[:, :, 1:129], op=ALU.subtract), reads=["hT"], writes=["xx"])

        def variant(i, buf):
            xb_, xk_ = xi[buf], "xi%d" % buf
            op("dve", lambda e: e.tensor_tensor(out=xb_, in0=xx, in1=vb(i), op=ALU.mult), reads=["xx", "vec"], writes=[xk_])
            op("dve", lambda e: e.tensor_tensor(out=xb_, in0=xb_, in1=hT[:, :, 1:129], op=ALU.add), reads=[xk_, "hT"], writes=[xk_])
            return xb_, xk_

        def proj(wname, xb_, xk_, grp):
            b0 = grp * 2
            for cc in range(8):
                b = b0 + cc // 4
                for kc in range(8):
                    op("pe", lambda e, cc=cc, kc=kc, b=b: e.matmul(ps[b][:, (cc % 4) * 128:(cc % 4 + 1) * 128], W[wname][:, kc, cc * 128:(cc + 1) * 128], xb_[:, kc, :], start=(kc == 0), stop=(kc == 7)),
                       reads=[wname, xk_], writes=["ps%d" % b])
            return b0

        def evac2(b0, fn_eng, mk):
            for hh in range(2):
                mk(hh, ps[b0 + hh][:, :].rearrange("p (c t) -> p c t", t=128), "ps%d" % (b0 + hh))

        xb_, xk_ = variant(0, 0)
        b0 = proj("rw_wr", xb_, xk_, 0)
        for hh in range(2):
            op("act", lambda e, hh=hh, b0=b0: e.activation(out=rS[:, hh * 4:(hh + 1) * 4, :], in_=ps[b0 + hh][:, :].rearrange("p (c t) -> p c t", t=128), func=AF.Copy), reads=["ps%d" % (b0 + hh)], writes=[rSk])
        yield
        xb_, xk_ = variant(2, 1)
        b0 = proj("rw_wk", xb_, xk_, 1)
        for hh in range(2):
            op("act", lambda e, hh=hh, b0=b0: e.activation(out=kS[:, hh * 4:(hh + 1) * 4, :], in_=ps[b0 + hh][:, :].rearrange("p (c t) -> p c t", t=128), func=AF.Copy), reads=["ps%d" % (b0 + hh)], writes=[kSk])
        yield
        xb_, xk_ = variant(3, 0)
        b0 = proj("rw_wv", xb_, xk_, 0)
        for hh in range(2):
            op("act", lambda e, hh=hh, b0=b0: e.activation(out=vS[:, hh * 4:(hh + 1) * 4, :], in_=ps[b0 + hh][:, :].rearrange("p (c t) -> p c t", t=128), func=AF.Copy), reads=["ps%d" % (b0 + hh)], writes=["vS" + kx])
        yield
        xb_, xk_ = variant(1, 1)
        b = nb()
        for kc in range(8):
            op("pe", lambda e, kc=kc, b=b, xb_=xb_: e.matmul(ps[b][0:64, 0:128], w1[:, kc, :], xb_[:, kc, :], start=(kc == 0), stop=(kc == 7)), reads=["w1", xk_], writes=["ps%d" % b])
        op("act", lambda e, b=b: e.activation(out=tw[0:64, :], in_=ps[b][0:64, 0:128], func=AF.Tanh), reads=["ps%d" % b], writes=["tw"])
        b0 = 2
        for cc in range(8):
            bb = b0 + cc // 4
            op("pe", lambda e, cc=cc, bb=bb: e.matmul(ps[bb][:, (cc % 4) * 128:(cc % 4 + 1) * 128], w2[0:64, cc * 128:(cc + 1) * 128], tw[0:64, :], start=True, stop=True), reads=["w2", "tw"], writes=["ps%d" % bb])
        for hh in range(2):
            op("dve", lambda e, hh=hh: e.tensor_tensor(out=sgd3[:, hh * 4:(hh + 1) * 4, :], in0=ps[2 + hh][:, :].rearrange("p (c t) -> p c t", t=128),
                                                      in1=bc(vec[:, 6, hh * 4:(hh + 1) * 4].unsqueeze(2), [128, 4, 128]), op=ALU.add), reads=["ps%d" % (2 + hh), "vec"], writes=[sgk])
        op("act", lambda e: e.activation(out=sgd, in_=sgd, func=AF.Sigmoid), reads=[sgk], writes=[sgk])
        yield
        xb_, xk_ = variant(4, 0)
        b = nb()
        for kc in range(8):
            op("pe", lambda e, kc=kc, b=b, xb_=xb_: e.matmul(ps[b][0:64, 0:128], a1[:, kc, :], xb_[:, kc, :], start=(kc == 0), stop=(kc == 7)), reads=["a1", xk_], writes=["ps%d" % b])
        op("act", lambda e, b=b: e.activation(out=ta[0:64, :], in_=ps[b][0:64, 0:128], func=AF.Copy), reads=["ps%d" % b], writes=["ta"])
        for cc in range(8):
            bb = cc // 4
            op("pe", lambda e, cc=cc, bb=bb: e.matmul(ps[bb][:, (cc % 4) * 128:(cc % 4 + 1) * 128], a2[0:64, cc * 128:(cc + 1) * 128], ta[0:64, :], start=True, stop=True), reads=["a2", "ta"], writes=["ps%d" % bb])
        for hh in range(2):
            op("dve", lambda e, hh=hh: e.tensor_tensor(out=aS[:, hh * 4:(hh + 1) * 4, :], in0=ps[hh][:, :].rearrange("p (c t) -> p c t", t=128),
                                                      in1=bc(vec[:, 7, hh * 4:(hh + 1) * 4].unsqueeze(2), [128, 4, 128]), op=ALU.add), reads=["ps%d" % hh, "vec"], writes=[aSk])
        op("act", lambda e: e.activation(out=aSf, in_=aSf, func=AF.Sigmoid), reads=[aSk], writes=[aSk])
        yield
        if own:
            xb_, xk_ = variant(5, 1)
            b = nb()
            for kc in range(8):
                op("pe", lambda e, kc=kc, b=b, xb_=xb_: e.matmul(ps[b][:, 0:128], g1[:, kc, 0:128], xb_[:, kc, :], start=(kc == 0), stop=(kc == 7)), reads=["g1", xk_], writes=["ps%d" % b])
            for kc in range(8):
                op("pe", lambda e, kc=kc, b=b, xb_=xb_: e.matmul(ps[b][0:32, 128:256], g1[:, kc, 128:160], xb_[:, kc, :], start=(kc == 0), stop=(kc == 7)), reads=["g1", xk_], writes=["ps%d" % b])
            op("act", lambda e, b=b: e.activation(out=tg, in_=ps[b][:, 0:128], func=AF.Sigmoid), reads=["ps%d" % b], writes=["tg"])
            op("act", lambda e, b=b: e.activation(out=tg2[0:32, :], in_=ps[b][0:32, 128:256], func=AF.Sigmoid), reads=["ps%d" % b], writes=["tg2"])
            for cc in range(8):
                bb = 2 + cc // 4
                op("pe", lambda e, cc=cc, bb=bb: e.matmul(ps[bb][:, (cc % 4) * 128:(cc % 4 + 1) * 128], g2a[:, cc * 128:(cc + 1) * 128], tg, start=True, stop=False), reads=["g2a", "tg"], writes=["ps%d" % bb])
                op("pe", lambda e, cc=cc, bb=bb: e.matmul(ps[bb][:, (cc % 4) * 128:(cc % 4 + 1) * 128], g2b[0:32, cc * 128:(cc + 1) * 128], tg2[0:32, :], start=False, stop=True), reads=["g2b", "tg2"], writes=["ps%d" % bb])
            for hh in range(2):
                op("act", lambda e, hh=hh: e.activation(out=gS[:, hh * 4:(hh + 1) * 4, :], in_=ps[2 + hh][:, :].rearrange("p (c t) -> p c t", t=128), func=AF.Copy), reads=["ps%d" % (2 + hh)], writes=["gS" + kx])

        op("pool", lambda e: e.tensor_copy(out=hT[:, :, 0:1], in_=hT[:, :, 128:129]), reads=["hT"], writes=["hT"])
        yield


    def back(ti):
        own = ti >= NTILE // 2
        par = ti % 2
        kx = "_%d" % par
        MIb, OWb, GC = MIs[par], OWs[par], GCs[par]
        PR, Qt, Kt, Qb, Kb, vS = MIb[:, :, 0:256], MIb[:, :, 256:384], MIb[:, :, 384:512], MIb[:, :, 512:640], MIb[:, :, 640:768], MIb[:, :, 768:896]
        gS, bonus = OWb[:, :, 0:128], OWb[:, :, 128:256]
        rS, rSk = rSs[par], "rS" + kx
        kSf, kSk = kSs[par], "kS" + kx
        kS = kSf.rearrange("p (c t) -> p c t", t=128)
        sgd, sgk = sgds[par], "sg" + kx
        sgd3 = sgd.rearrange("p (c t) -> p c t", t=128)
        aSf, aSk = aSs[par], "aS" + kx
        aS = aSf.rearrange("p (c t) -> p c t", t=128)
        Lp, Lpk = Tf[1], "T1"
        op("dve", lambda e: e.tensor_tensor_scan(out=Lp, data0=resetm, data1=sgd, initial=0.0, op0=ALU.mult, op1=ALU.add), reads=["resetm", sgk], writes=[Lpk])
        Lm, Lmk = Tf[2], "T2"
        op("dve", lambda e: e.tensor_tensor(out=Lm, in0=Lp, in1=sgd, op=ALU.subtract), reads=[Lpk, sgk], writes=[Lmk])
        Ld, Ldk = Tf[0], "T0"
        Lp64 = Lp.rearrange("p (c t) -> p c t", t=64)
        yield
        op("pool", lambda e: e.tensor_tensor(out=Ld.rearrange("p (c t) -> p c t", t=64), in0=bc(Lp64[:, :, 63:64], [128, 16, 64]), in1=Lp64, op=ALU.subtract), reads=[Lpk, Lmk], writes=[Ldk])
        E1, E1k = Tf[3], "T3"
        E2, E2k = Tf[4], "T4"
        op("act", lambda e: e.activation(out=E1, in_=Lp, func=AF.Exp, scale=-CDEC), reads=[Lpk], writes=[E1k])
        op("act", lambda e: e.activation(out=E2, in_=Lp, func=AF.Exp, scale=CDEC), reads=[Lpk], writes=[E2k])
        yield
        op("act", lambda e: e.activation(out=Lm, in_=Lm, func=AF.Exp, scale=-CDEC), reads=[Lmk], writes=[Lmk])
        op("act", lambda e: e.activation(out=Ld, in_=Ld, func=AF.Exp, scale=-CDEC), reads=[Ldk], writes=[Ldk])
        E3, E3k, E4, E4k = Lm, Lmk, Ld, Ldk
        op("pool", lambda e: e.tensor_copy(out=GC, in_=E1.rearrange("p (c t) -> p c t", t=64)[:, :, 63]), reads=[E1k], writes=["GC" + kx])
        kk, kkk = T3[1], "T1"
        yield
        op("dve", lambda e: e.tensor_tensor(out=kk, in0=kS, in1=vb(8), op=ALU.mult), reads=[kSk, "vec", E1k, E2k], writes=[kkk])
        op("pool", lambda e: e.tensor_tensor(out=B0, in0=kk, in1=kk, op=ALU.mult), reads=[kkk], writes=["B0"])
        for cc in range(8):
            bb = 6 + cc // 4
            op("pe", lambda e, cc=cc, bb=bb: e.matmul(ps[bb][:, (cc % 4) * 128:(cc % 4 + 1) * 128], bdones, B0[:, cc, :], start=True, stop=True), reads=["bdones", "B0"], writes=["ps%d" % bb])
        rn, rnk = T3[7], "T7"
        yield
        for hh in range(2):
            op("act", lambda e, hh=hh: e.activation(out=rn[:, hh * 4:(hh + 1) * 4, :], in_=ps[6 + hh][:, :].rearrange("p (c t) -> p c t", t=128), func=AF.Sqrt), reads=["ps%d" % (6 + hh)], writes=[rnk])
        op("dve", lambda e: e.reciprocal(out=Tf[7], in_=Tf[7]), reads=[rnk], writes=[rnk])
        op("dve", lambda e: e.tensor_tensor(out=kk, in0=kk, in1=rn, op=ALU.mult), reads=[kkk, rnk], writes=[kkk])
        km, kmk = T3[7], "T7"
        yield
        op("dve", lambda e: e.tensor_tensor(out=km, in0=aS, in1=vb(9), op=ALU.mult), reads=[aSk, "vec", kkk], writes=[kmk])
        op("pool", lambda e: e.tensor_tensor(out=km, in0=km, in1=vb(13), op=ALU.add), reads=[kmk, "vec"], writes=[kmk])
        op("dve", lambda e: e.tensor_tensor(out=km, in0=km, in1=kS, op=ALU.mult), reads=[kmk, kSk], writes=[kmk])
        q, qk = aS, aSk
        yield
        op("dve", lambda e: e.tensor_tensor(out=q, in0=aS, in1=kk, op=ALU.mult), reads=[aSk, kkk], writes=[qk])
        op("dve", lambda e: e.scalar_tensor_tensor(out=PR[:, :, 0:128], in0=kk, scalar=-1.0, in1=T3[2], op0=ALU.mult, op1=ALU.mult), reads=[kkk, E3k], writes=["PR" + kx])
        if own:
            op("pool", lambda e: e.tensor_tensor(out=PR[:, :, 128:256], in0=rS, in1=T3[3], op=ALU.mult), reads=[rSk, E1k], writes=["PR" + kx])
        else:
            op("pool", lambda e: e.tensor_copy(out=PR[:, :, 128:256], in_=rS), reads=[rSk], writes=["PR" + kx])
        op("dve", lambda e: e.tensor_tensor(out=Qt, in0=q, in1=T3[4], op=ALU.mult), reads=[qk, E2k], writes=["Qt" + kx])
        yield
        op("pool", lambda e: e.tensor_tensor(out=Kt, in0=km, in1=T3[4], op=ALU.mult), reads=[kmk, E2k], writes=["Kt" + kx])
        op("dve", lambda e: e.tensor_tensor(out=Qb, in0=q, in1=T3[0], op=ALU.mult), reads=[qk, E4k], writes=["Qb" + kx])
        op("pool", lambda e: e.tensor_tensor(out=Kb, in0=km, in1=T3[0], op=ALU.mult), reads=[kmk, E4k], writes=["Kb" + kx])
        if own:
            op("pool", lambda e: e.tensor_tensor(out=T3[5], in0=km, in1=vb(10), op=ALU.mult), reads=[kmk, "vec", kSk], writes=["T5"])
            op("pool", lambda e: e.tensor_tensor(out=B0, in0=T3[5], in1=rS, op=ALU.mult), reads=["T5", rSk], writes=["B0"])
            for cc in range(8):
                bb = 6 + cc // 4
                op("pe", lambda e, cc=cc, bb=bb: e.matmul(ps[bb][:, (cc % 4) * 128:(cc % 4 + 1) * 128], bdones, B0[:, cc, :], start=True, stop=True), reads=["bdones", "B0"], writes=["ps%d" % bb])
            for hh in range(2):
                op("dve", lambda e, hh=hh: e.tensor_tensor(out=bonus[:, hh * 4:(hh + 1) * 4, :], in0=ps[6 + hh][:, :].rearrange("p (c t) -> p c t", t=128), in1=vS[:, hh * 4:(hh + 1) * 4, :], op=ALU.mult),
                   reads=["ps%d" % (6 + hh), "vS" + kx], writes=["bonus" + kx])

        fw.dma("sp", S["MI"][ti], MIb, reads=["PR" + kx, "Qt" + kx, "Kt" + kx, "Qb" + kx, "Kb" + kx, "vS" + kx])
        fw.dma("sp", S["GCd"][ti], GC, reads=["GC" + kx])
        if own:
            fw.dma("sp", S["OW"][ti - NTILE // 2], OWb, reads=["gS" + kx, "bonus" + kx])


        yield


    prev = None
    for ti in tiles:
        gens = [front(ti)]
        if prev is not None:
            gens.append(back(prev))
        while gens:
            for g in list(gens):
                try:
                    next(g)
                except StopIteration:
                    gens.remove(g)
        prev = ti
    if prev is not None:
        for _ in back(prev):
            pass


def _phase_rwkv_chain(self):
    nc, fw, P, I, S, ar, ps = self.nc, self.fw, self.P, self.I, self.S, self.ar, self.ps
    fw.barrier()
    ar.reset()
    op = fw.op
    W = {"rw_wo": ar.alloc([8, D], BF16)}
    for kc in range(8):
        fw.dma("pool", W["rw_wo"][:, kc, :], I["rw_wo"][kc * 128:(kc + 1) * 128, :], writes=["rw_wo"])
    vec = ar.alloc([14, 8], F32)
    fw.dma("sp", vec[:, 0:13, :], I["rw_vec"], writes=["vec"])
    router = ar.alloc([8, 8], F32)
    fw.dma("sp", router, I["moe_router"].rearrange("(kc p) e -> p kc e", p=128), writes=["router"])
    mask1 = ar.alloc([2, 128], F32)
    maskT = ar.alloc([128], F32)
    bdones = ar.alloc([128], BF16)
    fw.dma("sp", mask1, I["mask1"], writes=["mask1"])
    fw.dma("sp", maskT, I["maskT"], writes=["maskT"])
    fw.dma("pool", bdones, I["bdones"], writes=["bdones"])

    def vb(i):
        return bc(vec[:, i, :].unsqueeze(2), [128, 8, 128])

    MIs = [ar.alloc([8, 896], BF16) for _ in range(2)]
    OWs = [ar.alloc([8, 256], BF16) for _ in range(2)]
    GCs = [ar.alloc([16], F32) for _ in range(2)]
    xts = [ar.alloc([D], F32) for _ in range(2)]
    Tf = {i: ar.alloc([1024], F32) for i in (1, 2, 6, 7)}
    T3 = {i: t.rearrange("p (c t) -> p c t", t=128) for i, t in Tf.items()}
    B0 = ar.alloc([8, 128], BF16)
    yg = ar.alloc([8, 128], BF16)
    Ysb = ar.alloc([8, 128], F32)
    Sbd = ar.alloc([8, 128], BF16)
    h32 = ar.alloc([8, 128], F32)
    hb = ar.alloc([8, 128], BF16)
    lg = ar.alloc([64], F32)
    scr = {"junk": Tf[6][:, 0:512].bitcast(BF16), "ss": lg[:, 32:34], "rstd": lg[:, 34:36], "xn": Tf[6][:, 512:1024].bitcast(BF16),
           "tmp": Tf[7], "ss2": lg[:, 36:38], "rstd2": lg[:, 38:40]}
    scr_keys = ["T6", "T7"]
    NSET = 8
    SETS = []
    for si in range(NSET):
        d = {}
        d["RHS"] = ar.alloc([2, 128], BF16)
        d["SPLA"] = ar.alloc([3, 128], BF16)
        d["SPLB"] = ar.alloc([3, 128], BF16)
        d["SP2A"] = ar.alloc([2, 128], BF16)
        d["SP2B"] = ar.alloc([2, 128], BF16)
        d["MA1"] = ar.alloc([2, 2, 128], BF16)
        d["MA2"] = ar.alloc([2, 2, 128], BF16)
        d["MT"] = ar.alloc([2, 128], BF16)
        d["Xb_"] = [ar.alloc([2, 128], BF16) for _ in range(2)]
        d["XTb_"] = [ar.alloc([2, 128], BF16) for _ in range(2)]
        d["Tb_"] = [ar.alloc([2, 128], BF16) for _ in range(2)]
        d["Rh"] = ar.alloc([128], BF16)
        d["Yloc"] = ar.alloc([128], F32)
        d["PQ"] = ar.alloc([2, 128], BF16)
        d["Sloc"] = ar.alloc([2, 128], BF16)
        SETS.append(d)
        for k_ in ("SPLA", "SPLB", "SP2A", "SP2B"):
            op("pool", lambda e, t_=d[k_]: e.memset(t_, 0.0), writes=[k_ + "_s%d" % si])
    op("pool", lambda e: e.memset(Sbd, 0.0), writes=["Sbd%d" % c for c in range(8)])

    bank_rr = [0]

    def nb():
        b = bank_rr[0] % 8
        bank_rr[0] += 1
        return b

    evac_rr = [0]

    def cp(dst, src, reads, writes):
        evac_rr[0] += 1
        if evac_rr[0] % 3:
            op("act", lambda e: e.activation(out=dst, in_=src, func=AF.Copy), reads=reads, writes=writes)
        else:
            op("dve", lambda e: e.tensor_copy(out=dst, in_=src), reads=reads, writes=writes)

    def mach(cc, own, kx, PR, Qt, Kt, Qb, Kb, vS, GC):
        d = SETS[cc % NSET]
        sx = "_s%d" % (cc % NSET)
        RHS, SPLA, SPLB, SP2A, SP2B, MA1, MA2, MT = d["RHS"], d["SPLA"], d["SPLB"], d["SP2A"], d["SP2B"], d["MA1"], d["MA2"], d["MT"]
        Xb_, XTb_, Tb_, Rh, Yloc, PQ, Sloc = d["Xb_"], d["XTb_"], d["Tb_"], d["Rh"], d["Yloc"], d["PQ"], d["Sloc"]
        sk = "Sbd%d" % cc
        b = nb()
        tp = ps[b][:, :].bitcast(BF16)
        srcs = (PR[:, cc, 0:128], Qb[:, cc, :], Kb[:, cc, :], vS[:, cc, :])
        skeys = ("PR" + kx, "Qb" + kx, "Kb" + kx, "vS" + kx)
        for i4 in range(4):
            op("pe", lambda e, i4=i4, tp=tp, srcs=srcs: e.transpose(tp[:, i4 * 128:(i4 + 1) * 128], srcs[i4], P["identb"][:]), reads=[skeys[i4], "identb"], writes=["ps%d" % b])
        tpv = tp[:, 128:512].rearrange("p (i j) -> p i j", j=128)
        op("act", lambda e, tpv=tpv: e.activation(out=SPLA[:, :, 0:64], in_=tpv[:, :, 0:64], func=AF.Copy), reads=["ps%d" % b], writes=["SPLA" + sx])
        op("dve", lambda e, tpv=tpv: e.tensor_copy(out=SPLB[:, :, 64:128], in_=tpv[:, :, 64:128]), reads=["ps%d" % b], writes=["SPLB" + sx])
        op("act", lambda e, tp=tp: e.activation(out=RHS[:, :, 0:64], in_=tp[:, 0:128].rearrange("p (h j) -> p h j", h=2), func=AF.Copy), reads=["ps%d" % b], writes=["RHS" + sx])
        yield
        bAh = [nb(), nb()]
        bBh = [nb(), nb()]
        for h in range(2):
            R_ = slice(64 * h, 64 * h + 64)
            op("pe", lambda e, h=h, R_=R_: e.matmul(ps[bAh[h]][:, 0:256], Qt[R_, cc, :], PR[R_, cc, :], start=True, stop=True), reads=["Qt" + kx, "PR" + kx], writes=["ps%d" % bAh[h]])
            op("pe", lambda e, h=h, R_=R_: e.matmul(ps[bAh[h]][:, 256:384], PR[R_, cc, 0:128], Qt[R_, cc, :], start=True, stop=True), reads=["Qt" + kx, "PR" + kx], writes=["ps%d" % bAh[h]])
            op("pe", lambda e, h=h, R_=R_: e.matmul(ps[bBh[h]][:, 0:256], Kt[R_, cc, :], PR[R_, cc, :], start=True, stop=True), reads=["Kt" + kx, "PR" + kx], writes=["ps%d" % bBh[h]])
        for h in range(2):
            op("dve", lambda e, h=h: e.tensor_tensor(out=MA1[:, h, :, :], in0=ps[bAh[h]][:, 0:256].rearrange("p (w t) -> p w t", w=2), in1=mask1, op=ALU.mult), reads=["ps%d" % bAh[h], "mask1"], writes=["MA1" + sx])
            op("dve", lambda e, h=h: e.tensor_tensor(out=MT[:, h, :], in0=ps[bAh[h]][:, 256:384], in1=maskT, op=ALU.mult), reads=["ps%d" % bAh[h], "maskT"], writes=["MT" + sx])
            op("dve", lambda e, h=h: e.tensor_tensor(out=MA2[:, h, :, :], in0=ps[bBh[h]][:, 0:256].rearrange("p (w t) -> p w t", w=2), in1=mask1, op=ALU.mult), reads=["ps%d" % bBh[h], "mask1"], writes=["MA2" + sx])
        yield
        Tc, Tck = Tb_[0], "Tb0" + sx
        op("pool", lambda e, Tc=Tc: e.tensor_tensor(out=Tc, in0=MA1[:, :, 0, :], in1=bc(P["ident"][:].unsqueeze(1), [128, 2, 128]), op=ALU.add), reads=["MA1" + sx, "ident"], writes=[Tck])
        Xc = [MA1[:, 0, 0, :], MA1[:, 1, 0, :]]
        Xck = "MA1" + sx
        XTc = [MT[:, 0, :], MT[:, 1, :]]
        XTck = "MT" + sx
        nlev = 5
        for lv in range(nlev):
            last = (lv == nlev - 1)
            Xn, Xnk = Xb_[lv % 2], "Xb%d" % (lv % 2) + sx
            XTn, XTnk = XTb_[lv % 2], "XTb%d" % (lv % 2) + sx
            Tn, Tnk = Tb_[(lv + 1) % 2], "Tb%d" % ((lv + 1) % 2) + sx
            if not last:
                bX = nb()
                for h in range(2):
                    op("pe", lambda e, h=h, bX=bX, XTc=XTc, Xc=Xc: e.matmul(ps[bX][:, h * 128:(h + 1) * 128], XTc[h], Xc[h], start=True, stop=True), reads=[Xck, XTck], writes=["ps%d" % bX])
                cp(Xn, ps[bX][:, 0:256].rearrange("p (h t) -> p h t", h=2), ["ps%d" % bX], [Xnk])
            bXT = nb()
            for h in range(2):
                op("pe", lambda e, h=h, bXT=bXT, XTc=XTc, Xc=Xc: e.matmul(ps[bXT][:, h * 128:(h + 1) * 128], Xc[h], XTc[h], start=True, stop=True), reads=[Xck, XTck], writes=["ps%d" % bXT])
            cp(XTn, ps[bXT][:, 0:256].rearrange("p (h t) -> p h t", h=2), ["ps%d" % bXT], [XTnk])
            bT = nb()
            for h in range(2):
                op("pe", lambda e, h=h, bT=bT, XTn=XTn, Tc=Tc: e.matmul(ps[bT][:, h * 128:(h + 1) * 128], XTn[:, h, :], Tc[:, h, :], start=True, stop=True), reads=[XTnk, Tck], writes=["ps%d" % bT])
            op("dve", lambda e, bT=bT, Tn=Tn, Tc=Tc: e.tensor_tensor(out=Tn, in0=ps[bT][:, 0:256].rearrange("p (h t) -> p h t", h=2), in1=Tc, op=ALU.add), reads=["ps%d" % bT, Tck], writes=[Tnk])
            Tc, Tck = Tn, Tnk
            yield
            if not last:
                Xc, Xck = [Xn[:, 0, :], Xn[:, 1, :]], Xnk
            XTc, XTck = [XTn[:, 0, :], XTn[:, 1, :]], XTnk
        yield
        bW = nb()
        op("pe", lambda e, bW=bW: e.matmul(ps[bW][:, 0:64], MA2[:, 0, 0, :], SPLA[:, 2, 0:64], start=True, stop=True), reads=["MA2" + sx, "SPLA" + sx], writes=["ps%d" % bW])
        op("pe", lambda e, bW=bW: e.matmul(ps[bW][:, 64:128], MA2[:, 1, 0, :], SPLB[:, 2, 64:128], start=True, stop=True), reads=["MA2" + sx, "SPLB" + sx], writes=["ps%d" % bW])
        cp(RHS[:, :, 64:128], ps[bW][:, 0:128].rearrange("p (h j) -> p h j", h=2), ["ps%d" % bW], ["RHS" + sx])
        yield
        bU = nb()
        for h in range(2):
            op("pe", lambda e, h=h, bU=bU, Tc=Tc: e.matmul(ps[bU][:, h * 128:(h + 1) * 128], Tc[:, h, :], RHS[:, h, :], start=True, stop=True), reads=[Tck, "RHS" + sx], writes=["ps%d" % bU])
        op("act", lambda e, bU=bU: e.activation(out=SP2A[:, :, 0:64], in_=ps[bU][:, 0:128].rearrange("p (w j) -> p w j", w=2), func=AF.Copy), reads=["ps%d" % bU], writes=["SP2A" + sx])
        op("dve", lambda e, bU=bU: e.tensor_copy(out=SP2B[:, :, 64:128], in_=ps[bU][:, 128:256].rearrange("p (w j) -> p w j", w=2)), reads=["ps%d" % bU], writes=["SP2B" + sx])
        yield
        if own:
            bR = nb()
            op("pe", lambda e, bR=bR: e.matmul(ps[bR][:, 0:128], SP2A[:, 0, :], MA1[:, 0, 1, :], start=True, stop=False), reads=["SP2A" + sx, "MA1" + sx], writes=["ps%d" % bR])
            op("pe", lambda e, bR=bR: e.matmul(ps[bR][:, 0:128], SP2B[:, 0, :], MA1[:, 1, 1, :], start=False, stop=True), reads=["SP2B" + sx, "MA1" + sx], writes=["ps%d" % bR])
            op("dve", lambda e, bR=bR: e.tensor_tensor(out=Rh, in0=ps[bR][:, 0:128], in1=PR[:, cc, 128:256], op=ALU.add), reads=["ps%d" % bR, "PR" + kx], writes=["Rh" + sx])
            bY = nb()
            op("pe", lambda e, bY=bY: e.matmul(ps[bY][:, 0:128], SP2A[:, 1, :], MA1[:, 0, 1, :], start=True, stop=False), reads=["SP2A" + sx, "MA1" + sx], writes=["ps%d" % bY])
            op("pe", lambda e, bY=bY: e.matmul(ps[bY][:, 0:128], SP2B[:, 1, :], MA1[:, 1, 1, :], start=False, stop=False), reads=["SP2B" + sx, "MA1" + sx], writes=["ps%d" % bY])
            op("pe", lambda e, bY=bY: e.matmul(ps[bY][:, 0:128], SPLA[:, 2, :], MA2[:, 0, 1, :], start=False, stop=False), reads=["SPLA" + sx, "MA2" + sx], writes=["ps%d" % bY])
            op("pe", lambda e, bY=bY: e.matmul(ps[bY][:, 0:128], SPLB[:, 2, :], MA2[:, 1, 1, :], start=False, stop=True), reads=["SPLB" + sx, "MA2" + sx], writes=["ps%d" % bY])
            cp(Yloc, ps[bY][:, 0:128], ["ps%d" % bY], ["Yloc" + sx])
        yield
        bQc = [nb(), nb()]
        for c in range(2):
            R_ = slice(64 * c, 64 * c + 64)
            bq = bQc[c]
            op("pe", lambda e, R_=R_, bq=bq: e.matmul(ps[bq][:, 0:128], SP2A[R_, 0, :], SPLA[R_, 0, :], start=True, stop=False), reads=["SP2A" + sx, "SPLA" + sx], writes=["ps%d" % bq])
            op("pe", lambda e, R_=R_, bq=bq: e.matmul(ps[bq][:, 0:128], SP2B[R_, 0, :], SPLB[R_, 0, :], start=False, stop=True), reads=["SP2B" + sx, "SPLB" + sx], writes=["ps%d" % bq])
            op("pe", lambda e, R_=R_, bq=bq: e.matmul(ps[bq][:, 128:256], SPLA[R_, 0, :], SP2A[R_, 1, :], start=True, stop=False), reads=["SP2A" + sx, "SPLA" + sx], writes=["ps%d" % bq])
            op("pe", lambda e, R_=R_, bq=bq: e.matmul(ps[bq][:, 128:256], SPLB[R_, 0, :], SP2B[R_, 1, :], start=False, stop=False), reads=["SP2B" + sx, "SPLB" + sx], writes=["ps%d" % bq])
            op("pe", lambda e, R_=R_, bq=bq: e.matmul(ps[bq][:, 128:256], SPLA[R_, 1, :], SPLA[R_, 2, :], start=False, stop=False), reads=["SPLA" + sx], writes=["ps%d" % bq])
            op("pe", lambda e, R_=R_, bq=bq: e.matmul(ps[bq][:, 128:256], SPLB[R_, 1, :], SPLB[R_, 2, :], start=False, stop=True), reads=["SPLB" + sx], writes=["ps%d" % bq])
        for c in range(2):
            cp(PQ[:, c, :], ps[bQc[c]][:, 0:128], ["ps%d" % bQc[c]], ["PQ" + sx])
            cp(Sloc[:, c, :], ps[bQc[c]][:, 128:256], ["ps%d" % bQc[c]], ["Sloc" + sx])
        yield
        for c in range(2):
            yield
            if own:
                bYc = nb()
                op("pe", lambda e, c=c, bYc=bYc: e.matmul(ps[bYc][:, 0:64], Sbd[:, cc, :], Rh[:, c * 64:(c + 1) * 64], start=True, stop=True), reads=[sk, "Rh" + sx], writes=["ps%d" % bYc])
                op("dve", lambda e, c=c, bYc=bYc: e.tensor_tensor(out=Ysb[:, cc, c * 64:(c + 1) * 64], in0=ps[bYc][:, 0:64], in1=Yloc[:, c * 64:(c + 1) * 64], op=ALU.add), reads=["ps%d" % bYc, "Yloc" + sx], writes=["Ysb"])
            bS2 = nb()
            op("pe", lambda e, c=c, bS2=bS2: e.matmul(ps[bS2][:, 0:128], PQ[:, c, :], Sbd[:, cc, :], start=True, stop=False), reads=["PQ" + sx, sk], writes=["ps%d" % bS2])
            op("pe", lambda e, c=c, bS2=bS2: e.matmul(ps[bS2][:, 0:128], P["identb"][:], Sloc[:, c, :], start=False, stop=True), reads=["identb", "Sloc" + sx], writes=["ps%d" % bS2])
            op("dve", lambda e, c=c, bS2=bS2: e.scalar_tensor_tensor(out=Sbd[:, cc, :], in0=Sbd[:, cc, :], scalar=GC[:, cc * 2 + c:cc * 2 + c + 1], in1=ps[bS2][:, 0:128], op0=ALU.mult, op1=ALU.add),
               reads=[sk, "GC" + kx, "ps%d" % bS2], writes=[sk])

    NTILE = SEQ // 128
    npre, nown = getattr(self, "rw_tiles", (NTILE // 2, NTILE // 2))
    tiles = list(range(npre)) + list(range(NTILE // 2, NTILE // 2 + nown))
    for tn, ti in enumerate(tiles):
        own = ti >= NTILE // 2
        par = tn % 2
        kx = "_%d" % par
        MIb, OWb, GC, xt = MIs[par], OWs[par], GCs[par], xts[par]
        xk = "xt" + kx
        PR, Qt, Kt, Qb, Kb, vS = MIb[:, :, 0:256], MIb[:, :, 256:384], MIb[:, :, 384:512], MIb[:, :, 512:640], MIb[:, :, 640:768], MIb[:, :, 768:896]
        gS, bonus = OWb[:, :, 0:128], OWb[:, :, 128:256]
        fw.dma("sp", MIb, S["MI"][ti], writes=["PR" + kx, "Qt" + kx, "Kt" + kx, "Qb" + kx, "Kb" + kx, "vS" + kx])
        fw.dma("sp", GC, S["GCd"][ti], writes=["GC" + kx])
        if own:
            fw.dma("sp", OWb, S["OW"][ti - NTILE // 2], writes=["gS" + kx, "bonus" + kx])
            fw.dma("sp", xt, S["x2"][ti * 128:(ti + 1) * 128, :], writes=[xk])
        if ti == NTILE // 2:
            op("pool", lambda e: e.tensor_scalar(out=Sbd, in0=Sbd, scalar1=P["flag"][:, 0:1], scalar2=None, op0=ALU.mult),
               reads=["flag"] + ["Sbd%d" % c for c in range(8)], writes=["Sbd%d" % c for c in range(8)])
        gens = [mach(cc, own, kx, PR, Qt, Kt, Qb, Kb, vS, GC) for cc in range(8)]
        while gens:
            for g in list(gens):
                try:
                    next(g)
                except StopIteration:
                    gens.remove(g)
        if not own:
            continue
        op("act", lambda e: e.activation(out=B0, in_=Ysb, func=AF.Copy), reads=["Ysb"], writes=["B0"])
        for cc in range(8):
            bb = cc // 4
            op("pe", lambda e, cc=cc, bb=bb: e.matmul(ps[bb][:, (cc % 4) * 128:(cc % 4 + 1) * 128], bdones, B0[:, cc, :], start=True, stop=True), reads=["bdones", "B0"], writes=["ps%d" % bb])
        dd, ddk = T3[1], "T1"
        for hh in range(2):
            op("dve", lambda e, hh=hh: e.scalar_tensor_tensor(out=dd[:, hh * 4:(hh + 1) * 4, :], in0=ps[hh][:, :].rearrange("p (c t) -> p c t", t=128), scalar=-1.0 / 64, in1=Ysb[:, hh * 4:(hh + 1) * 4, :], op0=ALU.mult, op1=ALU.add),
               reads=["ps%d" % hh, "Ysb"], writes=[ddk])
        op("pool", lambda e: e.tensor_tensor(out=B0, in0=dd, in1=dd, op=ALU.mult), reads=[ddk], writes=["B0"])
        for cc in range(8):
            bb = 2 + cc // 4
            op("pe", lambda e, cc=cc, bb=bb: e.matmul(ps[bb][:, (cc % 4) * 128:(cc % 4 + 1) * 128], bdones, B0[:, cc, :], start=True, stop=True), reads=["bdones", "B0"], writes=["ps%d" % bb])
        rs, rsk = T3[2], "T2"
        for hh in range(2):
            op("act", lambda e, hh=hh: e.activation(out=rs[:, hh * 4:(hh + 1) * 4, :], in_=ps[2 + hh][:, :].rearrange("p (c t) -> p c t", t=128), func=AF.Sqrt, scale=1.0 / 64, bias=P["eps"][:, 1:2]), reads=["ps%d" % (2 + hh), "eps"], writes=[rsk])
        op("dve", lambda e: e.reciprocal(out=Tf[2], in_=Tf[2]), reads=[rsk], writes=[rsk])
        op("pool", lambda e: e.tensor_tensor(out=dd, in0=dd, in1=rs, op=ALU.mult), reads=[ddk, rsk], writes=[ddk])
        op("pool", lambda e: e.tensor_tensor(out=dd, in0=dd, in1=vb(11), op=ALU.mult), reads=[ddk, "vec"], writes=[ddk])
        op("pool", lambda e: e.tensor_tensor(out=dd, in0=dd, in1=vb(12), op=ALU.add), reads=[ddk, "vec"], writes=[ddk])
        op("dve", lambda e: e.tensor_tensor(out=dd, in0=dd, in1=bonus, op=ALU.add), reads=[ddk, "bonus" + kx], writes=[ddk])
        op("dve", lambda e: e.tensor_tensor(out=yg, in0=dd, in1=gS, op=ALU.mult), reads=[ddk, "gS" + kx], writes=["yg"])
        for n in range(2):
            for kc in range(8):
                op("pe", lambda e, n=n, kc=kc: e.matmul(ps[2 + n][:, :], yg[:, kc, :], W["rw_wo"][:, kc, n * 512:(n + 1) * 512], start=(kc == 0), stop=(kc == 7)), reads=["yg", "rw_wo"], writes=["ps%d" % (2 + n)])
        self.postnorm_res2((2, 3), xt, xk, 2, scr, scr_keys)
        to = ti - NTILE // 2
        fw.dma("sp", S["x3"][to * 128:(to + 1) * 128, :], xt, reads=[xk])
        junk, ss, rstd = scr["junk"], scr["ss"], scr["rstd"]
        op("act", lambda e: e.activation(out=junk, in_=xt, func=AF.Square, accum_out=ss[:, 0:1]), reads=[xk], writes=["T6", "lg"])
        op("act", lambda e: e.activation(out=rstd[:, 0:1], in_=ss[:, 0:1], func=AF.Sqrt, scale=1.0 / D, bias=P["eps"][:, 0:1]), reads=["lg", "eps"], writes=["lg"])
        op("dve", lambda e: e.reciprocal(out=rstd[:, 0:1], in_=rstd[:, 0:1]), reads=["lg"], writes=["lg"])
        xn32 = Tf[7]
        op("act", lambda e: e.activation(out=xn32, in_=xt, func=AF.Copy, scale=rstd[:, 0:1]), reads=[xk, "lg"], writes=["T7"])
        for kc in range(8):
            bb = kc // 4
            op("pe", lambda e, kc=kc, bb=bb: e.transpose(ps[bb][:, (kc % 4) * 128:(kc % 4 + 1) * 128], xn32[:, kc * 128:(kc + 1) * 128], P["ident"][:]), reads=["T7", "ident"], writes=["ps%d" % bb])
        G1 = P["modT"][:, 4 + 2, :]
        sh = P["modT"][:, 4 + 3, :]
        for hh in range(2):
            op("dve", lambda e, hh=hh: e.tensor_tensor(out=h32[:, hh * 4:(hh + 1) * 4, :], in0=ps[hh][:, :].rearrange("p (c t) -> p c t", t=128), in1=bc(G1[:, hh * 4:(hh + 1) * 4].unsqueeze(2), [128, 4, 128]), op=ALU.mult),
               reads=["ps%d" % hh, "modT"], writes=["h32"])
        op("pool", lambda e: e.tensor_tensor(out=h32, in0=h32, in1=bc(sh.unsqueeze(2), [128, 8, 128]), op=ALU.add), reads=["h32", "modT"], writes=["h32"])
        op("act", lambda e: e.activation(out=hb, in_=h32, func=AF.Copy), reads=["h32"], writes=["hb"])
        fw.dma("sp", S["hTf1"][to // 2][:, :, (to % 2) * 128:(to % 2 + 1) * 128], hb, reads=["hb"])
        bL = nb()
        for kc in range(8):
            op("pe", lambda e, kc=kc, bL=bL: e.matmul(ps[bL][:, 0:8], h32[:, kc, :], router[:, kc, :], start=(kc == 0), stop=(kc == 7)), reads=["h32", "router"], writes=["ps%d" % bL])
        L8, m1, m2, eq, ex, sm = lg[:, 0:8], lg[:, 8:9], lg[:, 9:10], lg[:, 10:18], lg[:, 18:26], lg[:, 26:27]
        op("dve", lambda e, bL=bL: e.tensor_copy(out=L8, in_=ps[bL][:, 0:8]), reads=["ps%d" % bL], writes=["lg"])
        op("dve", lambda e: e.reduce_max(out=m1, in_=L8, axis=AX.X), reads=["lg"], writes=["lg"])
        op("dve", lambda e: e.tensor_scalar(out=eq, in0=L8, scalar1=m1, scalar2=-1e30, op0=ALU.is_equal, op1=ALU.mult), reads=["lg"], writes=["lg"])
        op("dve", lambda e: e.tensor_tensor(out=eq, in0=eq, in1=L8, op=ALU.add), reads=["lg"], writes=["lg"])
        op("dve", lambda e: e.reduce_max(out=m2, in_=eq, axis=AX.X), reads=["lg"], writes=["lg"])
        op("dve", lambda e: e.tensor_scalar(out=eq, in0=L8, scalar1=m2, scalar2=None, op0=ALU.is_ge), reads=["lg"], writes=["lg"])
        op("dve", lambda e: e.tensor_scalar(out=ex, in0=L8, scalar1=m1, scalar2=None, op0=ALU.subtract), reads=["lg"], writes=["lg"])
        op("act", lambda e: e.activation(out=ex, in_=ex, func=AF.Exp), reads=["lg"], writes=["lg"])
        op("dve", lambda e: e.tensor_tensor(out=ex, in0=ex, in1=eq, op=ALU.mult), reads=["lg"], writes=["lg"])
        op("dve", lambda e: e.reduce_sum(out=sm, in_=ex, axis=AX.X), reads=["lg"], writes=["lg"])
        op("dve", lambda e: e.reciprocal(out=sm, in_=sm), reads=["lg"], writes=["lg"])
        op("dve", lambda e: e.tensor_scalar(out=ex, in0=ex, scalar1=sm, scalar2=None, op0=ALU.mult), reads=["lg"], writes=["lg"])
        fw.dma("sp", S["comb"][to * 128:(to + 1) * 128, :], ex, reads=["lg"])


def _postnorm_sb2(self, y, ykey, xsub, xkey, gp_idx, scr, skeys):
    fw, P = self.fw, self.P
    junk, ss, rstd, tmp = scr["junk"], scr["ss2"], scr["rstd2"], scr["tmp"]
    fw.op("act", lambda e: e.activation(out=junk, in_=y, func=AF.Square, accum_out=ss[:, 0:1]), reads=[ykey], writes=[skeys[0], "lg"])
    fw.op("act", lambda e: e.activation(out=rstd[:, 0:1], in_=ss[:, 0:1], func=AF.Sqrt, scale=1.0 / D, bias=P["eps"][:, 0:1]), reads=["lg", "eps"], writes=["lg"])
    fw.op("dve", lambda e: e.reciprocal(out=rstd[:, 0:1], in_=rstd[:, 0:1]), reads=["lg"], writes=["lg"])
    fw.op("dve", lambda e: e.scalar_tensor_tensor(out=tmp, in0=y, scalar=rstd[:, 0:1], in1=P["GP"][:, gp_idx, :], op0=ALU.mult, op1=ALU.mult),
          reads=[ykey, "lg", "GP"], writes=[skeys[1]])
    fw.op("dve", lambda e: e.tensor_tensor(out=xsub, in0=xsub, in1=tmp, op=ALU.add), reads=[skeys[1], xkey], writes=[xkey])


def _prenorm_T2(self, xsub, xkey, l, sub, hT, hkey, col0, pbank, scr, skeys):
    fw, P, ps = self.fw, self.P, self.ps
    junk, ss, rstd, xn, tmp = scr["junk"], scr["ss"], scr["rstd"], scr["xn"], scr["tmp"]
    fw.op("act", lambda e: e.activation(out=junk, in_=xsub, func=AF.Square, accum_out=ss[:, 0:1]), reads=[xkey], writes=[skeys[0], "lg"])
    fw.op("act", lambda e: e.activation(out=rstd[:, 0:1], in_=ss[:, 0:1], func=AF.Sqrt, scale=1.0 / D, bias=P["eps"][:, 0:1]), reads=["lg", "eps"], writes=["lg"])
    fw.op("dve", lambda e: e.reciprocal(out=rstd[:, 0:1], in_=rstd[:, 0:1]), reads=["lg"], writes=["lg"])
    fw.op("act", lambda e: e.activation(out=xn, in_=xsub, func=AF.Copy, scale=rstd[:, 0:1]), reads=[xkey, "lg"], writes=[skeys[0]])
    pk = "ps%d" % pbank
    pbt = ps[pbank][:, :].bitcast(BF16)
    for kc in range(8):
        fw.op("pe", lambda e, kc=kc: e.transpose(pbt[:, kc * 128:(kc + 1) * 128], xn[:, kc * 128:(kc + 1) * 128], P["identb"][:]), reads=[skeys[0], "identb"], writes=[pk])
    G1 = P["modT"][:, l * 4 + sub * 2 + 0, :]
    sh = P["modT"][:, l * 4 + sub * 2 + 1, :]
    for kc in range(8):
        fw.op("act", lambda e, kc=kc: e.activation(out=hT[:, kc, col0:col0 + 128], in_=pbt[:, kc * 128:(kc + 1) * 128], func=AF.Identity, scale=G1[:, kc:kc + 1], bias=sh[:, kc:kc + 1]),
              reads=[pk, "modT"], writes=[hkey])


def _postnorm_res2(self, psb, xsub, xkey, gp_idx, scr, skeys):
    fw, P, ps = self.fw, self.P, self.ps
    junk, ss2, rstd, tmp = scr["junk"], scr["ss2"], scr["rstd2"], scr["tmp"]
    for n in range(2):
        fw.op("act", lambda e, n=n: e.activation(out=junk[:, 0:512], in_=ps[psb[n]][:, :], func=AF.Square, accum_out=ss2[:, n:n + 1]), reads=["ps%d" % psb[n]], writes=[skeys[0], "lg"])
    fw.op("pool", lambda e: e.tensor_tensor(out=rstd[:, 0:1], in0=ss2[:, 0:1], in1=ss2[:, 1:2], op=ALU.add), reads=["lg"], writes=["lg"])
    fw.op("act", lambda e: e.activation(out=rstd[:, 0:1], in_=rstd[:, 0:1], func=AF.Sqrt, scale=1.0 / D, bias=P["eps"][:, 0:1]), reads=["lg", "eps"], writes=["lg"])
    fw.op("dve", lambda e: e.reciprocal(out=rstd[:, 0:1], in_=rstd[:, 0:1]), reads=["lg"], writes=["lg"])
    for n in range(2):
        fw.op("dve", lambda e, n=n: e.scalar_tensor_tensor(out=tmp[:, n * 512:(n + 1) * 512], in0=ps[psb[n]][:, :], scalar=rstd[:, 0:1], in1=P["GP"][:, gp_idx, n * 512:(n + 1) * 512], op0=ALU.mult, op1=ALU.mult),
              reads=["ps%d" % psb[n], "lg", "GP"], writes=[skeys[1]])
    fw.op("dve", lambda e: e.tensor_tensor(out=xsub, in0=xsub, in1=tmp, op=ALU.add), reads=[skeys[1], xkey], writes=[xkey])


Builder.phase_rwkv_prep = _phase_rwkv_prep
Builder.phase_rwkv_chain = _phase_rwkv_chain
Builder.postnorm_sb2 = _postnorm_sb2
Builder.prenorm_T2 = _prenorm_T2
Builder.postnorm_res2 = _postnorm_res2
```
